# Optimizing a Trainium2 kernel written in Bass

```python
import numpy as np
import jax
import jax.numpy as jnp
from jax import lax

D_MODEL = 1024
BATCH = 8
SEQ = 4096
DEPTH = 2

HEAD_DIM = 64
N_MIXERS = 4
GROUP_W = D_MODEL // N_MIXERS
N_HEADS = GROUP_W // HEAD_DIM
MIX_W = N_MIXERS * GROUP_W

RWKV_DECAY_RANK = 64
RWKV_AAA_RANK = 64
RWKV_GATE_RANK = 128
RWKV_DECAY_SCALE = 0.606531
RWKV_GN_EPS = 64e-5

CONV_W = 3

DIL_CFG = ((128, 1), (512, 4), (2048, 16))

CMP_LEN = 32
CMP_STRIDE = 16
CMP_HIDDEN = 256
SEL_BLK = 64
SEL_TOP = 16
SWA_WIN = 512
FORCE_SCORE = 1e4

N_EXPERTS = 32
TOP_K = 4
EXPERT_FF = D_MODEL
SWIGLU_LIMIT = 7.0
SWIGLU_ALPHA = 1.702
MOE_BLK = 128

ROPE_THETA = 10000.0
NORM_EPS = 1e-6
QBLK = 128
NEG = -1e30

A_WIDTHS = (GROUP_W, GROUP_W, GROUP_W, RWKV_DECAY_RANK, RWKV_AAA_RANK, RWKV_GATE_RANK)
B_WIDTHS = (GROUP_W, GROUP_W, GROUP_W)
C_WIDTHS = (GROUP_W, GROUP_W, GROUP_W)
D_WIDTHS = (GROUP_W, HEAD_DIM, HEAD_DIM, HEAD_DIM, HEAD_DIM, HEAD_DIM, HEAD_DIM, 3 * N_HEADS)
SECTION_WIDTHS = (sum(A_WIDTHS), sum(B_WIDTHS), sum(C_WIDTHS), sum(D_WIDTHS))
PROJ_W = sum(SECTION_WIDTHS)

kernel_name = 'hybrid_rwkv7_shortconv_dilattn_nsa_moe'


def split_cols(z, widths):
    offs = np.cumsum((0,) + tuple(widths))
    return [z[..., int(offs[i]):int(offs[i + 1])] for i in range(len(widths))]


def rmsnorm(x, g):
    xf = x.astype(jnp.float32)
    y = xf * lax.rsqrt(jnp.mean(xf * xf, axis=-1, keepdims=True) + NORM_EPS)
    return (y * g.astype(jnp.float32)).astype(x.dtype)


def rope(x, pos):
    half = x.shape[-1] // 2
    inv = ROPE_THETA ** (-jnp.arange(half, dtype=jnp.float32) / half)
    ang = pos.astype(jnp.float32)[:, :, None, None] * inv
    cos, sin = jnp.cos(ang), jnp.sin(ang)
    xf = x.astype(jnp.float32)
    x1, x2 = xf[..., :half], xf[..., half:]
    return jnp.concatenate([x1 * cos - x2 * sin, x2 * cos + x1 * sin], axis=-1).astype(x.dtype)


def token_shift(z):
    return jnp.pad(z, ((0, 0), (1, 0), (0, 0)))[:, :-1]


def banded_attention(q, k, v, window):
    B, L, Hq, hd = q.shape
    Hk = k.shape[2]
    G = Hq // Hk
    nb = -(-L // QBLK)
    Lp = nb * QBLK
    npv = -(-window // QBLK)
    pad = ((0, 0), (0, Lp - L), (0, 0), (0, 0))
    q, k, v = jnp.pad(q, pad), jnp.pad(k, pad), jnp.pad(v, pad)
    bpad = ((0, 0), (npv, 0), (0, 0), (0, 0), (0, 0))
    kb = jnp.pad(k.reshape(B, nb, QBLK, Hk, hd), bpad)
    vb = jnp.pad(v.reshape(B, nb, QBLK, Hk, hd), bpad)
    bidx = np.arange(nb)[:, None] + np.arange(npv + 1)[None, :]
    J = (npv + 1) * QBLK
    kband = kb[:, bidx].reshape(B, nb, J, Hk, hd)
    vband = vb[:, bidx].reshape(B, nb, J, Hk, hd)
    qb = q.reshape(B, nb, QBLK, Hk, G, hd)
    s = jnp.einsum('bnqkgd,bnjkd->bnkgqj', qb, kband).astype(jnp.float32) * (hd ** -0.5)
    qpos = np.arange(nb)[:, None] * QBLK + np.arange(QBLK)[None, :]
    kpos = (np.arange(nb)[:, None] - npv) * QBLK + np.arange(J)[None, :]
    dist = qpos[:, :, None] - kpos[:, None, :]
    mask = (dist >= 0) & (dist <= window) & (kpos[:, None, :] >= 0)
    s = jnp.where(mask[None, :, None, None], s, NEG)
    lse = jax.nn.logsumexp(s, axis=-1)
    p = jnp.exp(s - lse[..., None]).astype(v.dtype)
    o = jnp.einsum('bnkgqj,bnjkd->bnqkgd', p, vband).reshape(B, Lp, Hq, hd)[:, :L]
    lse = jnp.transpose(lse, (0, 1, 4, 2, 3)).reshape(B, Lp, Hq)[:, :L]
    return o, lse


def decimate(t, d):
    B, S = t.shape[:2]
    t = t.reshape((B, S // d, d) + t.shape[2:])
    return jnp.moveaxis(t, 2, 1).reshape((B * d, S // d) + t.shape[3:])


def undecimate(t, d, B):
    Sd = t.shape[1]
    t = t.reshape((B, d, Sd) + t.shape[2:])
    return jnp.moveaxis(t, 1, 2).reshape((B, Sd * d) + t.shape[3:])


def rwkv7_scan(r, w, k, v, kk, b):
    B, S, H, N = r.shape
    tm = lambda t: jnp.moveaxis(t, 1, 0)

    def step(st, inp):
        r_t, w_t, k_t, v_t, kk_t, b_t = inp
        sa = jnp.einsum('bhij,bhj->bhi', st, -kk_t)
        st = st * w_t[:, :, None, :] + sa[..., None] * b_t[:, :, None, :] + v_t[..., None] * k_t[:, :, None, :]
        return st, jnp.einsum('bhij,bhj->bhi', st, r_t)

    s0 = jnp.zeros((B, H, N, N), jnp.float32)
    _, ys = lax.scan(step, s0, (tm(r), tm(w), tm(k), tm(v), tm(kk), tm(b)))
    return jnp.moveaxis(ys, 0, 1)


def rwkv7_mix(r, k, v, wd, ad, gd, w0, w2, a0, a2, g2, k_k, k_a, r_k, gn_w, gn_b):
    B, S, C = r.shape
    H = C // HEAD_DIM
    f32 = jnp.float32
    heads = lambda t: t.reshape(B, S, H, HEAD_DIM).astype(f32)
    w = jnp.exp(-RWKV_DECAY_SCALE * jax.nn.sigmoid((w0 + jnp.tanh(wd) @ w2).astype(f32)))
    a = jax.nn.sigmoid(a0 + ad @ a2)
    g = jax.nn.sigmoid(gd) @ g2
    kk = heads(k * k_k)
    kk = kk * lax.rsqrt(jnp.sum(kk * kk, axis=-1, keepdims=True) + 1e-12)
    k = k * (1.0 + (a - 1.0) * k_a)
    y = rwkv7_scan(heads(r), heads(w), heads(k), heads(v), kk, kk * heads(a))
    mu = jnp.mean(y, axis=-1, keepdims=True)
    var = jnp.mean(jnp.square(y - mu), axis=-1, keepdims=True)
    y = (y - mu) * lax.rsqrt(var + RWKV_GN_EPS) * gn_w.reshape(H, HEAD_DIM).astype(f32) + gn_b.reshape(H, HEAD_DIM).astype(f32)
    y = y + jnp.sum(heads(r) * heads(k) * r_k.astype(f32), axis=-1, keepdims=True) * heads(v)
    return (y.reshape(B, S, C) * g.astype(f32)).astype(r.dtype)


def short_conv_mix(bg, cg, xin, conv_w):
    u = cg * xin
    y = lax.conv_general_dilated(u, conv_w[:, None, :].astype(u.dtype), window_strides=(1,),
                                 padding=((CONV_W - 1, 0),), dimension_numbers=('NWC', 'WIO', 'NWC'),
                                 feature_group_count=u.shape[-1])
    return bg * y


def dilated_mix(q, k, v, q_g, k_g, pos):
    B, S, H, hd = q.shape
    q = rope(rmsnorm(q, q_g), pos)
    k = rope(rmsnorm(k, k_g), pos)
    outs, lses = [], []
    for window, dil in DIL_CFG:
        o, lse = banded_attention(decimate(q, dil), decimate(k, dil), decimate(v, dil), window // dil)
        outs.append(undecimate(o, dil, B))
        lses.append(undecimate(lse, dil, B))
    wts = jax.nn.softmax(jnp.stack(lses), axis=0)
    o = jnp.einsum('cbsh,cbshd->bshd', wts.astype(q.dtype), jnp.stack(outs))
    return o.reshape(B, S, H * hd)


def selection_overlap(n_c, n_sel):
    r = SEL_BLK // CMP_STRIDE
    m = CMP_LEN // CMP_STRIDE
    diff = np.arange(n_c)[:, None] - r * np.arange(n_sel)[None, :]
    offs = (np.arange(r)[:, None] - np.arange(m)[None, :]).reshape(-1)
    return (diff[..., None] == offs).sum(-1).astype(np.float32)


def nsa_mix(q, kc, vc, ks, vs, kw, vw, gl, q_g, kc_g, ks_g, kw_g, pe_k, pe_v, wk1, wk2, wv1, wv2, pos):
    B, S, H, hd = q.shape
    f32 = jnp.float32
    scale = hd ** -0.5
    q_n = rmsnorm(q, q_g)
    q_r = rope(q_n, pos)
    qidx = np.arange(S)
    n_c = (S - CMP_LEN) // CMP_STRIDE + 1
    cidx = np.arange(n_c)[:, None] * CMP_STRIDE + np.arange(CMP_LEN)[None, :]

    def compress(t, pe, w1, w2):
        blk = (t[:, cidx] + pe).reshape(B, n_c, CMP_LEN * hd)
        return jax.nn.gelu(blk @ w1) @ w2

    k_cmp = rmsnorm(compress(kc, pe_k, wk1, wk2), kc_g)
    v_cmp = compress(vc, pe_v, wv1, wv2)
    s = jnp.einsum('bshd,bcd->bhsc', q_n, k_cmp).astype(f32) * scale
    valid = cidx[:, -1][None, :] <= qidx[:, None]
    s = jnp.where(valid, s, NEG)
    p_cmp = jax.nn.softmax(s, axis=-1) * valid.any(-1, keepdims=True).astype(np.float32)
    o_cmp = jnp.einsum('bhsc,bcd->bshd', p_cmp.astype(v_cmp.dtype), v_cmp)
    n_sel = S // SEL_BLK
    n_top = min(SEL_TOP, n_sel)
    imp = jnp.einsum('bhsc,cj->bsj', p_cmp, selection_overlap(n_c, n_sel))
    cur = qidx[:, None] // SEL_BLK
    jb = np.arange(n_sel)[None, :]
    future = jb > cur
    forced = (jb == 0) | (jb == cur) | (jb == cur - 1)
    score = jnp.where(future, -1.0, jnp.where(forced, FORCE_SCORE, imp))
    _, sel = lax.top_k(score, n_top)
    ks_r = rope(rmsnorm(ks[:, :, None, :], ks_g), pos)[:, :, 0]
    kb = ks_r.reshape(B, n_sel, SEL_BLK, hd)
    vb = vs.reshape(B, n_sel, SEL_BLK, hd)
    nq = S // QBLK
    gather = jax.vmap(lambda t, i: t[i])

    def sel_block(args):
        qc, ic, tc = args
        kg = gather(kb, ic).reshape(B, QBLK, n_top * SEL_BLK, hd)
        vg = gather(vb, ic).reshape(B, QBLK, n_top * SEL_BLK, hd)
        kpos = (ic[..., None] * SEL_BLK + jnp.arange(SEL_BLK)).reshape(B, QBLK, n_top * SEL_BLK)
        sc = jnp.einsum('bqhd,bqjd->bhqj', qc, kg).astype(f32) * scale
        sc = jnp.where((kpos <= tc[None, :, None])[:, None], sc, NEG)
        pc = jax.nn.softmax(sc, axis=-1).astype(vg.dtype)
        return jnp.einsum('bhqj,bqjd->bqhd', pc, vg)

    o_slc = lax.map(sel_block, (jnp.moveaxis(q_r.reshape(B, nq, QBLK, H, hd), 1, 0),
                                jnp.moveaxis(sel.reshape(B, nq, QBLK, n_top), 1, 0),
                                jnp.asarray(qidx.reshape(nq, QBLK))))
    o_slc = jnp.moveaxis(o_slc, 0, 1).reshape(B, S, H, hd)
    kw_r = rope(rmsnorm(kw[:, :, None, :], kw_g), pos)
    o_swa, _ = banded_attention(q_r, kw_r, vw[:, :, None, :], SWA_WIN - 1)
    gt = jax.nn.sigmoid(gl.astype(f32)).reshape(B, S, H, 3).astype(q.dtype)
    o = gt[..., 0:1] * o_cmp + gt[..., 1:2] * o_slc + gt[..., 2:3] * o_swa
    return o.reshape(B, S, H * hd)


def hybrid_mixer(h, positions, w_in, w_out, rwkv_mu, rwkv_w0, rwkv_w2, rwkv_a0, rwkv_a2, rwkv_g2,
                 rwkv_kk, rwkv_ka, rwkv_rk, rwkv_gn_w, rwkv_gn_b, conv_w, dil_q_g, dil_k_g,
                 nsa_q_g, nsa_kc_g, nsa_ks_g, nsa_kw_g, nsa_pe_k, nsa_pe_v, nsa_wk1, nsa_wk2,
                 nsa_wv1, nsa_wv2, onorm_g):
    B, S, _ = h.shape
    proj = h @ w_in
    za, zb, zc, zd = split_cols(proj, SECTION_WIDTHS)
    za = za + (token_shift(za) - za) * rwkv_mu
    r, k, v, wd, ad, gd = split_cols(za, A_WIDTHS)
    y_a = rwkv7_mix(r, k, v, wd, ad, gd, rwkv_w0, rwkv_w2, rwkv_a0, rwkv_a2, rwkv_g2,
                    rwkv_kk, rwkv_ka, rwkv_rk, rwkv_gn_w, rwkv_gn_b)
    bg, cg, xin = split_cols(zb, B_WIDTHS)
    y_b = short_conv_mix(bg, cg, xin, conv_w)
    cq, ck, cv = [t.reshape(B, S, N_HEADS, HEAD_DIM) for t in split_cols(zc, C_WIDTHS)]
    y_c = dilated_mix(cq, ck, cv, dil_q_g, dil_k_g, positions)
    dq, dkc, dvc, dks, dvs, dkw, dvw, dg = split_cols(zd, D_WIDTHS)
    y_d = nsa_mix(dq.reshape(B, S, N_HEADS, HEAD_DIM), dkc, dvc, dks, dvs, dkw, dvw, dg,
                  nsa_q_g, nsa_kc_g, nsa_ks_g, nsa_kw_g, nsa_pe_k, nsa_pe_v,
                  nsa_wk1, nsa_wk2, nsa_wv1, nsa_wv2, positions)
    y_bcd = jnp.concatenate([y_b, y_c, y_d], axis=-1).reshape(B, S, 3 * N_HEADS, HEAD_DIM)
    y_bcd = rmsnorm(y_bcd, onorm_g.reshape(3 * N_HEADS, HEAD_DIM)).reshape(B, S, 3 * GROUP_W)
    return jnp.concatenate([y_a, y_bcd], axis=-1) @ w_out


def moe(h, w_router, b_router, w_gu, b_gu, w_down, b_down):
    B, S, D = h.shape
    T = B * S
    TK = T * TOP_K
    xt = h.reshape(T, D)
    logits = (xt @ w_router + b_router).astype(jnp.float32)
    top_val, top_idx = lax.top_k(logits, TOP_K)
    gates = jax.nn.softmax(top_val, axis=-1)
    e_flat = top_idx.reshape(TK)
    order = jnp.argsort(e_flat)
    e_s = e_flat[order]
    tok_s = order // TOP_K
    g_s = gates.reshape(TK)[order]
    counts = jnp.bincount(e_flat, length=N_EXPERTS)
    padded = (counts + MOE_BLK - 1) // MOE_BLK * MOE_BLK
    pad_end = jnp.cumsum(padded)
    pad_start = pad_end - padded
    start = jnp.cumsum(counts) - counts
    dest = pad_start[e_s] + jnp.arange(TK) - start[e_s]
    n_rows = TK + N_EXPERTS * MOE_BLK
    n_blk = n_rows // MOE_BLK
    buf = jnp.zeros((n_rows, D), h.dtype).at[dest].set(xt[tok_s])
    blk_e = jnp.minimum(jnp.searchsorted(pad_end, jnp.arange(n_blk) * MOE_BLK, side='right'), N_EXPERTS - 1)

    def expert_block(args):
        xb, e = args
        gu = xb @ w_gu[e] + b_gu[e]
        gate, up = gu[:, :EXPERT_FF], gu[:, EXPERT_FF:]
        gate = jnp.minimum(gate, SWIGLU_LIMIT)
        up = jnp.clip(up, -SWIGLU_LIMIT, SWIGLU_LIMIT)
        act = gate * jax.nn.sigmoid(SWIGLU_ALPHA * gate) * (up + 1.0)
        return act @ w_down[e] + b_down[e]

    ybuf = lax.map(expert_block, (buf.reshape(n_blk, MOE_BLK, D), blk_e)).reshape(n_rows, D)
    y = ybuf[dest] * g_s[:, None].astype(h.dtype)
    return jax.ops.segment_sum(y, tok_s, num_segments=T).reshape(B, S, D)


def setup_inputs(seed: int = 0) -> dict:
    key = jax.random.key(seed)
    ks = iter(jax.random.split(key, 64))

    def nrm(shape, scale):
        return scale * jax.random.normal(next(ks), shape, jnp.float32)

    L, H, hd, D, F, E = DEPTH, N_HEADS, HEAD_DIM, D_MODEL, EXPERT_FF, N_EXPERTS
    x = nrm((BATCH, SEQ, D), 1.0)
    c = nrm((BATCH, D), 1.0)
    positions = (jax.random.randint(next(ks), (BATCH, 1), 0, 1024) + jnp.arange(SEQ)[None, :]).astype(jnp.int32)
    return {
        'x': x,
        'c': c,
        'positions': positions,
        'w_ada': nrm((L, D, 6 * D), 0.5 * D ** -0.5),
        'b_ada': nrm((L, 6 * D), 0.01),
        'norm1_g': 1.0 + nrm((L, D), 0.05),
        'norm2_g': 1.0 + nrm((L, D), 0.05),
        'w_in': nrm((L, D, PROJ_W), D ** -0.5),
        'w_out': nrm((L, MIX_W, D), MIX_W ** -0.5),
        'rwkv_mu': jax.random.uniform(next(ks), (L, SECTION_WIDTHS[0]), jnp.float32),
        'rwkv_w0': nrm((L, GROUP_W), 0.5),
        'rwkv_w2': nrm((L, RWKV_DECAY_RANK, GROUP_W), 0.5 * RWKV_DECAY_RANK ** -0.5),
        'rwkv_a0': nrm((L, GROUP_W), 0.5),
        'rwkv_a2': nrm((L, RWKV_AAA_RANK, GROUP_W), 0.5 * RWKV_AAA_RANK ** -0.5),
        'rwkv_g2': nrm((L, RWKV_GATE_RANK, GROUP_W), RWKV_GATE_RANK ** -0.5),
        'rwkv_kk': 0.85 + nrm((L, GROUP_W), 0.05),
        'rwkv_ka': 1.0 + nrm((L, GROUP_W), 0.05),
        'rwkv_rk': nrm((L, H, hd), 0.1),
        'rwkv_gn_w': 1.0 + nrm((L, GROUP_W), 0.05),
        'rwkv_gn_b': nrm((L, GROUP_W), 0.01),
        'conv_w': nrm((L, CONV_W, GROUP_W), CONV_W ** -0.5),
        'dil_q_g': 1.0 + nrm((L, hd), 0.05),
        'dil_k_g': 1.0 + nrm((L, hd), 0.05),
        'nsa_q_g': 1.0 + nrm((L, hd), 0.05),
        'nsa_kc_g': 1.0 + nrm((L, hd), 0.05),
        'nsa_ks_g': 1.0 + nrm((L, hd), 0.05),
        'nsa_kw_g': 1.0 + nrm((L, hd), 0.05),
        'nsa_pe_k': nrm((L, CMP_LEN, hd), 0.1),
        'nsa_pe_v': nrm((L, CMP_LEN, hd), 0.1),
        'nsa_wk1': nrm((L, CMP_LEN * hd, CMP_HIDDEN), (CMP_LEN * hd) ** -0.5),
        'nsa_wk2': nrm((L, CMP_HIDDEN, hd), CMP_HIDDEN ** -0.5),
        'nsa_wv1': nrm((L, CMP_LEN * hd, CMP_HIDDEN), (CMP_LEN * hd) ** -0.5),
        'nsa_wv2': nrm((L, CMP_HIDDEN, hd), CMP_HIDDEN ** -0.5),
        'onorm_g': 1.0 + nrm((L, 3 * GROUP_W), 0.05),
        'w_router': nrm((L, D, E), D ** -0.5),
        'b_router': nrm((L, E), 0.01),
        'w_gu': nrm((L, E, D, 2 * F), D ** -0.5),
        'b_gu': nrm((L, E, 2 * F), 0.01),
        'w_down': nrm((L, E, F, D), F ** -0.5),
        'b_down': nrm((L, E, D), 0.01),
    }


def reference(x, c, positions, w_ada, b_ada, norm1_g, norm2_g, w_in, w_out, rwkv_mu, rwkv_w0,
              rwkv_w2, rwkv_a0, rwkv_a2, rwkv_g2, rwkv_kk, rwkv_ka, rwkv_rk, rwkv_gn_w, rwkv_gn_b,
              conv_w, dil_q_g, dil_k_g, nsa_q_g, nsa_kc_g, nsa_ks_g, nsa_kw_g, nsa_pe_k, nsa_pe_v,
              nsa_wk1, nsa_wk2, nsa_wv1, nsa_wv2, onorm_g, w_router, b_router, w_gu, b_gu,
              w_down, b_down):
    for l in range(DEPTH):
        mod = jax.nn.silu(c) @ w_ada[l] + b_ada[l]
        sh1, sc1, gt1, sh2, sc2, gt2 = [m[:, None, :] for m in jnp.split(mod, 6, axis=-1)]
        h = rmsnorm(x, norm1_g[l]) * (1.0 + sc1) + sh1
        y = hybrid_mixer(h, positions, w_in[l], w_out[l], rwkv_mu[l], rwkv_w0[l], rwkv_w2[l],
                         rwkv_a0[l], rwkv_a2[l], rwkv_g2[l], rwkv_kk[l], rwkv_ka[l], rwkv_rk[l],
                         rwkv_gn_w[l], rwkv_gn_b[l], conv_w[l], dil_q_g[l], dil_k_g[l],
                         nsa_q_g[l], nsa_kc_g[l], nsa_ks_g[l], nsa_kw_g[l], nsa_pe_k[l], nsa_pe_v[l],
                         nsa_wk1[l], nsa_wk2[l], nsa_wv1[l], nsa_wv2[l], onorm_g[l])
        x = x + gt1 * y
        h = rmsnorm(x, norm2_g[l]) * (1.0 + sc2) + sh2
        x = x + gt2 * moe(h, w_router[l], b_router[l], w_gu[l], b_gu[l], w_down[l], b_down[l])
    return x
```

```python
import contextlib
import numpy as np
import ml_dtypes
import concourse.bass as bass
import concourse.mybir as mybir
from concourse.bass_utils import run_bass_kernel_spmd

F32 = mybir.dt.float32
BF16 = mybir.dt.bfloat16
I32 = mybir.dt.int32
AF = mybir.ActivationFunctionType
ALU = mybir.AluOpType
AX = mybir.AxisListType


class KB:
    NDS = 8

    def __init__(self):
        self.nc = bass.Bass("TRN2", target_bir_lowering=False)
        nc = self.nc
        self.eng = {"pe": nc.tensor, "act": nc.scalar, "dve": nc.vector, "pool": nc.gpsimd, "sp": nc.sync}
        self.semh = {}
        for e in self.eng:
            self.semh[e] = nc.semaphore("s_" + e).__enter__()
        self.cnt = {e: 0 for e in self.eng}
        self.waited = {e: {} for e in self.eng}
        self.lastw = {}
        self.readers = {}
        self.dq = {}
        self.nwaits = 0
        self.nops = 0

    def _deps(self, eng, reads, writes):
        deps = {}

        def add(k, v):
            if deps.get(k, 0) < v:
                deps[k] = v

        for r in reads:
            p = self.lastw.get(r)
            if p is not None:
                if not (p[0] == eng and eng == "pe"):
                    add(*p)
            if isinstance(r, str) and r.startswith("ps") and r[2:].isdigit():
                for k, v in self.readers.get(r, {}).items():
                    if k != eng:
                        add(k, v)
        for w in writes:
            p = self.lastw.get(w)
            if p is not None and p[0] != eng:
                add(*p)
            for k, v in self.readers.get(w, {}).items():
                if k != eng:
                    add(k, v)
        return deps

    def _wait(self, eng, deps):
        wt = self.waited[eng]
        for k, v in deps.items():
            if wt.get(k, 0) >= v:
                continue
            self.eng[eng].wait_ge(self.semh[k], v)
            wt[k] = v
            self.nwaits += 1

    def _record(self, prod, reads, writes):
        k, v = prod
        for r in reads:
            d = self.readers.setdefault(r, {})
            if d.get(k, 0) < v:
                d[k] = v
        for w in writes:
            self.lastw[w] = prod
            self.readers[w] = {}

    def op(self, eng, fn, reads=(), writes=()):
        self._wait(eng, self._deps(eng, reads, writes))
        ins = fn(self.eng[eng])
        self.cnt[eng] += 1
        ins.then_inc(self.semh[eng], 1)
        self._record((eng, self.cnt[eng]), reads, writes)
        self.nops += 1
        return ins

    def dma(self, q, out, in_, reads=(), writes=(), **kw):
        st = self.dq.get(q)
        if st is None:
            st = {"n": 0, "sems": []}
            for i in range(self.NDS):
                key = ("dma", q, i)
                self.semh[key] = self.nc.semaphore("d_%s_%d" % (q, i)).__enter__()
                st["sems"].append(key)
            self.dq[q] = st
        i = st["n"]
        slot = i % self.NDS
        val = 16 * (i // self.NDS + 1)
        key = st["sems"][slot]
        deps = self._deps(q, reads, writes)
        if i >= self.NDS:
            if deps.get(key, 0) < val - 16:
                deps[key] = val - 16
        self._wait(q, deps)
        ins = self.eng[q].dma_start(out=out, in_=in_, **kw)
        ins.then_inc(self.semh[key], 16)
        st["n"] += 1
        self._record((key, val), reads, writes)
        return ins

    def barrier(self):
        deps = {}
        for e in self.eng:
            if self.cnt[e] > 0:
                deps[e] = self.cnt[e]
        for q, st in self.dq.items():
            n = st["n"]
            for slot in range(self.NDS):
                if n > slot:
                    cntslot = (n - 1 - slot) // self.NDS + 1
                    deps[st["sems"][slot]] = 16 * cntslot
        for e in self.eng:
            d = {k: v for k, v in deps.items() if k != e or e != "pe"}
            self._wait(e, d)

    def finish(self):
        self.barrier()


S = 4096
D = 1024
NT = 32
PROJ_W = 3212
L_DEPTH = 2
NORM_EPS = 1e-6
PI = float(np.pi)

PARAM_SHAPES = {
    'w_ada': (2, 1024, 6144), 'b_ada': (2, 6144), 'norm1_g': (2, 1024), 'norm2_g': (2, 1024),
    'w_in': (2, 1024, 3212), 'w_out': (2, 1024, 1024), 'rwkv_mu': (2, 1024), 'rwkv_w0': (2, 256),
    'rwkv_w2': (2, 64, 256), 'rwkv_a0': (2, 256), 'rwkv_a2': (2, 64, 256), 'rwkv_g2': (2, 128, 256),
    'rwkv_kk': (2, 256), 'rwkv_ka': (2, 256), 'rwkv_rk': (2, 4, 64), 'rwkv_gn_w': (2, 256), 'rwkv_gn_b': (2, 256),
    'conv_w': (2, 3, 256), 'dil_q_g': (2, 64), 'dil_k_g': (2, 64), 'nsa_q_g': (2, 64), 'nsa_kc_g': (2, 64),
    'nsa_ks_g': (2, 64), 'nsa_kw_g': (2, 64), 'nsa_pe_k': (2, 32, 64), 'nsa_pe_v': (2, 32, 64),
    'nsa_wk1': (2, 2048, 256), 'nsa_wk2': (2, 256, 64), 'nsa_wv1': (2, 2048, 256), 'nsa_wv2': (2, 256, 64),
    'onorm_g': (2, 768), 'w_router': (2, 1024, 32), 'b_router': (2, 32), 'w_gu': (2, 32, 1024, 2048),
    'b_gu': (2, 32, 2048), 'w_down': (2, 32, 1024, 1024), 'b_down': (2, 32, 1024),
}


def host_consts():
    c = {}
    c['ident_f'] = np.eye(128, dtype=np.float32)
    c['ident_b'] = np.eye(128).astype(ml_dtypes.bfloat16)
    c['ones_b'] = np.ones((128, 128)).astype(ml_dtypes.bfloat16)
    blk = np.zeros((128, 128), np.float32)
    blk[:64, :64] = 1.0
    blk[64:, 64:] = 1.0
    c['blk64_b'] = blk.astype(ml_dtypes.bfloat16)
    sel = np.zeros((32, 32, 128), np.float32)
    for e in range(32):
        sel[e, e, :] = 1.0
    c['sel_b'] = sel.astype(ml_dtypes.bfloat16)
    pr = np.zeros((128, 128), np.float32)
    for m in range(128):
        if m % 64 < 32:
            pr[m + 32, m] = -1.0
        else:
            pr[m - 32, m] = 1.0
    c['prot_b'] = pr.astype(ml_dtypes.bfloat16)
    c['invf'] = (10000.0 ** (-(np.arange(128) % 32) / 32.0)).astype(np.float32).reshape(128, 1)
    kl = np.arange(128)[:, None]
    ql = np.arange(512)[None, :]

    def toep(offs, fn):
        return np.stack([fn(128 * o + ql - kl) for o in offs]).astype(np.float32)

    def dil(d):
        m = ((d >= 0) & (d <= 128)).astype(np.float32)
        m += ((d >= 0) & (d % 4 == 0) & (d // 4 <= 128))
        m += ((d >= 0) & (d % 16 == 0) & (d // 16 <= 128))
        return m
    c['dil_mask'] = toep(range(-3, 17), dil).transpose(1, 0, 2).astype(ml_dtypes.bfloat16).copy()
    c['swa_mask'] = toep(range(-3, 5), lambda d: (d >= 0) & (d <= 511)).transpose(1, 0, 2).astype(ml_dtypes.bfloat16).copy()
    c['cau_mask'] = toep(range(-3, 1), lambda d: d >= 0).transpose(1, 0, 2).astype(ml_dtypes.bfloat16).copy()
    c['cmp_mask'] = np.stack([(16 * kl + 31 <= 512 * i + ql) for i in range(5)]).astype(np.float32).transpose(1, 0, 2) \
        .astype(ml_dtypes.bfloat16).copy()
    diff = np.arange(256)[:, None] - 4 * np.arange(64)[None, :]
    offs = (np.arange(4)[:, None] - np.arange(2)[None, :]).reshape(-1)
    ov = (diff[..., None] == offs).sum(-1).astype(np.float32)
    ov[255] = 0
    ov1 = np.concatenate([np.ones((256, 1), np.float32), ov], axis=1)
    ov1[255] = 0
    c['ovl1'] = ov1.reshape(2, 128, 65).transpose(1, 0, 2).astype(ml_dtypes.bfloat16).copy()
    tok = np.arange(S)[:, None]
    jb = np.arange(64)[None, :]
    cur = tok // 64
    fut = jb > cur
    forced = ((jb == 0) | (jb == cur) | (jb == cur - 1)) & ~fut
    keep = ~(fut | forced)
    c['sel_keep'] = keep.astype(np.float32).reshape(32, 128, 64).transpose(1, 0, 2).copy()
    c['sel_add'] = (-1.0 * fut + 1e4 * forced).astype(np.float32).reshape(32, 128, 64).transpose(1, 0, 2).copy()
    ex = np.zeros((64, 32, 128), np.float32)
    for kt in range(32):
        ex[2 * kt, kt, :64] = 1
        ex[2 * kt + 1, kt, 64:] = 1
    c['sel_exp'] = ex.astype(ml_dtypes.bfloat16)
    sam = np.zeros((128, 128), np.float32)
    sam[0:2, 0:64] = 1.0
    sam[2:4, 64:128] = 1.0
    c['sa_mask'] = sam
    hm = np.zeros((128, 4), np.float32)
    hm[0:64, 0] = -1.0
    hm[64:128, 1] = -1.0
    hm[0:64, 2] = 1.0
    hm[64:128, 3] = 1.0
    c['hl_mask'] = hm
    return c


RESIDENT_CONSTS = ('ident_f', 'ident_b', 'ones_b', 'blk64_b', 'sel_b', 'prot_b', 'invf')


class Ctx:
    pass


class LazyW:
    def __init__(self, nc):
        self.nc = nc
        self.d = {}

    def __getitem__(self, n):
        if n not in self.d:
            self.d[n] = self.nc.dram_tensor(n, list(PARAM_SHAPES[n]), F32, kind='ExternalInput').ap()
        return self.d[n]


def build_program(layers=(0, 1), taps=(), first=True, last=True, mixers='ABCD', moe=True):
    k = KB()
    nc = k.nc
    g = Ctx()
    g.k = k
    g.nc = nc
    g.taps = {}
    g.want = set(taps)
    g.mixers = mixers
    g.do_moe = moe
    g.x = nc.dram_tensor('x', [S, D], F32, kind='ExternalInput').ap()
    g.c = nc.dram_tensor('c', [D], F32, kind='ExternalInput').ap()
    g.pos = nc.dram_tensor('positions', [S], I32, kind='ExternalInput').ap()
    g.W = LazyW(nc)
    g.C = {}
    for n, a in host_consts().items():
        dt = F32 if a.dtype == np.float32 else BF16
        g.C[n] = nc.dram_tensor('const_' + n, list(a.shape), dt, kind='ExternalInput').ap()
    g.out = nc.dram_tensor('out', [S, D], F32, kind='ExternalOutput').ap()
    g.xT = [nc.dram_tensor('xT%d' % i, [D, S], F32, kind='Internal').ap() for i in range(2)]
    g.ycatT = nc.dram_tensor('ycatT', [D, S], BF16, kind='Internal').ap()
    g.h2Td = nc.dram_tensor('h2Td', [D, S], BF16, kind='Internal').ap()
    g.hTd = nc.dram_tensor('hTd', [D, S], BF16, kind='Internal').ap()
    g.rwscr = nc.dram_tensor('rwscr', [S, 768], BF16, kind='Internal').ap()

    with contextlib.ExitStack() as es:
        g.layer = 'i'
        g.nsb = 0

        def sb(name, shape, dt, stack=es):
            g.nsb += 1
            return stack.enter_context(nc.sbuf_tensor('%s_%d' % (name, g.nsb), list(shape), dt))
        g.sb = sb
        g.ps = [nc.alloc_psum_tensor('ps%d' % i, [128, 512], F32) for i in range(8)]
        g.psk = ['ps%d' % i for i in range(8)]
        g.cs = {}
        for n, ap in g.C.items():
            if n not in RESIDENT_CONSTS:
                continue
            t = sb('c_' + n, ap.shape, ap.dtype)
            k.dma('sp', t[:], ap, writes=['c_' + n])
            g.cs[n] = t
        g.mod = sb('mod', [128, 48], F32)
        g.GT = sb('GT', [32, S], BF16)
        if first:
            phase_pre(g)
        cur = 0
        for l in layers:
            g.layer = str(l)
            phase_mod(g, l)
            with contextlib.ExitStack() as les:
                phase_norm1(g, l, g.xT[cur])
                g.onorm = g.sb('onorm', [128, 6], F32, les)
                k.dma('sp', g.onorm[:], g.W['onorm_g'][l].rearrange('(j p) -> p j', p=128), writes=['onorm'],
                      allow_slow_non_contiguous=True)
                if 'B' in g.mixers:
                    mixer_conv(g, l)
                if 'C' in g.mixers:
                    mixer_dil(g, l)
                if 'D' in g.mixers:
                    mixer_nsa(g, l)
                if 'A' in g.mixers:
                    mixer_rwkv(g, l)
                zero_missing(g)
                t = tap(g, 'ycatT%d' % l, [D, S], BF16)
                if t is not None:
                    k.barrier()
                    k.dma('sp', t, g.ycatT, writes=['tap'])
                k.barrier()
            phase_wout(g, l, g.xT[cur], g.xT[cur ^ 1])
            phase_norm2_router(g, l, g.xT[cur ^ 1])
            phase_moe(g, l, g.xT[cur ^ 1], g.xT[cur], final=(last and l == layers[-1]))
        k.finish()
    return k, g


def tap(g, name, shape, dt=F32):
    if name not in g.want:
        return None
    t = g.nc.dram_tensor('tap_' + name, list(shape), dt, kind='ExternalOutput').ap()
    g.taps[name] = t
    return t


def phase_pre(g):
    k, nc = g.k, g.nc
    identf = g.cs['ident_f']
    xTd = g.xT[0].rearrange('(c p) t -> p c t', p=128)
    with contextlib.ExitStack() as es:
        xin = [g.sb('pre_x%d' % i, [128, D], F32, es) for i in range(2)]
        xo = [g.sb('pre_o%d' % i, [128, 8, 128], F32, es) for i in range(2)]
        for t in range(NT):
            b = t % 2
            k.dma('sp', xin[b][:], g.x[t * 128:(t + 1) * 128, :], writes=['pre_x%d' % b])
            for h in range(2):
                pi = (2 * t + h) % 4
                for j in range(4):
                    cc = h * 4 + j
                    k.op('pe', lambda e, cc=cc, j=j, pi=pi, b=b: e.transpose(
                        out=g.ps[pi][:, j * 128:(j + 1) * 128], in_=xin[b][:, cc * 128:(cc + 1) * 128], identity=identf[:]),
                        reads=['pre_x%d' % b, 'c_ident_f'], writes=[g.psk[pi]])
                eng = 'act' if h == 0 else 'dve'
                if eng == 'act':
                    k.op('act', lambda e, pi=pi, b=b, h=h: e.copy(
                        out=xo[b][:, h * 4:(h + 1) * 4, :], in_=g.ps[pi][:].rearrange('p (c t) -> p c t', c=4)),
                        reads=[g.psk[pi]], writes=['pre_o%d_%d' % (b, h)])
                else:
                    k.op('dve', lambda e, pi=pi, b=b, h=h: e.tensor_copy(
                        xo[b][:, h * 4:(h + 1) * 4, :], g.ps[pi][:].rearrange('p (c t) -> p c t', c=4)),
                        reads=[g.psk[pi]], writes=['pre_o%d_%d' % (b, h)])
            k.dma('sp', xTd[:, :, t * 128:(t + 1) * 128], xo[b][:], reads=['pre_o%d_0' % b, 'pre_o%d_1' % b],
                  writes=['xT0_%d' % (t // 4)])
        k.barrier()


def phase_mod(g, l):
    k, nc = g.k, g.nc
    with contextlib.ExitStack() as es:
        cT = g.sb('cT', [128, 8], F32, es)
        cs = g.sb('cs', [128, 8], BF16, es)
        bcol = g.sb('bcol', [128, 48], F32, es)
        wt = [g.sb('wada%d' % i, [128, 8, 512], BF16, es) for i in range(2)]
        k.dma('sp', cT[:], g.c.rearrange('(k p) -> p k', p=128), writes=['cT'], allow_slow_non_contiguous=True)
        k.dma('sp', bcol[:], g.W['b_ada'][l].rearrange('(k p) -> p k', p=128), writes=['bcol'], allow_slow_non_contiguous=True)
        k.op('act', lambda e: e.activation(out=cs[:], in_=cT[:], func=AF.Silu), reads=['cT'], writes=['cs'])
        psm = g.ps[7]
        for ct in range(12):
            b = ct % 2
            k.dma('pool', wt[b][:], g.W['w_ada'][l][:, ct * 512:(ct + 1) * 512].rearrange('(k p) c -> p k c', p=128),
                  writes=['wada%d' % b])
            for j in range(4):
                col = ct * 4 + j
                for kk in range(8):
                    k.op('pe', lambda e, b=b, j=j, kk=kk, col=col: e.matmul(
                        psm[:, col:col + 1], lhsT=wt[b][:, kk, j * 128:(j + 1) * 128], rhs=cs[:, kk:kk + 1],
                        start=(kk == 0), stop=(kk == 7)),
                        reads=['wada%d' % b, 'cs'], writes=[g.psk[7]])
        k.op('dve', lambda e: e.tensor_tensor(out=g.mod[:], in0=psm[:, 0:48], in1=bcol[:], op=ALU.add),
             reads=[g.psk[7], 'bcol'], writes=['mod'])
        t = tap(g, 'mod%d' % l, [128, 48])
        if t is not None:
            k.dma('sp', t, g.mod[:], reads=['mod'], writes=['tap'])
        k.barrier()


def phase_norm1(g, l, xTd):
    k, nc = g.k, g.nc
    xTv = xTd.rearrange('(c p) t -> p c t', p=128)
    with contextlib.ExitStack() as es:
        gcol = g.sb('n1_g', [128, 8], F32, es)
        g1s = g.sb('n1_gs', [128, 8], F32, es)
        k.dma('sp', gcol[:], g.W['norm1_g'][l].rearrange('(k p) -> p k', p=128), writes=['n1_g'], allow_slow_non_contiguous=True)
        k.op('dve', lambda e: e.scalar_tensor_tensor(out=g1s[:], in0=g.mod[:, 8:16], scalar=1.0, in1=gcol[:],
                                                      op0=ALU.add, op1=ALU.mult), reads=['mod', 'n1_g'], writes=['n1_gs'])
        hT = g.sb('hT', [128, 8, S], BF16, es)
        norm_tiles(g, es, xTv, g1s, g.mod[:, 0:8], ['n1_gs', 'mod'], hT, 'hT', 'n1')
        hv = g.hTd.rearrange('(c p) t -> p c t', p=128)
        for c in range(8):
            k.dma('sp', hv[:, c, :], hT[:, c, :], reads=['hT%d' % c], writes=['hTd'])
        t = tap(g, 'h%d' % l, [128, 8, S], BF16)
        if t is not None:
            k.dma('sp', t, hT[:], reads=['hT%d' % i for i in range(8)], writes=['tap'])
        k.barrier()


def norm_tiles(g, es, xTv, gs, sh, gkeys, hT, hkey, pfx, xsrc_keys=None):
    k = g.k
    xt = [g.sb(pfx + '_x%d' % i, [128, 8, 512], F32, es) for i in range(2)]
    sq = [g.sb(pfx + '_sq%d' % i, [128, 8, 512], BF16, es) for i in range(2)]
    rstd = [g.sb(pfx + '_rstd%d' % i, [128, 512], F32, es) for i in range(2)]
    tmp = [g.sb(pfx + '_tmp%d' % i, [128, 512], F32, es) for i in range(2)]
    ones = g.cs['ones_b']
    for tt in range(8):
        b = tt % 2
        ts = slice(tt * 512, (tt + 1) * 512)
        xk, sk, rk = pfx + '_x%d' % b, pfx + '_sq%d' % b, pfx + '_rstd%d' % b
        k.dma('sp', xt[b][:], xTv[:, :, ts], reads=(xsrc_keys or []), writes=[xk])
        k.op('act', lambda e, b=b: e.activation(out=sq[b][:], in_=xt[b][:], func=AF.Square), reads=[xk], writes=[sk])
        pi = tt % 2
        for c in range(8):
            k.op('pe', lambda e, b=b, c=c, pi=pi: e.matmul(g.ps[pi][:], lhsT=ones[:], rhs=sq[b][:, c, :],
                                                          start=(c == 0), stop=(c == 7)),
                 reads=[sk, 'c_ones_b'], writes=[g.psk[pi]])
        k.op('dve', lambda e, b=b, pi=pi: e.tensor_scalar(out=rstd[b][:], in0=g.ps[pi][:], scalar1=1.0 / D, scalar2=NORM_EPS,
                                                         op0=ALU.mult, op1=ALU.add), reads=[g.psk[pi]], writes=[rk])
        k.op('act', lambda e, b=b: e.activation(out=rstd[b][:], in_=rstd[b][:], func=AF.Sqrt), reads=[rk], writes=[rk])
        k.op('dve', lambda e, b=b: e.reciprocal(out=rstd[b][:], in_=rstd[b][:]), reads=[rk], writes=[rk])
        for c in range(8):
            tb = c % 2
            tk = pfx + '_tmp%d' % tb
            k.op('dve', lambda e, b=b, c=c, tb=tb: e.tensor_tensor(out=tmp[tb][:], in0=xt[b][:, c, :], in1=rstd[b][:], op=ALU.mult),
                 reads=[xk, rk], writes=[tk])
            k.op('act', lambda e, c=c, tb=tb, ts=ts: e.activation(out=hT[:, c, ts], in_=tmp[tb][:], func=AF.Identity,
                                                                  scale=gs[:, c:c + 1], bias=sh[:, c:c + 1]),
                 reads=[tk] + gkeys, writes=[hkey + '%d' % c])


def make_in_maps(inputs, g, n_cores=8):
    consts = host_consts()
    maps = []
    shared = {n: np.ascontiguousarray(inputs[n], dtype=np.float32) for n in g.W.d}
    for b in range(n_cores):
        m = {'x': np.ascontiguousarray(inputs['x'][b]), 'c': np.ascontiguousarray(inputs['c'][b]),
             'positions': np.ascontiguousarray(inputs['positions'][b]).astype(np.int32)}
        m.update(shared)
        for n, a in consts.items():
            m['const_' + n] = a
        maps.append(m)
    return maps


def kernel(**inputs):
    k, g = build_program()
    maps = make_in_maps(inputs, g)
    res = run_bass_kernel_spmd(k.nc, maps, core_ids=list(range(8)))
    return np.stack([r['out'] for r in res.results], axis=0)


def load_w_bf16(g, dst, dkey, src_ap):
    g.k.dma('pool', dst, src_ap.rearrange('(c p) n -> p c n', p=128), writes=[dkey])


def ht_loader(g, es, pfx):
    bufs = [g.sb(pfx + '_ht%d' % i, [128, 8, 512], BF16, es) for i in range(2)]
    hv = g.hTd.rearrange('(c p) t -> p c t', p=128)

    def load(tt):
        b = tt % 2
        g.k.dma('sp', bufs[b][:], hv[:, :, tt * 512:(tt + 1) * 512], reads=['hTd'], writes=[pfx + '_ht%d' % b])
        return bufs[b], pfx + '_ht%d' % b
    return load


def proj_fm(g, wsb, wkey, c0, ncols, ht, hkey, ps, pskey):
    for c in range(8):
        g.k.op('pe', lambda e, c=c: e.matmul(ps, lhsT=wsb[:, c, c0:c0 + ncols], rhs=ht[:, c, :],
                                             start=(c == 0), stop=(c == 7)),
               reads=[wkey, hkey], writes=[pskey])


def proj_tm(g, wsb, wkey, c0, ncols, ht, hkey, sub, ps, pskey):
    for c in range(8):
        g.k.op('pe', lambda e, c=c: e.matmul(ps, lhsT=ht[:, c, sub * 128:(sub + 1) * 128], rhs=wsb[:, c, c0:c0 + ncols],
                                             start=(c == 0), stop=(c == 7)),
               reads=[wkey, hkey], writes=[pskey])


def head_rmsnorm_fm(g, y, ykey, sq, sqkey, gcol, gkeys, out_ap, okey, pi, tmpf, tmpkey):
    k = g.k
    k.op('act', lambda e: e.activation(out=sq, in_=y, func=AF.Square), reads=[ykey], writes=[sqkey])
    k.op('pe', lambda e: e.matmul(g.ps[pi][:], lhsT=g.cs['blk64_b'][:], rhs=sq, start=True, stop=True),
         reads=[sqkey, 'c_blk64_b'], writes=[g.psk[pi]])
    k.op('dve', lambda e: e.tensor_scalar(out=tmpf, in0=g.ps[pi][:], scalar1=1.0 / 64, scalar2=NORM_EPS, op0=ALU.mult, op1=ALU.add),
         reads=[g.psk[pi]], writes=[tmpkey])
    k.op('act', lambda e: e.activation(out=tmpf, in_=tmpf, func=AF.Sqrt), reads=[tmpkey], writes=[tmpkey])
    k.op('dve', lambda e: e.reciprocal(out=tmpf, in_=tmpf), reads=[tmpkey], writes=[tmpkey])
    k.op('dve', lambda e: e.scalar_tensor_tensor(out=out_ap, in0=y, scalar=gcol, in1=tmpf, op0=ALU.mult, op1=ALU.mult),
         reads=[ykey, tmpkey] + gkeys, writes=[okey])


def mixer_conv(g, l):
    k = g.k
    ycv = g.ycatT.rearrange('(c p) t -> p c t', p=128)
    with contextlib.ExitStack() as es:
        wb = g.sb('cv_w', [128, 8, 768], BF16, es)
        load_w_bf16(g, wb[:], 'cv_w', g.W['w_in'][l][:, 1024:1792])
        cw = g.sb('cv_cw', [128, 2, 3], F32, es)
        for kk in range(3):
            k.dma('sp', cw[:, :, kk], g.W['conv_w'][l, kk].rearrange('(j p) -> p j', p=128), writes=['cv_cw'],
                  allow_slow_non_contiguous=True)
        bg = g.sb('cv_bg', [128, 2, S], BF16, es)
        u = g.sb('cv_u', [128, 2, S + 2], F32, es)
        cgt = [g.sb('cv_cg%d' % i, [128, 512], F32, es) for i in range(2)]
        k.op('pool', lambda e: e.memset(u[:, :, 0:2], 0.0), writes=['cv_u_h'])
        n = 0
        hload = ht_loader(g, es, 'cv')
        for tt in range(8):
            ts = slice(tt * 512, (tt + 1) * 512)
            ht, hk = hload(tt)
            for j in range(2):
                pa, pb, pc = n % 6, (n + 1) % 6, (n + 2) % 6
                n += 3
                proj_fm(g, wb, 'cv_w', j * 128, 128, ht, hk, g.ps[pa][:], g.psk[pa])
                proj_fm(g, wb, 'cv_w', 256 + j * 128, 128, ht, hk, g.ps[pb][:], g.psk[pb])
                proj_fm(g, wb, 'cv_w', 512 + j * 128, 128, ht, hk, g.ps[pc][:], g.psk[pc])
                k.op('act', lambda e, j=j, ts=ts, pa=pa: e.copy(out=bg[:, j, ts], in_=g.ps[pa][:]), reads=[g.psk[pa]],
                     writes=['cv_bg%d_%d' % (j, tt)])
                cb = n % 2
                k.op('act', lambda e, cb=cb, pb=pb: e.copy(out=cgt[cb][:], in_=g.ps[pb][:]), reads=[g.psk[pb]], writes=['cv_cg%d' % cb])
                k.op('dve', lambda e, j=j, tt=tt, cb=cb, pc=pc: e.tensor_tensor(out=u[:, j, 2 + tt * 512:2 + (tt + 1) * 512],
                                                                                 in0=g.ps[pc][:], in1=cgt[cb][:], op=ALU.mult),
                     reads=[g.psk[pc], 'cv_cg%d' % cb], writes=['cv_u%d_%d' % (j, tt)])
        y = [g.sb('cv_y%d' % i, [128, 512], F32, es) for i in range(2)]
        sq = [g.sb('cv_sq%d' % i, [128, 512], BF16, es) for i in range(2)]
        tf = [g.sb('cv_tf%d' % i, [128, 512], F32, es) for i in range(2)]
        yo = [g.sb('cv_yo%d' % i, [128, 2, 512], BF16, es) for i in range(2)]
        n = 0
        for tt in range(8):
            ts = slice(tt * 512, (tt + 1) * 512)
            ob = tt % 2
            for j in range(2):
                b = n % 2
                n += 1
                yk = 'cv_y%d' % b
                ukeys = ['cv_u%d_%d' % (j, tt), 'cv_u_h'] + (['cv_u%d_%d' % (j, tt - 1)] if tt > 0 else [])
                k.op('act', lambda e, b=b, j=j, tt=tt: e.activation(out=y[b][:], in_=u[:, j, 2 + tt * 512:2 + (tt + 1) * 512],
                                                                   func=AF.Copy, scale=cw[:, j, 2:3]),
                     reads=ukeys + ['cv_cw'], writes=[yk])
                for kk in (1, 0):
                    k.op('dve', lambda e, b=b, j=j, tt=tt, kk=kk: e.scalar_tensor_tensor(
                        out=y[b][:], in0=u[:, j, kk + tt * 512:kk + (tt + 1) * 512], scalar=cw[:, j, kk:kk + 1], in1=y[b][:],
                        op0=ALU.mult, op1=ALU.add), reads=ukeys + ['cv_cw', yk], writes=[yk])
                k.op('dve', lambda e, b=b, j=j, ts=ts: e.tensor_tensor(out=y[b][:], in0=y[b][:], in1=bg[:, j, ts], op=ALU.mult),
                     reads=[yk, 'cv_bg%d_%d' % (j, tt)], writes=[yk])
                head_rmsnorm_fm(g, y[b][:], yk, sq[b][:], 'cv_sq%d' % b, g.onorm[:, j:j + 1], ['onorm'], yo[ob][:, j, :],
                                'cv_yo%d_%d' % (ob, j), 6 + b, tf[b][:], 'cv_tf%d' % b)
            k.dma('sp', ycv[:, 2:4, ts], yo[ob][:], reads=['cv_yo%d_0' % ob, 'cv_yo%d_1' % ob], writes=['ycat_B%d' % tt])
        k.barrier()


def phase_wout(g, l, xT_old, xT_new):
    k = g.k
    ycv = g.ycatT.rearrange('(c p) t -> p c t', p=128)
    xo = xT_old.rearrange('(c p) t -> p c t', p=128)
    xn = xT_new.rearrange('(c p) t -> p c t', p=128)
    with contextlib.ExitStack() as es:
        wo = g.sb('wo_w', [128, 8, D], BF16, es)
        load_w_bf16(g, wo[:], 'wo_w', g.W['w_out'][l])
        yc = [g.sb('wo_yc%d' % i, [128, 8, 512], BF16, es) for i in range(2)]
        xt = [g.sb('wo_x%d' % i, [128, 8, 512], F32, es) for i in range(2)]
        n = 0
        for tt in range(8):
            b = tt % 2
            ts = slice(tt * 512, (tt + 1) * 512)
            k.dma('sp', yc[b][:], ycv[:, :, ts], writes=['wo_yc%d' % b])
            k.dma('sp', xt[b][:], xo[:, :, ts], writes=['wo_x%d' % b])
            for dc in range(8):
                pi = n % 4
                n += 1
                for c in range(8):
                    k.op('pe', lambda e, b=b, c=c, dc=dc, pi=pi: e.matmul(g.ps[pi][:], lhsT=wo[:, c, dc * 128:(dc + 1) * 128],
                                                                         rhs=yc[b][:, c, :], start=(c == 0), stop=(c == 7)),
                         reads=['wo_w', 'wo_yc%d' % b], writes=[g.psk[pi]])
                k.op('dve', lambda e, b=b, dc=dc, pi=pi: e.scalar_tensor_tensor(
                    out=xt[b][:, dc, :], in0=g.ps[pi][:], scalar=g.mod[:, 16 + dc:17 + dc], in1=xt[b][:, dc, :],
                    op0=ALU.mult, op1=ALU.add), reads=[g.psk[pi], 'mod', 'wo_x%d' % b], writes=['wo_x%d' % b])
            k.dma('sp', xn[:, :, ts], xt[b][:], reads=['wo_x%d' % b], writes=['xTn_%d' % tt])
        t = tap(g, 'x1T%d' % l, [128, 8, S])
        if t is not None:
            k.barrier()
            k.dma('sp', t, xn, writes=['tap'])
        k.barrier()


def phase_norm2_router(g, l, xT1):
    k = g.k
    xv = xT1.rearrange('(c p) t -> p c t', p=128)
    h2v = g.h2Td.rearrange('(c p) t -> p c t', p=128)
    with contextlib.ExitStack() as es:
        gcol = g.sb('n2_g', [128, 8], F32, es)
        g2s = g.sb('n2_gs', [128, 8], F32, es)
        k.dma('sp', gcol[:], g.W['norm2_g'][l].rearrange('(k p) -> p k', p=128), writes=['n2_g'], allow_slow_non_contiguous=True)
        k.op('dve', lambda e: e.scalar_tensor_tensor(out=g2s[:], in0=g.mod[:, 32:40], scalar=1.0, in1=gcol[:],
                                                      op0=ALU.add, op1=ALU.mult), reads=['mod', 'n2_g'], writes=['n2_gs'])
        h2 = g.sb('n2_h', [128, 8, S], BF16, es)
        norm_tiles(g, es, xv, g2s, g.mod[:, 24:32], ['n2_gs', 'mod'], h2, 'n2_h', 'n2')
        for c in range(8):
            k.dma('sp', h2v[:, c, :], h2[:, c, :], reads=['n2_h%d' % c], writes=['h2Td%d' % c])
        t = tap(g, 'h2T%d' % l, [128, 8, S], BF16)
        if t is not None:
            k.dma('sp', t, h2[:], reads=['n2_h%d' % c for c in range(8)], writes=['tap'])
        wr = g.sb('rt_w', [128, 8, 32], BF16, es)
        load_w_bf16(g, wr[:], 'rt_w', g.W['w_router'][l])
        br = g.sb('rt_b', [1, 32], BF16, es)
        k.dma('pool', br[:], g.W['b_router'][l:l + 1, :], writes=['rt_b'])
        lg = [g.sb('rt_lg%d' % i, [128, 32], F32, es) for i in range(2)]
        m8 = [g.sb('rt_m8%d' % i, [128, 8], F32, es) for i in range(2)]
        nm = [g.sb('rt_nm%d' % i, [128, 1], F32, es) for i in range(2)]
        ex = [g.sb('rt_ex%d' % i, [128, 32], F32, es) for i in range(2)]
        mk = [g.sb('rt_mk%d' % i, [128, 32], F32, es) for i in range(2)]
        sm = [g.sb('rt_sm%d' % i, [128, 1], F32, es) for i in range(2)]
        Gt = [g.sb('rt_G%d' % i, [128, 32], F32, es) for i in range(2)]
        gtap = tap(g, 'G%d' % l, [S, 32])
        for t_ in range(NT):
            b = t_ % 2
            pi = t_ % 2
            tk = slice(t_ * 128, (t_ + 1) * 128)
            for c in range(8):
                k.op('pe', lambda e, c=c, tk=tk, pi=pi: e.matmul(g.ps[pi][:, 0:32], lhsT=h2[:, c, tk], rhs=wr[:, c, :],
                                                               start=(c == 0), stop=False),
                     reads=['n2_h%d' % c, 'rt_w'], writes=[g.psk[pi]])
            k.op('pe', lambda e, pi=pi: e.matmul(g.ps[pi][:, 0:32], lhsT=g.cs['ones_b'][0:1, :], rhs=br[:], start=False, stop=True),
                 reads=['c_ones_b', 'rt_b'], writes=[g.psk[pi]])
            s = str(b)
            k.op('act', lambda e, b=b, pi=pi: e.copy(out=lg[b][:], in_=g.ps[pi][:, 0:32]), reads=[g.psk[pi]], writes=['rt_lg' + s])
            k.op('dve', lambda e, b=b: e.max(out=m8[b][:], in_=lg[b][:]), reads=['rt_lg' + s], writes=['rt_m8' + s])
            k.op('dve', lambda e, b=b: e.tensor_scalar(out=nm[b][:], in0=m8[b][:, 0:1], scalar1=-1.0, scalar2=None, op0=ALU.mult),
                 reads=['rt_m8' + s], writes=['rt_nm' + s])
            k.op('act', lambda e, b=b: e.activation(out=ex[b][:], in_=lg[b][:], func=AF.Exp, bias=nm[b][:], scale=1.0),
                 reads=['rt_lg' + s, 'rt_nm' + s], writes=['rt_ex' + s])
            k.op('dve', lambda e, b=b: e.tensor_scalar(out=mk[b][:], in0=lg[b][:], scalar1=m8[b][:, 3:4], scalar2=None, op0=ALU.is_ge),
                 reads=['rt_lg' + s, 'rt_m8' + s], writes=['rt_mk' + s])
            k.op('dve', lambda e, b=b: e.tensor_tensor(out=ex[b][:], in0=ex[b][:], in1=mk[b][:], op=ALU.mult),
                 reads=['rt_ex' + s, 'rt_mk' + s], writes=['rt_ex' + s])
            k.op('dve', lambda e, b=b: e.reduce_sum(out=sm[b][:], in_=ex[b][:], axis=AX.X), reads=['rt_ex' + s], writes=['rt_sm' + s])
            k.op('dve', lambda e, b=b: e.reciprocal(out=sm[b][:], in_=sm[b][:]), reads=['rt_sm' + s], writes=['rt_sm' + s])
            k.op('dve', lambda e, b=b: e.tensor_scalar(out=Gt[b][:], in0=ex[b][:], scalar1=sm[b][:], scalar2=None, op0=ALU.mult),
                 reads=['rt_ex' + s, 'rt_sm' + s], writes=['rt_G' + s])
            if gtap is not None:
                k.dma('sp', gtap[tk, :], Gt[b][:], reads=['rt_G' + s], writes=['tap'])
            pj = 2 + t_ % 2
            k.op('pe', lambda e, b=b, pj=pj: e.transpose(out=g.ps[pj][0:32, 0:128], in_=Gt[b][:], identity=g.cs['ident_f'][:]),
                 reads=['rt_G' + s, 'c_ident_f'], writes=[g.psk[pj]])
            k.op('act', lambda e, tk=tk, pj=pj: e.copy(out=g.GT[:, tk], in_=g.ps[pj][0:32, 0:128]), reads=[g.psk[pj]],
                 writes=['GT%d' % (t_ // 4)])
        k.barrier()


def phase_moe(g, l, xT1, xT2, final):
    k = g.k
    x1v = xT1.rearrange('(c p) t -> p c t', p=128)
    x2v = xT2.rearrange('(c p) t -> p c t', p=128)
    h2v = g.h2Td.rearrange('(c p) t -> p c t', p=128)
    NE = 32 if g.do_moe else 0
    QT = 1024
    with contextlib.ExitStack() as es:
        bgc = g.sb('me_bgc', [128, 16, 32], F32, es)
        bdr = g.sb('me_bdr', [32, D], BF16, es)
        k.dma('pool', bdr[:], g.W['b_down'][l], writes=['me_bdr'])
        with contextlib.ExitStack() as es2:
            bgr = g.sb('me_bgr', [32, 2048], F32, es2)
            k.dma('sp', bgr[:], g.W['b_gu'][l], writes=['me_bgr'])
            for j in range(16):
                pi = j % 2
                k.op('pe', lambda e, j=j, pi=pi: e.transpose(out=g.ps[pi][:, 0:32], in_=bgr[:, j * 128:(j + 1) * 128],
                                                             identity=g.cs['ident_f'][0:32, 0:32]),
                     reads=['me_bgr', 'c_ident_f'], writes=[g.psk[pi]])
                k.op('act', lambda e, j=j, pi=pi: e.copy(out=bgc[:, j, :], in_=g.ps[pi][:, 0:32]), reads=[g.psk[pi]], writes=['me_bgc'])
            k.barrier()
        wgu = [g.sb('me_wgu%d' % i, [128, 8, 2048], BF16, es) for i in range(2)]
        wdn = [g.sb('me_wdn%d' % i, [128, 8, D], BF16, es) for i in range(2)]
        h2q = g.sb('me_h2', [128, 8, QT], BF16, es)
        yacc = g.sb('me_yacc', [128, 8, QT], F32, es)
        act = [g.sb('me_act%d' % i, [128, 8, 512], BF16, es) for i in range(2)]
        gbc = [g.sb('me_gbc%d' % i, [128, 512], BF16, es) for i in range(2)]
        gq = [g.sb('me_gq%d' % i, [128, 512], F32, es) for i in range(2)]
        sg = [g.sb('me_sg%d' % i, [128, 512], F32, es) for i in range(2)]
        uq = [g.sb('me_uq%d' % i, [128, 512], F32, es) for i in range(2)]
        xrow = [g.sb('me_xr%d' % i, [128, D], F32, es) for i in range(2)] if final else None
        nw = 0
        nit = 0
        npsum = 0
        for q in range(S // QT):
            qs = slice(q * QT, (q + 1) * QT)
            k.dma('sp', h2q[:], h2v[:, :, qs], reads=['h2Td%d' % c for c in range(8)], writes=['me_h2'])
            for e_ in range(NE):
                wb = nw % 2
                nw += 1
                gk, dk = 'me_wgu%d' % wb, 'me_wdn%d' % wb
                load_w_bf16(g, wgu[wb][:], gk, g.W['w_gu'][l, e_])
                load_w_bf16(g, wdn[wb][:], dk, g.W['w_down'][l, e_])
                for tl in range(QT // 512):
                    ts = slice(tl * 512, (tl + 1) * 512)
                    gts = slice(q * QT + tl * 512, q * QT + (tl + 1) * 512)
                    ab = nit % 2
                    nit += 1
                    ak = 'me_act%d' % ab
                    pg = 6 + ab
                    k.op('pe', lambda e, e_=e_, gts=gts, pg=pg: e.matmul(g.ps[pg][:], lhsT=g.cs['sel_b'][:, e_, :], rhs=g.GT[:, gts],
                                                                        start=True, stop=True),
                         reads=['c_sel_b'] + ['GT%d' % i for i in range(8)], writes=[g.psk[pg]])
                    k.op('act', lambda e, ab=ab, pg=pg: e.copy(out=gbc[ab][:], in_=g.ps[pg][:]), reads=[g.psk[pg]], writes=['me_gbc%d' % ab])
                    for f in range(8):
                        pa, pb = (2 * f) % 4, (2 * f + 1) % 4
                        fb = f % 2
                        for c in range(8):
                            k.op('pe', lambda e, c=c, f=f, wb=wb, ts=ts, pa=pa: e.matmul(
                                g.ps[pa][:], lhsT=wgu[wb][:, c, f * 128:(f + 1) * 128], rhs=h2q[:, c, ts], start=(c == 0), stop=(c == 7)),
                                reads=[gk, 'me_h2'], writes=[g.psk[pa]])
                        for c in range(8):
                            k.op('pe', lambda e, c=c, f=f, wb=wb, ts=ts, pb=pb: e.matmul(
                                g.ps[pb][:], lhsT=wgu[wb][:, c, 1024 + f * 128:1024 + (f + 1) * 128], rhs=h2q[:, c, ts],
                                start=(c == 0), stop=(c == 7)), reads=[gk, 'me_h2'], writes=[g.psk[pb]])
                        fs = str(fb)
                        k.op('dve', lambda e, f=f, fb=fb, pa=pa, e_=e_: e.tensor_scalar(
                            out=gq[fb][:], in0=g.ps[pa][:], scalar1=bgc[:, f, e_:e_ + 1], scalar2=7.0, op0=ALU.add, op1=ALU.min),
                            reads=[g.psk[pa], 'me_bgc'], writes=['me_gq' + fs])
                        k.op('act', lambda e, fb=fb: e.activation(out=sg[fb][:], in_=gq[fb][:], func=AF.Sigmoid, scale=1.702),
                             reads=['me_gq' + fs], writes=['me_sg' + fs])
                        k.op('dve', lambda e, f=f, fb=fb, pb=pb, e_=e_: e.tensor_scalar(
                            out=uq[fb][:], in0=g.ps[pb][:], scalar1=bgc[:, 8 + f, e_:e_ + 1], scalar2=-7.0, op0=ALU.add, op1=ALU.max),
                            reads=[g.psk[pb], 'me_bgc'], writes=['me_uq' + fs])
                        k.op('pool', lambda e, fb=fb: e.tensor_scalar(out=uq[fb][:], in0=uq[fb][:], scalar1=7.0, scalar2=1.0,
                                                                      op0=ALU.min, op1=ALU.add), reads=['me_uq' + fs], writes=['me_uq' + fs])
                        k.op('pool', lambda e, fb=fb: e.tensor_tensor(out=sg[fb][:], in0=sg[fb][:], in1=gq[fb][:], op=ALU.mult),
                             reads=['me_sg' + fs, 'me_gq' + fs], writes=['me_sg' + fs])
                        k.op('pool', lambda e, fb=fb: e.tensor_tensor(out=sg[fb][:], in0=sg[fb][:], in1=uq[fb][:], op=ALU.mult),
                             reads=['me_sg' + fs, 'me_uq' + fs], writes=['me_sg' + fs])
                        k.op('dve', lambda e, f=f, fb=fb, ab=ab: e.tensor_tensor(out=act[ab][:, f, :], in0=sg[fb][:], in1=gbc[ab][:], op=ALU.mult),
                             reads=['me_sg' + fs, 'me_gbc%d' % ab], writes=[ak])
                    for dc in range(8):
                        pd = 4 + dc % 2
                        for f in range(8):
                            k.op('pe', lambda e, f=f, dc=dc, wb=wb, ab=ab, pd=pd: e.matmul(
                                g.ps[pd][:], lhsT=wdn[wb][:, f, dc * 128:(dc + 1) * 128], rhs=act[ab][:, f, :],
                                start=(f == 0), stop=(f == 7 and e_ != 0)), reads=[dk, ak], writes=[g.psk[pd]])
                        if e_ == 0:
                            k.op('pe', lambda e, dc=dc, gts=gts, pd=pd: e.matmul(
                                g.ps[pd][:], lhsT=bdr[:, dc * 128:(dc + 1) * 128], rhs=g.GT[:, gts], start=False, stop=True),
                                reads=['me_bdr'] + ['GT%d' % i for i in range(8)], writes=[g.psk[pd]])
                            k.op('dve', lambda e, dc=dc, ts=ts, pd=pd: e.tensor_copy(yacc[:, dc, ts], g.ps[pd][:]),
                                 reads=[g.psk[pd]], writes=['me_yacc'])
                        else:
                            k.op('dve', lambda e, dc=dc, ts=ts, pd=pd: e.tensor_tensor(out=yacc[:, dc, ts], in0=yacc[:, dc, ts],
                                                                                    in1=g.ps[pd][:], op=ALU.add),
                                 reads=[g.psk[pd], 'me_yacc'], writes=['me_yacc'])
            if NE == 0:
                k.op('pool', lambda e: e.memset(yacc[:], 0.0), writes=['me_yacc'])
            for c in range(8):
                for tl in range(QT // 512):
                    ts = slice(tl * 512, (tl + 1) * 512)
                    gts = slice(q * QT + tl * 512, q * QT + (tl + 1) * 512)
                    fb = (c * 2 + tl) % 2
                    k.dma('sp', gq[fb][:], x1v[:, c, gts], writes=['me_gq%d' % fb])
                    k.op('dve', lambda e, c=c, ts=ts, fb=fb: e.scalar_tensor_tensor(
                        out=yacc[:, c, ts], in0=yacc[:, c, ts], scalar=g.mod[:, 40 + c:41 + c], in1=gq[fb][:], op0=ALU.mult, op1=ALU.add),
                        reads=['me_yacc', 'mod', 'me_gq%d' % fb], writes=['me_yacc'])
            if not final:
                k.dma('sp', x2v[:, :, qs], yacc[:], reads=['me_yacc'], writes=['xT2_%d' % q])
            else:
                for t_ in range(QT // 128):
                    tg = q * (QT // 128) + t_
                    ob = t_ % 2
                    for h in range(2):
                        pi = (2 * t_ + h) % 4
                        for j in range(4):
                            cc = h * 4 + j
                            k.op('pe', lambda e, cc=cc, j=j, pi=pi, t_=t_: e.transpose(
                                out=g.ps[pi][:, j * 128:(j + 1) * 128], in_=yacc[:, cc, t_ * 128:(t_ + 1) * 128],
                                identity=g.cs['ident_f'][:]), reads=['me_yacc', 'c_ident_f'], writes=[g.psk[pi]])
                        k.op('act' if h == 0 else 'dve',
                             (lambda e, pi=pi, ob=ob, h=h: e.copy(out=xrow[ob][:, h * 512:(h + 1) * 512], in_=g.ps[pi][:])) if h == 0 else
                             (lambda e, pi=pi, ob=ob, h=h: e.tensor_copy(xrow[ob][:, h * 512:(h + 1) * 512], g.ps[pi][:])),
                             reads=[g.psk[pi]], writes=['me_xr%d_%d' % (ob, h)])
                    k.dma('sp', g.out[tg * 128:(tg + 1) * 128, :], xrow[ob][:], reads=['me_xr%d_0' % ob, 'me_xr%d_1' % ob], writes=['out'])
            tp = tap(g, 'x2T%d_q%d' % (l, q), [128, 8, QT])
            if tp is not None:
                k.dma('sp', tp, yacc[:], reads=['me_yacc'], writes=['tap'])
        k.barrier()


def zero_missing(g):
    k = g.k
    miss = [i for i, m in enumerate('ABCD') if m not in g.mixers]
    if not miss:
        return
    with contextlib.ExitStack() as es:
        z = g.sb('zz', [128, S], BF16, es)
        k.op('pool', lambda e: e.memset(z[:], 0.0), writes=['zz'])
        for i in miss:
            for j in range(2):
                r0 = i * 256 + j * 128
                k.dma('sp', g.ycatT[r0:r0 + 128, :], z[:], reads=['zz'], writes=['ycat_z'])
        k.barrier()


def load_const(g, es, name):
    ap = g.C[name]
    t = g.sb('c_' + name, ap.shape, ap.dtype, es)
    g.k.dma('sp', t[:], ap, writes=['c_' + name])
    return t


def rope_tables(g, es):
    k = g.k
    cos = g.sb('rp_cos', [128, S], F32, es)
    sin = g.sb('rp_sin', [128, S], F32, es)
    invf = g.cs['invf']
    CH = 1024
    with contextlib.ExitStack() as es2:
        posi = g.sb('rp_pi', [128, CH], I32, es2)
        ang = g.sb('rp_ang', [128, CH], F32, es2)
        tq = g.sb('rp_t', [128, CH], F32, es2)
        ti = g.sb('rp_ti', [128, CH], I32, es2)
        r = g.sb('rp_r', [128, CH], F32, es2)
        m = g.sb('rp_m', [128, CH], F32, es2)
        for ch in range(S // CH):
            cs_ = slice(ch * CH, (ch + 1) * CH)
            k.dma('sp', posi[:], g.pos[cs_].partition_broadcast(128), writes=['rp_pi'])
            k.op('dve', lambda e: e.tensor_copy(ang[:], posi[:]), reads=['rp_pi'], writes=['rp_ang'])
            k.op('dve', lambda e: e.tensor_scalar(out=ang[:], in0=ang[:], scalar1=invf[:, 0:1], scalar2=None, op0=ALU.mult),
                 reads=['rp_ang', 'c_invf'], writes=['rp_ang'])
            for which, dst, dkey in ((0, sin, 'rp_sin'), (1, cos, 'rp_cos')):
                shift = 0.0 if which == 0 else PI / 2
                k.op('dve', lambda e, shift=shift: e.tensor_scalar(out=tq[:], in0=ang[:], scalar1=shift, scalar2=1.0 / (2 * PI),
                                                                    op0=ALU.add, op1=ALU.mult), reads=['rp_ang'], writes=['rp_t'])
                k.op('dve', lambda e: e.tensor_copy(ti[:], tq[:]), reads=['rp_t'], writes=['rp_ti'])
                k.op('dve', lambda e: e.tensor_copy(tq[:], ti[:]), reads=['rp_ti'], writes=['rp_t'])
                k.op('dve', lambda e: e.scalar_tensor_tensor(out=r[:], in0=tq[:], scalar=-2 * PI, in1=ang[:], op0=ALU.mult, op1=ALU.add),
                     reads=['rp_t', 'rp_ang'], writes=['rp_r'])
                if shift != 0.0:
                    k.op('dve', lambda e, shift=shift: e.tensor_scalar(out=r[:], in0=r[:], scalar1=shift, scalar2=None, op0=ALU.add),
                         reads=['rp_r'], writes=['rp_r'])
                k.op('dve', lambda e: e.tensor_scalar(out=m[:], in0=r[:], scalar1=PI, scalar2=-2 * PI, op0=ALU.is_gt, op1=ALU.mult),
                     reads=['rp_r'], writes=['rp_m'])
                k.op('dve', lambda e: e.tensor_tensor(out=r[:], in0=r[:], in1=m[:], op=ALU.add), reads=['rp_r', 'rp_m'], writes=['rp_r'])
                k.op('dve', lambda e: e.tensor_scalar(out=m[:], in0=r[:], scalar1=-PI, scalar2=2 * PI, op0=ALU.is_lt, op1=ALU.mult),
                     reads=['rp_r'], writes=['rp_m'])
                k.op('dve', lambda e: e.tensor_tensor(out=r[:], in0=r[:], in1=m[:], op=ALU.add), reads=['rp_r', 'rp_m'], writes=['rp_r'])
                k.op('act', lambda e, dst=dst, cs_=cs_: e.activation(out=dst[:, cs_], in_=r[:], func=AF.Sin), reads=['rp_r'],
                     writes=[dkey])
        k.barrier()
    return cos, sin


class NormRope:
    def __init__(self, g, es, pfx):
        self.g = g
        self.pfx = pfx
        self.n = 0
        mk = lambda nm, dt: [g.sb('%s_%s%d' % (pfx, nm, i), [128, 512], dt, es) for i in range(2)]
        self.xf = mk('xf', F32)
        self.sq = mk('sq', BF16)
        self.rs = mk('rs', F32)
        self.xn = mk('xn', F32)
        self.xb = mk('xb', BF16)
        self.t1 = mk('t1', F32)

    def run(self, ps, pskey, gcol, gkeys, ts, cos=None, sin=None, out_r=None, okr=None, out_n=None, okn=None, pbank=2):
        g, k, pfx = self.g, self.g.k, self.pfx
        b = self.n % 2
        self.n += 1
        K_ = lambda nm: '%s_%s%d' % (pfx, nm, b)
        xf, sq, rs, xn, xb, t1 = self.xf[b], self.sq[b], self.rs[b], self.xn[b], self.xb[b], self.t1[b]
        k.op('act', lambda e: e.copy(out=xf[:], in_=ps), reads=[pskey], writes=[K_('xf')])
        k.op('act', lambda e: e.activation(out=sq[:], in_=xf[:], func=AF.Square), reads=[K_('xf')], writes=[K_('sq')])
        pi = pbank + b
        k.op('pe', lambda e: e.matmul(g.ps[pi][:], lhsT=g.cs['blk64_b'][:], rhs=sq[:], start=True, stop=True),
             reads=[K_('sq'), 'c_blk64_b'], writes=[g.psk[pi]])
        k.op('dve', lambda e: e.tensor_scalar(out=rs[:], in0=g.ps[pi][:], scalar1=1.0 / 64, scalar2=NORM_EPS, op0=ALU.mult, op1=ALU.add),
             reads=[g.psk[pi]], writes=[K_('rs')])
        k.op('act', lambda e: e.activation(out=rs[:], in_=rs[:], func=AF.Sqrt), reads=[K_('rs')], writes=[K_('rs')])
        k.op('dve', lambda e: e.reciprocal(out=rs[:], in_=rs[:]), reads=[K_('rs')], writes=[K_('rs')])
        k.op('dve', lambda e: e.scalar_tensor_tensor(out=xn[:], in0=xf[:], scalar=gcol, in1=rs[:], op0=ALU.mult, op1=ALU.mult),
             reads=[K_('xf'), K_('rs')] + gkeys, writes=[K_('xn')])
        if out_n is not None:
            k.op('pool', lambda e: e.tensor_copy(out_n, xn[:]), reads=[K_('xn')], writes=[okn])
        if out_r is not None:
            k.op('act', lambda e: e.copy(out=xb[:], in_=xn[:]), reads=[K_('xn')], writes=[K_('xb')])
            k.op('pe', lambda e: e.matmul(g.ps[pi][:], lhsT=g.cs['prot_b'][:], rhs=xb[:], start=True, stop=True),
                 reads=[K_('xb'), 'c_prot_b'], writes=[g.psk[pi]])
            k.op('dve', lambda e: e.tensor_tensor(out=t1[:], in0=g.ps[pi][:], in1=sin[:, ts], op=ALU.mult),
                 reads=[g.psk[pi], 'rp_sin'], writes=[K_('t1')])
            k.op('pool', lambda e: e.tensor_tensor(out=xn[:], in0=xn[:], in1=cos[:, ts], op=ALU.mult),
                 reads=[K_('xn'), 'rp_cos'], writes=[K_('xn')])
            k.op('dve', lambda e: e.tensor_tensor(out=out_r, in0=xn[:], in1=t1[:], op=ALU.add),
                 reads=[K_('xn'), K_('t1')], writes=[okr])


def attention(g, es, pfx, heads, ngroups, ktiles, qT, kT, vfn, ncols, masks, epilogue, scale=0.125):
    k = g.k
    pt = [g.sb('%s_pt%d' % (pfx, i), [128, 512], BF16, es) for i in range(3)]
    n = 0
    for h in heads:
        for gi in range(ngroups):
            kts = ktiles(gi)
            if not kts:
                continue
            for idx, kt in enumerate(kts):
                sbk = n % 2
                pb = n % 3
                n += 1
                qa, qk = qT(h, gi)
                ka, kk, nk = kT(h, kt)
                k.op('pe', lambda e, sbk=sbk, ka=ka, qa=qa, nk=nk: e.matmul(g.ps[sbk][0:nk, :], lhsT=ka, rhs=qa, start=True, stop=True),
                     reads=qk + kk, writes=[g.psk[sbk]])
                ptk = '%s_pt%d' % (pfx, pb)
                k.op('act', lambda e, sbk=sbk, pb=pb, nk=nk: e.activation(out=pt[pb][0:nk, :], in_=g.ps[sbk][0:nk, :], func=AF.Exp, scale=scale),
                     reads=[g.psk[sbk]], writes=[ptk])
                for (ma, mk_, is_ps) in masks(h, gi, kt):
                    eng = 'dve' if (is_ps or n % 2 == 0) else 'pool'
                    k.op(eng, lambda e, pb=pb, ma=ma, nk=nk: e.tensor_tensor(out=pt[pb][0:nk, :], in0=pt[pb][0:nk, :], in1=ma[0:nk, :], op=ALU.mult),
                         reads=[ptk] + mk_, writes=[ptk])
                va, vk = vfn(h, kt)
                for sub in range(4):
                    k.op('pe', lambda e, sub=sub, pb=pb, va=va, nk=nk, idx=idx: e.matmul(
                        g.ps[4 + sub][:, 0:ncols], lhsT=pt[pb][0:nk, sub * 128:(sub + 1) * 128], rhs=va,
                        start=(idx == 0), stop=(idx == len(kts) - 1)), reads=[ptk] + vk, writes=[g.psk[4 + sub]])
            for sub in range(4):
                epilogue(h, gi, sub, g.ps[4 + sub], g.psk[4 + sub])


def finish_tm(g, es, pfx, ytm, ykeyfn, gb, gbkey, row0):
    k = g.k
    ycv = g.ycatT.rearrange('(c p) t -> p c t', p=128)
    sq = [g.sb(pfx + '_fsq%d' % i, [128, 256], F32, es) for i in range(2)]
    ss = [g.sb(pfx + '_fss%d' % i, [128, 4], F32, es) for i in range(2)]
    yb = [g.sb(pfx + '_fyb%d' % i, [128, 256], BF16, es) for i in range(2)]
    yT = [g.sb(pfx + '_fyT%d' % i, [128, 2, 128], BF16, es) for i in range(2)]
    psb = [g.ps[2][:].bitcast(BF16), g.ps[3][:].bitcast(BF16)]
    for t_ in range(NT):
        b = t_ % 2
        s_ = str(b)
        yk = ykeyfn(t_)
        k.op('pool', lambda e, b=b, t_=t_: e.tensor_tensor(out=sq[b][:], in0=ytm[:, t_, :], in1=ytm[:, t_, :], op=ALU.mult),
             reads=yk, writes=[pfx + '_fsq' + s_])
        k.op('dve', lambda e, b=b: e.reduce_sum(out=ss[b][:], in_=sq[b][:].rearrange('p (h d) -> p h d', d=64), axis=AX.X),
             reads=[pfx + '_fsq' + s_], writes=[pfx + '_fss' + s_])
        k.op('dve', lambda e, b=b: e.tensor_scalar(out=ss[b][:], in0=ss[b][:], scalar1=1.0 / 64, scalar2=NORM_EPS, op0=ALU.mult, op1=ALU.add),
             reads=[pfx + '_fss' + s_], writes=[pfx + '_fss' + s_])
        k.op('act', lambda e, b=b: e.activation(out=ss[b][:], in_=ss[b][:], func=AF.Sqrt), reads=[pfx + '_fss' + s_], writes=[pfx + '_fss' + s_])
        k.op('dve', lambda e, b=b: e.reciprocal(out=ss[b][:], in_=ss[b][:]), reads=[pfx + '_fss' + s_], writes=[pfx + '_fss' + s_])
        for h in range(4):
            k.op('dve', lambda e, b=b, h=h, t_=t_: e.scalar_tensor_tensor(
                out=yb[b][:, h * 64:(h + 1) * 64], in0=ytm[:, t_, h * 64:(h + 1) * 64], scalar=ss[b][:, h:h + 1],
                in1=gb[:, h * 64:(h + 1) * 64], op0=ALU.mult, op1=ALU.mult),
                reads=yk + [pfx + '_fss' + s_, gbkey], writes=[pfx + '_fyb' + s_])
        for j in range(2):
            k.op('pe', lambda e, b=b, j=j: e.transpose(out=psb[b][:, j * 128:(j + 1) * 128], in_=yb[b][:, j * 128:(j + 1) * 128],
                                                       identity=g.cs['ident_b'][:]),
                 reads=[pfx + '_fyb' + s_, 'c_ident_b'], writes=[g.psk[2 + b]])
        k.op('act', lambda e, b=b: e.copy(out=yT[b][:], in_=psb[b][:, 0:256].rearrange('p (j t) -> p j t', j=2)),
             reads=[g.psk[2 + b]], writes=[pfx + '_fyT' + s_])
        c0 = row0 // 128
        k.dma('sp', ycv[:, c0:c0 + 2, t_ * 128:(t_ + 1) * 128], yT[b][:], reads=[pfx + '_fyT' + s_], writes=['ycat_%s%d' % (pfx, t_)])


def mixer_dil(g, l):
    k = g.k
    with contextlib.ExitStack() as es:
        qT = g.sb('dl_qT', [128, 2, S], BF16, es)
        kT = g.sb('dl_kT', [128, 2, S], BF16, es)
        V = g.sb('dl_V', [128, NT, 4, 65], BF16, es)
        ytm = g.sb('dl_ytm', [128, NT, 256], F32, es)
        gb = g.sb('dl_gb', [128, 256], F32, es)
        k.dma('sp', gb[:], g.W['onorm_g'][l, 256:512].partition_broadcast(128), writes=['dl_gb'])
        k.op('pool', lambda e: e.memset(V[:, :, :, 64:65], 1.0), writes=['dl_Vone'])
        with contextlib.ExitStack() as es2:
            cos, sin = rope_tables(g, es2)
            w = g.sb('dl_w', [128, 8, 768], BF16, es2)
            load_w_bf16(g, w[:], 'dl_w', g.W['w_in'][l][:, 1792:2560])
            gq = g.sb('dl_gq', [128, 1], F32, es2)
            gk = g.sb('dl_gk', [128, 1], F32, es2)
            for hh in range(2):
                k.dma('sp', gq[hh * 64:(hh + 1) * 64, :], g.W['dil_q_g'][l].rearrange('(p o) -> p o', o=1), writes=['dl_gq'],
                      allow_slow_non_contiguous=True)
                k.dma('sp', gk[hh * 64:(hh + 1) * 64, :], g.W['dil_k_g'][l].rearrange('(p o) -> p o', o=1), writes=['dl_gk'],
                      allow_slow_non_contiguous=True)
            nr = NormRope(g, es2, 'dl')
            hload = ht_loader(g, es2, 'dl')
            n = 0
            for tt in range(8):
                ts = slice(tt * 512, (tt + 1) * 512)
                ht, hk = hload(tt)
                for j in range(2):
                    for (dst, dkey, c0, gcol, gkey) in ((qT, 'dl_qT', 0, gq, 'dl_gq'), (kT, 'dl_kT', 256, gk, 'dl_gk')):
                        pi = n % 2
                        n += 1
                        proj_fm(g, w, 'dl_w', c0 + j * 128, 128, ht, hk, g.ps[pi][:], g.psk[pi])
                        nr.run(g.ps[pi][:], g.psk[pi], gcol[:, 0:1], [gkey], ts, cos, sin, dst[:, j, ts], '%s%d_%d' % (dkey, j, tt))
                for sub in range(4):
                    pi = 4 + sub
                    t_ = tt * 4 + sub
                    proj_tm(g, w, 'dl_w', 512, 256, ht, hk, sub, g.ps[pi][:, 0:256], g.psk[pi])
                    k.op('act', lambda e, pi=pi, t_=t_: e.copy(out=V[:, t_, :, 0:64], in_=g.ps[pi][:, 0:256].rearrange('p (h d) -> p h d', d=64)),
                         reads=[g.psk[pi]], writes=['dl_V%d' % t_])
            k.barrier()
        msk = load_const(g, es, 'dil_mask')
        rd = [g.sb('dl_rd%d' % i, [128, 1], F32, es) for i in range(2)]
        cnt = [0]

        def q_of(h, gi):
            j, hp = h // 2, h % 2
            return qT[hp * 64:(hp + 1) * 64, j, gi * 512:(gi + 1) * 512], ['dl_qT%d_%d' % (j, gi)]

        def k_of(h, kt):
            j, hp = h // 2, h % 2
            return kT[hp * 64:(hp + 1) * 64, j, kt * 128:(kt + 1) * 128], ['dl_kT%d_%d' % (j, kt // 4)], 128

        def v_of(h, kt):
            return V[:, kt, h, :], ['dl_V%d' % kt, 'dl_Vone']

        def ktiles(gi):
            return [kt for kt in range(max(0, 4 * gi - 16), 4 * gi + 4)]

        def masks(h, gi, kt):
            return [(msk[:, (4 * gi - kt) + 3, :], ['c_dil_mask'], False)]

        def epi(h, gi, sub, acc, acck):
            b = cnt[0] % 2
            cnt[0] += 1
            t_ = gi * 4 + sub
            k.op('dve', lambda e: e.reciprocal(out=rd[b][:], in_=acc[:, 64:65]), reads=[acck], writes=['dl_rd%d' % b])
            k.op('dve', lambda e: e.tensor_scalar(out=ytm[:, t_, h * 64:(h + 1) * 64], in0=acc[:, 0:64], scalar1=rd[b][:, 0:1],
                                                   scalar2=None, op0=ALU.mult), reads=[acck, 'dl_rd%d' % b], writes=['dl_ytm%d_%d' % (t_, h)])

        attention(g, es, 'dl', range(4), 8, ktiles, q_of, k_of, v_of, 65, masks, epi)
        finish_tm(g, es, 'dl', ytm, lambda t_: ['dl_ytm%d_%d' % (t_, h) for h in range(4)], gb, 'dl_gb', 512)
        k.barrier()


NSA_STOP = None
NSA_SKIP = ''


def mixer_nsa(g, l):
    k = g.k
    W = g.W
    with contextlib.ExitStack() as es:
        qn = g.sb('ns_qn', [128, 2, S], BF16, es)
        qr = g.sb('ns_qr', [128, 2, S], BF16, es)
        ksT = g.sb('ns_ks', [128, S], BF16, es)
        kwT = g.sb('ns_kw', [128, S], BF16, es)
        kcvc = g.sb('ns_kcvc', [128, S], BF16, es)
        Vs = g.sb('ns_Vs', [128, NT, 66], BF16, es)
        Vw = g.sb('ns_Vw', [128, NT, 66], BF16, es)
        gate = g.sb('ns_gate', [128, NT, 12], F32, es)
        ytm = g.sb('ns_ytm', [128, NT, 256], F32, es)
        gb = g.sb('ns_gb', [128, 256], F32, es)
        k.dma('sp', gb[:], W['onorm_g'][l, 512:768].partition_broadcast(128), writes=['ns_gb'])
        k.op('pool', lambda e: e.memset(Vs[:, :, 64:65], 1.0), writes=['ns_Vs1'])
        k.op('pool', lambda e: e.memset(Vw[:, :, 64:65], 1.0), writes=['ns_Vw1'])
        with contextlib.ExitStack() as es2:
            cos, sin = rope_tables(g, es2)
            w = g.sb('ns_w', [128, 8, 652], BF16, es2)
            load_w_bf16(g, w[:], 'ns_w', W['w_in'][l][:, 2560:3212])
            wks = g.sb('ns_wks', [128, 8, 128], BF16, es2)
            wkw = g.sb('ns_wkw', [128, 8, 128], BF16, es2)
            for hh in range(2):
                load_w_bf16(g, wks[:, :, hh * 64:(hh + 1) * 64], 'ns_wks', W['w_in'][l][:, 2944:3008])
                load_w_bf16(g, wkw[:, :, hh * 64:(hh + 1) * 64], 'ns_wkw', W['w_in'][l][:, 3072:3136])
            gq = g.sb('ns_gq', [128, 1], F32, es2)
            gks = g.sb('ns_gks', [128, 1], F32, es2)
            gkw = g.sb('ns_gkw', [128, 1], F32, es2)
            for hh in range(2):
                for (dst, nm, key) in ((gq, 'nsa_q_g', 'ns_gq'), (gks, 'nsa_ks_g', 'ns_gks'), (gkw, 'nsa_kw_g', 'ns_gkw')):
                    k.dma('sp', dst[hh * 64:(hh + 1) * 64, :], W[nm][l].rearrange('(p o) -> p o', o=1), writes=[key],
                          allow_slow_non_contiguous=True)
            nr = NormRope(g, es2, 'ns')
            hload = ht_loader(g, es2, 'ns')
            n = 0
            for tt in range(8):
                ts = slice(tt * 512, (tt + 1) * 512)
                ht, hk = hload(tt)
                for j in range(2):
                    pi = n % 2
                    n += 1
                    proj_fm(g, w, 'ns_w', j * 128, 128, ht, hk, g.ps[pi][:], g.psk[pi])
                    nr.run(g.ps[pi][:], g.psk[pi], gq[:, 0:1], ['ns_gq'], ts, cos, sin, qr[:, j, ts], 'ns_qr%d_%d' % (j, tt),
                           None if 'a' in NSA_SKIP else qn[:, j, ts], 'ns_qn%d_%d' % (j, tt))
                for (wsb, wkey, gcol, gkey, dst, dkey) in ((wks, 'ns_wks', gks, 'ns_gks', ksT, 'ns_ks'), (wkw, 'ns_wkw', gkw, 'ns_gkw', kwT, 'ns_kw')):
                    if 'c' in NSA_SKIP:
                        continue
                    pi = n % 2
                    n += 1
                    proj_fm(g, wsb, wkey, 0, 128, ht, hk, g.ps[pi][:], g.psk[pi])
                    nr.run(g.ps[pi][:], g.psk[pi], gcol[:, 0:1], [gkey], ts, cos, sin, dst[:, ts], '%s_%d' % (dkey, tt))
                pi = n % 2
                n += 1
                proj_fm(g, w, 'ns_w', 256, 128, ht, hk, g.ps[pi][:], g.psk[pi])
                k.op('act', lambda e, pi=pi, ts=ts: e.copy(out=kcvc[:, ts], in_=g.ps[pi][:]), reads=[g.psk[pi]], writes=['ns_kcvc%d' % tt])
                for sub in range(4):
                    if 'b' in NSA_SKIP:
                        continue
                    pi = 4 + sub
                    t_ = tt * 4 + sub
                    proj_tm(g, w, 'ns_w', 448, 204, ht, hk, sub, g.ps[pi][:, 0:204], g.psk[pi])
                    k.op('act', lambda e, pi=pi, t_=t_: e.copy(out=Vs[:, t_, 0:64], in_=g.ps[pi][:, 0:64]), reads=[g.psk[pi]],
                         writes=['ns_Vs%d' % t_])
                    if 'e' not in NSA_SKIP:
                        k.op('dve', lambda e, pi=pi, t_=t_: e.tensor_copy(Vw[:, t_, 0:64], g.ps[pi][:, 128:192]), reads=[g.psk[pi]],
                             writes=['ns_Vw%d' % t_])
                    if 'd' not in NSA_SKIP:
                        k.op('act', lambda e, pi=pi, t_=t_: e.activation(out=gate[:, t_, :], in_=g.ps[pi][:, 192:204], func=AF.Sigmoid),
                             reads=[g.psk[pi]], writes=['ns_gate%d' % t_])
            k.barrier()
        if NSA_STOP == 'proj':
            return
        kcT = g.sb('ns_kcT', [128, 512], BF16, es)
        Vc = [g.sb('ns_Vc%d' % j, [128, 129], BF16, es) for j in range(2)]
        ovl1 = load_const(g, es, 'ovl1')
        with contextlib.ExitStack() as es3:
            W1 = g.sb('ns_W1', [128, 32, 256], BF16, es3)
            peT = g.sb('ns_peT', [128, 32], BF16, es3)
            W2k = g.sb('ns_W2k', [128, 2, 128], BF16, es3)
            W2v = g.sb('ns_W2v', [128, 2, 64], BF16, es3)
            gkc = g.sb('ns_gkc', [128, 1], F32, es3)
            for (pb, w1n, pen) in ((0, 'nsa_wk1', 'nsa_pe_k'), (64, 'nsa_wv1', 'nsa_pe_v')):
                k.dma('pool', W1[pb:pb + 64, :, :], W[w1n][l].rearrange('(l d) m -> d l m', d=64), writes=['ns_W1'])
                k.dma('pool', peT[pb:pb + 64, :], W[pen][l].rearrange('l d -> d l'), writes=['ns_peT'], allow_slow_non_contiguous=True)
            for hh in range(2):
                k.dma('pool', W2k[:, :, hh * 64:(hh + 1) * 64], W['nsa_wk2'][l].rearrange('(c p) d -> p c d', p=128), writes=['ns_W2k'])
                k.dma('sp', gkc[hh * 64:(hh + 1) * 64, :], W['nsa_kc_g'][l].rearrange('(p o) -> p o', o=1), writes=['ns_gkc'],
                      allow_slow_non_contiguous=True)
            k.dma('pool', W2v[:], W['nsa_wv2'][l].rearrange('(c p) d -> p c d', p=128), writes=['ns_W2v'])
            hid = {}
            bia = g.sb('ns_bia', [128, 4], F32, es3)
            xs = g.sb('ns_xs', [128, 256], F32, es3)
            x2 = g.sb('ns_x2', [128, 256], F32, es3)
            th = g.sb('ns_th', [128, 256], F32, es3)
            n = 0
            allkc = ['ns_kcvc%d' % i for i in range(8)]
            for kv, pb in (('k', 0), ('v', 64)):
                for mc in range(2):
                    hd_ = g.sb('ns_hid%s%d' % (kv, mc), [128, 256], BF16, es3)
                    hid[(kv, mc)] = hd_
                    hk_ = 'ns_hid%s%d' % (kv, mc)
                    k.op('pool', lambda e, hd_=hd_: e.memset(hd_[:], 0.0), writes=[hk_])
                    pi, pj = n % 2, 2 + n % 2
                    for l_ in range(32):
                        k.op('pe', lambda e, l_=l_, pb=pb, mc=mc, pi=pi: e.matmul(
                            g.ps[pi][:, 0:255], lhsT=W1[pb:pb + 64, l_, mc * 128:(mc + 1) * 128],
                            rhs=kcvc[pb:pb + 64, l_:l_ + 16 * 254 + 1:16], start=(l_ == 0), stop=(l_ == 31)),
                            reads=['ns_W1'] + allkc, writes=[g.psk[pi]])
                    for l_ in range(32):
                        k.op('pe', lambda e, l_=l_, pb=pb, mc=mc, pj=pj: e.matmul(
                            g.ps[pj][:, 0:1], lhsT=W1[pb:pb + 64, l_, mc * 128:(mc + 1) * 128], rhs=peT[pb:pb + 64, l_:l_ + 1],
                            start=(l_ == 0), stop=(l_ == 31)), reads=['ns_W1', 'ns_peT'], writes=[g.psk[pj]])
                    k.op('act', lambda e, n=n, pj=pj: e.copy(out=bia[:, n:n + 1], in_=g.ps[pj][:, 0:1]), reads=[g.psk[pj]], writes=['ns_bia'])
                    k.op('act', lambda e, n=n, pi=pi: e.activation(out=xs[:, 0:255], in_=g.ps[pi][:, 0:255], func=AF.Identity,
                                                                   bias=bia[:, n:n + 1], scale=1.0), reads=[g.psk[pi], 'ns_bia'], writes=['ns_xs'])
                    k.op('dve', lambda e: e.tensor_tensor(out=x2[:, 0:255], in0=xs[:, 0:255], in1=xs[:, 0:255], op=ALU.mult), reads=['ns_xs'], writes=['ns_x2'])
                    k.op('dve', lambda e: e.tensor_scalar(out=x2[:, 0:255], in0=x2[:, 0:255], scalar1=0.044715, scalar2=1.0, op0=ALU.mult, op1=ALU.add),
                         reads=['ns_x2'], writes=['ns_x2'])
                    k.op('dve', lambda e: e.tensor_tensor(out=x2[:, 0:255], in0=x2[:, 0:255], in1=xs[:, 0:255], op=ALU.mult), reads=['ns_x2', 'ns_xs'], writes=['ns_x2'])
                    k.op('act', lambda e: e.activation(out=th[:, 0:255], in_=x2[:, 0:255], func=AF.Tanh, scale=0.7978845608028654),
                         reads=['ns_x2'], writes=['ns_th'])
                    k.op('dve', lambda e: e.scalar_tensor_tensor(out=th[:, 0:255], in0=th[:, 0:255], scalar=1.0, in1=xs[:, 0:255], op0=ALU.add, op1=ALU.mult),
                         reads=['ns_th', 'ns_xs'], writes=['ns_th'])
                    k.op('act', lambda e, hd_=hd_: e.mul(out=hd_[:, 0:255], in_=th[:, 0:255], mul=0.5), reads=['ns_th'], writes=[hk_])
                    n += 1
            for mc in range(2):
                k.op('pe', lambda e, mc=mc: e.matmul(g.ps[0][:, 0:256], lhsT=W2k[:, mc, :], rhs=hid[('k', mc)][:], start=(mc == 0), stop=(mc == 1)),
                     reads=['ns_W2k', 'ns_hidk%d' % mc], writes=[g.psk[0]])
            nr2 = NormRope(g, es3, 'nc')
            nr2.run(g.ps[0][:], g.psk[0], gkc[:, 0:1], ['ns_gkc'], slice(0, 512), out_n=kcT[:], okn='ns_kcT')
            k.op('pool', lambda e: e.memset(kcT[:, 255:512], 0.0), reads=['ns_kcT'], writes=['ns_kcT'])
            for j in range(2):
                for mc in range(2):
                    k.op('pe', lambda e, j=j, mc=mc: e.matmul(g.ps[1][:, 0:64], lhsT=hid[('v', mc)][:, j * 128:(j + 1) * 128], rhs=W2v[:, mc, :],
                                                           start=(mc == 0), stop=(mc == 1)), reads=['ns_W2v', 'ns_hidv%d' % mc], writes=[g.psk[1]])
                k.op('act', lambda e, j=j: e.copy(out=Vc[j][:, 0:64], in_=g.ps[1][:, 0:64]), reads=[g.psk[1]], writes=['ns_Vc%d' % j])
                k.op('pool', lambda e, j=j: e.tensor_copy(Vc[j][:, 64:129], ovl1[:, j, :]), reads=['c_ovl1'], writes=['ns_Vc%d_o' % j])
            k.barrier()
        if NSA_STOP == 'cmpkv':
            return
        imp = g.sb('ns_imp', [128, NT, 64], F32, es)
        selT = g.sb('ns_selT', [64, S], BF16, es)
        rd = [g.sb('ns_rd%d' % i, [128, 1], F32, es) for i in range(2)]
        cf = [g.sb('ns_cf%d' % i, [128, 1], F32, es) for i in range(2)]
        cnt = [0]

        def q_of_n(h, gi):
            j, hp = h // 2, h % 2
            return qn[hp * 64:(hp + 1) * 64, j, gi * 512:(gi + 1) * 512], ['ns_qn%d_%d' % (j, gi)]

        def q_of_r(h, gi):
            j, hp = h // 2, h % 2
            return qr[hp * 64:(hp + 1) * 64, j, gi * 512:(gi + 1) * 512], ['ns_qr%d_%d' % (j, gi)]

        def make_epi(br, first):
            def epi(h, gi, sub, acc, acck):
                b = cnt[0] % 2
                cnt[0] += 1
                t_ = gi * 4 + sub
                rk, ck = 'ns_rd%d' % b, 'ns_cf%d' % b
                k.op('dve', lambda e: e.tensor_scalar(out=rd[b][:], in0=acc[:, 64:65], scalar1=1e-30, scalar2=None, op0=ALU.max),
                     reads=[acck], writes=[rk])
                k.op('dve', lambda e: e.reciprocal(out=rd[b][:], in_=rd[b][:]), reads=[rk], writes=[rk])
                k.op('dve', lambda e: e.tensor_tensor(out=cf[b][:], in0=rd[b][:], in1=gate[:, t_, h * 3 + br:h * 3 + br + 1], op=ALU.mult),
                     reads=[rk, 'ns_gate%d' % t_], writes=[ck])
                yk = 'ns_ytm%d_%d' % (t_, h)
                if first:
                    k.op('dve', lambda e: e.tensor_scalar(out=ytm[:, t_, h * 64:(h + 1) * 64], in0=acc[:, 0:64], scalar1=cf[b][:, 0:1],
                                                           scalar2=None, op0=ALU.mult), reads=[acck, ck], writes=[yk])
                    ik = 'ns_imp%d' % t_
                    if h == 0:
                        k.op('dve', lambda e: e.tensor_scalar(out=imp[:, t_, :], in0=acc[:, 65:129], scalar1=rd[b][:, 0:1], scalar2=None,
                                                               op0=ALU.mult), reads=[acck, rk], writes=[ik])
                    else:
                        k.op('dve', lambda e: e.scalar_tensor_tensor(out=imp[:, t_, :], in0=acc[:, 65:129], scalar=rd[b][:, 0:1], in1=imp[:, t_, :],
                                                                      op0=ALU.mult, op1=ALU.add), reads=[acck, rk, ik], writes=[ik])
                else:
                    k.op('dve', lambda e: e.scalar_tensor_tensor(out=ytm[:, t_, h * 64:(h + 1) * 64], in0=acc[:, 0:64], scalar=cf[b][:, 0:1],
                                                                  in1=ytm[:, t_, h * 64:(h + 1) * 64], op0=ALU.mult, op1=ALU.add),
                         reads=[acck, ck, yk], writes=[yk])
            return epi

        with contextlib.ExitStack() as es4:
            cmsk = load_const(g, es4, 'cmp_mask')

            def k_cmp(h, kt):
                hp = h % 2
                return kcT[hp * 64:(hp + 1) * 64, kt * 128:(kt + 1) * 128], ['ns_kcT'], 128

            def v_cmp(h, kt):
                return Vc[kt][:, :], ['ns_Vc%d' % kt, 'ns_Vc%d_o' % kt]

            def kt_cmp(gi):
                return [0] + ([1] if gi >= 4 else [])

            def m_cmp(h, gi, kt):
                i = gi - 4 * kt
                return [] if i >= 5 else [(cmsk[:, i, :], ['c_cmp_mask'], False)]

            attention(g, es4, 'nc', range(4), 8, kt_cmp, q_of_n, k_cmp, v_cmp, 129, m_cmp, make_epi(0, True))
            if NSA_STOP == 'cmpattn':
                k.barrier()
                return
            keep = load_const(g, es4, 'sel_keep')
            addc = load_const(g, es4, 'sel_add')
            sc = [g.sb('ns_sc%d' % i, [128, 64], F32, es4) for i in range(2)]
            wk2_ = [g.sb('ns_wk%d' % i, [128, 64], F32, es4) for i in range(2)]
            m8 = [g.sb('ns_m8%d' % i, [128, 8], F32, es4) for i in range(2)]
            sm = [g.sb('ns_sm%d' % i, [128, 64], BF16, es4) for i in range(2)]
            psb = [g.ps[2][:].bitcast(BF16), g.ps[3][:].bitcast(BF16)]
            for t_ in range(NT):
                b = t_ % 2
                s_ = str(b)
                k.op('dve', lambda e, b=b, t_=t_: e.tensor_tensor(out=sc[b][:], in0=imp[:, t_, :], in1=keep[:, t_, :], op=ALU.mult),
                     reads=['ns_imp%d' % t_, 'c_sel_keep'], writes=['ns_sc' + s_])
                k.op('dve', lambda e, b=b, t_=t_: e.tensor_tensor(out=sc[b][:], in0=sc[b][:], in1=addc[:, t_, :], op=ALU.add),
                     reads=['ns_sc' + s_, 'c_sel_add'], writes=['ns_sc' + s_])
                k.op('dve', lambda e, b=b: e.max(out=m8[b][:], in_=sc[b][:]), reads=['ns_sc' + s_], writes=['ns_m8' + s_])
                k.op('dve', lambda e, b=b: e.match_replace(out=wk2_[b][:], in_to_replace=m8[b][:], in_values=sc[b][:], imm_value=-1e30),
                     reads=['ns_sc' + s_, 'ns_m8' + s_], writes=['ns_wk' + s_])
                k.op('dve', lambda e, b=b: e.max(out=m8[b][:], in_=wk2_[b][:]), reads=['ns_wk' + s_], writes=['ns_m8' + s_])
                k.op('dve', lambda e, b=b: e.tensor_scalar(out=sm[b][:], in0=sc[b][:], scalar1=m8[b][:, 7:8], scalar2=None, op0=ALU.is_ge),
                     reads=['ns_sc' + s_, 'ns_m8' + s_], writes=['ns_sm' + s_])
                k.op('pe', lambda e, b=b: e.transpose(out=psb[b][0:64, 0:128], in_=sm[b][:], identity=g.cs['ident_b'][:]),
                     reads=['ns_sm' + s_, 'c_ident_b'], writes=[g.psk[2 + b]])
                k.op('act', lambda e, b=b, t_=t_: e.copy(out=selT[:, t_ * 128:(t_ + 1) * 128], in_=psb[b][0:64, 0:128]),
                     reads=[g.psk[2 + b]], writes=['ns_selT%d' % (t_ // 4)])
            tp = tap(g, 'selT%d' % l, [64, S], BF16)
            if tp is not None:
                k.dma('sp', tp, selT[:], reads=['ns_selT%d' % i for i in range(8)], writes=['tap'])
            k.barrier()
        if NSA_STOP == 'topk':
            return
        with contextlib.ExitStack() as es5:
            cau = load_const(g, es5, 'cau_mask')
            sexp = load_const(g, es5, 'sel_exp')
            mc_ = [0]

            def k_s(h, kt):
                hp = h % 2
                return ksT[hp * 64:(hp + 1) * 64, kt * 128:(kt + 1) * 128], ['ns_ks_%d' % (kt // 4)], 128

            def v_s(h, kt):
                return Vs[:, kt, 0:65], ['ns_Vs%d' % kt, 'ns_Vs1']

            def m_s(h, gi, kt):
                pm = 2 + mc_[0] % 2
                mc_[0] += 1
                k.op('pe', lambda e: e.matmul(g.ps[pm][:], lhsT=sexp[:, kt, :], rhs=selT[:, gi * 512:(gi + 1) * 512], start=True, stop=True),
                     reads=['c_sel_exp', 'ns_selT%d' % gi], writes=[g.psk[pm]])
                ms = [(g.ps[pm], [g.psk[pm]], True)]
                if kt >= 4 * gi:
                    ms.append((cau[:, (4 * gi - kt) + 3, :], ['c_cau_mask'], False))
                return ms

            attention(g, es5, 'nl', range(4), 8, lambda gi: list(range(0, 4 * gi + 4)), q_of_r, k_s, v_s, 65, m_s, make_epi(1, False))
            k.barrier()
        if NSA_STOP == 'sel':
            return
        with contextlib.ExitStack() as es6:
            swm = load_const(g, es6, 'swa_mask')

            def k_w(h, kt):
                hp = h % 2
                return kwT[hp * 64:(hp + 1) * 64, kt * 128:(kt + 1) * 128], ['ns_kw_%d' % (kt // 4)], 128

            def v_w(h, kt):
                return Vw[:, kt, 0:65], ['ns_Vw%d' % kt, 'ns_Vw1']

            attention(g, es6, 'nw', range(4), 8, lambda gi: list(range(max(0, 4 * gi - 4), 4 * gi + 4)), q_of_r, k_w, v_w, 65,
                      lambda h, gi, kt: [(swm[:, (4 * gi - kt) + 3, :], ['c_swa_mask'], False)], make_epi(2, False))
            finish_tm(g, es6, 'ns', ytm, lambda t_: ['ns_ytm%d_%d' % (t_, h) for h in range(4)], gb, 'ns_gb', 768)
            k.barrier()


RW_STEPS = S


def mixer_rwkv(g, l):
    k = g.k
    W = g.W
    ycv = g.ycatT.rearrange('(c p) t -> p c t', p=128)
    with contextlib.ExitStack() as es:
        rT = g.sb('rw_rT', [128, 2, S], BF16, es)
        kkT = g.sb('rw_kkT', [128, 2, S], BF16, es)
        wT = g.sb('rw_wT', [128, 2, S], F32, es)
        vtm = g.sb('rw_vtm', [128, NT, 256], BF16, es)
        gtm = g.sb('rw_gtm', [128, NT, 256], BF16, es)
        bon = g.sb('rw_bon', [128, NT, 4], F32, es)
        with contextlib.ExitStack() as es2:
            wa = g.sb('rw_wa', [128, 8, 1024], BF16, es2)
            wb = g.sb('rw_wb', [128, 8, 1024], BF16, es2)
            mub = g.sb('rw_mub', [128, 1024], F32, es2)
            omb = g.sb('rw_omb', [128, 1024], F32, es2)
            load_w_bf16(g, wa[:], 'rw_wa', W['w_in'][l][:, 0:1024])
            k.dma('sp', mub[:], W['rwkv_mu'][l].partition_broadcast(128), writes=['rw_mub'])
            k.op('dve', lambda e: e.tensor_scalar(out=omb[:], in0=mub[:], scalar1=-1.0, scalar2=1.0, op0=ALU.mult, op1=ALU.add),
                 reads=['rw_mub'], writes=['rw_omb'])
            for c in range(8):
                k.op('dve', lambda e, c=c: e.tensor_tensor(out=wb[:, c, :], in0=wa[:, c, :], in1=mub[:], op=ALU.mult),
                     reads=['rw_wa', 'rw_mub'], writes=['rw_wb'])
            for c in range(8):
                k.op('pool', lambda e, c=c: e.tensor_tensor(out=wa[:, c, :], in0=wa[:, c, :], in1=omb[:], op=ALU.mult),
                     reads=['rw_wa', 'rw_omb', 'rw_wb'], writes=['rw_wa'])
            w2 = g.sb('rw_w2', [64, 256], BF16, es2)
            a2 = g.sb('rw_a2', [128, 256], BF16, es2)
            g2 = g.sb('rw_g2', [128, 256], BF16, es2)
            a0r = g.sb('rw_a0r', [1, 256], BF16, es2)
            w0c = g.sb('rw_w0c', [128, 2], F32, es2)
            kkc = g.sb('rw_kkc', [128, 2], F32, es2)
            k.dma('pool', w2[:], W['rwkv_w2'][l], writes=['rw_w2'])
            k.dma('pool', a2[64:128, :], W['rwkv_a2'][l], writes=['rw_a2'])
            k.dma('pool', g2[:], W['rwkv_g2'][l], writes=['rw_g2'])
            k.dma('pool', a0r[:], W['rwkv_a0'][l:l + 1, :], writes=['rw_a0r'])
            k.dma('sp', w0c[:], W['rwkv_w0'][l].rearrange('(c p) -> p c', p=128), writes=['rw_w0c'], allow_slow_non_contiguous=True)
            k.dma('sp', kkc[:], W['rwkv_kk'][l].rearrange('(c p) -> p c', p=128), writes=['rw_kkc'], allow_slow_non_contiguous=True)
            bc = {}
            for nm, src in (('kk', W['rwkv_kk'][l]), ('ka', W['rwkv_ka'][l]), ('rk', W['rwkv_rk'][l].rearrange('h d -> (h d)'))):
                t = g.sb('rw_bc_' + nm, [128, 256], F32, es2)
                k.dma('sp', t[:], src.partition_broadcast(128), writes=['rw_bc_' + nm])
                bc[nm] = t
            hv = g.hTd.rearrange('(c p) t -> p c t', p=128)
            hb = [g.sb('rw_ht%d' % i, [128, 8, 513], BF16, es2) for i in range(2)]
            k.op('pool', lambda e: e.memset(hb[0][:, :, 0:1], 0.0), writes=['rw_ht0'])
            tnh = [g.sb('rw_tnh%d' % i, [128, 512], BF16, es2) for i in range(2)]
            sgd = [g.sb('rw_sgd%d' % i, [128, 512], BF16, es2) for i in range(2)]
            kx = [g.sb('rw_kx%d' % i, [128, 512], F32, es2) for i in range(2)]
            sq = [g.sb('rw_sq%d' % i, [128, 512], BF16, es2) for i in range(2)]
            rs = [g.sb('rw_rs%d' % i, [128, 512], F32, es2) for i in range(2)]
            tm = {nm: [g.sb('rw_%s%d' % (nm, i), [128, 256], F32, es2) for i in range(2)] for nm in ('a', 'r', 'kxm', 'k2', 'tq')}
            s4 = [g.sb('rw_s4%d' % i, [128, 4], F32, es2) for i in range(2)]
            ob = [g.sb('rw_ob%d' % i, [128, 768], BF16, es2) for i in range(2)]

            def fm2(ht, hk, c0, ncols, ps, pskey):
                for c in range(8):
                    k.op('pe', lambda e, c=c: e.matmul(ps, lhsT=wa[:, c, c0:c0 + ncols], rhs=ht[:, c, 1:513], start=(c == 0), stop=False),
                         reads=['rw_wa', hk], writes=[pskey])
                for c in range(8):
                    k.op('pe', lambda e, c=c: e.matmul(ps, lhsT=wb[:, c, c0:c0 + ncols], rhs=ht[:, c, 0:512], start=False, stop=(c == 7)),
                         reads=['rw_wb', hk], writes=[pskey])

            nps = 0
            for tt in range(8):
                b = tt % 2
                ts = slice(tt * 512, (tt + 1) * 512)
                hk = 'rw_ht%d' % b
                ht = hb[b]
                if tt == 0:
                    k.dma('sp', ht[:, :, 1:513], hv[:, :, 0:512], reads=['hTd'], writes=[hk])
                else:
                    k.dma('sp', ht[:], hv[:, :, tt * 512 - 1:(tt + 1) * 512], reads=['hTd'], writes=[hk])
                for j in range(2):
                    pi = nps % 2
                    nps += 1
                    fm2(ht, hk, j * 128, 128, g.ps[pi][:], g.psk[pi])
                    k.op('act', lambda e, pi=pi, j=j, ts=ts: e.copy(out=rT[:, j, ts], in_=g.ps[pi][:]), reads=[g.psk[pi]],
                         writes=['rw_rT%d_%d' % (j, tt)])
                    pi = nps % 2
                    nps += 1
                    fm2(ht, hk, 256 + j * 128, 128, g.ps[pi][:], g.psk[pi])
                    kb = nps % 2
                    ks_ = str(kb)
                    k.op('act', lambda e, pi=pi, j=j, kb=kb: e.activation(out=kx[kb][:], in_=g.ps[pi][:], func=AF.Copy, scale=kkc[:, j:j + 1]),
                         reads=[g.psk[pi], 'rw_kkc'], writes=['rw_kx' + ks_])
                    k.op('act', lambda e, kb=kb: e.activation(out=sq[kb][:], in_=kx[kb][:], func=AF.Square), reads=['rw_kx' + ks_], writes=['rw_sq' + ks_])
                    pj = 2 + kb
                    k.op('pe', lambda e, kb=kb, pj=pj: e.matmul(g.ps[pj][:], lhsT=g.cs['blk64_b'][:], rhs=sq[kb][:], start=True, stop=True),
                         reads=['rw_sq' + ks_, 'c_blk64_b'], writes=[g.psk[pj]])
                    k.op('dve', lambda e, kb=kb, pj=pj: e.tensor_scalar(out=rs[kb][:], in0=g.ps[pj][:], scalar1=1e-12, scalar2=None, op0=ALU.add),
                         reads=[g.psk[pj]], writes=['rw_rs' + ks_])
                    k.op('act', lambda e, kb=kb: e.activation(out=rs[kb][:], in_=rs[kb][:], func=AF.Sqrt), reads=['rw_rs' + ks_], writes=['rw_rs' + ks_])
                    k.op('dve', lambda e, kb=kb: e.reciprocal(out=rs[kb][:], in_=rs[kb][:]), reads=['rw_rs' + ks_], writes=['rw_rs' + ks_])
                    k.op('dve', lambda e, kb=kb, j=j, ts=ts: e.tensor_tensor(out=kkT[:, j, ts], in0=kx[kb][:], in1=rs[kb][:], op=ALU.mult),
                         reads=['rw_kx' + ks_, 'rw_rs' + ks_], writes=['rw_kkT%d_%d' % (j, tt)])
                pi = nps % 2
                nps += 1
                fm2(ht, hk, 768, 128, g.ps[pi][:], g.psk[pi])
                k.op('act', lambda e, pi=pi, b=b: e.activation(out=tnh[b][0:64, :], in_=g.ps[pi][0:64, :], func=AF.Tanh),
                     reads=[g.psk[pi]], writes=['rw_tnh%d' % b])
                k.op('act', lambda e, pi=pi, b=b: e.copy(out=tnh[b][64:128, :], in_=g.ps[pi][64:128, :]),
                     reads=[g.psk[pi]], writes=['rw_tnhb%d' % b])
                pi = nps % 2
                nps += 1
                fm2(ht, hk, 896, 128, g.ps[pi][:], g.psk[pi])
                k.op('act', lambda e, pi=pi, b=b: e.activation(out=sgd[b][:], in_=g.ps[pi][:], func=AF.Sigmoid), reads=[g.psk[pi]],
                     writes=['rw_sgd%d' % b])
                for j in range(2):
                    pi = nps % 2
                    nps += 1
                    k.op('pe', lambda e, pi=pi, j=j, b=b: e.matmul(g.ps[pi][:], lhsT=w2[:, j * 128:(j + 1) * 128], rhs=tnh[b][0:64, :],
                                                                   start=True, stop=True), reads=['rw_w2', 'rw_tnh%d' % b], writes=[g.psk[pi]])
                    k.op('act', lambda e, pi=pi, j=j, ts=ts: e.activation(out=wT[:, j, ts], in_=g.ps[pi][:], func=AF.Sigmoid, bias=w0c[:, j:j + 1], scale=1.0),
                         reads=[g.psk[pi], 'rw_w0c'], writes=['rw_wT%d_%d' % (j, tt)])
                    k.op('act', lambda e, j=j, ts=ts: e.activation(out=wT[:, j, ts], in_=wT[:, j, ts], func=AF.Exp, scale=-0.606531),
                         reads=['rw_wT%d_%d' % (j, tt)], writes=['rw_wT%d_%d' % (j, tt)])
                for sub in range(4):
                    t_ = tt * 4 + sub
                    q = t_ % 2
                    qs = str(q)
                    cs_ = slice(sub * 128, (sub + 1) * 128)
                    pA, pB, pC, pD = 4, 5, 6, 7
                    for (ps_, c0, n_) in ((pA, 0, 512), (pB, 512, 256)):
                        for c in range(8):
                            k.op('pe', lambda e, c=c, ps_=ps_, c0=c0, n_=n_, sub=sub: e.matmul(
                                g.ps[ps_][:, 0:n_], lhsT=ht[:, c, 1 + sub * 128:1 + (sub + 1) * 128], rhs=wa[:, c, c0:c0 + n_],
                                start=(c == 0), stop=False), reads=['rw_wa', hk], writes=[g.psk[ps_]])
                        for c in range(8):
                            k.op('pe', lambda e, c=c, ps_=ps_, c0=c0, n_=n_, sub=sub: e.matmul(
                                g.ps[ps_][:, 0:n_], lhsT=ht[:, c, sub * 128:(sub + 1) * 128], rhs=wb[:, c, c0:c0 + n_],
                                start=False, stop=(c == 7)), reads=['rw_wb', hk], writes=[g.psk[ps_]])
                    k.op('pe', lambda e, b=b, cs_=cs_: e.matmul(g.ps[pC][:, 0:256], lhsT=tnh[b][64:128, cs_], rhs=a2[64:128, :], start=True, stop=False),
                         reads=['rw_tnhb%d' % b, 'rw_a2'], writes=[g.psk[pC]])
                    k.op('pe', lambda e: e.matmul(g.ps[pC][:, 0:256], lhsT=g.cs['ones_b'][0:1, :], rhs=a0r[:], start=False, stop=True),
                         reads=['c_ones_b', 'rw_a0r'], writes=[g.psk[pC]])
                    k.op('pe', lambda e, b=b, cs_=cs_: e.matmul(g.ps[pD][:, 0:256], lhsT=sgd[b][:, cs_], rhs=g2[:], start=True, stop=True),
                         reads=['rw_sgd%d' % b, 'rw_g2'], writes=[g.psk[pD]])
                    a_, r_, kxm, k2, tq = (tm[nm][q] for nm in ('a', 'r', 'kxm', 'k2', 'tq'))
                    K_ = lambda nm: 'rw_%s%s' % (nm, qs)
                    k.op('act', lambda e: e.activation(out=a_[:], in_=g.ps[pC][:, 0:256], func=AF.Sigmoid), reads=[g.psk[pC]], writes=[K_('a')])
                    k.op('act', lambda e, t_=t_: e.copy(out=gtm[:, t_, :], in_=g.ps[pD][:, 0:256]), reads=[g.psk[pD]], writes=['rw_gtm%d' % t_])
                    k.op('act', lambda e: e.copy(out=r_[:], in_=g.ps[pA][:, 0:256]), reads=[g.psk[pA]], writes=[K_('r')])
                    k.op('act', lambda e, t_=t_: e.copy(out=vtm[:, t_, :], in_=g.ps[pB][:, 0:256]), reads=[g.psk[pB]], writes=['rw_vtm%d' % t_])
                    k.op('act', lambda e, q=q: e.copy(out=ob[q][:, 512:768], in_=g.ps[pB][:, 0:256]), reads=[g.psk[pB]], writes=['rw_obv' + qs])
                    k.op('dve', lambda e: e.tensor_tensor(out=kxm[:], in0=g.ps[pA][:, 256:512], in1=bc['kk'][:], op=ALU.mult),
                         reads=[g.psk[pA], 'rw_bc_kk'], writes=[K_('kxm')])
                    k.op('pool', lambda e: e.tensor_tensor(out=tq[:], in0=kxm[:], in1=kxm[:], op=ALU.mult), reads=[K_('kxm')], writes=[K_('tq')])
                    k.op('dve', lambda e, q=q: e.reduce_sum(out=s4[q][:], in_=tq[:].rearrange('p (h d) -> p h d', d=64), axis=AX.X),
                         reads=[K_('tq')], writes=['rw_s4' + qs])
                    k.op('dve', lambda e, q=q: e.tensor_scalar(out=s4[q][:], in0=s4[q][:], scalar1=1e-12, scalar2=None, op0=ALU.add),
                         reads=['rw_s4' + qs], writes=['rw_s4' + qs])
                    k.op('act', lambda e, q=q: e.activation(out=s4[q][:], in_=s4[q][:], func=AF.Sqrt), reads=['rw_s4' + qs], writes=['rw_s4' + qs])
                    k.op('dve', lambda e, q=q: e.reciprocal(out=s4[q][:], in_=s4[q][:]), reads=['rw_s4' + qs], writes=['rw_s4' + qs])
                    for h in range(4):
                        hs = slice(h * 64, (h + 1) * 64)
                        k.op('dve', lambda e, h=h, hs=hs, q=q: e.scalar_tensor_tensor(out=ob[q][:, hs], in0=kxm[:, hs], scalar=s4[q][:, h:h + 1],
                                                                                      in1=a_[:, hs], op0=ALU.mult, op1=ALU.mult),
                             reads=[K_('kxm'), 'rw_s4' + qs, K_('a')], writes=['rw_obb' + qs])
                    k.op('dve', lambda e: e.scalar_tensor_tensor(out=tq[:], in0=a_[:], scalar=-1.0, in1=bc['ka'][:], op0=ALU.add, op1=ALU.mult),
                         reads=[K_('a'), 'rw_bc_ka', K_('tq')], writes=[K_('tq')])
                    k.op('dve', lambda e: e.scalar_tensor_tensor(out=k2[:], in0=tq[:], scalar=1.0, in1=g.ps[pA][:, 256:512], op0=ALU.add, op1=ALU.mult),
                         reads=[K_('tq'), g.psk[pA]], writes=[K_('k2')])
                    k.op('pool', lambda e, q=q: e.tensor_copy(ob[q][:, 256:512], k2[:]), reads=[K_('k2')], writes=['rw_obk' + qs])
                    k.op('pool', lambda e: e.tensor_tensor(out=tq[:], in0=r_[:], in1=k2[:], op=ALU.mult), reads=[K_('r'), K_('k2'), K_('tq')], writes=[K_('tq')])
                    k.op('pool', lambda e: e.tensor_tensor(out=tq[:], in0=tq[:], in1=bc['rk'][:], op=ALU.mult), reads=[K_('tq'), 'rw_bc_rk'], writes=[K_('tq')])
                    k.op('dve', lambda e, t_=t_: e.reduce_sum(out=bon[:, t_, :], in_=tq[:].rearrange('p (h d) -> p h d', d=64), axis=AX.X),
                         reads=[K_('tq')], writes=['rw_bon%d' % t_])
                    k.dma('sp', g.rwscr[t_ * 128:(t_ + 1) * 128, :], ob[q][:], reads=['rw_obv' + qs, 'rw_obb' + qs, 'rw_obk' + qs],
                          writes=['rwscr%d' % t_])
            k.barrier()
        TC = 64
        ST = g.sb('rw_ST', [128, 128], BF16, es)
        Lb = [g.sb('rw_L%d' % i, [36, TC, 128], BF16, es) for i in range(2)]
        Rb = [g.sb('rw_R%d' % i, [36, TC, 128], BF16, es) for i in range(2)]
        KKn = [g.sb('rw_KKn%d' % i, [128, TC, 4], BF16, es) for i in range(2)]
        Rr = [g.sb('rw_Rr%d' % i, [128, TC, 4], BF16, es) for i in range(2)]
        sam = load_const(g, es, 'sa_mask')
        hlm = load_const(g, es, 'hl_mask')
        Yc = [g.sb('rw_Yc%d' % i, [128, 2, 128], F32, es) for i in range(2)]
        ytm = g.sb('rw_ytm', [128, 256], F32, es)
        yo = [g.sb('rw_yo%d' % i, [128, 256], BF16, es) for i in range(2)]
        yT = [g.sb('rw_yT%d' % i, [128, 2, 128], BF16, es) for i in range(2)]
        st8 = g.sb('rw_st8', [128, 8], F32, es)
        tq2 = g.sb('rw_tq2', [128, 256], F32, es)
        gnw = g.sb('rw_gnw', [128, 256], F32, es)
        gnb = g.sb('rw_gnb', [128, 256], F32, es)
        k.dma('sp', gnw[:], W['rwkv_gn_w'][l].partition_broadcast(128), writes=['rw_gnw'])
        k.dma('sp', gnb[:], W['rwkv_gn_b'][l].partition_broadcast(128), writes=['rw_gnb'])
        k.op('pool', lambda e: e.memset(ST[:], 0.0), writes=['rw_ST'])
        for i in range(2):
            k.op('pool', lambda e, i=i: e.memset(Lb[i][:], 0.0), writes=['rw_L%d' % i])
            k.op('pool', lambda e, i=i: e.memset(Rb[i][:], 0.0), writes=['rw_R%d' % i])
        psb = [g.ps[6][:].bitcast(BF16), g.ps[7][:].bitcast(BF16)]
        nsteps = RW_STEPS
        for ch in range(nsteps // TC):
            cb = ch % 2
            t0 = ch * TC
            cbs = str(cb)
            tt = t0 // 512
            for h in range(4):
                hl, hh = h % 2, h // 2
                src = g.rwscr[t0:t0 + TC, :]
                k.dma('sp', Lb[cb][h:h + 1, :, hl * 64:(hl + 1) * 64], src[:, h * 64:(h + 1) * 64].rearrange('(o t) d -> o t d', o=1),
                      reads=['rwscr%d' % (t0 // 128)], writes=['rw_L' + cbs])
                k.dma('sp', Lb[cb][32 + h:33 + h, :, hl * 64:(hl + 1) * 64], src[:, 256 + h * 64:256 + (h + 1) * 64].rearrange('(o t) d -> o t d', o=1),
                      reads=['rwscr%d' % (t0 // 128)], writes=['rw_L' + cbs])
                k.dma('sp', Rb[cb][32 + h:33 + h, :, hh * 64:(hh + 1) * 64], src[:, 512 + h * 64:512 + (h + 1) * 64].rearrange('(o t) d -> o t d', o=1),
                      reads=['rwscr%d' % (t0 // 128)], writes=['rw_Rv' + cbs])
                k.op('pool', lambda e, h=h, hh=hh, hl=hl, cb=cb, t0=t0: e.tensor_scalar(
                    out=KKn[cb][:, :, h], in0=kkT[:, hh, t0:t0 + TC], scalar1=hlm[:, hl:hl + 1], scalar2=None, op0=ALU.mult),
                    reads=['rw_kkT%d_%d' % (hh, tt), 'c_hl_mask'], writes=['rw_KKn' + cbs])
                k.op('pool', lambda e, h=h, hh=hh, hl=hl, cb=cb, t0=t0: e.tensor_scalar(
                    out=Rr[cb][:, :, h], in0=rT[:, hh, t0:t0 + TC], scalar1=hlm[:, 2 + hl:3 + hl], scalar2=None, op0=ALU.mult),
                    reads=['rw_rT%d_%d' % (hh, tt), 'c_hl_mask'], writes=['rw_Rr' + cbs])
            for s_ in range(TC):
                t = t0 + s_
                pa = t % 2
                pu = 2 + t % 2
                py = 4 + (t // 128) % 2
                k.op('pe', lambda e, cb=cb, s_=s_, pa=pa: e.matmul(g.ps[pa][0:4, 0:128], lhsT=KKn[cb][:, s_, :], rhs=ST[:], start=True, stop=True),
                     reads=['rw_KKn' + cbs, 'rw_ST'], writes=[g.psk[pa]])
                k.op('dve', lambda e, cb=cb, s_=s_, pa=pa: e.tensor_tensor(out=Rb[cb][0:4, s_, :], in0=g.ps[pa][0:4, 0:128], in1=sam[0:4, :], op=ALU.mult),
                     reads=[g.psk[pa], 'c_sa_mask'], writes=['rw_R' + cbs])
                k.op('pe', lambda e, cb=cb, s_=s_, pu=pu: e.matmul(g.ps[pu][:, 0:128], lhsT=Lb[cb][:, s_, :], rhs=Rb[cb][:, s_, :], start=True, stop=True),
                     reads=['rw_L' + cbs, 'rw_R' + cbs, 'rw_Rv' + cbs], writes=[g.psk[pu]])
                for hh in range(2):
                    k.op('dve', lambda e, hh=hh, t=t, pu=pu: e.scalar_tensor_tensor(
                        out=ST[:, hh * 64:(hh + 1) * 64], in0=ST[:, hh * 64:(hh + 1) * 64], scalar=wT[:, hh, t:t + 1],
                        in1=g.ps[pu][:, hh * 64:(hh + 1) * 64], op0=ALU.mult, op1=ALU.add),
                        reads=['rw_ST', g.psk[pu], 'rw_wT%d_%d' % (hh, tt)], writes=['rw_ST'])
                k.op('pe', lambda e, cb=cb, s_=s_, t=t, py=py: e.matmul(g.ps[py][:, (t % 128) * 4:(t % 128) * 4 + 4], lhsT=ST[:], rhs=Rr[cb][:, s_, :],
                                                                   start=True, stop=True), reads=['rw_ST', 'rw_Rr' + cbs], writes=[g.psk[py]])
                if t % 128 == 127:
                    t_ = t // 128
                    yb_ = t_ % 2
                    ys = str(yb_)
                    yv = g.ps[py][:].rearrange('p (t h) -> p h t', h=4)
                    for hl in range(2):
                        k.op('act', lambda e, hl=hl, yb_=yb_: e.copy(out=Yc[yb_][0:64, hl, :], in_=yv[0:64, hl, :]), reads=[g.psk[py]],
                             writes=['rw_Yc%s_%d' % (ys, hl)])
                        k.op('act', lambda e, hl=hl, yb_=yb_: e.copy(out=Yc[yb_][64:128, hl, :], in_=yv[64:128, 2 + hl, :]), reads=[g.psk[py]],
                             writes=['rw_Ycb%s_%d' % (ys, hl)])
                    ytv = ytm[:].rearrange('p (hh hl i) -> p hl hh i', hh=2, hl=2)
                    for hl in range(2):
                        pt_ = 6 + hl
                        k.op('pe', lambda e, hl=hl, yb_=yb_, pt_=pt_: e.transpose(out=g.ps[pt_][:, 0:128], in_=Yc[yb_][:, hl, :], identity=g.cs['ident_f'][:]),
                             reads=['rw_Yc%s_%d' % (ys, hl), 'rw_Ycb%s_%d' % (ys, hl), 'c_ident_f'], writes=[g.psk[pt_]])
                        k.op('act', lambda e, hl=hl, pt_=pt_: e.copy(out=ytv[:, hl, :, :], in_=g.ps[pt_][:, 0:128].rearrange('p (hh i) -> p hh i', hh=2)),
                             reads=[g.psk[pt_]], writes=['rw_ytm'])
                    k.op('dve', lambda e: e.reduce_sum(out=st8[:, 0:4], in_=ytm[:].rearrange('p (h d) -> p h d', d=64), axis=AX.X),
                         reads=['rw_ytm'], writes=['rw_st8'])
                    k.op('pool', lambda e: e.tensor_tensor(out=tq2[:], in0=ytm[:], in1=ytm[:], op=ALU.mult), reads=['rw_ytm'], writes=['rw_tq2'])
                    k.op('dve', lambda e: e.reduce_sum(out=st8[:, 4:8], in_=tq2[:].rearrange('p (h d) -> p h d', d=64), axis=AX.X),
                         reads=['rw_tq2'], writes=['rw_st8'])
                    k.op('dve', lambda e: e.tensor_scalar(out=st8[:], in0=st8[:], scalar1=1.0 / 64, scalar2=None, op0=ALU.mult),
                         reads=['rw_st8'], writes=['rw_st8'])
                    k.op('dve', lambda e: e.tensor_tensor(out=tq2[:, 0:4], in0=st8[:, 0:4], in1=st8[:, 0:4], op=ALU.mult), reads=['rw_st8', 'rw_tq2'],
                         writes=['rw_tq2'])
                    k.op('dve', lambda e: e.tensor_tensor(out=st8[:, 4:8], in0=st8[:, 4:8], in1=tq2[:, 0:4], op=ALU.subtract), reads=['rw_st8', 'rw_tq2'],
                         writes=['rw_st8'])
                    k.op('dve', lambda e: e.tensor_scalar(out=st8[:, 4:8], in0=st8[:, 4:8], scalar1=64e-5, scalar2=None, op0=ALU.add),
                         reads=['rw_st8'], writes=['rw_st8'])
                    k.op('act', lambda e: e.activation(out=st8[:, 4:8], in_=st8[:, 4:8], func=AF.Sqrt), reads=['rw_st8'], writes=['rw_st8'])
                    k.op('dve', lambda e: e.reciprocal(out=st8[:, 4:8], in_=st8[:, 4:8]), reads=['rw_st8'], writes=['rw_st8'])
                    for h in range(4):
                        hs = slice(h * 64, (h + 1) * 64)
                        k.op('dve', lambda e, h=h, hs=hs: e.tensor_scalar(out=tq2[:, hs], in0=ytm[:, hs], scalar1=st8[:, h:h + 1], scalar2=st8[:, 4 + h:5 + h],
                                                                         op0=ALU.subtract, op1=ALU.mult), reads=['rw_ytm', 'rw_st8', 'rw_tq2'], writes=['rw_tq2'])
                    k.op('pool', lambda e: e.tensor_tensor(out=tq2[:], in0=tq2[:], in1=gnw[:], op=ALU.mult), reads=['rw_tq2', 'rw_gnw'], writes=['rw_tq2'])
                    k.op('pool', lambda e: e.tensor_tensor(out=tq2[:], in0=tq2[:], in1=gnb[:], op=ALU.add), reads=['rw_tq2', 'rw_gnb'], writes=['rw_tq2'])
                    for h in range(4):
                        hs = slice(h * 64, (h + 1) * 64)
                        k.op('dve', lambda e, h=h, hs=hs, t_=t_: e.scalar_tensor_tensor(out=tq2[:, hs], in0=vtm[:, t_, hs], scalar=bon[:, t_, h:h + 1],
                                                                                       in1=tq2[:, hs], op0=ALU.mult, op1=ALU.add),
                             reads=['rw_vtm%d' % t_, 'rw_bon%d' % t_, 'rw_tq2'], writes=['rw_tq2'])
                    k.op('dve', lambda e, yb_=yb_, t_=t_: e.tensor_tensor(out=yo[yb_][:], in0=tq2[:], in1=gtm[:, t_, :], op=ALU.mult),
                         reads=['rw_tq2', 'rw_gtm%d' % t_], writes=['rw_yo' + ys])
                    for j in range(2):
                        k.op('pe', lambda e, j=j, yb_=yb_: e.transpose(out=psb[yb_][:, j * 128:(j + 1) * 128], in_=yo[yb_][:, j * 128:(j + 1) * 128],
                                                                      identity=g.cs['ident_b'][:]), reads=['rw_yo' + ys, 'c_ident_b'], writes=[g.psk[6 + yb_]])
                    k.op('act', lambda e, yb_=yb_: e.copy(out=yT[yb_][:], in_=psb[yb_][:, 0:256].rearrange('p (j t) -> p j t', j=2)),
                         reads=[g.psk[6 + yb_]], writes=['rw_yT' + ys])
                    k.dma('sp', ycv[:, 0:2, t_ * 128:(t_ + 1) * 128], yT[yb_][:], reads=['rw_yT' + ys], writes=['ycat_rw%d' % t_])
        k.barrier()
```

```python
import contextlib
import numpy as np
import ml_dtypes
import concourse.bass as bass
import concourse.mybir as mybir
from concourse.bass_utils import run_bass_kernel_spmd

F32 = mybir.dt.float32
BF16 = mybir.dt.bfloat16
I32 = mybir.dt.int32
AF = mybir.ActivationFunctionType
ALU = mybir.AluOpType
AX = mybir.AxisListType


class KB:
    NDS = 8

    def __init__(self):
        self.nc = bass.Bass("TRN2", target_bir_lowering=False)
        nc = self.nc
        self.eng = {"pe": nc.tensor, "act": nc.scalar, "dve": nc.vector, "pool": nc.gpsimd, "sp": nc.sync}
        self.semh = {}
        for e in self.eng:
            self.semh[e] = nc.semaphore("s_" + e).__enter__()
        self.cnt = {e: 0 for e in self.eng}
        self.waited = {e: {} for e in self.eng}
        self.lastw = {}
        self.readers = {}
        self.dq = {}
        self.nwaits = 0
        self.nops = 0

    def _deps(self, eng, reads, writes):
        deps = {}

        def add(k, v):
            if deps.get(k, 0) < v:
                deps[k] = v

        for r in reads:
            p = self.lastw.get(r)
            if p is not None:
                if not (p[0] == eng and eng == "pe"):
                    add(*p)
            if isinstance(r, str) and r.startswith("ps") and r[2:].isdigit():
                for k, v in self.readers.get(r, {}).items():
                    if k != eng:
                        add(k, v)
        for w in writes:
            p = self.lastw.get(w)
            if p is not None and p[0] != eng:
                add(*p)
            for k, v in self.readers.get(w, {}).items():
                if k != eng:
                    add(k, v)
        return deps

    def _wait(self, eng, deps):
        wt = self.waited[eng]
        for k, v in deps.items():
            if wt.get(k, 0) >= v:
                continue
            self.eng[eng].wait_ge(self.semh[k], v)
            wt[k] = v
            self.nwaits += 1

    def _record(self, prod, reads, writes):
        k, v = prod
        for r in reads:
            d = self.readers.setdefault(r, {})
            if d.get(k, 0) < v:
                d[k] = v
        for w in writes:
            self.lastw[w] = prod
            self.readers[w] = {}

    def op(self, eng, fn, reads=(), writes=()):
        self._wait(eng, self._deps(eng, reads, writes))
        ins = fn(self.eng[eng])
        self.cnt[eng] += 1
        ins.then_inc(self.semh[eng], 1)
        self._record((eng, self.cnt[eng]), reads, writes)
        self.nops += 1
        return ins

    def dma(self, q, out, in_, reads=(), writes=(), **kw):
        st = self.dq.get(q)
        if st is None:
            st = {"n": 0, "sems": []}
            for i in range(self.NDS):
                key = ("dma", q, i)
                self.semh[key] = self.nc.semaphore("d_%s_%d" % (q, i)).__enter__()
                st["sems"].append(key)
            self.dq[q] = st
        i = st["n"]
        slot = i % self.NDS
        val = 16 * (i // self.NDS + 1)
        key = st["sems"][slot]
        deps = self._deps(q, reads, writes)
        if i >= self.NDS:
            if deps.get(key, 0) < val - 16:
                deps[key] = val - 16
        self._wait(q, deps)
        ins = self.eng[q].dma_start(out=out, in_=in_, **kw)
        ins.then_inc(self.semh[key], 16)
        st["n"] += 1
        self._record((key, val), reads, writes)
        return ins

    def barrier(self):
        deps = {}
        for e in self.eng:
            if self.cnt[e] > 0:
                deps[e] = self.cnt[e]
        for q, st in self.dq.items():
            n = st["n"]
            for slot in range(self.NDS):
                if n > slot:
                    cntslot = (n - 1 - slot) // self.NDS + 1
                    deps[st["sems"][slot]] = 16 * cntslot
        for e in self.eng:
            d = {k: v for k, v in deps.items() if k != e or e != "pe"}
            self._wait(e, d)

    def finish(self):
        self.barrier()


S = 4096
D = 1024
NT = 32
PROJ_W = 3212
L_DEPTH = 2
NORM_EPS = 1e-6
PI = float(np.pi)

PARAM_SHAPES = {
    'w_ada': (2, 1024, 6144), 'b_ada': (2, 6144), 'norm1_g': (2, 1024), 'norm2_g': (2, 1024),
    'w_in': (2, 1024, 3212), 'w_out': (2, 1024, 1024), 'rwkv_mu': (2, 1024), 'rwkv_w0': (2, 256),
    'rwkv_w2': (2, 64, 256), 'rwkv_a0': (2, 256), 'rwkv_a2': (2, 64, 256), 'rwkv_g2': (2, 128, 256),
    'rwkv_kk': (2, 256), 'rwkv_ka': (2, 256), 'rwkv_rk': (2, 4, 64), 'rwkv_gn_w': (2, 256), 'rwkv_gn_b': (2, 256),
    'conv_w': (2, 3, 256), 'dil_q_g': (2, 64), 'dil_k_g': (2, 64), 'nsa_q_g': (2, 64), 'nsa_kc_g': (2, 64),
    'nsa_ks_g': (2, 64), 'nsa_kw_g': (2, 64), 'nsa_pe_k': (2, 32, 64), 'nsa_pe_v': (2, 32, 64),
    'nsa_wk1': (2, 2048, 256), 'nsa_wk2': (2, 256, 64), 'nsa_wv1': (2, 2048, 256), 'nsa_wv2': (2, 256, 64),
    'onorm_g': (2, 768), 'w_router': (2, 1024, 32), 'b_router': (2, 32), 'w_gu': (2, 32, 1024, 2048),
    'b_gu': (2, 32, 2048), 'w_down': (2, 32, 1024, 1024), 'b_down': (2, 32, 1024),
}


def host_consts():
    c = {}
    c['ident_f'] = np.eye(128, dtype=np.float32)
    c['ident_b'] = np.eye(128).astype(ml_dtypes.bfloat16)
    c['ones_b'] = np.ones((128, 128)).astype(ml_dtypes.bfloat16)
    blk = np.zeros((128, 128), np.float32)
    blk[:64, :64] = 1.0
    blk[64:, 64:] = 1.0
    c['blk64_b'] = blk.astype(ml_dtypes.bfloat16)
    sel = np.zeros((32, 32, 128), np.float32)
    for e in range(32):
        sel[e, e, :] = 1.0
    c['sel_b'] = sel.astype(ml_dtypes.bfloat16)
    pr = np.zeros((128, 128), np.float32)
    for m in range(128):
        if m % 64 < 32:
            pr[m + 32, m] = -1.0
        else:
            pr[m - 32, m] = 1.0
    c['prot_b'] = pr.astype(ml_dtypes.bfloat16)
    c['invf'] = (10000.0 ** (-(np.arange(128) % 32) / 32.0)).astype(np.float32).reshape(128, 1)
    kl = np.arange(128)[:, None]
    ql = np.arange(512)[None, :]

    def toep(offs, fn):
        return np.stack([fn(128 * o + ql - kl) for o in offs]).astype(np.float32)

    def dil(d):
        m = ((d >= 0) & (d <= 128)).astype(np.float32)
        m += ((d >= 0) & (d % 4 == 0) & (d // 4 <= 128))
        m += ((d >= 0) & (d % 16 == 0) & (d // 16 <= 128))
        return m
    c['dil_mask'] = toep(range(-3, 17), dil).transpose(1, 0, 2).astype(ml_dtypes.bfloat16).copy()
    c['swa_mask'] = toep(range(-3, 5), lambda d: (d >= 0) & (d <= 511)).transpose(1, 0, 2).astype(ml_dtypes.bfloat16).copy()
    c['cau_mask'] = toep(range(-3, 1), lambda d: d >= 0).transpose(1, 0, 2).astype(ml_dtypes.bfloat16).copy()
    c['cmp_mask'] = np.stack([(16 * kl + 31 <= 512 * i + ql) for i in range(5)]).astype(np.float32).transpose(1, 0, 2) \
        .astype(ml_dtypes.bfloat16).copy()
    diff = np.arange(256)[:, None] - 4 * np.arange(64)[None, :]
    offs = (np.arange(4)[:, None] - np.arange(2)[None, :]).reshape(-1)
    ov = (diff[..., None] == offs).sum(-1).astype(np.float32)
    ov[255] = 0
    ov1 = np.concatenate([np.ones((256, 1), np.float32), ov], axis=1)
    ov1[255] = 0
    c['ovl1'] = ov1.reshape(2, 128, 65).transpose(1, 0, 2).astype(ml_dtypes.bfloat16).copy()
    tok = np.arange(S)[:, None]
    jb = np.arange(64)[None, :]
    cur = tok // 64
    fut = jb > cur
    forced = ((jb == 0) | (jb == cur) | (jb == cur - 1)) & ~fut
    keep = ~(fut | forced)
    c['sel_keep'] = keep.astype(np.float32).reshape(32, 128, 64).transpose(1, 0, 2).copy()
    c['sel_add'] = (-1.0 * fut + 1e4 * forced).astype(np.float32).reshape(32, 128, 64).transpose(1, 0, 2).copy()
    ex = np.zeros((64, 32, 128), np.float32)
    for kt in range(32):
        ex[2 * kt, kt, :64] = 1
        ex[2 * kt + 1, kt, 64:] = 1
    c['sel_exp'] = ex.astype(ml_dtypes.bfloat16)
    sam = np.zeros((128, 128), np.float32)
    sam[0:2, 0:64] = 1.0
    sam[2:4, 64:128] = 1.0
    c['sa_mask'] = sam
    hm = np.zeros((128, 4), np.float32)
    hm[0:64, 0] = -1.0
    hm[64:128, 1] = -1.0
    hm[0:64, 2] = 1.0
    hm[64:128, 3] = 1.0
    c['hl_mask'] = hm
    return c


RESIDENT_CONSTS = ('ident_f', 'ident_b', 'ones_b', 'blk64_b', 'sel_b', 'prot_b', 'invf')


class Ctx:
    pass


class LazyW:
    def __init__(self, nc):
        self.nc = nc
        self.d = {}

    def __getitem__(self, n):
        if n not in self.d:
            self.d[n] = self.nc.dram_tensor(n, list(PARAM_SHAPES[n]), F32, kind='ExternalInput').ap()
        return self.d[n]


def build_program(layers=(0, 1), taps=(), first=True, last=True, mixers='ABCD', moe=True):
    k = KB()
    nc = k.nc
    g = Ctx()
    g.k = k
    g.nc = nc
    g.taps = {}
    g.want = set(taps)
    g.mixers = mixers
    g.do_moe = moe
    g.x = nc.dram_tensor('x', [S, D], F32, kind='ExternalInput').ap()
    g.c = nc.dram_tensor('c', [D], F32, kind='ExternalInput').ap()
    g.pos = nc.dram_tensor('positions', [S], I32, kind='ExternalInput').ap()
    g.W = LazyW(nc)
    g.C = {}
    for n, a in host_consts().items():
        dt = F32 if a.dtype == np.float32 else BF16
        g.C[n] = nc.dram_tensor('const_' + n, list(a.shape), dt, kind='ExternalInput').ap()
    g.out = nc.dram_tensor('out', [S, D], F32, kind='ExternalOutput').ap()
    g.xT = [nc.dram_tensor('xT%d' % i, [D, S], F32, kind='Internal').ap() for i in range(2)]
    g.ycatT = nc.dram_tensor('ycatT', [D, S], BF16, kind='Internal').ap()
    g.h2Td = nc.dram_tensor('h2Td', [D, S], BF16, kind='Internal').ap()
    g.hTd = nc.dram_tensor('hTd', [D, S], BF16, kind='Internal').ap()
    g.rwscr = nc.dram_tensor('rwscr', [S, 768], BF16, kind='Internal').ap()

    with contextlib.ExitStack() as es:
        g.layer = 'i'
        g.nsb = 0

        def sb(name, shape, dt, stack=es):
            g.nsb += 1
            return stack.enter_context(nc.sbuf_tensor('%s_%d' % (name, g.nsb), list(shape), dt))
        g.sb = sb
        g.ps = [nc.alloc_psum_tensor('ps%d' % i, [128, 512], F32) for i in range(8)]
        g.psk = ['ps%d' % i for i in range(8)]
        g.cs = {}
        for n, ap in g.C.items():
            if n not in RESIDENT_CONSTS:
                continue
            t = sb('c_' + n, ap.shape, ap.dtype)
            k.dma('sp', t[:], ap, writes=['c_' + n])
            g.cs[n] = t
        g.mod = sb('mod', [128, 48], F32)
        g.GT = sb('GT', [32, S], BF16)
        if first:
            phase_pre(g)
        cur = 0
        for l in layers:
            g.layer = str(l)
            phase_mod(g, l)
            with contextlib.ExitStack() as les:
                phase_norm1(g, l, g.xT[cur])
                g.onorm = g.sb('onorm', [128, 6], F32, les)
                k.dma('sp', g.onorm[:], g.W['onorm_g'][l].rearrange('(j p) -> p j', p=128), writes=['onorm'],
                      allow_slow_non_contiguous=True)
                if 'B' in g.mixers:
                    mixer_conv(g, l)
                if 'C' in g.mixers:
                    mixer_dil(g, l)
                if 'D' in g.mixers:
                    mixer_nsa(g, l)
                if 'A' in g.mixers:
                    mixer_rwkv(g, l)
                zero_missing(g)
                t = tap(g, 'ycatT%d' % l, [D, S], BF16)
                if t is not None:
                    k.barrier()
                    k.dma('sp', t, g.ycatT, writes=['tap'])
                k.barrier()
            phase_wout(g, l, g.xT[cur], g.xT[cur ^ 1])
            phase_norm2_router(g, l, g.xT[cur ^ 1])
            phase_moe(g, l, g.xT[cur ^ 1], g.xT[cur], final=(last and l == layers[-1]))
        k.finish()
    return k, g


def tap(g, name, shape, dt=F32):
    if name not in g.want:
        return None
    t = g.nc.dram_tensor('tap_' + name, list(shape), dt, kind='ExternalOutput').ap()
    g.taps[name] = t
    return t


def phase_pre(g):
    k, nc = g.k, g.nc
    identf = g.cs['ident_f']
    xTd = g.xT[0].rearrange('(c p) t -> p c t', p=128)
    with contextlib.ExitStack() as es:
        xin = [g.sb('pre_x%d' % i, [128, D], F32, es) for i in range(2)]
        xo = [g.sb('pre_o%d' % i, [128, 8, 128], F32, es) for i in range(2)]
        for t in range(NT):
            b = t % 2
            k.dma('sp', xin[b][:], g.x[t * 128:(t + 1) * 128, :], writes=['pre_x%d' % b])
            for h in range(2):
                pi = (2 * t + h) % 4
                for j in range(4):
                    cc = h * 4 + j
                    k.op('pe', lambda e, cc=cc, j=j, pi=pi, b=b: e.transpose(
                        out=g.ps[pi][:, j * 128:(j + 1) * 128], in_=xin[b][:, cc * 128:(cc + 1) * 128], identity=identf[:]),
                        reads=['pre_x%d' % b, 'c_ident_f'], writes=[g.psk[pi]])
                eng = 'act' if h == 0 else 'dve'
                if eng == 'act':
                    k.op('act', lambda e, pi=pi, b=b, h=h: e.copy(
                        out=xo[b][:, h * 4:(h + 1) * 4, :], in_=g.ps[pi][:].rearrange('p (c t) -> p c t', c=4)),
                        reads=[g.psk[pi]], writes=['pre_o%d_%d' % (b, h)])
                else:
                    k.op('dve', lambda e, pi=pi, b=b, h=h: e.tensor_copy(
                        xo[b][:, h * 4:(h + 1) * 4, :], g.ps[pi][:].rearrange('p (c t) -> p c t', c=4)),
                        reads=[g.psk[pi]], writes=['pre_o%d_%d' % (b, h)])
            k.dma('sp', xTd[:, :, t * 128:(t + 1) * 128], xo[b][:], reads=['pre_o%d_0' % b, 'pre_o%d_1' % b],
                  writes=['xT0_%d' % (t // 4)])
        k.barrier()


def phase_mod(g, l):
    k, nc = g.k, g.nc
    with contextlib.ExitStack() as es:
        cT = g.sb('cT', [128, 8], F32, es)
        cs = g.sb('cs', [128, 8], BF16, es)
        bcol = g.sb('bcol', [128, 48], F32, es)
        wt = [g.sb('wada%d' % i, [128, 8, 512], BF16, es) for i in range(2)]
        k.dma('sp', cT[:], g.c.rearrange('(k p) -> p k', p=128), writes=['cT'], allow_slow_non_contiguous=True)
        k.dma('sp', bcol[:], g.W['b_ada'][l].rearrange('(k p) -> p k', p=128), writes=['bcol'], allow_slow_non_contiguous=True)
        k.op('act', lambda e: e.activation(out=cs[:], in_=cT[:], func=AF.Silu), reads=['cT'], writes=['cs'])
        psm = g.ps[7]
        for ct in range(12):
            b = ct % 2
            k.dma('pool', wt[b][:], g.W['w_ada'][l][:, ct * 512:(ct + 1) * 512].rearrange('(k p) c -> p k c', p=128),
                  writes=['wada%d' % b])
            for j in range(4):
                col = ct * 4 + j
                for kk in range(8):
                    k.op('pe', lambda e, b=b, j=j, kk=kk, col=col: e.matmul(
                        psm[:, col:col + 1], lhsT=wt[b][:, kk, j * 128:(j + 1) * 128], rhs=cs[:, kk:kk + 1],
                        start=(kk == 0), stop=(kk == 7)),
                        reads=['wada%d' % b, 'cs'], writes=[g.psk[7]])
        k.op('dve', lambda e: e.tensor_tensor(out=g.mod[:], in0=psm[:, 0:48], in1=bcol[:], op=ALU.add),
             reads=[g.psk[7], 'bcol'], writes=['mod'])
        t = tap(g, 'mod%d' % l, [128, 48])
        if t is not None:
            k.dma('sp', t, g.mod[:], reads=['mod'], writes=['tap'])
        k.barrier()


def phase_norm1(g, l, xTd):
    k, nc = g.k, g.nc
    xTv = xTd.rearrange('(c p) t -> p c t', p=128)
    with contextlib.ExitStack() as es:
        gcol = g.sb('n1_g', [128, 8], F32, es)
        g1s = g.sb('n1_gs', [128, 8], F32, es)
        k.dma('sp', gcol[:], g.W['norm1_g'][l].rearrange('(k p) -> p k', p=128), writes=['n1_g'], allow_slow_non_contiguous=True)
        k.op('dve', lambda e: e.scalar_tensor_tensor(out=g1s[:], in0=g.mod[:, 8:16], scalar=1.0, in1=gcol[:],
                                                      op0=ALU.add, op1=ALU.mult), reads=['mod', 'n1_g'], writes=['n1_gs'])
        hT = g.sb('hT', [128, 8, S], BF16, es)
        norm_tiles(g, es, xTv, g1s, g.mod[:, 0:8], ['n1_gs', 'mod'], hT, 'hT', 'n1')
        hv = g.hTd.rearrange('(c p) t -> p c t', p=128)
        for c in range(8):
            k.dma('sp', hv[:, c, :], hT[:, c, :], reads=['hT%d' % c], writes=['hTd'])
        t = tap(g, 'h%d' % l, [128, 8, S], BF16)
        if t is not None:
            k.dma('sp', t, hT[:], reads=['hT%d' % i for i in range(8)], writes=['tap'])
        k.barrier()


def norm_tiles(g, es, xTv, gs, sh, gkeys, hT, hkey, pfx, xsrc_keys=None):
    k = g.k
    xt = [g.sb(pfx + '_x%d' % i, [128, 8, 512], F32, es) for i in range(2)]
    sq = [g.sb(pfx + '_sq%d' % i, [128, 8, 512], BF16, es) for i in range(2)]
    rstd = [g.sb(pfx + '_rstd%d' % i, [128, 512], F32, es) for i in range(2)]
    tmp = [g.sb(pfx + '_tmp%d' % i, [128, 512], F32, es) for i in range(2)]
    ones = g.cs['ones_b']
    for tt in range(8):
        b = tt % 2
        ts = slice(tt * 512, (tt + 1) * 512)
        xk, sk, rk = pfx + '_x%d' % b, pfx + '_sq%d' % b, pfx + '_rstd%d' % b
        k.dma('sp', xt[b][:], xTv[:, :, ts], reads=(xsrc_keys or []), writes=[xk])
        k.op('act', lambda e, b=b: e.activation(out=sq[b][:], in_=xt[b][:], func=AF.Square), reads=[xk], writes=[sk])
        pi = tt % 2
        for c in range(8):
            k.op('pe', lambda e, b=b, c=c, pi=pi: e.matmul(g.ps[pi][:], lhsT=ones[:], rhs=sq[b][:, c, :],
                                                          start=(c == 0), stop=(c == 7)),
                 reads=[sk, 'c_ones_b'], writes=[g.psk[pi]])
        k.op('dve', lambda e, b=b, pi=pi: e.tensor_scalar(out=rstd[b][:], in0=g.ps[pi][:], scalar1=1.0 / D, scalar2=NORM_EPS,
                                                         op0=ALU.mult, op1=ALU.add), reads=[g.psk[pi]], writes=[rk])
        k.op('act', lambda e, b=b: e.activation(out=rstd[b][:], in_=rstd[b][:], func=AF.Sqrt), reads=[rk], writes=[rk])
        k.op('dve', lambda e, b=b: e.reciprocal(out=rstd[b][:], in_=rstd[b][:]), reads=[rk], writes=[rk])
        for c in range(8):
            tb = c % 2
            tk = pfx + '_tmp%d' % tb
            k.op('dve', lambda e, b=b, c=c, tb=tb: e.tensor_tensor(out=tmp[tb][:], in0=xt[b][:, c, :], in1=rstd[b][:], op=ALU.mult),
                 reads=[xk, rk], writes=[tk])
            k.op('act', lambda e, c=c, tb=tb, ts=ts: e.activation(out=hT[:, c, ts], in_=tmp[tb][:], func=AF.Identity,
                                                                  scale=gs[:, c:c + 1], bias=sh[:, c:c + 1]),
                 reads=[tk] + gkeys, writes=[hkey + '%d' % c])


def make_in_maps(inputs, g, n_cores=8):
    consts = host_consts()
    maps = []
    shared = {n: np.ascontiguousarray(inputs[n], dtype=np.float32) for n in g.W.d}
    for b in range(n_cores):
        m = {'x': np.ascontiguousarray(inputs['x'][b]), 'c': np.ascontiguousarray(inputs['c'][b]),
             'positions': np.ascontiguousarray(inputs['positions'][b]).astype(np.int32)}
        m.update(shared)
        for n, a in consts.items():
            m['const_' + n] = a
        maps.append(m)
    return maps


def kernel(**inputs):
    k, g = build_program()
    maps = make_in_maps(inputs, g)
    res = run_bass_kernel_spmd(k.nc, maps, core_ids=list(range(8)))
    return np.stack([r['out'] for r in res.results], axis=0)


def load_w_bf16(g, dst, dkey, src_ap):
    g.k.dma('pool', dst, src_ap.rearrange('(c p) n -> p c n', p=128), writes=[dkey])


def ht_loader(g, es, pfx):
    bufs = [g.sb(pfx + '_ht%d' % i, [128, 8, 512], BF16, es) for i in range(2)]
    hv = g.hTd.rearrange('(c p) t -> p c t', p=128)

    def load(tt):
        b = tt % 2
        g.k.dma('sp', bufs[b][:], hv[:, :, tt * 512:(tt + 1) * 512], reads=['hTd'], writes=[pfx + '_ht%d' % b])
        return bufs[b], pfx + '_ht%d' % b
    return load


def proj_fm(g, wsb, wkey, c0, ncols, ht, hkey, ps, pskey):
    for c in range(8):
        g.k.op('pe', lambda e, c=c: e.matmul(ps, lhsT=wsb[:, c, c0:c0 + ncols], rhs=ht[:, c, :],
                                             start=(c == 0), stop=(c == 7)),
               reads=[wkey, hkey], writes=[pskey])


def proj_tm(g, wsb, wkey, c0, ncols, ht, hkey, sub, ps, pskey):
    for c in range(8):
        g.k.op('pe', lambda e, c=c: e.matmul(ps, lhsT=ht[:, c, sub * 128:(sub + 1) * 128], rhs=wsb[:, c, c0:c0 + ncols],
                                             start=(c == 0), stop=(c == 7)),
               reads=[wkey, hkey], writes=[pskey])


def head_rmsnorm_fm(g, y, ykey, sq, sqkey, gcol, gkeys, out_ap, okey, pi, tmpf, tmpkey):
    k = g.k
    k.op('act', lambda e: e.activation(out=sq, in_=y, func=AF.Square), reads=[ykey], writes=[sqkey])
    k.op('pe', lambda e: e.matmul(g.ps[pi][:], lhsT=g.cs['blk64_b'][:], rhs=sq, start=True, stop=True),
         reads=[sqkey, 'c_blk64_b'], writes=[g.psk[pi]])
    k.op('dve', lambda e: e.tensor_scalar(out=tmpf, in0=g.ps[pi][:], scalar1=1.0 / 64, scalar2=NORM_EPS, op0=ALU.mult, op1=ALU.add),
         reads=[g.psk[pi]], writes=[tmpkey])
    k.op('act', lambda e: e.activation(out=tmpf, in_=tmpf, func=AF.Sqrt), reads=[tmpkey], writes=[tmpkey])
    k.op('dve', lambda e: e.reciprocal(out=tmpf, in_=tmpf), reads=[tmpkey], writes=[tmpkey])
    k.op('dve', lambda e: e.scalar_tensor_tensor(out=out_ap, in0=y, scalar=gcol, in1=tmpf, op0=ALU.mult, op1=ALU.mult),
         reads=[ykey, tmpkey] + gkeys, writes=[okey])


def mixer_conv(g, l):
    k = g.k
    ycv = g.ycatT.rearrange('(c p) t -> p c t', p=128)
    with contextlib.ExitStack() as es:
        wb = g.sb('cv_w', [128, 8, 768], BF16, es)
        load_w_bf16(g, wb[:], 'cv_w', g.W['w_in'][l][:, 1024:1792])
        cw = g.sb('cv_cw', [128, 2, 3], F32, es)
        for kk in range(3):
            k.dma('sp', cw[:, :, kk], g.W['conv_w'][l, kk].rearrange('(j p) -> p j', p=128), writes=['cv_cw'],
                  allow_slow_non_contiguous=True)
        bg = g.sb('cv_bg', [128, 2, S], BF16, es)
        u = g.sb('cv_u', [128, 2, S + 2], F32, es)
        cgt = [g.sb('cv_cg%d' % i, [128, 512], F32, es) for i in range(2)]
        k.op('pool', lambda e: e.memset(u[:, :, 0:2], 0.0), writes=['cv_u_h'])
        n = 0
        hload = ht_loader(g, es, 'cv')
        for tt in range(8):
            ts = slice(tt * 512, (tt + 1) * 512)
            ht, hk = hload(tt)
            for j in range(2):
                pa, pb, pc = n % 6, (n + 1) % 6, (n + 2) % 6
                n += 3
                proj_fm(g, wb, 'cv_w', j * 128, 128, ht, hk, g.ps[pa][:], g.psk[pa])
                proj_fm(g, wb, 'cv_w', 256 + j * 128, 128, ht, hk, g.ps[pb][:], g.psk[pb])
                proj_fm(g, wb, 'cv_w', 512 + j * 128, 128, ht, hk, g.ps[pc][:], g.psk[pc])
                k.op('act', lambda e, j=j, ts=ts, pa=pa: e.copy(out=bg[:, j, ts], in_=g.ps[pa][:]), reads=[g.psk[pa]],
                     writes=['cv_bg%d_%d' % (j, tt)])
                cb = n % 2
                k.op('act', lambda e, cb=cb, pb=pb: e.copy(out=cgt[cb][:], in_=g.ps[pb][:]), reads=[g.psk[pb]], writes=['cv_cg%d' % cb])
                k.op('dve', lambda e, j=j, tt=tt, cb=cb, pc=pc: e.tensor_tensor(out=u[:, j, 2 + tt * 512:2 + (tt + 1) * 512],
                                                                                 in0=g.ps[pc][:], in1=cgt[cb][:], op=ALU.mult),
                     reads=[g.psk[pc], 'cv_cg%d' % cb], writes=['cv_u%d_%d' % (j, tt)])
        y = [g.sb('cv_y%d' % i, [128, 512], F32, es) for i in range(2)]
        sq = [g.sb('cv_sq%d' % i, [128, 512], BF16, es) for i in range(2)]
        tf = [g.sb('cv_tf%d' % i, [128, 512], F32, es) for i in range(2)]
        yo = [g.sb('cv_yo%d' % i, [128, 2, 512], BF16, es) for i in range(2)]
        n = 0
        for tt in range(8):
            ts = slice(tt * 512, (tt + 1) * 512)
            ob = tt % 2
            for j in range(2):
                b = n % 2
                n += 1
                yk = 'cv_y%d' % b
                ukeys = ['cv_u%d_%d' % (j, tt), 'cv_u_h'] + (['cv_u%d_%d' % (j, tt - 1)] if tt > 0 else [])
                k.op('act', lambda e, b=b, j=j, tt=tt: e.activation(out=y[b][:], in_=u[:, j, 2 + tt * 512:2 + (tt + 1) * 512],
                                                                   func=AF.Copy, scale=cw[:, j, 2:3]),
                     reads=ukeys + ['cv_cw'], writes=[yk])
                for kk in (1, 0):
                    k.op('dve', lambda e, b=b, j=j, tt=tt, kk=kk: e.scalar_tensor_tensor(
                        out=y[b][:], in0=u[:, j, kk + tt * 512:kk + (tt + 1) * 512], scalar=cw[:, j, kk:kk + 1], in1=y[b][:],
                        op0=ALU.mult, op1=ALU.add), reads=ukeys + ['cv_cw', yk], writes=[yk])
                k.op('dve', lambda e, b=b, j=j, ts=ts: e.tensor_tensor(out=y[b][:], in0=y[b][:], in1=bg[:, j, ts], op=ALU.mult),
                     reads=[yk, 'cv_bg%d_%d' % (j, tt)], writes=[yk])
                head_rmsnorm_fm(g, y[b][:], yk, sq[b][:], 'cv_sq%d' % b, g.onorm[:, j:j + 1], ['onorm'], yo[ob][:, j, :],
                                'cv_yo%d_%d' % (ob, j), 6 + b, tf[b][:], 'cv_tf%d' % b)
            k.dma('sp', ycv[:, 2:4, ts], yo[ob][:], reads=['cv_yo%d_0' % ob, 'cv_yo%d_1' % ob], writes=['ycat_B%d' % tt])
        k.barrier()


def phase_wout(g, l, xT_old, xT_new):
    k = g.k
    ycv = g.ycatT.rearrange('(c p) t -> p c t', p=128)
    xo = xT_old.rearrange('(c p) t -> p c t', p=128)
    xn = xT_new.rearrange('(c p) t -> p c t', p=128)
    with contextlib.ExitStack() as es:
        wo = g.sb('wo_w', [128, 8, D], BF16, es)
        load_w_bf16(g, wo[:], 'wo_w', g.W['w_out'][l])
        yc = [g.sb('wo_yc%d' % i, [128, 8, 512], BF16, es) for i in range(2)]
        xt = [g.sb('wo_x%d' % i, [128, 8, 512], F32, es) for i in range(2)]
        n = 0
        for tt in range(8):
            b = tt % 2
            ts = slice(tt * 512, (tt + 1) * 512)
            k.dma('sp', yc[b][:], ycv[:, :, ts], writes=['wo_yc%d' % b])
            k.dma('sp', xt[b][:], xo[:, :, ts], writes=['wo_x%d' % b])
            for dc in range(8):
                pi = n % 4
                n += 1
                for c in range(8):
                    k.op('pe', lambda e, b=b, c=c, dc=dc, pi=pi: e.matmul(g.ps[pi][:], lhsT=wo[:, c, dc * 128:(dc + 1) * 128],
                                                                         rhs=yc[b][:, c, :], start=(c == 0), stop=(c == 7)),
                         reads=['wo_w', 'wo_yc%d' % b], writes=[g.psk[pi]])
                k.op('dve', lambda e, b=b, dc=dc, pi=pi: e.scalar_tensor_tensor(
                    out=xt[b][:, dc, :], in0=g.ps[pi][:], scalar=g.mod[:, 16 + dc:17 + dc], in1=xt[b][:, dc, :],
                    op0=ALU.mult, op1=ALU.add), reads=[g.psk[pi], 'mod', 'wo_x%d' % b], writes=['wo_x%d' % b])
            k.dma('sp', xn[:, :, ts], xt[b][:], reads=['wo_x%d' % b], writes=['xTn_%d' % tt])
        t = tap(g, 'x1T%d' % l, [128, 8, S])
        if t is not None:
            k.barrier()
            k.dma('sp', t, xn, writes=['tap'])
        k.barrier()


def phase_norm2_router(g, l, xT1):
    k = g.k
    xv = xT1.rearrange('(c p) t -> p c t', p=128)
    h2v = g.h2Td.rearrange('(c p) t -> p c t', p=128)
    with contextlib.ExitStack() as es:
        gcol = g.sb('n2_g', [128, 8], F32, es)
        g2s = g.sb('n2_gs', [128, 8], F32, es)
        k.dma('sp', gcol[:], g.W['norm2_g'][l].rearrange('(k p) -> p k', p=128), writes=['n2_g'], allow_slow_non_contiguous=True)
        k.op('dve', lambda e: e.scalar_tensor_tensor(out=g2s[:], in0=g.mod[:, 32:40], scalar=1.0, in1=gcol[:],
                                                      op0=ALU.add, op1=ALU.mult), reads=['mod', 'n2_g'], writes=['n2_gs'])
        h2 = g.sb('n2_h', [128, 8, S], BF16, es)
        norm_tiles(g, es, xv, g2s, g.mod[:, 24:32], ['n2_gs', 'mod'], h2, 'n2_h', 'n2')
        for c in range(8):
            k.dma('sp', h2v[:, c, :], h2[:, c, :], reads=['n2_h%d' % c], writes=['h2Td%d' % c])
        t = tap(g, 'h2T%d' % l, [128, 8, S], BF16)
        if t is not None:
            k.dma('sp', t, h2[:], reads=['n2_h%d' % c for c in range(8)], writes=['tap'])
        wr = g.sb('rt_w', [128, 8, 32], BF16, es)
        load_w_bf16(g, wr[:], 'rt_w', g.W['w_router'][l])
        br = g.sb('rt_b', [1, 32], BF16, es)
        k.dma('pool', br[:], g.W['b_router'][l:l + 1, :], writes=['rt_b'])
        lg = [g.sb('rt_lg%d' % i, [128, 32], F32, es) for i in range(2)]
        m8 = [g.sb('rt_m8%d' % i, [128, 8], F32, es) for i in range(2)]
        nm = [g.sb('rt_nm%d' % i, [128, 1], F32, es) for i in range(2)]
        ex = [g.sb('rt_ex%d' % i, [128, 32], F32, es) for i in range(2)]
        mk = [g.sb('rt_mk%d' % i, [128, 32], F32, es) for i in range(2)]
        sm = [g.sb('rt_sm%d' % i, [128, 1], F32, es) for i in range(2)]
        Gt = [g.sb('rt_G%d' % i, [128, 32], F32, es) for i in range(2)]
        gtap = tap(g, 'G%d' % l, [S, 32])
        for t_ in range(NT):
            b = t_ % 2
            pi = t_ % 2
            tk = slice(t_ * 128, (t_ + 1) * 128)
            for c in range(8):
                k.op('pe', lambda e, c=c, tk=tk, pi=pi: e.matmul(g.ps[pi][:, 0:32], lhsT=h2[:, c, tk], rhs=wr[:, c, :],
                                                               start=(c == 0), stop=False),
                     reads=['n2_h%d' % c, 'rt_w'], writes=[g.psk[pi]])
            k.op('pe', lambda e, pi=pi: e.matmul(g.ps[pi][:, 0:32], lhsT=g.cs['ones_b'][0:1, :], rhs=br[:], start=False, stop=True),
                 reads=['c_ones_b', 'rt_b'], writes=[g.psk[pi]])
            s = str(b)
            k.op('act', lambda e, b=b, pi=pi: e.copy(out=lg[b][:], in_=g.ps[pi][:, 0:32]), reads=[g.psk[pi]], writes=['rt_lg' + s])
            k.op('dve', lambda e, b=b: e.max(out=m8[b][:], in_=lg[b][:]), reads=['rt_lg' + s], writes=['rt_m8' + s])
            k.op('dve', lambda e, b=b: e.tensor_scalar(out=nm[b][:], in0=m8[b][:, 0:1], scalar1=-1.0, scalar2=None, op0=ALU.mult),
                 reads=['rt_m8' + s], writes=['rt_nm' + s])
            k.op('act', lambda e, b=b: e.activation(out=ex[b][:], in_=lg[b][:], func=AF.Exp, bias=nm[b][:], scale=1.0),
                 reads=['rt_lg' + s, 'rt_nm' + s], writes=['rt_ex' + s])
            k.op('dve', lambda e, b=b: e.tensor_scalar(out=mk[b][:], in0=lg[b][:], scalar1=m8[b][:, 3:4], scalar2=None, op0=ALU.is_ge),
                 reads=['rt_lg' + s, 'rt_m8' + s], writes=['rt_mk' + s])
            k.op('dve', lambda e, b=b: e.tensor_tensor(out=ex[b][:], in0=ex[b][:], in1=mk[b][:], op=ALU.mult),
                 reads=['rt_ex' + s, 'rt_mk' + s], writes=['rt_ex' + s])
            k.op('dve', lambda e, b=b: e.reduce_sum(out=sm[b][:], in_=ex[b][:], axis=AX.X), reads=['rt_ex' + s], writes=['rt_sm' + s])
            k.op('dve', lambda e, b=b: e.reciprocal(out=sm[b][:], in_=sm[b][:]), reads=['rt_sm' + s], writes=['rt_sm' + s])
            k.op('dve', lambda e, b=b: e.tensor_scalar(out=Gt[b][:], in0=ex[b][:], scalar1=sm[b][:], scalar2=None, op0=ALU.mult),
                 reads=['rt_ex' + s, 'rt_sm' + s], writes=['rt_G' + s])
            if gtap is not None:
                k.dma('sp', gtap[tk, :], Gt[b][:], reads=['rt_G' + s], writes=['tap'])
            pj = 2 + t_ % 2
            k.op('pe', lambda e, b=b, pj=pj: e.transpose(out=g.ps[pj][0:32, 0:128], in_=Gt[b][:], identity=g.cs['ident_f'][:]),
                 reads=['rt_G' + s, 'c_ident_f'], writes=[g.psk[pj]])
            k.op('act', lambda e, tk=tk, pj=pj: e.copy(out=g.GT[:, tk], in_=g.ps[pj][0:32, 0:128]), reads=[g.psk[pj]],
                 writes=['GT%d' % (t_ // 4)])
        k.barrier()


def phase_moe(g, l, xT1, xT2, final):
    k = g.k
    x1v = xT1.rearrange('(c p) t -> p c t', p=128)
    x2v = xT2.rearrange('(c p) t -> p c t', p=128)
    h2v = g.h2Td.rearrange('(c p) t -> p c t', p=128)
    NE = 32 if g.do_moe else 0
    QT = 1024
    with contextlib.ExitStack() as es:
        bgc = g.sb('me_bgc', [128, 16, 32], F32, es)
        bdr = g.sb('me_bdr', [32, D], BF16, es)
        k.dma('pool', bdr[:], g.W['b_down'][l], writes=['me_bdr'])
        with contextlib.ExitStack() as es2:
            bgr = g.sb('me_bgr', [32, 2048], F32, es2)
            k.dma('sp', bgr[:], g.W['b_gu'][l], writes=['me_bgr'])
            for j in range(16):
                pi = j % 2
                k.op('pe', lambda e, j=j, pi=pi: e.transpose(out=g.ps[pi][:, 0:32], in_=bgr[:, j * 128:(j + 1) * 128],
                                                             identity=g.cs['ident_f'][0:32, 0:32]),
                     reads=['me_bgr', 'c_ident_f'], writes=[g.psk[pi]])
                k.op('act', lambda e, j=j, pi=pi: e.copy(out=bgc[:, j, :], in_=g.ps[pi][:, 0:32]), reads=[g.psk[pi]], writes=['me_bgc'])
            k.barrier()
        wgu = [g.sb('me_wgu%d' % i, [128, 8, 2048], BF16, es) for i in range(2)]
        wdn = [g.sb('me_wdn%d' % i, [128, 8, D], BF16, es) for i in range(2)]
        h2q = g.sb('me_h2', [128, 8, QT], BF16, es)
        yacc = g.sb('me_yacc', [128, 8, QT], F32, es)
        act = [g.sb('me_act%d' % i, [128, 8, 512], BF16, es) for i in range(2)]
        gbc = [g.sb('me_gbc%d' % i, [128, 512], BF16, es) for i in range(2)]
        NSET = 3
        gq = [g.sb('me_gq%d' % i, [128, 512], F32, es) for i in range(NSET)]
        sg = [g.sb('me_sg%d' % i, [128, 512], F32, es) for i in range(NSET)]
        uq = [g.sb('me_uq%d' % i, [128, 512], F32, es) for i in range(NSET)]
        k.op('dve', lambda e: e.tensor_scalar(out=bgc[:, 8:16, :], in0=bgc[:, 8:16, :], scalar1=1.0, scalar2=None, op0=ALU.add),
             reads=['me_bgc'], writes=['me_bgc'])
        nf = 0
        xrow = [g.sb('me_xr%d' % i, [128, D], F32, es) for i in range(1)] if final else None
        nw = 0
        nit = 0
        npsum = 0
        for q in range(S // QT):
            qs = slice(q * QT, (q + 1) * QT)
            k.dma('sp', h2q[:], h2v[:, :, qs], reads=['h2Td%d' % c for c in range(8)], writes=['me_h2'])
            for e_ in range(NE):
                wb = nw % 2
                nw += 1
                gk, dk = 'me_wgu%d' % wb, 'me_wdn%d' % wb
                load_w_bf16(g, wgu[wb][:], gk, g.W['w_gu'][l, e_])
                load_w_bf16(g, wdn[wb][:], dk, g.W['w_down'][l, e_])
                for tl in range(QT // 512):
                    ts = slice(tl * 512, (tl + 1) * 512)
                    gts = slice(q * QT + tl * 512, q * QT + (tl + 1) * 512)
                    ab = nit % 2
                    nit += 1
                    ak = 'me_act%d' % ab
                    pg = 7
                    k.op('pe', lambda e, e_=e_, gts=gts, pg=pg: e.matmul(g.ps[pg][:], lhsT=g.cs['sel_b'][:, e_, :], rhs=g.GT[:, gts],
                                                                        start=True, stop=True),
                         reads=['c_sel_b'] + ['GT%d' % i for i in range(8)], writes=[g.psk[pg]])
                    k.op('act', lambda e, ab=ab, pg=pg: e.copy(out=gbc[ab][:], in_=g.ps[pg][:]), reads=[g.psk[pg]], writes=['me_gbc%d' % ab])
                    for f in range(8):
                        fb = nf % NSET
                        nf += 1
                        pa, pb = 2 * fb, 2 * fb + 1
                        for c in range(8):
                            k.op('pe', lambda e, c=c, f=f, wb=wb, ts=ts, pa=pa: e.matmul(
                                g.ps[pa][:], lhsT=wgu[wb][:, c, f * 128:(f + 1) * 128], rhs=h2q[:, c, ts], start=(c == 0), stop=(c == 7)),
                                reads=[gk, 'me_h2'], writes=[g.psk[pa]])
                        for c in range(8):
                            k.op('pe', lambda e, c=c, f=f, wb=wb, ts=ts, pb=pb: e.matmul(
                                g.ps[pb][:], lhsT=wgu[wb][:, c, 1024 + f * 128:1024 + (f + 1) * 128], rhs=h2q[:, c, ts],
                                start=(c == 0), stop=(c == 7)), reads=[gk, 'me_h2'], writes=[g.psk[pb]])
                        fs = str(fb)
                        k.op('dve', lambda e, f=f, fb=fb, pa=pa, e_=e_: e.tensor_scalar(
                            out=gq[fb][:], in0=g.ps[pa][:], scalar1=bgc[:, f, e_:e_ + 1], scalar2=7.0, op0=ALU.add, op1=ALU.min),
                            reads=[g.psk[pa], 'me_bgc'], writes=['me_gq' + fs])
                        k.op('act', lambda e, fb=fb: e.activation(out=sg[fb][:], in_=gq[fb][:], func=AF.Sigmoid, scale=1.702),
                             reads=['me_gq' + fs], writes=['me_sg' + fs])
                        k.op('dve', lambda e, f=f, fb=fb, pb=pb, e_=e_: e.tensor_scalar(
                            out=uq[fb][:], in0=g.ps[pb][:], scalar1=bgc[:, 8 + f, e_:e_ + 1], scalar2=-6.0, op0=ALU.add, op1=ALU.max),
                            reads=[g.psk[pb], 'me_bgc'], writes=['me_uq' + fs])
                        k.op('pool', lambda e, fb=fb: e.tensor_tensor(out=sg[fb][:], in0=sg[fb][:], in1=gq[fb][:], op=ALU.mult),
                             reads=['me_sg' + fs, 'me_gq' + fs], writes=['me_sg' + fs])
                        k.op('dve', lambda e, fb=fb: e.scalar_tensor_tensor(out=uq[fb][:], in0=uq[fb][:], scalar=8.0, in1=sg[fb][:],
                                                                            op0=ALU.min, op1=ALU.mult),
                             reads=['me_sg' + fs, 'me_uq' + fs], writes=['me_uq' + fs])
                        k.op('pool', lambda e, f=f, fb=fb, ab=ab: e.tensor_tensor(out=act[ab][:, f, :], in0=uq[fb][:], in1=gbc[ab][:], op=ALU.mult),
                             reads=['me_uq' + fs, 'me_gbc%d' % ab], writes=[ak])
                    for dc in range(8):
                        pd = 6 + dc % 2
                        for f in range(8):
                            k.op('pe', lambda e, f=f, dc=dc, wb=wb, ab=ab, pd=pd: e.matmul(
                                g.ps[pd][:], lhsT=wdn[wb][:, f, dc * 128:(dc + 1) * 128], rhs=act[ab][:, f, :],
                                start=(f == 0), stop=(f == 7 and e_ != 0)), reads=[dk, ak], writes=[g.psk[pd]])
                        if e_ == 0:
                            k.op('pe', lambda e, dc=dc, gts=gts, pd=pd: e.matmul(
                                g.ps[pd][:], lhsT=bdr[:, dc * 128:(dc + 1) * 128], rhs=g.GT[:, gts], start=False, stop=True),
                                reads=['me_bdr'] + ['GT%d' % i for i in range(8)], writes=[g.psk[pd]])
                            k.op('dve', lambda e, dc=dc, ts=ts, pd=pd: e.tensor_copy(yacc[:, dc, ts], g.ps[pd][:]),
                                 reads=[g.psk[pd]], writes=['me_yacc'])
                        else:
                            k.op('dve', lambda e, dc=dc, ts=ts, pd=pd: e.tensor_tensor(out=yacc[:, dc, ts], in0=yacc[:, dc, ts],
                                                                                    in1=g.ps[pd][:], op=ALU.add),
                                 reads=[g.psk[pd], 'me_yacc'], writes=['me_yacc'])
            if NE == 0:
                k.op('pool', lambda e: e.memset(yacc[:], 0.0), writes=['me_yacc'])
            for c in range(8):
                for tl in range(QT // 512):
                    ts = slice(tl * 512, (tl + 1) * 512)
                    gts = slice(q * QT + tl * 512, q * QT + (tl + 1) * 512)
                    fb = (c * 2 + tl) % 2
                    k.dma('sp', gq[fb][:], x1v[:, c, gts], writes=['me_gq%d' % fb])
                    k.op('dve', lambda e, c=c, ts=ts, fb=fb: e.scalar_tensor_tensor(
                        out=yacc[:, c, ts], in0=yacc[:, c, ts], scalar=g.mod[:, 40 + c:41 + c], in1=gq[fb][:], op0=ALU.mult, op1=ALU.add),
                        reads=['me_yacc', 'mod', 'me_gq%d' % fb], writes=['me_yacc'])
            if not final:
                k.dma('sp', x2v[:, :, qs], yacc[:], reads=['me_yacc'], writes=['xT2_%d' % q])
            else:
                for t_ in range(QT // 128):
                    tg = q * (QT // 128) + t_
                    ob = 0
                    for h in range(2):
                        pi = (2 * t_ + h) % 4
                        for j in range(4):
                            cc = h * 4 + j
                            k.op('pe', lambda e, cc=cc, j=j, pi=pi, t_=t_: e.transpose(
                                out=g.ps[pi][:, j * 128:(j + 1) * 128], in_=yacc[:, cc, t_ * 128:(t_ + 1) * 128],
                                identity=g.cs['ident_f'][:]), reads=['me_yacc', 'c_ident_f'], writes=[g.psk[pi]])
                        k.op('act' if h == 0 else 'dve',
                             (lambda e, pi=pi, ob=ob, h=h: e.copy(out=xrow[ob][:, h * 512:(h + 1) * 512], in_=g.ps[pi][:])) if h == 0 else
                             (lambda e, pi=pi, ob=ob, h=h: e.tensor_copy(xrow[ob][:, h * 512:(h + 1) * 512], g.ps[pi][:])),
                             reads=[g.psk[pi]], writes=['me_xr%d_%d' % (ob, h)])
                    k.dma('sp', g.out[tg * 128:(tg + 1) * 128, :], xrow[ob][:], reads=['me_xr%d_0' % ob, 'me_xr%d_1' % ob], writes=['out'])
            tp = tap(g, 'x2T%d_q%d' % (l, q), [128, 8, QT])
            if tp is not None:
                k.dma('sp', tp, yacc[:], reads=['me_yacc'], writes=['tap'])
        k.barrier()


def zero_missing(g):
    k = g.k
    miss = [i for i, m in enumerate('ABCD') if m not in g.mixers]
    if not miss:
        return
    with contextlib.ExitStack() as es:
        z = g.sb('zz', [128, S], BF16, es)
        k.op('pool', lambda e: e.memset(z[:], 0.0), writes=['zz'])
        for i in miss:
            for j in range(2):
                r0 = i * 256 + j * 128
                k.dma('sp', g.ycatT[r0:r0 + 128, :], z[:], reads=['zz'], writes=['ycat_z'])
        k.barrier()


def load_const(g, es, name):
    ap = g.C[name]
    t = g.sb('c_' + name, ap.shape, ap.dtype, es)
    g.k.dma('sp', t[:], ap, writes=['c_' + name])
    return t


def rope_tables(g, es):
    k = g.k
    cos = g.sb('rp_cos', [128, S], F32, es)
    sin = g.sb('rp_sin', [128, S], F32, es)
    invf = g.cs['invf']
    CH = 1024
    with contextlib.ExitStack() as es2:
        posi = g.sb('rp_pi', [128, CH], I32, es2)
        ang = g.sb('rp_ang', [128, CH], F32, es2)
        tq = g.sb('rp_t', [128, CH], F32, es2)
        ti = g.sb('rp_ti', [128, CH], I32, es2)
        r = g.sb('rp_r', [128, CH], F32, es2)
        m = g.sb('rp_m', [128, CH], F32, es2)
        for ch in range(S // CH):
            cs_ = slice(ch * CH, (ch + 1) * CH)
            k.dma('sp', posi[:], g.pos[cs_].partition_broadcast(128), writes=['rp_pi'])
            k.op('dve', lambda e: e.tensor_copy(ang[:], posi[:]), reads=['rp_pi'], writes=['rp_ang'])
            k.op('dve', lambda e: e.tensor_scalar(out=ang[:], in0=ang[:], scalar1=invf[:, 0:1], scalar2=None, op0=ALU.mult),
                 reads=['rp_ang', 'c_invf'], writes=['rp_ang'])
            for which, dst, dkey in ((0, sin, 'rp_sin'), (1, cos, 'rp_cos')):
                shift = 0.0 if which == 0 else PI / 2
                k.op('dve', lambda e, shift=shift: e.tensor_scalar(out=tq[:], in0=ang[:], scalar1=shift, scalar2=1.0 / (2 * PI),
                                                                    op0=ALU.add, op1=ALU.mult), reads=['rp_ang'], writes=['rp_t'])
                k.op('dve', lambda e: e.tensor_copy(ti[:], tq[:]), reads=['rp_t'], writes=['rp_ti'])
                k.op('dve', lambda e: e.tensor_copy(tq[:], ti[:]), reads=['rp_ti'], writes=['rp_t'])
                k.op('dve', lambda e: e.scalar_tensor_tensor(out=r[:], in0=tq[:], scalar=-2 * PI, in1=ang[:], op0=ALU.mult, op1=ALU.add),
                     reads=['rp_t', 'rp_ang'], writes=['rp_r'])
                if shift != 0.0:
                    k.op('dve', lambda e, shift=shift: e.tensor_scalar(out=r[:], in0=r[:], scalar1=shift, scalar2=None, op0=ALU.add),
                         reads=['rp_r'], writes=['rp_r'])
                k.op('dve', lambda e: e.tensor_scalar(out=m[:], in0=r[:], scalar1=PI, scalar2=-2 * PI, op0=ALU.is_gt, op1=ALU.mult),
                     reads=['rp_r'], writes=['rp_m'])
                k.op('dve', lambda e: e.tensor_tensor(out=r[:], in0=r[:], in1=m[:], op=ALU.add), reads=['rp_r', 'rp_m'], writes=['rp_r'])
                k.op('dve', lambda e: e.tensor_scalar(out=m[:], in0=r[:], scalar1=-PI, scalar2=2 * PI, op0=ALU.is_lt, op1=ALU.mult),
                     reads=['rp_r'], writes=['rp_m'])
                k.op('dve', lambda e: e.tensor_tensor(out=r[:], in0=r[:], in1=m[:], op=ALU.add), reads=['rp_r', 'rp_m'], writes=['rp_r'])
                k.op('act', lambda e, dst=dst, cs_=cs_: e.activation(out=dst[:, cs_], in_=r[:], func=AF.Sin), reads=['rp_r'],
                     writes=[dkey])
        k.barrier()
    return cos, sin


class NormRope:
    def __init__(self, g, es, pfx):
        self.g = g
        self.pfx = pfx
        self.n = 0
        mk = lambda nm, dt: [g.sb('%s_%s%d' % (pfx, nm, i), [128, 512], dt, es) for i in range(2)]
        self.xf = mk('xf', F32)
        self.sq = mk('sq', BF16)
        self.rs = mk('rs', F32)
        self.xn = mk('xn', F32)
        self.xb = mk('xb', BF16)
        self.t1 = mk('t1', F32)

    def run(self, ps, pskey, gcol, gkeys, ts, cos=None, sin=None, out_r=None, okr=None, out_n=None, okn=None, pbank=2):
        g, k, pfx = self.g, self.g.k, self.pfx
        b = self.n % 2
        self.n += 1
        K_ = lambda nm: '%s_%s%d' % (pfx, nm, b)
        xf, sq, rs, xn, xb, t1 = self.xf[b], self.sq[b], self.rs[b], self.xn[b], self.xb[b], self.t1[b]
        k.op('act', lambda e: e.copy(out=xf[:], in_=ps), reads=[pskey], writes=[K_('xf')])
        k.op('act', lambda e: e.activation(out=sq[:], in_=xf[:], func=AF.Square), reads=[K_('xf')], writes=[K_('sq')])
        pi = pbank + b
        k.op('pe', lambda e: e.matmul(g.ps[pi][:], lhsT=g.cs['blk64_b'][:], rhs=sq[:], start=True, stop=True),
             reads=[K_('sq'), 'c_blk64_b'], writes=[g.psk[pi]])
        k.op('dve', lambda e: e.tensor_scalar(out=rs[:], in0=g.ps[pi][:], scalar1=1.0 / 64, scalar2=NORM_EPS, op0=ALU.mult, op1=ALU.add),
             reads=[g.psk[pi]], writes=[K_('rs')])
        k.op('act', lambda e: e.activation(out=rs[:], in_=rs[:], func=AF.Sqrt), reads=[K_('rs')], writes=[K_('rs')])
        k.op('dve', lambda e: e.reciprocal(out=rs[:], in_=rs[:]), reads=[K_('rs')], writes=[K_('rs')])
        k.op('dve', lambda e: e.scalar_tensor_tensor(out=xn[:], in0=xf[:], scalar=gcol, in1=rs[:], op0=ALU.mult, op1=ALU.mult),
             reads=[K_('xf'), K_('rs')] + gkeys, writes=[K_('xn')])
        if out_n is not None:
            k.op('pool', lambda e: e.tensor_copy(out_n, xn[:]), reads=[K_('xn')], writes=[okn])
        if out_r is not None:
            k.op('act', lambda e: e.copy(out=xb[:], in_=xn[:]), reads=[K_('xn')], writes=[K_('xb')])
            k.op('pe', lambda e: e.matmul(g.ps[pi][:], lhsT=g.cs['prot_b'][:], rhs=xb[:], start=True, stop=True),
                 reads=[K_('xb'), 'c_prot_b'], writes=[g.psk[pi]])
            k.op('dve', lambda e: e.tensor_tensor(out=t1[:], in0=g.ps[pi][:], in1=sin[:, ts], op=ALU.mult),
                 reads=[g.psk[pi], 'rp_sin'], writes=[K_('t1')])
            k.op('pool', lambda e: e.tensor_tensor(out=xn[:], in0=xn[:], in1=cos[:, ts], op=ALU.mult),
                 reads=[K_('xn'), 'rp_cos'], writes=[K_('xn')])
            k.op('dve', lambda e: e.tensor_tensor(out=out_r, in0=xn[:], in1=t1[:], op=ALU.add),
                 reads=[K_('xn'), K_('t1')], writes=[okr])


def attention(g, es, pfx, heads, ngroups, ktiles, qT, kT, vfn, ncols, masks, epilogue, scale=0.125):
    k = g.k
    pt = [g.sb('%s_pt%d' % (pfx, i), [128, 512], BF16, es) for i in range(3)]
    n = 0
    for h in heads:
        for gi in range(ngroups):
            kts = ktiles(gi)
            if not kts:
                continue
            for idx, kt in enumerate(kts):
                sbk = n % 2
                pb = n % 3
                n += 1
                qa, qk = qT(h, gi)
                ka, kk, nk = kT(h, kt)
                k.op('pe', lambda e, sbk=sbk, ka=ka, qa=qa, nk=nk: e.matmul(g.ps[sbk][0:nk, :], lhsT=ka, rhs=qa, start=True, stop=True),
                     reads=qk + kk, writes=[g.psk[sbk]])
                ptk = '%s_pt%d' % (pfx, pb)
                k.op('act', lambda e, sbk=sbk, pb=pb, nk=nk: e.activation(out=pt[pb][0:nk, :], in_=g.ps[sbk][0:nk, :], func=AF.Exp, scale=scale),
                     reads=[g.psk[sbk]], writes=[ptk])
                for (ma, mk_, is_ps) in masks(h, gi, kt):
                    eng = 'dve' if (is_ps or n % 2 == 0) else 'pool'
                    k.op(eng, lambda e, pb=pb, ma=ma, nk=nk: e.tensor_tensor(out=pt[pb][0:nk, :], in0=pt[pb][0:nk, :], in1=ma[0:nk, :], op=ALU.mult),
                         reads=[ptk] + mk_, writes=[ptk])
                va, vk = vfn(h, kt)
                for sub in range(4):
                    k.op('pe', lambda e, sub=sub, pb=pb, va=va, nk=nk, idx=idx: e.matmul(
                        g.ps[4 + sub][:, 0:ncols], lhsT=pt[pb][0:nk, sub * 128:(sub + 1) * 128], rhs=va,
                        start=(idx == 0), stop=(idx == len(kts) - 1)), reads=[ptk] + vk, writes=[g.psk[4 + sub]])
            for sub in range(4):
                epilogue(h, gi, sub, g.ps[4 + sub], g.psk[4 + sub])


def finish_tm(g, es, pfx, ytm, ykeyfn, gb, gbkey, row0):
    k = g.k
    ycv = g.ycatT.rearrange('(c p) t -> p c t', p=128)
    sq = [g.sb(pfx + '_fsq%d' % i, [128, 256], F32, es) for i in range(2)]
    ss = [g.sb(pfx + '_fss%d' % i, [128, 4], F32, es) for i in range(2)]
    yb = [g.sb(pfx + '_fyb%d' % i, [128, 256], BF16, es) for i in range(2)]
    yT = [g.sb(pfx + '_fyT%d' % i, [128, 2, 128], BF16, es) for i in range(2)]
    psb = [g.ps[2][:].bitcast(BF16), g.ps[3][:].bitcast(BF16)]
    for t_ in range(NT):
        b = t_ % 2
        s_ = str(b)
        yk = ykeyfn(t_)
        k.op('pool', lambda e, b=b, t_=t_: e.tensor_tensor(out=sq[b][:], in0=ytm[:, t_, :], in1=ytm[:, t_, :], op=ALU.mult),
             reads=yk, writes=[pfx + '_fsq' + s_])
        k.op('dve', lambda e, b=b: e.reduce_sum(out=ss[b][:], in_=sq[b][:].rearrange('p (h d) -> p h d', d=64), axis=AX.X),
             reads=[pfx + '_fsq' + s_], writes=[pfx + '_fss' + s_])
        k.op('dve', lambda e, b=b: e.tensor_scalar(out=ss[b][:], in0=ss[b][:], scalar1=1.0 / 64, scalar2=NORM_EPS, op0=ALU.mult, op1=ALU.add),
             reads=[pfx + '_fss' + s_], writes=[pfx + '_fss' + s_])
        k.op('act', lambda e, b=b: e.activation(out=ss[b][:], in_=ss[b][:], func=AF.Sqrt), reads=[pfx + '_fss' + s_], writes=[pfx + '_fss' + s_])
        k.op('dve', lambda e, b=b: e.reciprocal(out=ss[b][:], in_=ss[b][:]), reads=[pfx + '_fss' + s_], writes=[pfx + '_fss' + s_])
        for h in range(4):
            k.op('dve', lambda e, b=b, h=h, t_=t_: e.scalar_tensor_tensor(
                out=yb[b][:, h * 64:(h + 1) * 64], in0=ytm[:, t_, h * 64:(h + 1) * 64], scalar=ss[b][:, h:h + 1],
                in1=gb[:, h * 64:(h + 1) * 64], op0=ALU.mult, op1=ALU.mult),
                reads=yk + [pfx + '_fss' + s_, gbkey], writes=[pfx + '_fyb' + s_])
        for j in range(2):
            k.op('pe', lambda e, b=b, j=j: e.transpose(out=psb[b][:, j * 128:(j + 1) * 128], in_=yb[b][:, j * 128:(j + 1) * 128],
                                                       identity=g.cs['ident_b'][:]),
                 reads=[pfx + '_fyb' + s_, 'c_ident_b'], writes=[g.psk[2 + b]])
        k.op('act', lambda e, b=b: e.copy(out=yT[b][:], in_=psb[b][:, 0:256].rearrange('p (j t) -> p j t', j=2)),
             reads=[g.psk[2 + b]], writes=[pfx + '_fyT' + s_])
        c0 = row0 // 128
        k.dma('sp', ycv[:, c0:c0 + 2, t_ * 128:(t_ + 1) * 128], yT[b][:], reads=[pfx + '_fyT' + s_], writes=['ycat_%s%d' % (pfx, t_)])


def mixer_dil(g, l):
    k = g.k
    with contextlib.ExitStack() as es:
        qT = g.sb('dl_qT', [128, 2, S], BF16, es)
        kT = g.sb('dl_kT', [128, 2, S], BF16, es)
        V = g.sb('dl_V', [128, NT, 4, 65], BF16, es)
        ytm = g.sb('dl_ytm', [128, NT, 256], F32, es)
        gb = g.sb('dl_gb', [128, 256], F32, es)
        k.dma('sp', gb[:], g.W['onorm_g'][l, 256:512].partition_broadcast(128), writes=['dl_gb'])
        k.op('pool', lambda e: e.memset(V[:, :, :, 64:65], 1.0), writes=['dl_Vone'])
        with contextlib.ExitStack() as es2:
            cos, sin = rope_tables(g, es2)
            w = g.sb('dl_w', [128, 8, 768], BF16, es2)
            load_w_bf16(g, w[:], 'dl_w', g.W['w_in'][l][:, 1792:2560])
            gq = g.sb('dl_gq', [128, 1], F32, es2)
            gk = g.sb('dl_gk', [128, 1], F32, es2)
            for hh in range(2):
                k.dma('sp', gq[hh * 64:(hh + 1) * 64, :], g.W['dil_q_g'][l].rearrange('(p o) -> p o', o=1), writes=['dl_gq'],
                      allow_slow_non_contiguous=True)
                k.dma('sp', gk[hh * 64:(hh + 1) * 64, :], g.W['dil_k_g'][l].rearrange('(p o) -> p o', o=1), writes=['dl_gk'],
                      allow_slow_non_contiguous=True)
            nr = NormRope(g, es2, 'dl')
            hload = ht_loader(g, es2, 'dl')
            n = 0
            for tt in range(8):
                ts = slice(tt * 512, (tt + 1) * 512)
                ht, hk = hload(tt)
                for j in range(2):
                    for (dst, dkey, c0, gcol, gkey) in ((qT, 'dl_qT', 0, gq, 'dl_gq'), (kT, 'dl_kT', 256, gk, 'dl_gk')):
                        pi = n % 2
                        n += 1
                        proj_fm(g, w, 'dl_w', c0 + j * 128, 128, ht, hk, g.ps[pi][:], g.psk[pi])
                        nr.run(g.ps[pi][:], g.psk[pi], gcol[:, 0:1], [gkey], ts, cos, sin, dst[:, j, ts], '%s%d_%d' % (dkey, j, tt))
                for sub in range(4):
                    pi = 4 + sub
                    t_ = tt * 4 + sub
                    proj_tm(g, w, 'dl_w', 512, 256, ht, hk, sub, g.ps[pi][:, 0:256], g.psk[pi])
                    k.op('act', lambda e, pi=pi, t_=t_: e.copy(out=V[:, t_, :, 0:64], in_=g.ps[pi][:, 0:256].rearrange('p (h d) -> p h d', d=64)),
                         reads=[g.psk[pi]], writes=['dl_V%d' % t_])
            k.barrier()
        msk = load_const(g, es, 'dil_mask')
        rd = [g.sb('dl_rd%d' % i, [128, 1], F32, es) for i in range(2)]
        cnt = [0]

        def q_of(h, gi):
            j, hp = h // 2, h % 2
            return qT[hp * 64:(hp + 1) * 64, j, gi * 512:(gi + 1) * 512], ['dl_qT%d_%d' % (j, gi)]

        def k_of(h, kt):
            j, hp = h // 2, h % 2
            return kT[hp * 64:(hp + 1) * 64, j, kt * 128:(kt + 1) * 128], ['dl_kT%d_%d' % (j, kt // 4)], 128

        def v_of(h, kt):
            return V[:, kt, h, :], ['dl_V%d' % kt, 'dl_Vone']

        def ktiles(gi):
            return [kt for kt in range(max(0, 4 * gi - 16), 4 * gi + 4)]

        def masks(h, gi, kt):
            return [(msk[:, (4 * gi - kt) + 3, :], ['c_dil_mask'], False)]

        def epi(h, gi, sub, acc, acck):
            b = cnt[0] % 2
            cnt[0] += 1
            t_ = gi * 4 + sub
            k.op('dve', lambda e: e.reciprocal(out=rd[b][:], in_=acc[:, 64:65]), reads=[acck], writes=['dl_rd%d' % b])
            k.op('dve', lambda e: e.tensor_scalar(out=ytm[:, t_, h * 64:(h + 1) * 64], in0=acc[:, 0:64], scalar1=rd[b][:, 0:1],
                                                   scalar2=None, op0=ALU.mult), reads=[acck, 'dl_rd%d' % b], writes=['dl_ytm%d_%d' % (t_, h)])

        attention(g, es, 'dl', range(4), 8, ktiles, q_of, k_of, v_of, 65, masks, epi)
        finish_tm(g, es, 'dl', ytm, lambda t_: ['dl_ytm%d_%d' % (t_, h) for h in range(4)], gb, 'dl_gb', 512)
        k.barrier()


NSA_STOP = None
NSA_SKIP = ''


def mixer_nsa(g, l):
    k = g.k
    W = g.W
    with contextlib.ExitStack() as es:
        qn = g.sb('ns_qn', [128, 2, S], BF16, es)
        qr = g.sb('ns_qr', [128, 2, S], BF16, es)
        ksT = g.sb('ns_ks', [128, S], BF16, es)
        kwT = g.sb('ns_kw', [128, S], BF16, es)
        kcvc = g.sb('ns_kcvc', [128, S], BF16, es)
        Vs = g.sb('ns_Vs', [128, NT, 66], BF16, es)
        Vw = g.sb('ns_Vw', [128, NT, 66], BF16, es)
        gate = g.sb('ns_gate', [128, NT, 12], F32, es)
        ytm = g.sb('ns_ytm', [128, NT, 256], F32, es)
        gb = g.sb('ns_gb', [128, 256], F32, es)
        k.dma('sp', gb[:], W['onorm_g'][l, 512:768].partition_broadcast(128), writes=['ns_gb'])
        k.op('pool', lambda e: e.memset(Vs[:, :, 64:65], 1.0), writes=['ns_Vs1'])
        k.op('pool', lambda e: e.memset(Vw[:, :, 64:65], 1.0), writes=['ns_Vw1'])
        with contextlib.ExitStack() as es2:
            cos, sin = rope_tables(g, es2)
            w = g.sb('ns_w', [128, 8, 652], BF16, es2)
            load_w_bf16(g, w[:], 'ns_w', W['w_in'][l][:, 2560:3212])
            wks = g.sb('ns_wks', [128, 8, 128], BF16, es2)
            wkw = g.sb('ns_wkw', [128, 8, 128], BF16, es2)
            for hh in range(2):
                load_w_bf16(g, wks[:, :, hh * 64:(hh + 1) * 64], 'ns_wks', W['w_in'][l][:, 2944:3008])
                load_w_bf16(g, wkw[:, :, hh * 64:(hh + 1) * 64], 'ns_wkw', W['w_in'][l][:, 3072:3136])
            gq = g.sb('ns_gq', [128, 1], F32, es2)
            gks = g.sb('ns_gks', [128, 1], F32, es2)
            gkw = g.sb('ns_gkw', [128, 1], F32, es2)
            for hh in range(2):
                for (dst, nm, key) in ((gq, 'nsa_q_g', 'ns_gq'), (gks, 'nsa_ks_g', 'ns_gks'), (gkw, 'nsa_kw_g', 'ns_gkw')):
                    k.dma('sp', dst[hh * 64:(hh + 1) * 64, :], W[nm][l].rearrange('(p o) -> p o', o=1), writes=[key],
                          allow_slow_non_contiguous=True)
            nr = NormRope(g, es2, 'ns')
            hload = ht_loader(g, es2, 'ns')
            n = 0
            for tt in range(8):
                ts = slice(tt * 512, (tt + 1) * 512)
                ht, hk = hload(tt)
                for j in range(2):
                    pi = n % 2
                    n += 1
                    proj_fm(g, w, 'ns_w', j * 128, 128, ht, hk, g.ps[pi][:], g.psk[pi])
                    nr.run(g.ps[pi][:], g.psk[pi], gq[:, 0:1], ['ns_gq'], ts, cos, sin, qr[:, j, ts], 'ns_qr%d_%d' % (j, tt),
                           None if 'a' in NSA_SKIP else qn[:, j, ts], 'ns_qn%d_%d' % (j, tt))
                for (wsb, wkey, gcol, gkey, dst, dkey) in ((wks, 'ns_wks', gks, 'ns_gks', ksT, 'ns_ks'), (wkw, 'ns_wkw', gkw, 'ns_gkw', kwT, 'ns_kw')):
                    if 'c' in NSA_SKIP:
                        continue
                    pi = n % 2
                    n += 1
                    proj_fm(g, wsb, wkey, 0, 128, ht, hk, g.ps[pi][:], g.psk[pi])
                    nr.run(g.ps[pi][:], g.psk[pi], gcol[:, 0:1], [gkey], ts, cos, sin, dst[:, ts], '%s_%d' % (dkey, tt))
                pi = n % 2
                n += 1
                proj_fm(g, w, 'ns_w', 256, 128, ht, hk, g.ps[pi][:], g.psk[pi])
                k.op('act', lambda e, pi=pi, ts=ts: e.copy(out=kcvc[:, ts], in_=g.ps[pi][:]), reads=[g.psk[pi]], writes=['ns_kcvc%d' % tt])
                for sub in range(4):
                    if 'b' in NSA_SKIP:
                        continue
                    pi = 4 + sub
                    t_ = tt * 4 + sub
                    proj_tm(g, w, 'ns_w', 448, 204, ht, hk, sub, g.ps[pi][:, 0:204], g.psk[pi])
                    k.op('act', lambda e, pi=pi, t_=t_: e.copy(out=Vs[:, t_, 0:64], in_=g.ps[pi][:, 0:64]), reads=[g.psk[pi]],
                         writes=['ns_Vs%d' % t_])
                    if 'e' not in NSA_SKIP:
                        k.op('dve', lambda e, pi=pi, t_=t_: e.tensor_copy(Vw[:, t_, 0:64], g.ps[pi][:, 128:192]), reads=[g.psk[pi]],
                             writes=['ns_Vw%d' % t_])
                    if 'd' not in NSA_SKIP:
                        k.op('act', lambda e, pi=pi, t_=t_: e.activation(out=gate[:, t_, :], in_=g.ps[pi][:, 192:204], func=AF.Sigmoid),
                             reads=[g.psk[pi]], writes=['ns_gate%d' % t_])
            k.barrier()
        if NSA_STOP == 'proj':
            return
        kcT = g.sb('ns_kcT', [128, 512], BF16, es)
        Vc = [g.sb('ns_Vc%d' % j, [128, 129], BF16, es) for j in range(2)]
        ovl1 = load_const(g, es, 'ovl1')
        with contextlib.ExitStack() as es3:
            W1 = g.sb('ns_W1', [128, 32, 256], BF16, es3)
            peT = g.sb('ns_peT', [128, 32], BF16, es3)
            W2k = g.sb('ns_W2k', [128, 2, 128], BF16, es3)
            W2v = g.sb('ns_W2v', [128, 2, 64], BF16, es3)
            gkc = g.sb('ns_gkc', [128, 1], F32, es3)
            for (pb, w1n, pen) in ((0, 'nsa_wk1', 'nsa_pe_k'), (64, 'nsa_wv1', 'nsa_pe_v')):
                k.dma('pool', W1[pb:pb + 64, :, :], W[w1n][l].rearrange('(l d) m -> d l m', d=64), writes=['ns_W1'])
                k.dma('pool', peT[pb:pb + 64, :], W[pen][l].rearrange('l d -> d l'), writes=['ns_peT'], allow_slow_non_contiguous=True)
            for hh in range(2):
                k.dma('pool', W2k[:, :, hh * 64:(hh + 1) * 64], W['nsa_wk2'][l].rearrange('(c p) d -> p c d', p=128), writes=['ns_W2k'])
                k.dma('sp', gkc[hh * 64:(hh + 1) * 64, :], W['nsa_kc_g'][l].rearrange('(p o) -> p o', o=1), writes=['ns_gkc'],
                      allow_slow_non_contiguous=True)
            k.dma('pool', W2v[:], W['nsa_wv2'][l].rearrange('(c p) d -> p c d', p=128), writes=['ns_W2v'])
            hid = {}
            bia = g.sb('ns_bia', [128, 4], F32, es3)
            xs = g.sb('ns_xs', [128, 256], F32, es3)
            x2 = g.sb('ns_x2', [128, 256], F32, es3)
            th = g.sb('ns_th', [128, 256], F32, es3)
            n = 0
            allkc = ['ns_kcvc%d' % i for i in range(8)]
            for kv, pb in (('k', 0), ('v', 64)):
                for mc in range(2):
                    hd_ = g.sb('ns_hid%s%d' % (kv, mc), [128, 256], BF16, es3)
                    hid[(kv, mc)] = hd_
                    hk_ = 'ns_hid%s%d' % (kv, mc)
                    k.op('pool', lambda e, hd_=hd_: e.memset(hd_[:], 0.0), writes=[hk_])
                    pi, pj = n % 2, 2 + n % 2
                    for l_ in range(32):
                        k.op('pe', lambda e, l_=l_, pb=pb, mc=mc, pi=pi: e.matmul(
                            g.ps[pi][:, 0:255], lhsT=W1[pb:pb + 64, l_, mc * 128:(mc + 1) * 128],
                            rhs=kcvc[pb:pb + 64, l_:l_ + 16 * 254 + 1:16], start=(l_ == 0), stop=(l_ == 31)),
                            reads=['ns_W1'] + allkc, writes=[g.psk[pi]])
                    for l_ in range(32):
                        k.op('pe', lambda e, l_=l_, pb=pb, mc=mc, pj=pj: e.matmul(
                            g.ps[pj][:, 0:1], lhsT=W1[pb:pb + 64, l_, mc * 128:(mc + 1) * 128], rhs=peT[pb:pb + 64, l_:l_ + 1],
                            start=(l_ == 0), stop=(l_ == 31)), reads=['ns_W1', 'ns_peT'], writes=[g.psk[pj]])
                    k.op('act', lambda e, n=n, pj=pj: e.copy(out=bia[:, n:n + 1], in_=g.ps[pj][:, 0:1]), reads=[g.psk[pj]], writes=['ns_bia'])
                    k.op('act', lambda e, n=n, pi=pi: e.activation(out=xs[:, 0:255], in_=g.ps[pi][:, 0:255], func=AF.Identity,
                                                                   bias=bia[:, n:n + 1], scale=1.0), reads=[g.psk[pi], 'ns_bia'], writes=['ns_xs'])
                    k.op('dve', lambda e: e.tensor_tensor(out=x2[:, 0:255], in0=xs[:, 0:255], in1=xs[:, 0:255], op=ALU.mult), reads=['ns_xs'], writes=['ns_x2'])
                    k.op('dve', lambda e: e.tensor_scalar(out=x2[:, 0:255], in0=x2[:, 0:255], scalar1=0.044715, scalar2=1.0, op0=ALU.mult, op1=ALU.add),
                         reads=['ns_x2'], writes=['ns_x2'])
                    k.op('dve', lambda e: e.tensor_tensor(out=x2[:, 0:255], in0=x2[:, 0:255], in1=xs[:, 0:255], op=ALU.mult), reads=['ns_x2', 'ns_xs'], writes=['ns_x2'])
                    k.op('act', lambda e: e.activation(out=th[:, 0:255], in_=x2[:, 0:255], func=AF.Tanh, scale=0.7978845608028654),
                         reads=['ns_x2'], writes=['ns_th'])
                    k.op('dve', lambda e: e.scalar_tensor_tensor(out=th[:, 0:255], in0=th[:, 0:255], scalar=1.0, in1=xs[:, 0:255], op0=ALU.add, op1=ALU.mult),
                         reads=['ns_th', 'ns_xs'], writes=['ns_th'])
                    k.op('act', lambda e, hd_=hd_: e.mul(out=hd_[:, 0:255], in_=th[:, 0:255], mul=0.5), reads=['ns_th'], writes=[hk_])
                    n += 1
            for mc in range(2):
                k.op('pe', lambda e, mc=mc: e.matmul(g.ps[0][:, 0:256], lhsT=W2k[:, mc, :], rhs=hid[('k', mc)][:], start=(mc == 0), stop=(mc == 1)),
                     reads=['ns_W2k', 'ns_hidk%d' % mc], writes=[g.psk[0]])
            nr2 = NormRope(g, es3, 'nc')
            nr2.run(g.ps[0][:], g.psk[0], gkc[:, 0:1], ['ns_gkc'], slice(0, 512), out_n=kcT[:], okn='ns_kcT')
            k.op('pool', lambda e: e.memset(kcT[:, 255:512], 0.0), reads=['ns_kcT'], writes=['ns_kcT'])
            for j in range(2):
                for mc in range(2):
                    k.op('pe', lambda e, j=j, mc=mc: e.matmul(g.ps[1][:, 0:64], lhsT=hid[('v', mc)][:, j * 128:(j + 1) * 128], rhs=W2v[:, mc, :],
                                                           start=(mc == 0), stop=(mc == 1)), reads=['ns_W2v', 'ns_hidv%d' % mc], writes=[g.psk[1]])
                k.op('act', lambda e, j=j: e.copy(out=Vc[j][:, 0:64], in_=g.ps[1][:, 0:64]), reads=[g.psk[1]], writes=['ns_Vc%d' % j])
                k.op('pool', lambda e, j=j: e.tensor_copy(Vc[j][:, 64:129], ovl1[:, j, :]), reads=['c_ovl1'], writes=['ns_Vc%d_o' % j])
            k.barrier()
        if NSA_STOP == 'cmpkv':
            return
        imp = g.sb('ns_imp', [128, NT, 64], F32, es)
        selT = g.sb('ns_selT', [64, S], BF16, es)
        rd = [g.sb('ns_rd%d' % i, [128, 1], F32, es) for i in range(2)]
        cf = [g.sb('ns_cf%d' % i, [128, 1], F32, es) for i in range(2)]
        cnt = [0]

        def q_of_n(h, gi):
            j, hp = h // 2, h % 2
            return qn[hp * 64:(hp + 1) * 64, j, gi * 512:(gi + 1) * 512], ['ns_qn%d_%d' % (j, gi)]

        def q_of_r(h, gi):
            j, hp = h // 2, h % 2
            return qr[hp * 64:(hp + 1) * 64, j, gi * 512:(gi + 1) * 512], ['ns_qr%d_%d' % (j, gi)]

        def make_epi(br, first):
            def epi(h, gi, sub, acc, acck):
                b = cnt[0] % 2
                cnt[0] += 1
                t_ = gi * 4 + sub
                rk, ck = 'ns_rd%d' % b, 'ns_cf%d' % b
                k.op('dve', lambda e: e.tensor_scalar(out=rd[b][:], in0=acc[:, 64:65], scalar1=1e-30, scalar2=None, op0=ALU.max),
                     reads=[acck], writes=[rk])
                k.op('dve', lambda e: e.reciprocal(out=rd[b][:], in_=rd[b][:]), reads=[rk], writes=[rk])
                k.op('dve', lambda e: e.tensor_tensor(out=cf[b][:], in0=rd[b][:], in1=gate[:, t_, h * 3 + br:h * 3 + br + 1], op=ALU.mult),
                     reads=[rk, 'ns_gate%d' % t_], writes=[ck])
                yk = 'ns_ytm%d_%d' % (t_, h)
                if first:
                    k.op('dve', lambda e: e.tensor_scalar(out=ytm[:, t_, h * 64:(h + 1) * 64], in0=acc[:, 0:64], scalar1=cf[b][:, 0:1],
                                                           scalar2=None, op0=ALU.mult), reads=[acck, ck], writes=[yk])
                    ik = 'ns_imp%d' % t_
                    if h == 0:
                        k.op('dve', lambda e: e.tensor_scalar(out=imp[:, t_, :], in0=acc[:, 65:129], scalar1=rd[b][:, 0:1], scalar2=None,
                                                               op0=ALU.mult), reads=[acck, rk], writes=[ik])
                    else:
                        k.op('dve', lambda e: e.scalar_tensor_tensor(out=imp[:, t_, :], in0=acc[:, 65:129], scalar=rd[b][:, 0:1], in1=imp[:, t_, :],
                                                                      op0=ALU.mult, op1=ALU.add), reads=[acck, rk, ik], writes=[ik])
                else:
                    k.op('dve', lambda e: e.scalar_tensor_tensor(out=ytm[:, t_, h * 64:(h + 1) * 64], in0=acc[:, 0:64], scalar=cf[b][:, 0:1],
                                                                  in1=ytm[:, t_, h * 64:(h + 1) * 64], op0=ALU.mult, op1=ALU.add),
                         reads=[acck, ck, yk], writes=[yk])
            return epi

        with contextlib.ExitStack() as es4:
            cmsk = load_const(g, es4, 'cmp_mask')

            def k_cmp(h, kt):
                hp = h % 2
                return kcT[hp * 64:(hp + 1) * 64, kt * 128:(kt + 1) * 128], ['ns_kcT'], 128

            def v_cmp(h, kt):
                return Vc[kt][:, :], ['ns_Vc%d' % kt, 'ns_Vc%d_o' % kt]

            def kt_cmp(gi):
                return [0] + ([1] if gi >= 4 else [])

            def m_cmp(h, gi, kt):
                i = gi - 4 * kt
                return [] if i >= 5 else [(cmsk[:, i, :], ['c_cmp_mask'], False)]

            attention(g, es4, 'nc', range(4), 8, kt_cmp, q_of_n, k_cmp, v_cmp, 129, m_cmp, make_epi(0, True))
            if NSA_STOP == 'cmpattn':
                k.barrier()
                return
            keep = load_const(g, es4, 'sel_keep')
            addc = load_const(g, es4, 'sel_add')
            sc = [g.sb('ns_sc%d' % i, [128, 64], F32, es4) for i in range(2)]
            wk2_ = [g.sb('ns_wk%d' % i, [128, 64], F32, es4) for i in range(2)]
            m8 = [g.sb('ns_m8%d' % i, [128, 8], F32, es4) for i in range(2)]
            sm = [g.sb('ns_sm%d' % i, [128, 64], BF16, es4) for i in range(2)]
            psb = [g.ps[2][:].bitcast(BF16), g.ps[3][:].bitcast(BF16)]
            for t_ in range(NT):
                b = t_ % 2
                s_ = str(b)
                k.op('dve', lambda e, b=b, t_=t_: e.tensor_tensor(out=sc[b][:], in0=imp[:, t_, :], in1=keep[:, t_, :], op=ALU.mult),
                     reads=['ns_imp%d' % t_, 'c_sel_keep'], writes=['ns_sc' + s_])
                k.op('dve', lambda e, b=b, t_=t_: e.tensor_tensor(out=sc[b][:], in0=sc[b][:], in1=addc[:, t_, :], op=ALU.add),
                     reads=['ns_sc' + s_, 'c_sel_add'], writes=['ns_sc' + s_])
                k.op('dve', lambda e, b=b: e.max(out=m8[b][:], in_=sc[b][:]), reads=['ns_sc' + s_], writes=['ns_m8' + s_])
                k.op('dve', lambda e, b=b: e.match_replace(out=wk2_[b][:], in_to_replace=m8[b][:], in_values=sc[b][:], imm_value=-1e30),
                     reads=['ns_sc' + s_, 'ns_m8' + s_], writes=['ns_wk' + s_])
                k.op('dve', lambda e, b=b: e.max(out=m8[b][:], in_=wk2_[b][:]), reads=['ns_wk' + s_], writes=['ns_m8' + s_])
                k.op('dve', lambda e, b=b: e.tensor_scalar(out=sm[b][:], in0=sc[b][:], scalar1=m8[b][:, 7:8], scalar2=None, op0=ALU.is_ge),
                     reads=['ns_sc' + s_, 'ns_m8' + s_], writes=['ns_sm' + s_])
                k.op('pe', lambda e, b=b: e.transpose(out=psb[b][0:64, 0:128], in_=sm[b][:], identity=g.cs['ident_b'][:]),
                     reads=['ns_sm' + s_, 'c_ident_b'], writes=[g.psk[2 + b]])
                k.op('act', lambda e, b=b, t_=t_: e.copy(out=selT[:, t_ * 128:(t_ + 1) * 128], in_=psb[b][0:64, 0:128]),
                     reads=[g.psk[2 + b]], writes=['ns_selT%d' % (t_ // 4)])
            tp = tap(g, 'selT%d' % l, [64, S], BF16)
            if tp is not None:
                k.dma('sp', tp, selT[:], reads=['ns_selT%d' % i for i in range(8)], writes=['tap'])
            k.barrier()
        if NSA_STOP == 'topk':
            return
        with contextlib.ExitStack() as es5:
            cau = load_const(g, es5, 'cau_mask')
            sexp = load_const(g, es5, 'sel_exp')
            mc_ = [0]

            def k_s(h, kt):
                hp = h % 2
                return ksT[hp * 64:(hp + 1) * 64, kt * 128:(kt + 1) * 128], ['ns_ks_%d' % (kt // 4)], 128

            def v_s(h, kt):
                return Vs[:, kt, 0:65], ['ns_Vs%d' % kt, 'ns_Vs1']

            def m_s(h, gi, kt):
                pm = 2 + mc_[0] % 2
                mc_[0] += 1
                k.op('pe', lambda e: e.matmul(g.ps[pm][:], lhsT=sexp[:, kt, :], rhs=selT[:, gi * 512:(gi + 1) * 512], start=True, stop=True),
                     reads=['c_sel_exp', 'ns_selT%d' % gi], writes=[g.psk[pm]])
                ms = [(g.ps[pm], [g.psk[pm]], True)]
                if kt >= 4 * gi:
                    ms.append((cau[:, (4 * gi - kt) + 3, :], ['c_cau_mask'], False))
                return ms

            attention(g, es5, 'nl', range(4), 8, lambda gi: list(range(0, 4 * gi + 4)), q_of_r, k_s, v_s, 65, m_s, make_epi(1, False))
            k.barrier()
        if NSA_STOP == 'sel':
            return
        with contextlib.ExitStack() as es6:
            swm = load_const(g, es6, 'swa_mask')

            def k_w(h, kt):
                hp = h % 2
                return kwT[hp * 64:(hp + 1) * 64, kt * 128:(kt + 1) * 128], ['ns_kw_%d' % (kt // 4)], 128

            def v_w(h, kt):
                return Vw[:, kt, 0:65], ['ns_Vw%d' % kt, 'ns_Vw1']

            attention(g, es6, 'nw', range(4), 8, lambda gi: list(range(max(0, 4 * gi - 4), 4 * gi + 4)), q_of_r, k_w, v_w, 65,
                      lambda h, gi, kt: [(swm[:, (4 * gi - kt) + 3, :], ['c_swa_mask'], False)], make_epi(2, False))
            finish_tm(g, es6, 'ns', ytm, lambda t_: ['ns_ytm%d_%d' % (t_, h) for h in range(4)], gb, 'ns_gb', 768)
            k.barrier()


RW_STEPS = S


def mixer_rwkv(g, l):
    k = g.k
    W = g.W
    ycv = g.ycatT.rearrange('(c p) t -> p c t', p=128)
    with contextlib.ExitStack() as es:
        rT = g.sb('rw_rT', [128, 2, S], BF16, es)
        kkT = g.sb('rw_kkT', [128, 2, S], BF16, es)
        wT = g.sb('rw_wT', [128, 2, S], F32, es)
        vtm = g.sb('rw_vtm', [128, NT, 256], BF16, es)
        gtm = g.sb('rw_gtm', [128, NT, 256], BF16, es)
        bon = g.sb('rw_bon', [128, NT, 4], F32, es)
        with contextlib.ExitStack() as es2:
            wa = g.sb('rw_wa', [128, 8, 1024], BF16, es2)
            wb = g.sb('rw_wb', [128, 8, 1024], BF16, es2)
            mub = g.sb('rw_mub', [128, 1024], F32, es2)
            omb = g.sb('rw_omb', [128, 1024], F32, es2)
            load_w_bf16(g, wa[:], 'rw_wa', W['w_in'][l][:, 0:1024])
            k.dma('sp', mub[:], W['rwkv_mu'][l].partition_broadcast(128), writes=['rw_mub'])
            k.op('dve', lambda e: e.tensor_scalar(out=omb[:], in0=mub[:], scalar1=-1.0, scalar2=1.0, op0=ALU.mult, op1=ALU.add),
                 reads=['rw_mub'], writes=['rw_omb'])
            for c in range(8):
                k.op('dve', lambda e, c=c: e.tensor_tensor(out=wb[:, c, :], in0=wa[:, c, :], in1=mub[:], op=ALU.mult),
                     reads=['rw_wa', 'rw_mub'], writes=['rw_wb'])
            for c in range(8):
                k.op('pool', lambda e, c=c: e.tensor_tensor(out=wa[:, c, :], in0=wa[:, c, :], in1=omb[:], op=ALU.mult),
                     reads=['rw_wa', 'rw_omb', 'rw_wb'], writes=['rw_wa'])
            w2 = g.sb('rw_w2', [64, 256], BF16, es2)
            a2 = g.sb('rw_a2', [128, 256], BF16, es2)
            g2 = g.sb('rw_g2', [128, 256], BF16, es2)
            a0r = g.sb('rw_a0r', [1, 256], BF16, es2)
            w0c = g.sb('rw_w0c', [128, 2], F32, es2)
            kkc = g.sb('rw_kkc', [128, 2], F32, es2)
            k.dma('pool', w2[:], W['rwkv_w2'][l], writes=['rw_w2'])
            k.dma('pool', a2[64:128, :], W['rwkv_a2'][l], writes=['rw_a2'])
            k.dma('pool', g2[:], W['rwkv_g2'][l], writes=['rw_g2'])
            k.dma('pool', a0r[:], W['rwkv_a0'][l:l + 1, :], writes=['rw_a0r'])
            k.dma('sp', w0c[:], W['rwkv_w0'][l].rearrange('(c p) -> p c', p=128), writes=['rw_w0c'], allow_slow_non_contiguous=True)
            k.dma('sp', kkc[:], W['rwkv_kk'][l].rearrange('(c p) -> p c', p=128), writes=['rw_kkc'], allow_slow_non_contiguous=True)
            bc = {}
            for nm, src in (('kk', W['rwkv_kk'][l]), ('ka', W['rwkv_ka'][l]), ('rk', W['rwkv_rk'][l].rearrange('h d -> (h d)'))):
                t = g.sb('rw_bc_' + nm, [128, 256], F32, es2)
                k.dma('sp', t[:], src.partition_broadcast(128), writes=['rw_bc_' + nm])
                bc[nm] = t
            hv = g.hTd.rearrange('(c p) t -> p c t', p=128)
            hb = [g.sb('rw_ht%d' % i, [128, 8, 513], BF16, es2) for i in range(2)]
            k.op('pool', lambda e: e.memset(hb[0][:, :, 0:1], 0.0), writes=['rw_ht0'])
            tnh = [g.sb('rw_tnh%d' % i, [128, 512], BF16, es2) for i in range(2)]
            sgd = [g.sb('rw_sgd%d' % i, [128, 512], BF16, es2) for i in range(2)]
            kx = [g.sb('rw_kx%d' % i, [128, 512], F32, es2) for i in range(2)]
            sq = [g.sb('rw_sq%d' % i, [128, 512], BF16, es2) for i in range(2)]
            rs = [g.sb('rw_rs%d' % i, [128, 512], F32, es2) for i in range(2)]
            tm = {nm: [g.sb('rw_%s%d' % (nm, i), [128, 256], F32, es2) for i in range(2)] for nm in ('a', 'r', 'kxm', 'k2', 'tq')}
            s4 = [g.sb('rw_s4%d' % i, [128, 4], F32, es2) for i in range(2)]
            ob = [g.sb('rw_ob%d' % i, [128, 768], BF16, es2) for i in range(2)]

            def fm2(ht, hk, c0, ncols, ps, pskey):
                for c in range(8):
                    k.op('pe', lambda e, c=c: e.matmul(ps, lhsT=wa[:, c, c0:c0 + ncols], rhs=ht[:, c, 1:513], start=(c == 0), stop=False),
                         reads=['rw_wa', hk], writes=[pskey])
                for c in range(8):
                    k.op('pe', lambda e, c=c: e.matmul(ps, lhsT=wb[:, c, c0:c0 + ncols], rhs=ht[:, c, 0:512], start=False, stop=(c == 7)),
                         reads=['rw_wb', hk], writes=[pskey])

            nps = 0
            for tt in range(8):
                b = tt % 2
                ts = slice(tt * 512, (tt + 1) * 512)
                hk = 'rw_ht%d' % b
                ht = hb[b]
                if tt == 0:
                    k.dma('sp', ht[:, :, 1:513], hv[:, :, 0:512], reads=['hTd'], writes=[hk])
                else:
                    k.dma('sp', ht[:], hv[:, :, tt * 512 - 1:(tt + 1) * 512], reads=['hTd'], writes=[hk])
                for j in range(2):
                    pi = nps % 2
                    nps += 1
                    fm2(ht, hk, j * 128, 128, g.ps[pi][:], g.psk[pi])
                    k.op('act', lambda e, pi=pi, j=j, ts=ts: e.copy(out=rT[:, j, ts], in_=g.ps[pi][:]), reads=[g.psk[pi]],
                         writes=['rw_rT%d_%d' % (j, tt)])
                    pi = nps % 2
                    nps += 1
                    fm2(ht, hk, 256 + j * 128, 128, g.ps[pi][:], g.psk[pi])
                    kb = nps % 2
                    ks_ = str(kb)
                    k.op('act', lambda e, pi=pi, j=j, kb=kb: e.activation(out=kx[kb][:], in_=g.ps[pi][:], func=AF.Copy, scale=kkc[:, j:j + 1]),
                         reads=[g.psk[pi], 'rw_kkc'], writes=['rw_kx' + ks_])
                    k.op('act', lambda e, kb=kb: e.activation(out=sq[kb][:], in_=kx[kb][:], func=AF.Square), reads=['rw_kx' + ks_], writes=['rw_sq' + ks_])
                    pj = 2 + kb
                    k.op('pe', lambda e, kb=kb, pj=pj: e.matmul(g.ps[pj][:], lhsT=g.cs['blk64_b'][:], rhs=sq[kb][:], start=True, stop=True),
                         reads=['rw_sq' + ks_, 'c_blk64_b'], writes=[g.psk[pj]])
                    k.op('dve', lambda e, kb=kb, pj=pj: e.tensor_scalar(out=rs[kb][:], in0=g.ps[pj][:], scalar1=1e-12, scalar2=None, op0=ALU.add),
                         reads=[g.psk[pj]], writes=['rw_rs' + ks_])
                    k.op('act', lambda e, kb=kb: e.activation(out=rs[kb][:], in_=rs[kb][:], func=AF.Sqrt), reads=['rw_rs' + ks_], writes=['rw_rs' + ks_])
                    k.op('dve', lambda e, kb=kb: e.reciprocal(out=rs[kb][:], in_=rs[kb][:]), reads=['rw_rs' + ks_], writes=['rw_rs' + ks_])
                    k.op('dve', lambda e, kb=kb, j=j, ts=ts: e.tensor_tensor(out=kkT[:, j, ts], in0=kx[kb][:], in1=rs[kb][:], op=ALU.mult),
                         reads=['rw_kx' + ks_, 'rw_rs' + ks_], writes=['rw_kkT%d_%d' % (j, tt)])
                pi = nps % 2
                nps += 1
                fm2(ht, hk, 768, 128, g.ps[pi][:], g.psk[pi])
                k.op('act', lambda e, pi=pi, b=b: e.activation(out=tnh[b][0:64, :], in_=g.ps[pi][0:64, :], func=AF.Tanh),
                     reads=[g.psk[pi]], writes=['rw_tnh%d' % b])
                k.op('act', lambda e, pi=pi, b=b: e.copy(out=tnh[b][64:128, :], in_=g.ps[pi][64:128, :]),
                     reads=[g.psk[pi]], writes=['rw_tnhb%d' % b])
                pi = nps % 2
                nps += 1
                fm2(ht, hk, 896, 128, g.ps[pi][:], g.psk[pi])
                k.op('act', lambda e, pi=pi, b=b: e.activation(out=sgd[b][:], in_=g.ps[pi][:], func=AF.Sigmoid), reads=[g.psk[pi]],
                     writes=['rw_sgd%d' % b])
                for j in range(2):
                    pi = nps % 2
                    nps += 1
                    k.op('pe', lambda e, pi=pi, j=j, b=b: e.matmul(g.ps[pi][:], lhsT=w2[:, j * 128:(j + 1) * 128], rhs=tnh[b][0:64, :],
                                                                   start=True, stop=True), reads=['rw_w2', 'rw_tnh%d' % b], writes=[g.psk[pi]])
                    k.op('act', lambda e, pi=pi, j=j, ts=ts: e.activation(out=wT[:, j, ts], in_=g.ps[pi][:], func=AF.Sigmoid, bias=w0c[:, j:j + 1], scale=1.0),
                         reads=[g.psk[pi], 'rw_w0c'], writes=['rw_wT%d_%d' % (j, tt)])
                    k.op('act', lambda e, j=j, ts=ts: e.activation(out=wT[:, j, ts], in_=wT[:, j, ts], func=AF.Exp, scale=-0.606531),
                         reads=['rw_wT%d_%d' % (j, tt)], writes=['rw_wT%d_%d' % (j, tt)])
                for sub in range(4):
                    t_ = tt * 4 + sub
                    q = t_ % 2
                    qs = str(q)
                    cs_ = slice(sub * 128, (sub + 1) * 128)
                    pA, pB, pC, pD = 4, 5, 6, 7
                    for (ps_, c0, n_) in ((pA, 0, 512), (pB, 512, 256)):
                        for c in range(8):
                            k.op('pe', lambda e, c=c, ps_=ps_, c0=c0, n_=n_, sub=sub: e.matmul(
                                g.ps[ps_][:, 0:n_], lhsT=ht[:, c, 1 + sub * 128:1 + (sub + 1) * 128], rhs=wa[:, c, c0:c0 + n_],
                                start=(c == 0), stop=False), reads=['rw_wa', hk], writes=[g.psk[ps_]])
                        for c in range(8):
                            k.op('pe', lambda e, c=c, ps_=ps_, c0=c0, n_=n_, sub=sub: e.matmul(
                                g.ps[ps_][:, 0:n_], lhsT=ht[:, c, sub * 128:(sub + 1) * 128], rhs=wb[:, c, c0:c0 + n_],
                                start=False, stop=(c == 7)), reads=['rw_wb', hk], writes=[g.psk[ps_]])
                    k.op('pe', lambda e, b=b, cs_=cs_: e.matmul(g.ps[pC][:, 0:256], lhsT=tnh[b][64:128, cs_], rhs=a2[64:128, :], start=True, stop=False),
                         reads=['rw_tnhb%d' % b, 'rw_a2'], writes=[g.psk[pC]])
                    k.op('pe', lambda e: e.matmul(g.ps[pC][:, 0:256], lhsT=g.cs['ones_b'][0:1, :], rhs=a0r[:], start=False, stop=True),
                         reads=['c_ones_b', 'rw_a0r'], writes=[g.psk[pC]])
                    k.op('pe', lambda e, b=b, cs_=cs_: e.matmul(g.ps[pD][:, 0:256], lhsT=sgd[b][:, cs_], rhs=g2[:], start=True, stop=True),
                         reads=['rw_sgd%d' % b, 'rw_g2'], writes=[g.psk[pD]])
                    a_, r_, kxm, k2, tq = (tm[nm][q] for nm in ('a', 'r', 'kxm', 'k2', 'tq'))
                    K_ = lambda nm: 'rw_%s%s' % (nm, qs)
                    k.op('act', lambda e: e.activation(out=a_[:], in_=g.ps[pC][:, 0:256], func=AF.Sigmoid), reads=[g.psk[pC]], writes=[K_('a')])
                    k.op('act', lambda e, t_=t_: e.copy(out=gtm[:, t_, :], in_=g.ps[pD][:, 0:256]), reads=[g.psk[pD]], writes=['rw_gtm%d' % t_])
                    k.op('act', lambda e: e.copy(out=r_[:], in_=g.ps[pA][:, 0:256]), reads=[g.psk[pA]], writes=[K_('r')])
                    k.op('act', lambda e, t_=t_: e.copy(out=vtm[:, t_, :], in_=g.ps[pB][:, 0:256]), reads=[g.psk[pB]], writes=['rw_vtm%d' % t_])
                    k.op('act', lambda e, q=q: e.copy(out=ob[q][:, 512:768], in_=g.ps[pB][:, 0:256]), reads=[g.psk[pB]], writes=['rw_obv' + qs])
                    k.op('dve', lambda e: e.tensor_tensor(out=kxm[:], in0=g.ps[pA][:, 256:512], in1=bc['kk'][:], op=ALU.mult),
                         reads=[g.psk[pA], 'rw_bc_kk'], writes=[K_('kxm')])
                    k.op('pool', lambda e: e.tensor_tensor(out=tq[:], in0=kxm[:], in1=kxm[:], op=ALU.mult), reads=[K_('kxm')], writes=[K_('tq')])
                    k.op('dve', lambda e, q=q: e.reduce_sum(out=s4[q][:], in_=tq[:].rearrange('p (h d) -> p h d', d=64), axis=AX.X),
                         reads=[K_('tq')], writes=['rw_s4' + qs])
                    k.op('dve', lambda e, q=q: e.tensor_scalar(out=s4[q][:], in0=s4[q][:], scalar1=1e-12, scalar2=None, op0=ALU.add),
                         reads=['rw_s4' + qs], writes=['rw_s4' + qs])
                    k.op('act', lambda e, q=q: e.activation(out=s4[q][:], in_=s4[q][:], func=AF.Sqrt), reads=['rw_s4' + qs], writes=['rw_s4' + qs])
                    k.op('dve', lambda e, q=q: e.reciprocal(out=s4[q][:], in_=s4[q][:]), reads=['rw_s4' + qs], writes=['rw_s4' + qs])
                    for h in range(4):
                        hs = slice(h * 64, (h + 1) * 64)
                        k.op('dve', lambda e, h=h, hs=hs, q=q: e.scalar_tensor_tensor(out=ob[q][:, hs], in0=kxm[:, hs], scalar=s4[q][:, h:h + 1],
                                                                                      in1=a_[:, hs], op0=ALU.mult, op1=ALU.mult),
                             reads=[K_('kxm'), 'rw_s4' + qs, K_('a')], writes=['rw_obb' + qs])
                    k.op('dve', lambda e: e.scalar_tensor_tensor(out=tq[:], in0=a_[:], scalar=-1.0, in1=bc['ka'][:], op0=ALU.add, op1=ALU.mult),
                         reads=[K_('a'), 'rw_bc_ka', K_('tq')], writes=[K_('tq')])
                    k.op('dve', lambda e: e.scalar_tensor_tensor(out=k2[:], in0=tq[:], scalar=1.0, in1=g.ps[pA][:, 256:512], op0=ALU.add, op1=ALU.mult),
                         reads=[K_('tq'), g.psk[pA]], writes=[K_('k2')])
                    k.op('pool', lambda e, q=q: e.tensor_copy(ob[q][:, 256:512], k2[:]), reads=[K_('k2')], writes=['rw_obk' + qs])
                    k.op('pool', lambda e: e.tensor_tensor(out=tq[:], in0=r_[:], in1=k2[:], op=ALU.mult), reads=[K_('r'), K_('k2'), K_('tq')], writes=[K_('tq')])
                    k.op('pool', lambda e: e.tensor_tensor(out=tq[:], in0=tq[:], in1=bc['rk'][:], op=ALU.mult), reads=[K_('tq'), 'rw_bc_rk'], writes=[K_('tq')])
                    k.op('dve', lambda e, t_=t_: e.reduce_sum(out=bon[:, t_, :], in_=tq[:].rearrange('p (h d) -> p h d', d=64), axis=AX.X),
                         reads=[K_('tq')], writes=['rw_bon%d' % t_])
                    k.dma('sp', g.rwscr[t_ * 128:(t_ + 1) * 128, :], ob[q][:], reads=['rw_obv' + qs, 'rw_obb' + qs, 'rw_obk' + qs],
                          writes=['rwscr%d' % t_])
            k.barrier()
        TC = 64
        ST = g.sb('rw_ST', [128, 128], BF16, es)
        Lb = [g.sb('rw_L%d' % i, [36, TC, 128], BF16, es) for i in range(2)]
        Rb = [g.sb('rw_R%d' % i, [36, TC, 128], BF16, es) for i in range(2)]
        KKn = [g.sb('rw_KKn%d' % i, [128, TC, 4], BF16, es) for i in range(2)]
        Rr = [g.sb('rw_Rr%d' % i, [128, TC, 4], BF16, es) for i in range(2)]
        sam = load_const(g, es, 'sa_mask')
        hlm = load_const(g, es, 'hl_mask')
        Yc = [g.sb('rw_Yc%d' % i, [128, 2, 128], F32, es) for i in range(2)]
        ytm = g.sb('rw_ytm', [128, 256], F32, es)
        yo = [g.sb('rw_yo%d' % i, [128, 256], BF16, es) for i in range(2)]
        yT = [g.sb('rw_yT%d' % i, [128, 2, 128], BF16, es) for i in range(2)]
        st8 = g.sb('rw_st8', [128, 8], F32, es)
        tq2 = g.sb('rw_tq2', [128, 256], F32, es)
        gnw = g.sb('rw_gnw', [128, 256], F32, es)
        gnb = g.sb('rw_gnb', [128, 256], F32, es)
        k.dma('sp', gnw[:], W['rwkv_gn_w'][l].partition_broadcast(128), writes=['rw_gnw'])
        k.dma('sp', gnb[:], W['rwkv_gn_b'][l].partition_broadcast(128), writes=['rw_gnb'])
        k.op('pool', lambda e: e.memset(ST[:], 0.0), writes=['rw_ST'])
        for i in range(2):
            k.op('pool', lambda e, i=i: e.memset(Lb[i][:], 0.0), writes=['rw_L%d' % i])
            k.op('pool', lambda e, i=i: e.memset(Rb[i][:], 0.0), writes=['rw_R%d' % i])
        psb = [g.ps[6][:].bitcast(BF16), g.ps[7][:].bitcast(BF16)]
        nsteps = RW_STEPS
        for ch in range(nsteps // TC):
            cb = ch % 2
            t0 = ch * TC
            cbs = str(cb)
            tt = t0 // 512
            for h in range(4):
                hl, hh = h % 2, h // 2
                src = g.rwscr[t0:t0 + TC, :]
                k.dma('sp', Lb[cb][h:h + 1, :, hl * 64:(hl + 1) * 64], src[:, h * 64:(h + 1) * 64].rearrange('(o t) d -> o t d', o=1),
                      reads=['rwscr%d' % (t0 // 128)], writes=['rw_L' + cbs])
                k.dma('sp', Lb[cb][32 + h:33 + h, :, hl * 64:(hl + 1) * 64], src[:, 256 + h * 64:256 + (h + 1) * 64].rearrange('(o t) d -> o t d', o=1),
                      reads=['rwscr%d' % (t0 // 128)], writes=['rw_L' + cbs])
                k.dma('sp', Rb[cb][32 + h:33 + h, :, hh * 64:(hh + 1) * 64], src[:, 512 + h * 64:512 + (h + 1) * 64].rearrange('(o t) d -> o t d', o=1),
                      reads=['rwscr%d' % (t0 // 128)], writes=['rw_Rv' + cbs])
                k.op('pool', lambda e, h=h, hh=hh, hl=hl, cb=cb, t0=t0: e.tensor_scalar(
                    out=KKn[cb][:, :, h], in0=kkT[:, hh, t0:t0 + TC], scalar1=hlm[:, hl:hl + 1], scalar2=None, op0=ALU.mult),
                    reads=['rw_kkT%d_%d' % (hh, tt), 'c_hl_mask'], writes=['rw_KKn' + cbs])
                k.op('pool', lambda e, h=h, hh=hh, hl=hl, cb=cb, t0=t0: e.tensor_scalar(
                    out=Rr[cb][:, :, h], in0=rT[:, hh, t0:t0 + TC], scalar1=hlm[:, 2 + hl:3 + hl], scalar2=None, op0=ALU.mult),
                    reads=['rw_rT%d_%d' % (hh, tt), 'c_hl_mask'], writes=['rw_Rr' + cbs])
            for s_ in range(TC):
                t = t0 + s_
                pa = t % 2
                pu = 2 + t % 2
                py = 4 + (t // 128) % 2
                k.op('pe', lambda e, cb=cb, s_=s_, pa=pa: e.matmul(g.ps[pa][0:4, 0:128], lhsT=KKn[cb][:, s_, :], rhs=ST[:], start=True, stop=True),
                     reads=['rw_KKn' + cbs, 'rw_ST'], writes=[g.psk[pa]])
                k.op('dve', lambda e, cb=cb, s_=s_, pa=pa: e.tensor_tensor(out=Rb[cb][0:4, s_, :], in0=g.ps[pa][0:4, 0:128], in1=sam[0:4, :], op=ALU.mult),
                     reads=[g.psk[pa], 'c_sa_mask'], writes=['rw_R' + cbs])
                k.op('pe', lambda e, cb=cb, s_=s_, pu=pu: e.matmul(g.ps[pu][:, 0:128], lhsT=Lb[cb][:, s_, :], rhs=Rb[cb][:, s_, :], start=True, stop=True),
                     reads=['rw_L' + cbs, 'rw_R' + cbs, 'rw_Rv' + cbs], writes=[g.psk[pu]])
                for hh in range(2):
                    k.op('dve', lambda e, hh=hh, t=t, pu=pu: e.scalar_tensor_tensor(
                        out=ST[:, hh * 64:(hh + 1) * 64], in0=ST[:, hh * 64:(hh + 1) * 64], scalar=wT[:, hh, t:t + 1],
                        in1=g.ps[pu][:, hh * 64:(hh + 1) * 64], op0=ALU.mult, op1=ALU.add),
                        reads=['rw_ST', g.psk[pu], 'rw_wT%d_%d' % (hh, tt)], writes=['rw_ST'])
                k.op('pe', lambda e, cb=cb, s_=s_, t=t, py=py: e.matmul(g.ps[py][:, (t % 128) * 4:(t % 128) * 4 + 4], lhsT=ST[:], rhs=Rr[cb][:, s_, :],
                                                                   start=True, stop=True), reads=['rw_ST', 'rw_Rr' + cbs], writes=[g.psk[py]])
                if t % 128 == 127:
                    t_ = t // 128
                    yb_ = t_ % 2
                    ys = str(yb_)
                    yv = g.ps[py][:].rearrange('p (t h) -> p h t', h=4)
                    for hl in range(2):
                        k.op('act', lambda e, hl=hl, yb_=yb_: e.copy(out=Yc[yb_][0:64, hl, :], in_=yv[0:64, hl, :]), reads=[g.psk[py]],
                             writes=['rw_Yc%s_%d' % (ys, hl)])
                        k.op('act', lambda e, hl=hl, yb_=yb_: e.copy(out=Yc[yb_][64:128, hl, :], in_=yv[64:128, 2 + hl, :]), reads=[g.psk[py]],
                             writes=['rw_Ycb%s_%d' % (ys, hl)])
                    ytv = ytm[:].rearrange('p (hh hl i) -> p hl hh i', hh=2, hl=2)
                    for hl in range(2):
                        pt_ = 6 + hl
                        k.op('pe', lambda e, hl=hl, yb_=yb_, pt_=pt_: e.transpose(out=g.ps[pt_][:, 0:128], in_=Yc[yb_][:, hl, :], identity=g.cs['ident_f'][:]),
                             reads=['rw_Yc%s_%d' % (ys, hl), 'rw_Ycb%s_%d' % (ys, hl), 'c_ident_f'], writes=[g.psk[pt_]])
                        k.op('act', lambda e, hl=hl, pt_=pt_: e.copy(out=ytv[:, hl, :, :], in_=g.ps[pt_][:, 0:128].rearrange('p (hh i) -> p hh i', hh=2)),
                             reads=[g.psk[pt_]], writes=['rw_ytm'])
                    k.op('dve', lambda e: e.reduce_sum(out=st8[:, 0:4], in_=ytm[:].rearrange('p (h d) -> p h d', d=64), axis=AX.X),
                         reads=['rw_ytm'], writes=['rw_st8'])
                    k.op('pool', lambda e: e.tensor_tensor(out=tq2[:], in0=ytm[:], in1=ytm[:], op=ALU.mult), reads=['rw_ytm'], writes=['rw_tq2'])
                    k.op('dve', lambda e: e.reduce_sum(out=st8[:, 4:8], in_=tq2[:].rearrange('p (h d) -> p h d', d=64), axis=AX.X),
                         reads=['rw_tq2'], writes=['rw_st8'])
                    k.op('dve', lambda e: e.tensor_scalar(out=st8[:], in0=st8[:], scalar1=1.0 / 64, scalar2=None, op0=ALU.mult),
                         reads=['rw_st8'], writes=['rw_st8'])
                    k.op('dve', lambda e: e.tensor_tensor(out=tq2[:, 0:4], in0=st8[:, 0:4], in1=st8[:, 0:4], op=ALU.mult), reads=['rw_st8', 'rw_tq2'],
                         writes=['rw_tq2'])
                    k.op('dve', lambda e: e.tensor_tensor(out=st8[:, 4:8], in0=st8[:, 4:8], in1=tq2[:, 0:4], op=ALU.subtract), reads=['rw_st8', 'rw_tq2'],
                         writes=['rw_st8'])
                    k.op('dve', lambda e: e.tensor_scalar(out=st8[:, 4:8], in0=st8[:, 4:8], scalar1=64e-5, scalar2=None, op0=ALU.add),
                         reads=['rw_st8'], writes=['rw_st8'])
                    k.op('act', lambda e: e.activation(out=st8[:, 4:8], in_=st8[:, 4:8], func=AF.Sqrt), reads=['rw_st8'], writes=['rw_st8'])
                    k.op('dve', lambda e: e.reciprocal(out=st8[:, 4:8], in_=st8[:, 4:8]), reads=['rw_st8'], writes=['rw_st8'])
                    for h in range(4):
                        hs = slice(h * 64, (h + 1) * 64)
                        k.op('dve', lambda e, h=h, hs=hs: e.tensor_scalar(out=tq2[:, hs], in0=ytm[:, hs], scalar1=st8[:, h:h + 1], scalar2=st8[:, 4 + h:5 + h],
                                                                         op0=ALU.subtract, op1=ALU.mult), reads=['rw_ytm', 'rw_st8', 'rw_tq2'], writes=['rw_tq2'])
                    k.op('pool', lambda e: e.tensor_tensor(out=tq2[:], in0=tq2[:], in1=gnw[:], op=ALU.mult), reads=['rw_tq2', 'rw_gnw'], writes=['rw_tq2'])
                    k.op('pool', lambda e: e.tensor_tensor(out=tq2[:], in0=tq2[:], in1=gnb[:], op=ALU.add), reads=['rw_tq2', 'rw_gnb'], writes=['rw_tq2'])
                    for h in range(4):
                        hs = slice(h * 64, (h + 1) * 64)
                        k.op('dve', lambda e, h=h, hs=hs, t_=t_: e.scalar_tensor_tensor(out=tq2[:, hs], in0=vtm[:, t_, hs], scalar=bon[:, t_, h:h + 1],
                                                                                       in1=tq2[:, hs], op0=ALU.mult, op1=ALU.add),
                             reads=['rw_vtm%d' % t_, 'rw_bon%d' % t_, 'rw_tq2'], writes=['rw_tq2'])
                    k.op('dve', lambda e, yb_=yb_, t_=t_: e.tensor_tensor(out=yo[yb_][:], in0=tq2[:], in1=gtm[:, t_, :], op=ALU.mult),
                         reads=['rw_tq2', 'rw_gtm%d' % t_], writes=['rw_yo' + ys])
                    for j in range(2):
                        k.op('pe', lambda e, j=j, yb_=yb_: e.transpose(out=psb[yb_][:, j * 128:(j + 1) * 128], in_=yo[yb_][:, j * 128:(j + 1) * 128],
                                                                      identity=g.cs['ident_b'][:]), reads=['rw_yo' + ys, 'c_ident_b'], writes=[g.psk[6 + yb_]])
                    k.op('act', lambda e, yb_=yb_: e.copy(out=yT[yb_][:], in_=psb[yb_][:, 0:256].rearrange('p (j t) -> p j t', j=2)),
                         reads=[g.psk[6 + yb_]], writes=['rw_yT' + ys])
                    k.dma('sp', ycv[:, 0:2, t_ * 128:(t_ + 1) * 128], yT[yb_][:], reads=['rw_yT' + ys], writes=['ycat_rw%d' % t_])
        k.barrier()
```

```python
import contextlib
import numpy as np
import ml_dtypes
import concourse.bass as bass
import concourse.mybir as mybir
from concourse.bass_utils import run_bass_kernel_spmd

F32 = mybir.dt.float32
BF16 = mybir.dt.bfloat16
I32 = mybir.dt.int32
AF = mybir.ActivationFunctionType
ALU = mybir.AluOpType
AX = mybir.AxisListType


class KB:
    NDS = 8

    def __init__(self):
        self.nc = bass.Bass("TRN2", target_bir_lowering=False)
        nc = self.nc
        self.eng = {"pe": nc.tensor, "act": nc.scalar, "dve": nc.vector, "pool": nc.gpsimd, "sp": nc.sync}
        self.semh = {}
        for e in self.eng:
            self.semh[e] = nc.semaphore("s_" + e).__enter__()
        self.cnt = {e: 0 for e in self.eng}
        self.waited = {e: {} for e in self.eng}
        self.lastw = {}
        self.readers = {}
        self.dq = {}
        self.nwaits = 0
        self.nops = 0

    def _deps(self, eng, reads, writes):
        deps = {}

        def add(k, v):
            if deps.get(k, 0) < v:
                deps[k] = v

        for r in reads:
            p = self.lastw.get(r)
            if p is not None:
                if not (p[0] == eng and eng == "pe"):
                    add(*p)
            if isinstance(r, str) and r.startswith("ps") and r[2:].isdigit():
                for k, v in self.readers.get(r, {}).items():
                    if k != eng:
                        add(k, v)
        for w in writes:
            p = self.lastw.get(w)
            if p is not None and p[0] != eng:
                add(*p)
            for k, v in self.readers.get(w, {}).items():
                if k != eng:
                    add(k, v)
        return deps

    def _wait(self, eng, deps):
        wt = self.waited[eng]
        for k, v in deps.items():
            if wt.get(k, 0) >= v:
                continue
            self.eng[eng].wait_ge(self.semh[k], v)
            wt[k] = v
            self.nwaits += 1

    def _record(self, prod, reads, writes):
        k, v = prod
        for r in reads:
            d = self.readers.setdefault(r, {})
            if d.get(k, 0) < v:
                d[k] = v
        for w in writes:
            self.lastw[w] = prod
            self.readers[w] = {}

    def op(self, eng, fn, reads=(), writes=()):
        self._wait(eng, self._deps(eng, reads, writes))
        ins = fn(self.eng[eng])
        self.cnt[eng] += 1
        ins.then_inc(self.semh[eng], 1)
        self._record((eng, self.cnt[eng]), reads, writes)
        self.nops += 1
        return ins

    def dma(self, q, out, in_, reads=(), writes=(), **kw):
        st = self.dq.get(q)
        if st is None:
            st = {"n": 0, "sems": []}
            for i in range(self.NDS):
                key = ("dma", q, i)
                self.semh[key] = self.nc.semaphore("d_%s_%d" % (q, i)).__enter__()
                st["sems"].append(key)
            self.dq[q] = st
        i = st["n"]
        slot = i % self.NDS
        val = 16 * (i // self.NDS + 1)
        key = st["sems"][slot]
        deps = self._deps(q, reads, writes)
        if i >= self.NDS:
            if deps.get(key, 0) < val - 16:
                deps[key] = val - 16
        self._wait(q, deps)
        ins = self.eng[q].dma_start(out=out, in_=in_, **kw)
        ins.then_inc(self.semh[key], 16)
        st["n"] += 1
        self._record((key, val), reads, writes)
        return ins

    def barrier(self):
        deps = {}
        for e in self.eng:
            if self.cnt[e] > 0:
                deps[e] = self.cnt[e]
        for q, st in self.dq.items():
            n = st["n"]
            for slot in range(self.NDS):
                if n > slot:
                    cntslot = (n - 1 - slot) // self.NDS + 1
                    deps[st["sems"][slot]] = 16 * cntslot
        for e in self.eng:
            d = {k: v for k, v in deps.items() if k != e or e != "pe"}
            self._wait(e, d)

    def finish(self):
        self.barrier()


S = 4096
D = 1024
NT = 32
PROJ_W = 3212
L_DEPTH = 2
NORM_EPS = 1e-6
PI = float(np.pi)

PARAM_SHAPES = {
    'w_ada': (2, 1024, 6144), 'b_ada': (2, 6144), 'norm1_g': (2, 1024), 'norm2_g': (2, 1024),
    'w_in': (2, 1024, 3212), 'w_out': (2, 1024, 1024), 'rwkv_mu': (2, 1024), 'rwkv_w0': (2, 256),
    'rwkv_w2': (2, 64, 256), 'rwkv_a0': (2, 256), 'rwkv_a2': (2, 64, 256), 'rwkv_g2': (2, 128, 256),
    'rwkv_kk': (2, 256), 'rwkv_ka': (2, 256), 'rwkv_rk': (2, 4, 64), 'rwkv_gn_w': (2, 256), 'rwkv_gn_b': (2, 256),
    'conv_w': (2, 3, 256), 'dil_q_g': (2, 64), 'dil_k_g': (2, 64), 'nsa_q_g': (2, 64), 'nsa_kc_g': (2, 64),
    'nsa_ks_g': (2, 64), 'nsa_kw_g': (2, 64), 'nsa_pe_k': (2, 32, 64), 'nsa_pe_v': (2, 32, 64),
    'nsa_wk1': (2, 2048, 256), 'nsa_wk2': (2, 256, 64), 'nsa_wv1': (2, 2048, 256), 'nsa_wv2': (2, 256, 64),
    'onorm_g': (2, 768), 'w_router': (2, 1024, 32), 'b_router': (2, 32), 'w_gu': (2, 32, 1024, 2048),
    'b_gu': (2, 32, 2048), 'w_down': (2, 32, 1024, 1024), 'b_down': (2, 32, 1024),
}


def host_consts():
    c = {}
    c['ident_f'] = np.eye(128, dtype=np.float32)
    c['ident_b'] = np.eye(128).astype(ml_dtypes.bfloat16)
    c['ones_b'] = np.ones((128, 128)).astype(ml_dtypes.bfloat16)
    blk = np.zeros((128, 128), np.float32)
    blk[:64, :64] = 1.0
    blk[64:, 64:] = 1.0
    c['blk64_b'] = blk.astype(ml_dtypes.bfloat16)
    sel = np.zeros((32, 32, 128), np.float32)
    for e in range(32):
        sel[e, e, :] = 1.0
    c['sel_b'] = sel.astype(ml_dtypes.bfloat16)
    pr = np.zeros((128, 128), np.float32)
    for m in range(128):
        if m % 64 < 32:
            pr[m + 32, m] = -1.0
        else:
            pr[m - 32, m] = 1.0
    c['prot_b'] = pr.astype(ml_dtypes.bfloat16)
    c['invf'] = (10000.0 ** (-(np.arange(128) % 32) / 32.0)).astype(np.float32).reshape(128, 1)
    kl = np.arange(128)[:, None]
    ql = np.arange(512)[None, :]

    def toep(offs, fn):
        return np.stack([fn(128 * o + ql - kl) for o in offs]).astype(np.float32)

    def dil(d):
        m = ((d >= 0) & (d <= 128)).astype(np.float32)
        m += ((d >= 0) & (d % 4 == 0) & (d // 4 <= 128))
        m += ((d >= 0) & (d % 16 == 0) & (d // 16 <= 128))
        return m
    c['dil_mask'] = toep(range(-3, 17), dil).transpose(1, 0, 2).astype(ml_dtypes.bfloat16).copy()
    c['swa_mask'] = toep(range(-3, 5), lambda d: (d >= 0) & (d <= 511)).transpose(1, 0, 2).astype(ml_dtypes.bfloat16).copy()
    c['cau_mask'] = toep(range(-3, 1), lambda d: d >= 0).transpose(1, 0, 2).astype(ml_dtypes.bfloat16).copy()
    c['cmp_mask'] = np.stack([(16 * kl + 31 <= 512 * i + ql) for i in range(5)]).astype(np.float32).transpose(1, 0, 2) \
        .astype(ml_dtypes.bfloat16).copy()
    diff = np.arange(256)[:, None] - 4 * np.arange(64)[None, :]
    offs = (np.arange(4)[:, None] - np.arange(2)[None, :]).reshape(-1)
    ov = (diff[..., None] == offs).sum(-1).astype(np.float32)
    ov[255] = 0
    ov1 = np.concatenate([np.ones((256, 1), np.float32), ov], axis=1)
    ov1[255] = 0
    c['ovl1'] = ov1.reshape(2, 128, 65).transpose(1, 0, 2).astype(ml_dtypes.bfloat16).copy()
    tok = np.arange(S)[:, None]
    jb = np.arange(64)[None, :]
    cur = tok // 64
    fut = jb > cur
    forced = ((jb == 0) | (jb == cur) | (jb == cur - 1)) & ~fut
    keep = ~(fut | forced)
    c['sel_keep'] = keep.astype(np.float32).reshape(32, 128, 64).transpose(1, 0, 2).copy()
    c['sel_add'] = (-1.0 * fut + 1e4 * forced).astype(np.float32).reshape(32, 128, 64).transpose(1, 0, 2).copy()
    ex = np.zeros((64, 32, 128), np.float32)
    for kt in range(32):
        ex[2 * kt, kt, :64] = 1
        ex[2 * kt + 1, kt, 64:] = 1
    c['sel_exp'] = ex.astype(ml_dtypes.bfloat16)
    sam = np.zeros((128, 128), np.float32)
    sam[0:2, 0:64] = 1.0
    sam[2:4, 64:128] = 1.0
    c['sa_mask'] = sam
    hm = np.zeros((128, 4), np.float32)
    hm[0:64, 0] = -1.0
    hm[64:128, 1] = -1.0
    hm[0:64, 2] = 1.0
    hm[64:128, 3] = 1.0
    c['hl_mask'] = hm
    return c


RESIDENT_CONSTS = ('ident_f', 'ident_b', 'ones_b', 'blk64_b', 'sel_b', 'prot_b', 'invf')


class Ctx:
    pass


class LazyW:
    def __init__(self, nc):
        self.nc = nc
        self.d = {}

    def __getitem__(self, n):
        if n not in self.d:
            self.d[n] = self.nc.dram_tensor(n, list(PARAM_SHAPES[n]), F32, kind='ExternalInput').ap()
        return self.d[n]


def build_program(layers=(0, 1), taps=(), first=True, last=True, mixers='ABCD', moe=True):
    k = KB()
    nc = k.nc
    g = Ctx()
    g.k = k
    g.nc = nc
    g.taps = {}
    g.want = set(taps)
    g.mixers = mixers
    g.do_moe = moe
    g.x = nc.dram_tensor('x', [S, D], F32, kind='ExternalInput').ap()
    g.c = nc.dram_tensor('c', [D], F32, kind='ExternalInput').ap()
    g.pos = nc.dram_tensor('positions', [S], I32, kind='ExternalInput').ap()
    g.W = LazyW(nc)
    g.C = {}
    for n, a in host_consts().items():
        dt = F32 if a.dtype == np.float32 else BF16
        g.C[n] = nc.dram_tensor('const_' + n, list(a.shape), dt, kind='ExternalInput').ap()
    g.out = nc.dram_tensor('out', [S, D], F32, kind='ExternalOutput').ap()
    g.xT = [nc.dram_tensor('xT%d' % i, [D, S], F32, kind='Internal').ap() for i in range(2)]
    g.ycatT = nc.dram_tensor('ycatT', [D, S], BF16, kind='Internal').ap()
    g.h2Td = nc.dram_tensor('h2Td', [D, S], BF16, kind='Internal').ap()
    g.hTd = nc.dram_tensor('hTd', [D, S], BF16, kind='Internal').ap()
    g.rwscr = nc.dram_tensor('rwscr', [S, 768], BF16, kind='Internal').ap()

    with contextlib.ExitStack() as es:
        g.layer = 'i'
        g.nsb = 0

        def sb(name, shape, dt, stack=es):
            g.nsb += 1
            return stack.enter_context(nc.sbuf_tensor('%s_%d' % (name, g.nsb), list(shape), dt))
        g.sb = sb
        g.ps = [nc.alloc_psum_tensor('ps%d' % i, [128, 512], F32) for i in range(8)]
        g.psk = ['ps%d' % i for i in range(8)]
        g.cs = {}
        for n, ap in g.C.items():
            if n not in RESIDENT_CONSTS:
                continue
            t = sb('c_' + n, ap.shape, ap.dtype)
            k.dma('sp', t[:], ap, writes=['c_' + n])
            g.cs[n] = t
        g.mod = sb('mod', [128, 48], F32)
        g.GT = sb('GT', [32, S], BF16)
        if first:
            phase_pre(g)
        cur = 0
        for l in layers:
            g.layer = str(l)
            phase_mod(g, l)
            with contextlib.ExitStack() as les:
                phase_norm1(g, l, g.xT[cur])
                g.onorm = g.sb('onorm', [128, 6], F32, les)
                k.dma('sp', g.onorm[:], g.W['onorm_g'][l].rearrange('(j p) -> p j', p=128), writes=['onorm'],
                      allow_slow_non_contiguous=True)
                if 'B' in g.mixers:
                    mixer_conv(g, l)
                if 'C' in g.mixers:
                    mixer_dil(g, l)
                if 'D' in g.mixers:
                    mixer_nsa(g, l)
                if 'A' in g.mixers:
                    mixer_rwkv(g, l)
                zero_missing(g)
                t = tap(g, 'ycatT%d' % l, [D, S], BF16)
                if t is not None:
                    k.barrier()
                    k.dma('sp', t, g.ycatT, writes=['tap'])
                k.barrier()
            phase_wout(g, l, g.xT[cur], g.xT[cur ^ 1])
            phase_norm2_router(g, l, g.xT[cur ^ 1])
            phase_moe(g, l, g.xT[cur ^ 1], g.xT[cur], final=(last and l == layers[-1]))
        k.finish()
    return k, g


def tap(g, name, shape, dt=F32):
    if name not in g.want:
        return None
    t = g.nc.dram_tensor('tap_' + name, list(shape), dt, kind='ExternalOutput').ap()
    g.taps[name] = t
    return t


def phase_pre(g):
    k, nc = g.k, g.nc
    identf = g.cs['ident_f']
    xTd = g.xT[0].rearrange('(c p) t -> p c t', p=128)
    with contextlib.ExitStack() as es:
        xin = [g.sb('pre_x%d' % i, [128, D], F32, es) for i in range(2)]
        xo = [g.sb('pre_o%d' % i, [128, 8, 128], F32, es) for i in range(2)]
        for t in range(NT):
            b = t % 2
            k.dma('sp', xin[b][:], g.x[t * 128:(t + 1) * 128, :], writes=['pre_x%d' % b])
            for h in range(2):
                pi = (2 * t + h) % 4
                for j in range(4):
                    cc = h * 4 + j
                    k.op('pe', lambda e, cc=cc, j=j, pi=pi, b=b: e.transpose(
                        out=g.ps[pi][:, j * 128:(j + 1) * 128], in_=xin[b][:, cc * 128:(cc + 1) * 128], identity=identf[:]),
                        reads=['pre_x%d' % b, 'c_ident_f'], writes=[g.psk[pi]])
                eng = 'act' if h == 0 else 'dve'
                if eng == 'act':
                    k.op('act', lambda e, pi=pi, b=b, h=h: e.copy(
                        out=xo[b][:, h * 4:(h + 1) * 4, :], in_=g.ps[pi][:].rearrange('p (c t) -> p c t', c=4)),
                        reads=[g.psk[pi]], writes=['pre_o%d_%d' % (b, h)])
                else:
                    k.op('dve', lambda e, pi=pi, b=b, h=h: e.tensor_copy(
                        xo[b][:, h * 4:(h + 1) * 4, :], g.ps[pi][:].rearrange('p (c t) -> p c t', c=4)),
                        reads=[g.psk[pi]], writes=['pre_o%d_%d' % (b, h)])
            k.dma('sp', xTd[:, :, t * 128:(t + 1) * 128], xo[b][:], reads=['pre_o%d_0' % b, 'pre_o%d_1' % b],
                  writes=['xT0_%d' % (t // 4)])
        k.barrier()


def phase_mod(g, l):
    k, nc = g.k, g.nc
    with contextlib.ExitStack() as es:
        cT = g.sb('cT', [128, 8], F32, es)
        cs = g.sb('cs', [128, 8], BF16, es)
        bcol = g.sb('bcol', [128, 48], F32, es)
        wt = [g.sb('wada%d' % i, [128, 8, 512], BF16, es) for i in range(2)]
        k.dma('sp', cT[:], g.c.rearrange('(k p) -> p k', p=128), writes=['cT'], allow_slow_non_contiguous=True)
        k.dma('sp', bcol[:], g.W['b_ada'][l].rearrange('(k p) -> p k', p=128), writes=['bcol'], allow_slow_non_contiguous=True)
        k.op('act', lambda e: e.activation(out=cs[:], in_=cT[:], func=AF.Silu), reads=['cT'], writes=['cs'])
        psm = g.ps[7]
        for ct in range(12):
            b = ct % 2
            k.dma('pool', wt[b][:], g.W['w_ada'][l][:, ct * 512:(ct + 1) * 512].rearrange('(k p) c -> p k c', p=128),
                  writes=['wada%d' % b])
            for j in range(4):
                col = ct * 4 + j
                for kk in range(8):
                    k.op('pe', lambda e, b=b, j=j, kk=kk, col=col: e.matmul(
                        psm[:, col:col + 1], lhsT=wt[b][:, kk, j * 128:(j + 1) * 128], rhs=cs[:, kk:kk + 1],
                        start=(kk == 0), stop=(kk == 7)),
                        reads=['wada%d' % b, 'cs'], writes=[g.psk[7]])
        k.op('dve', lambda e: e.tensor_tensor(out=g.mod[:], in0=psm[:, 0:48], in1=bcol[:], op=ALU.add),
             reads=[g.psk[7], 'bcol'], writes=['mod'])
        t = tap(g, 'mod%d' % l, [128, 48])
        if t is not None:
            k.dma('sp', t, g.mod[:], reads=['mod'], writes=['tap'])
        k.barrier()


def phase_norm1(g, l, xTd):
    k, nc = g.k, g.nc
    xTv = xTd.rearrange('(c p) t -> p c t', p=128)
    with contextlib.ExitStack() as es:
        gcol = g.sb('n1_g', [128, 8], F32, es)
        g1s = g.sb('n1_gs', [128, 8], F32, es)
        k.dma('sp', gcol[:], g.W['norm1_g'][l].rearrange('(k p) -> p k', p=128), writes=['n1_g'], allow_slow_non_contiguous=True)
        k.op('dve', lambda e: e.scalar_tensor_tensor(out=g1s[:], in0=g.mod[:, 8:16], scalar=1.0, in1=gcol[:],
                                                      op0=ALU.add, op1=ALU.mult), reads=['mod', 'n1_g'], writes=['n1_gs'])
        hT = g.sb('hT', [128, 8, S], BF16, es)
        norm_tiles(g, es, xTv, g1s, g.mod[:, 0:8], ['n1_gs', 'mod'], hT, 'hT', 'n1')
        hv = g.hTd.rearrange('(c p) t -> p c t', p=128)
        for c in range(8):
            k.dma('sp', hv[:, c, :], hT[:, c, :], reads=['hT%d' % c], writes=['hTd'])
        t = tap(g, 'h%d' % l, [128, 8, S], BF16)
        if t is not None:
            k.dma('sp', t, hT[:], reads=['hT%d' % i for i in range(8)], writes=['tap'])
        k.barrier()


def norm_tiles(g, es, xTv, gs, sh, gkeys, hT, hkey, pfx, xsrc_keys=None):
    k = g.k
    xt = [g.sb(pfx + '_x%d' % i, [128, 8, 512], F32, es) for i in range(2)]
    sq = [g.sb(pfx + '_sq%d' % i, [128, 8, 512], BF16, es) for i in range(2)]
    rstd = [g.sb(pfx + '_rstd%d' % i, [128, 512], F32, es) for i in range(2)]
    tmp = [g.sb(pfx + '_tmp%d' % i, [128, 512], F32, es) for i in range(2)]
    ones = g.cs['ones_b']
    for tt in range(8):
        b = tt % 2
        ts = slice(tt * 512, (tt + 1) * 512)
        xk, sk, rk = pfx + '_x%d' % b, pfx + '_sq%d' % b, pfx + '_rstd%d' % b
        k.dma('sp', xt[b][:], xTv[:, :, ts], reads=(xsrc_keys or []), writes=[xk])
        k.op('act', lambda e, b=b: e.activation(out=sq[b][:], in_=xt[b][:], func=AF.Square), reads=[xk], writes=[sk])
        pi = tt % 2
        for c in range(8):
            k.op('pe', lambda e, b=b, c=c, pi=pi: e.matmul(g.ps[pi][:], lhsT=ones[:], rhs=sq[b][:, c, :],
                                                          start=(c == 0), stop=(c == 7)),
                 reads=[sk, 'c_ones_b'], writes=[g.psk[pi]])
        k.op('dve', lambda e, b=b, pi=pi: e.tensor_scalar(out=rstd[b][:], in0=g.ps[pi][:], scalar1=1.0 / D, scalar2=NORM_EPS,
                                                         op0=ALU.mult, op1=ALU.add), reads=[g.psk[pi]], writes=[rk])
        k.op('act', lambda e, b=b: e.activation(out=rstd[b][:], in_=rstd[b][:], func=AF.Sqrt), reads=[rk], writes=[rk])
        k.op('dve', lambda e, b=b: e.reciprocal(out=rstd[b][:], in_=rstd[b][:]), reads=[rk], writes=[rk])
        for c in range(8):
            tb = c % 2
            tk = pfx + '_tmp%d' % tb
            k.op('dve', lambda e, b=b, c=c, tb=tb: e.tensor_tensor(out=tmp[tb][:], in0=xt[b][:, c, :], in1=rstd[b][:], op=ALU.mult),
                 reads=[xk, rk], writes=[tk])
            k.op('act', lambda e, c=c, tb=tb, ts=ts: e.activation(out=hT[:, c, ts], in_=tmp[tb][:], func=AF.Identity,
                                                                  scale=gs[:, c:c + 1], bias=sh[:, c:c + 1]),
                 reads=[tk] + gkeys, writes=[hkey + '%d' % c])


def make_in_maps(inputs, g, n_cores=8):
    consts = host_consts()
    maps = []
    shared = {n: np.ascontiguousarray(inputs[n], dtype=np.float32) for n in g.W.d}
    for b in range(n_cores):
        m = {'x': np.ascontiguousarray(inputs['x'][b]), 'c': np.ascontiguousarray(inputs['c'][b]),
             'positions': np.ascontiguousarray(inputs['positions'][b]).astype(np.int32)}
        m.update(shared)
        for n, a in consts.items():
            m['const_' + n] = a
        maps.append(m)
    return maps


def kernel(**inputs):
    k, g = build_program()
    maps = make_in_maps(inputs, g)
    res = run_bass_kernel_spmd(k.nc, maps, core_ids=list(range(8)))
    return np.stack([r['out'] for r in res.results], axis=0)


def load_w_bf16(g, dst, dkey, src_ap):
    g.k.dma('pool', dst, src_ap.rearrange('(c p) n -> p c n', p=128), writes=[dkey])


def ht_loader(g, es, pfx):
    bufs = [g.sb(pfx + '_ht%d' % i, [128, 8, 512], BF16, es) for i in range(2)]
    hv = g.hTd.rearrange('(c p) t -> p c t', p=128)

    def load(tt):
        b = tt % 2
        g.k.dma('sp', bufs[b][:], hv[:, :, tt * 512:(tt + 1) * 512], reads=['hTd'], writes=[pfx + '_ht%d' % b])
        return bufs[b], pfx + '_ht%d' % b
    return load


def proj_fm(g, wsb, wkey, c0, ncols, ht, hkey, ps, pskey):
    for c in range(8):
        g.k.op('pe', lambda e, c=c: e.matmul(ps, lhsT=wsb[:, c, c0:c0 + ncols], rhs=ht[:, c, :],
                                             start=(c == 0), stop=(c == 7)),
               reads=[wkey, hkey], writes=[pskey])


def proj_tm(g, wsb, wkey, c0, ncols, ht, hkey, sub, ps, pskey):
    for c in range(8):
        g.k.op('pe', lambda e, c=c: e.matmul(ps, lhsT=ht[:, c, sub * 128:(sub + 1) * 128], rhs=wsb[:, c, c0:c0 + ncols],
                                             start=(c == 0), stop=(c == 7)),
               reads=[wkey, hkey], writes=[pskey])


def head_rmsnorm_fm(g, y, ykey, sq, sqkey, gcol, gkeys, out_ap, okey, pi, tmpf, tmpkey):
    k = g.k
    k.op('act', lambda e: e.activation(out=sq, in_=y, func=AF.Square), reads=[ykey], writes=[sqkey])
    k.op('pe', lambda e: e.matmul(g.ps[pi][:], lhsT=g.cs['blk64_b'][:], rhs=sq, start=True, stop=True),
         reads=[sqkey, 'c_blk64_b'], writes=[g.psk[pi]])
    k.op('dve', lambda e: e.tensor_scalar(out=tmpf, in0=g.ps[pi][:], scalar1=1.0 / 64, scalar2=NORM_EPS, op0=ALU.mult, op1=ALU.add),
         reads=[g.psk[pi]], writes=[tmpkey])
    k.op('act', lambda e: e.activation(out=tmpf, in_=tmpf, func=AF.Sqrt), reads=[tmpkey], writes=[tmpkey])
    k.op('dve', lambda e: e.reciprocal(out=tmpf, in_=tmpf), reads=[tmpkey], writes=[tmpkey])
    k.op('dve', lambda e: e.scalar_tensor_tensor(out=out_ap, in0=y, scalar=gcol, in1=tmpf, op0=ALU.mult, op1=ALU.mult),
         reads=[ykey, tmpkey] + gkeys, writes=[okey])


def mixer_conv(g, l):
    k = g.k
    ycv = g.ycatT.rearrange('(c p) t -> p c t', p=128)
    with contextlib.ExitStack() as es:
        wb = g.sb('cv_w', [128, 8, 768], BF16, es)
        load_w_bf16(g, wb[:], 'cv_w', g.W['w_in'][l][:, 1024:1792])
        cw = g.sb('cv_cw', [128, 2, 3], F32, es)
        for kk in range(3):
            k.dma('sp', cw[:, :, kk], g.W['conv_w'][l, kk].rearrange('(j p) -> p j', p=128), writes=['cv_cw'],
                  allow_slow_non_contiguous=True)
        bg = g.sb('cv_bg', [128, 2, S], BF16, es)
        u = g.sb('cv_u', [128, 2, S + 2], F32, es)
        cgt = [g.sb('cv_cg%d' % i, [128, 512], F32, es) for i in range(2)]
        k.op('pool', lambda e: e.memset(u[:, :, 0:2], 0.0), writes=['cv_u_h'])
        n = 0
        hload = ht_loader(g, es, 'cv')
        for tt in range(8):
            ts = slice(tt * 512, (tt + 1) * 512)
            ht, hk = hload(tt)
            for j in range(2):
                pa, pb, pc = n % 6, (n + 1) % 6, (n + 2) % 6
                n += 3
                proj_fm(g, wb, 'cv_w', j * 128, 128, ht, hk, g.ps[pa][:], g.psk[pa])
                proj_fm(g, wb, 'cv_w', 256 + j * 128, 128, ht, hk, g.ps[pb][:], g.psk[pb])
                proj_fm(g, wb, 'cv_w', 512 + j * 128, 128, ht, hk, g.ps[pc][:], g.psk[pc])
                k.op('act', lambda e, j=j, ts=ts, pa=pa: e.copy(out=bg[:, j, ts], in_=g.ps[pa][:]), reads=[g.psk[pa]],
                     writes=['cv_bg%d_%d' % (j, tt)])
                cb = n % 2
                k.op('act', lambda e, cb=cb, pb=pb: e.copy(out=cgt[cb][:], in_=g.ps[pb][:]), reads=[g.psk[pb]], writes=['cv_cg%d' % cb])
                k.op('dve', lambda e, j=j, tt=tt, cb=cb, pc=pc: e.tensor_tensor(out=u[:, j, 2 + tt * 512:2 + (tt + 1) * 512],
                                                                                 in0=g.ps[pc][:], in1=cgt[cb][:], op=ALU.mult),
                     reads=[g.psk[pc], 'cv_cg%d' % cb], writes=['cv_u%d_%d' % (j, tt)])
        y = [g.sb('cv_y%d' % i, [128, 512], F32, es) for i in range(2)]
        sq = [g.sb('cv_sq%d' % i, [128, 512], BF16, es) for i in range(2)]
        tf = [g.sb('cv_tf%d' % i, [128, 512], F32, es) for i in range(2)]
        yo = [g.sb('cv_yo%d' % i, [128, 2, 512], BF16, es) for i in range(2)]
        n = 0
        for tt in range(8):
            ts = slice(tt * 512, (tt + 1) * 512)
            ob = tt % 2
            for j in range(2):
                b = n % 2
                n += 1
                yk = 'cv_y%d' % b
                ukeys = ['cv_u%d_%d' % (j, tt), 'cv_u_h'] + (['cv_u%d_%d' % (j, tt - 1)] if tt > 0 else [])
                k.op('act', lambda e, b=b, j=j, tt=tt: e.activation(out=y[b][:], in_=u[:, j, 2 + tt * 512:2 + (tt + 1) * 512],
                                                                   func=AF.Copy, scale=cw[:, j, 2:3]),
                     reads=ukeys + ['cv_cw'], writes=[yk])
                for kk in (1, 0):
                    k.op('dve', lambda e, b=b, j=j, tt=tt, kk=kk: e.scalar_tensor_tensor(
                        out=y[b][:], in0=u[:, j, kk + tt * 512:kk + (tt + 1) * 512], scalar=cw[:, j, kk:kk + 1], in1=y[b][:],
                        op0=ALU.mult, op1=ALU.add), reads=ukeys + ['cv_cw', yk], writes=[yk])
                k.op('dve', lambda e, b=b, j=j, ts=ts: e.tensor_tensor(out=y[b][:], in0=y[b][:], in1=bg[:, j, ts], op=ALU.mult),
                     reads=[yk, 'cv_bg%d_%d' % (j, tt)], writes=[yk])
                head_rmsnorm_fm(g, y[b][:], yk, sq[b][:], 'cv_sq%d' % b, g.onorm[:, j:j + 1], ['onorm'], yo[ob][:, j, :],
                                'cv_yo%d_%d' % (ob, j), 6 + b, tf[b][:], 'cv_tf%d' % b)
            k.dma('sp', ycv[:, 2:4, ts], yo[ob][:], reads=['cv_yo%d_0' % ob, 'cv_yo%d_1' % ob], writes=['ycat_B%d' % tt])
        k.barrier()


def phase_wout(g, l, xT_old, xT_new):
    k = g.k
    ycv = g.ycatT.rearrange('(c p) t -> p c t', p=128)
    xo = xT_old.rearrange('(c p) t -> p c t', p=128)
    xn = xT_new.rearrange('(c p) t -> p c t', p=128)
    with contextlib.ExitStack() as es:
        wo = g.sb('wo_w', [128, 8, D], BF16, es)
        load_w_bf16(g, wo[:], 'wo_w', g.W['w_out'][l])
        yc = [g.sb('wo_yc%d' % i, [128, 8, 512], BF16, es) for i in range(2)]
        xt = [g.sb('wo_x%d' % i, [128, 8, 512], F32, es) for i in range(2)]
        n = 0
        for tt in range(8):
            b = tt % 2
            ts = slice(tt * 512, (tt + 1) * 512)
            k.dma('sp', yc[b][:], ycv[:, :, ts], writes=['wo_yc%d' % b])
            k.dma('sp', xt[b][:], xo[:, :, ts], writes=['wo_x%d' % b])
            for dc in range(8):
                pi = n % 4
                n += 1
                for c in range(8):
                    k.op('pe', lambda e, b=b, c=c, dc=dc, pi=pi: e.matmul(g.ps[pi][:], lhsT=wo[:, c, dc * 128:(dc + 1) * 128],
                                                                         rhs=yc[b][:, c, :], start=(c == 0), stop=(c == 7)),
                         reads=['wo_w', 'wo_yc%d' % b], writes=[g.psk[pi]])
                k.op('dve', lambda e, b=b, dc=dc, pi=pi: e.scalar_tensor_tensor(
                    out=xt[b][:, dc, :], in0=g.ps[pi][:], scalar=g.mod[:, 16 + dc:17 + dc], in1=xt[b][:, dc, :],
                    op0=ALU.mult, op1=ALU.add), reads=[g.psk[pi], 'mod', 'wo_x%d' % b], writes=['wo_x%d' % b])
            k.dma('sp', xn[:, :, ts], xt[b][:], reads=['wo_x%d' % b], writes=['xTn_%d' % tt])
        t = tap(g, 'x1T%d' % l, [128, 8, S])
        if t is not None:
            k.barrier()
            k.dma('sp', t, xn, writes=['tap'])
        k.barrier()


def phase_norm2_router(g, l, xT1):
    k = g.k
    xv = xT1.rearrange('(c p) t -> p c t', p=128)
    h2v = g.h2Td.rearrange('(c p) t -> p c t', p=128)
    with contextlib.ExitStack() as es:
        gcol = g.sb('n2_g', [128, 8], F32, es)
        g2s = g.sb('n2_gs', [128, 8], F32, es)
        k.dma('sp', gcol[:], g.W['norm2_g'][l].rearrange('(k p) -> p k', p=128), writes=['n2_g'], allow_slow_non_contiguous=True)
        k.op('dve', lambda e: e.scalar_tensor_tensor(out=g2s[:], in0=g.mod[:, 32:40], scalar=1.0, in1=gcol[:],
                                                      op0=ALU.add, op1=ALU.mult), reads=['mod', 'n2_g'], writes=['n2_gs'])
        h2 = g.sb('n2_h', [128, 8, S], BF16, es)
        norm_tiles(g, es, xv, g2s, g.mod[:, 24:32], ['n2_gs', 'mod'], h2, 'n2_h', 'n2')
        for c in range(8):
            k.dma('sp', h2v[:, c, :], h2[:, c, :], reads=['n2_h%d' % c], writes=['h2Td%d' % c])
        t = tap(g, 'h2T%d' % l, [128, 8, S], BF16)
        if t is not None:
            k.dma('sp', t, h2[:], reads=['n2_h%d' % c for c in range(8)], writes=['tap'])
        wr = g.sb('rt_w', [128, 8, 32], BF16, es)
        load_w_bf16(g, wr[:], 'rt_w', g.W['w_router'][l])
        br = g.sb('rt_b', [1, 32], BF16, es)
        k.dma('pool', br[:], g.W['b_router'][l:l + 1, :], writes=['rt_b'])
        lg = [g.sb('rt_lg%d' % i, [128, 32], F32, es) for i in range(2)]
        m8 = [g.sb('rt_m8%d' % i, [128, 8], F32, es) for i in range(2)]
        nm = [g.sb('rt_nm%d' % i, [128, 1], F32, es) for i in range(2)]
        ex = [g.sb('rt_ex%d' % i, [128, 32], F32, es) for i in range(2)]
        mk = [g.sb('rt_mk%d' % i, [128, 32], F32, es) for i in range(2)]
        sm = [g.sb('rt_sm%d' % i, [128, 1], F32, es) for i in range(2)]
        Gt = [g.sb('rt_G%d' % i, [128, 32], F32, es) for i in range(2)]
        gtap = tap(g, 'G%d' % l, [S, 32])
        for t_ in range(NT):
            b = t_ % 2
            pi = t_ % 2
            tk = slice(t_ * 128, (t_ + 1) * 128)
            for c in range(8):
                k.op('pe', lambda e, c=c, tk=tk, pi=pi: e.matmul(g.ps[pi][:, 0:32], lhsT=h2[:, c, tk], rhs=wr[:, c, :],
                                                               start=(c == 0), stop=False),
                     reads=['n2_h%d' % c, 'rt_w'], writes=[g.psk[pi]])
            k.op('pe', lambda e, pi=pi: e.matmul(g.ps[pi][:, 0:32], lhsT=g.cs['ones_b'][0:1, :], rhs=br[:], start=False, stop=True),
                 reads=['c_ones_b', 'rt_b'], writes=[g.psk[pi]])
            s = str(b)
            k.op('act', lambda e, b=b, pi=pi: e.copy(out=lg[b][:], in_=g.ps[pi][:, 0:32]), reads=[g.psk[pi]], writes=['rt_lg' + s])
            k.op('dve', lambda e, b=b: e.max(out=m8[b][:], in_=lg[b][:]), reads=['rt_lg' + s], writes=['rt_m8' + s])
            k.op('dve', lambda e, b=b: e.tensor_scalar(out=nm[b][:], in0=m8[b][:, 0:1], scalar1=-1.0, scalar2=None, op0=ALU.mult),
                 reads=['rt_m8' + s], writes=['rt_nm' + s])
            k.op('act', lambda e, b=b: e.activation(out=ex[b][:], in_=lg[b][:], func=AF.Exp, bias=nm[b][:], scale=1.0),
                 reads=['rt_lg' + s, 'rt_nm' + s], writes=['rt_ex' + s])
            k.op('dve', lambda e, b=b: e.tensor_scalar(out=mk[b][:], in0=lg[b][:], scalar1=m8[b][:, 3:4], scalar2=None, op0=ALU.is_ge),
                 reads=['rt_lg' + s, 'rt_m8' + s], writes=['rt_mk' + s])
            k.op('dve', lambda e, b=b: e.tensor_tensor(out=ex[b][:], in0=ex[b][:], in1=mk[b][:], op=ALU.mult),
                 reads=['rt_ex' + s, 'rt_mk' + s], writes=['rt_ex' + s])
            k.op('dve', lambda e, b=b: e.reduce_sum(out=sm[b][:], in_=ex[b][:], axis=AX.X), reads=['rt_ex' + s], writes=['rt_sm' + s])
            k.op('dve', lambda e, b=b: e.reciprocal(out=sm[b][:], in_=sm[b][:]), reads=['rt_sm' + s], writes=['rt_sm' + s])
            k.op('dve', lambda e, b=b: e.tensor_scalar(out=Gt[b][:], in0=ex[b][:], scalar1=sm[b][:], scalar2=None, op0=ALU.mult),
                 reads=['rt_ex' + s, 'rt_sm' + s], writes=['rt_G' + s])
            if gtap is not None:
                k.dma('sp', gtap[tk, :], Gt[b][:], reads=['rt_G' + s], writes=['tap'])
            pj = 2 + t_ % 2
            k.op('pe', lambda e, b=b, pj=pj: e.transpose(out=g.ps[pj][0:32, 0:128], in_=Gt[b][:], identity=g.cs['ident_f'][:]),
                 reads=['rt_G' + s, 'c_ident_f'], writes=[g.psk[pj]])
            k.op('act', lambda e, tk=tk, pj=pj: e.copy(out=g.GT[:, tk], in_=g.ps[pj][0:32, 0:128]), reads=[g.psk[pj]],
                 writes=['GT%d' % (t_ // 4)])
        k.barrier()


def phase_moe(g, l, xT1, xT2, final):
    k = g.k
    x1v = xT1.rearrange('(c p) t -> p c t', p=128)
    x2v = xT2.rearrange('(c p) t -> p c t', p=128)
    h2v = g.h2Td.rearrange('(c p) t -> p c t', p=128)
    NE = 32 if g.do_moe else 0
    QT = 1024
    with contextlib.ExitStack() as es:
        bgc = g.sb('me_bgc', [128, 16, 32], F32, es)
        bdr = g.sb('me_bdr', [32, D], BF16, es)
        k.dma('pool', bdr[:], g.W['b_down'][l], writes=['me_bdr'])
        with contextlib.ExitStack() as es2:
            bgr = g.sb('me_bgr', [32, 2048], F32, es2)
            k.dma('sp', bgr[:], g.W['b_gu'][l], writes=['me_bgr'])
            for j in range(16):
                pi = j % 2
                k.op('pe', lambda e, j=j, pi=pi: e.transpose(out=g.ps[pi][:, 0:32], in_=bgr[:, j * 128:(j + 1) * 128],
                                                             identity=g.cs['ident_f'][0:32, 0:32]),
                     reads=['me_bgr', 'c_ident_f'], writes=[g.psk[pi]])
                k.op('act', lambda e, j=j, pi=pi: e.copy(out=bgc[:, j, :], in_=g.ps[pi][:, 0:32]), reads=[g.psk[pi]], writes=['me_bgc'])
            k.barrier()
        wgu = [g.sb('me_wgu%d' % i, [128, 8, 2048], BF16, es) for i in range(2)]
        wdn = [g.sb('me_wdn%d' % i, [128, 8, D], BF16, es) for i in range(2)]
        h2q = g.sb('me_h2', [128, 8, QT], BF16, es)
        yacc = g.sb('me_yacc', [128, 8, QT], F32, es)
        act = [g.sb('me_act%d' % i, [128, 8, 512], BF16, es) for i in range(2)]
        gbc = [g.sb('me_gbc%d' % i, [128, 512], BF16, es) for i in range(2)]
        NSET = 3
        gq = [g.sb('me_gq%d' % i, [128, 512], F32, es) for i in range(NSET)]
        sg = [g.sb('me_sg%d' % i, [128, 512], F32, es) for i in range(NSET)]
        uq = [g.sb('me_uq%d' % i, [128, 512], F32, es) for i in range(NSET)]
        k.op('dve', lambda e: e.tensor_scalar(out=bgc[:, 8:16, :], in0=bgc[:, 8:16, :], scalar1=1.0, scalar2=None, op0=ALU.add),
             reads=['me_bgc'], writes=['me_bgc'])
        nf = 0
        xrow = [g.sb('me_xr%d' % i, [128, D], F32, es) for i in range(1)] if final else None
        nw = 0
        nit = 0
        npsum = 0
        for q in range(S // QT):
            qs = slice(q * QT, (q + 1) * QT)
            k.dma('sp', h2q[:], h2v[:, :, qs], reads=['h2Td%d' % c for c in range(8)], writes=['me_h2'])
            for e_ in range(NE):
                wb = nw % 2
                nw += 1
                gk, dk = 'me_wgu%d' % wb, 'me_wdn%d' % wb
                load_w_bf16(g, wgu[wb][:], gk, g.W['w_gu'][l, e_])
                load_w_bf16(g, wdn[wb][:], dk, g.W['w_down'][l, e_])
                for tl in range(QT // 512):
                    ts = slice(tl * 512, (tl + 1) * 512)
                    gts = slice(q * QT + tl * 512, q * QT + (tl + 1) * 512)
                    ab = nit % 2
                    nit += 1
                    ak = 'me_act%d' % ab
                    pg = 7
                    k.op('pe', lambda e, e_=e_, gts=gts, pg=pg: e.matmul(g.ps[pg][:], lhsT=g.cs['sel_b'][:, e_, :], rhs=g.GT[:, gts],
                                                                        start=True, stop=True),
                         reads=['c_sel_b'] + ['GT%d' % i for i in range(8)], writes=[g.psk[pg]])
                    k.op('act', lambda e, ab=ab, pg=pg: e.copy(out=gbc[ab][:], in_=g.ps[pg][:]), reads=[g.psk[pg]], writes=['me_gbc%d' % ab])
                    for f in range(8):
                        fb = nf % NSET
                        nf += 1
                        pa, pb = 2 * fb, 2 * fb + 1
                        for c in range(8):
                            k.op('pe', lambda e, c=c, f=f, wb=wb, ts=ts, pa=pa: e.matmul(
                                g.ps[pa][:], lhsT=wgu[wb][:, c, f * 128:(f + 1) * 128], rhs=h2q[:, c, ts], start=(c == 0), stop=(c == 7)),
                                reads=[gk, 'me_h2'], writes=[g.psk[pa]])
                        for c in range(8):
                            k.op('pe', lambda e, c=c, f=f, wb=wb, ts=ts, pb=pb: e.matmul(
                                g.ps[pb][:], lhsT=wgu[wb][:, c, 1024 + f * 128:1024 + (f + 1) * 128], rhs=h2q[:, c, ts],
                                start=(c == 0), stop=(c == 7)), reads=[gk, 'me_h2'], writes=[g.psk[pb]])
                        fs = str(fb)
                        k.op('dve', lambda e, f=f, fb=fb, pa=pa, e_=e_: e.tensor_scalar(
                            out=gq[fb][:], in0=g.ps[pa][:], scalar1=bgc[:, f, e_:e_ + 1], scalar2=7.0, op0=ALU.add, op1=ALU.min),
                            reads=[g.psk[pa], 'me_bgc'], writes=['me_gq' + fs])
                        k.op('act', lambda e, fb=fb: e.activation(out=sg[fb][:], in_=gq[fb][:], func=AF.Sigmoid, scale=1.702),
                             reads=['me_gq' + fs], writes=['me_sg' + fs])
                        k.op('dve', lambda e, f=f, fb=fb, pb=pb, e_=e_: e.tensor_scalar(
                            out=uq[fb][:], in0=g.ps[pb][:], scalar1=bgc[:, 8 + f, e_:e_ + 1], scalar2=-6.0, op0=ALU.add, op1=ALU.max),
                            reads=[g.psk[pb], 'me_bgc'], writes=['me_uq' + fs])
                        k.op('pool', lambda e, fb=fb: e.tensor_tensor(out=sg[fb][:], in0=sg[fb][:], in1=gq[fb][:], op=ALU.mult),
                             reads=['me_sg' + fs, 'me_gq' + fs], writes=['me_sg' + fs])
                        k.op('dve', lambda e, fb=fb: e.scalar_tensor_tensor(out=uq[fb][:], in0=uq[fb][:], scalar=8.0, in1=sg[fb][:],
                                                                            op0=ALU.min, op1=ALU.mult),
                             reads=['me_sg' + fs, 'me_uq' + fs], writes=['me_uq' + fs])
                        k.op('pool', lambda e, f=f, fb=fb, ab=ab: e.tensor_tensor(out=act[ab][:, f, :], in0=uq[fb][:], in1=gbc[ab][:], op=ALU.mult),
                             reads=['me_uq' + fs, 'me_gbc%d' % ab], writes=[ak])
                    for dc in range(8):
                        pd = 6 + dc % 2
                        for f in range(8):
                            k.op('pe', lambda e, f=f, dc=dc, wb=wb, ab=ab, pd=pd: e.matmul(
                                g.ps[pd][:], lhsT=wdn[wb][:, f, dc * 128:(dc + 1) * 128], rhs=act[ab][:, f, :],
                                start=(f == 0), stop=(f == 7 and e_ != 0)), reads=[dk, ak], writes=[g.psk[pd]])
                        if e_ == 0:
                            k.op('pe', lambda e, dc=dc, gts=gts, pd=pd: e.matmul(
                                g.ps[pd][:], lhsT=bdr[:, dc * 128:(dc + 1) * 128], rhs=g.GT[:, gts], start=False, stop=True),
                                reads=['me_bdr'] + ['GT%d' % i for i in range(8)], writes=[g.psk[pd]])
                            k.op('dve', lambda e, dc=dc, ts=ts, pd=pd: e.tensor_copy(yacc[:, dc, ts], g.ps[pd][:]),
                                 reads=[g.psk[pd]], writes=['me_yacc'])
                        else:
                            k.op('dve', lambda e, dc=dc, ts=ts, pd=pd: e.tensor_tensor(out=yacc[:, dc, ts], in0=yacc[:, dc, ts],
                                                                                    in1=g.ps[pd][:], op=ALU.add),
                                 reads=[g.psk[pd], 'me_yacc'], writes=['me_yacc'])
            if NE == 0:
                k.op('pool', lambda e: e.memset(yacc[:], 0.0), writes=['me_yacc'])
            for c in range(8):
                for tl in range(QT // 512):
                    ts = slice(tl * 512, (tl + 1) * 512)
                    gts = slice(q * QT + tl * 512, q * QT + (tl + 1) * 512)
                    fb = (c * 2 + tl) % 2
                    k.dma('sp', gq[fb][:], x1v[:, c, gts], writes=['me_gq%d' % fb])
                    k.op('dve', lambda e, c=c, ts=ts, fb=fb: e.scalar_tensor_tensor(
                        out=yacc[:, c, ts], in0=yacc[:, c, ts], scalar=g.mod[:, 40 + c:41 + c], in1=gq[fb][:], op0=ALU.mult, op1=ALU.add),
                        reads=['me_yacc', 'mod', 'me_gq%d' % fb], writes=['me_yacc'])
            if not final:
                k.dma('sp', x2v[:, :, qs], yacc[:], reads=['me_yacc'], writes=['xT2_%d' % q])
            else:
                for t_ in range(QT // 128):
                    tg = q * (QT // 128) + t_
                    ob = 0
                    for h in range(2):
                        pi = (2 * t_ + h) % 4
                        for j in range(4):
                            cc = h * 4 + j
                            k.op('pe', lambda e, cc=cc, j=j, pi=pi, t_=t_: e.transpose(
                                out=g.ps[pi][:, j * 128:(j + 1) * 128], in_=yacc[:, cc, t_ * 128:(t_ + 1) * 128],
                                identity=g.cs['ident_f'][:]), reads=['me_yacc', 'c_ident_f'], writes=[g.psk[pi]])
                        k.op('act' if h == 0 else 'dve',
                             (lambda e, pi=pi, ob=ob, h=h: e.copy(out=xrow[ob][:, h * 512:(h + 1) * 512], in_=g.ps[pi][:])) if h == 0 else
                             (lambda e, pi=pi, ob=ob, h=h: e.tensor_copy(xrow[ob][:, h * 512:(h + 1) * 512], g.ps[pi][:])),
                             reads=[g.psk[pi]], writes=['me_xr%d_%d' % (ob, h)])
                    k.dma('sp', g.out[tg * 128:(tg + 1) * 128, :], xrow[ob][:], reads=['me_xr%d_0' % ob, 'me_xr%d_1' % ob], writes=['out'])
            tp = tap(g, 'x2T%d_q%d' % (l, q), [128, 8, QT])
            if tp is not None:
                k.dma('sp', tp, yacc[:], reads=['me_yacc'], writes=['tap'])
        k.barrier()


def zero_missing(g):
    k = g.k
    miss = [i for i, m in enumerate('ABCD') if m not in g.mixers]
    if not miss:
        return
    with contextlib.ExitStack() as es:
        z = g.sb('zz', [128, S], BF16, es)
        k.op('pool', lambda e: e.memset(z[:], 0.0), writes=['zz'])
        for i in miss:
            for j in range(2):
                r0 = i * 256 + j * 128
                k.dma('sp', g.ycatT[r0:r0 + 128, :], z[:], reads=['zz'], writes=['ycat_z'])
        k.barrier()


def load_const(g, es, name):
    ap = g.C[name]
    t = g.sb('c_' + name, ap.shape, ap.dtype, es)
    g.k.dma('sp', t[:], ap, writes=['c_' + name])
    return t


def rope_tables(g, es):
    k = g.k
    cos = g.sb('rp_cos', [128, S], F32, es)
    sin = g.sb('rp_sin', [128, S], F32, es)
    invf = g.cs['invf']
    CH = 1024
    with contextlib.ExitStack() as es2:
        posi = g.sb('rp_pi', [128, CH], I32, es2)
        ang = g.sb('rp_ang', [128, CH], F32, es2)
        tq = g.sb('rp_t', [128, CH], F32, es2)
        ti = g.sb('rp_ti', [128, CH], I32, es2)
        r = g.sb('rp_r', [128, CH], F32, es2)
        m = g.sb('rp_m', [128, CH], F32, es2)
        for ch in range(S // CH):
            cs_ = slice(ch * CH, (ch + 1) * CH)
            k.dma('sp', posi[:], g.pos[cs_].partition_broadcast(128), writes=['rp_pi'])
            k.op('dve', lambda e: e.tensor_copy(ang[:], posi[:]), reads=['rp_pi'], writes=['rp_ang'])
            k.op('dve', lambda e: e.tensor_scalar(out=ang[:], in0=ang[:], scalar1=invf[:, 0:1], scalar2=None, op0=ALU.mult),
                 reads=['rp_ang', 'c_invf'], writes=['rp_ang'])
            for which, dst, dkey in ((0, sin, 'rp_sin'), (1, cos, 'rp_cos')):
                shift = 0.0 if which == 0 else PI / 2
                k.op('dve', lambda e, shift=shift: e.tensor_scalar(out=tq[:], in0=ang[:], scalar1=shift, scalar2=1.0 / (2 * PI),
                                                                    op0=ALU.add, op1=ALU.mult), reads=['rp_ang'], writes=['rp_t'])
                k.op('dve', lambda e: e.tensor_copy(ti[:], tq[:]), reads=['rp_t'], writes=['rp_ti'])
                k.op('dve', lambda e: e.tensor_copy(tq[:], ti[:]), reads=['rp_ti'], writes=['rp_t'])
                k.op('dve', lambda e: e.scalar_tensor_tensor(out=r[:], in0=tq[:], scalar=-2 * PI, in1=ang[:], op0=ALU.mult, op1=ALU.add),
                     reads=['rp_t', 'rp_ang'], writes=['rp_r'])
                if shift != 0.0:
                    k.op('dve', lambda e, shift=shift: e.tensor_scalar(out=r[:], in0=r[:], scalar1=shift, scalar2=None, op0=ALU.add),
                         reads=['rp_r'], writes=['rp_r'])
                k.op('dve', lambda e: e.tensor_scalar(out=m[:], in0=r[:], scalar1=PI, scalar2=-2 * PI, op0=ALU.is_gt, op1=ALU.mult),
                     reads=['rp_r'], writes=['rp_m'])
                k.op('dve', lambda e: e.tensor_tensor(out=r[:], in0=r[:], in1=m[:], op=ALU.add), reads=['rp_r', 'rp_m'], writes=['rp_r'])
                k.op('dve', lambda e: e.tensor_scalar(out=m[:], in0=r[:], scalar1=-PI, scalar2=2 * PI, op0=ALU.is_lt, op1=ALU.mult),
                     reads=['rp_r'], writes=['rp_m'])
                k.op('dve', lambda e: e.tensor_tensor(out=r[:], in0=r[:], in1=m[:], op=ALU.add), reads=['rp_r', 'rp_m'], writes=['rp_r'])
                k.op('act', lambda e, dst=dst, cs_=cs_: e.activation(out=dst[:, cs_], in_=r[:], func=AF.Sin), reads=['rp_r'],
                     writes=[dkey])
        k.barrier()
    return cos, sin


class NormRope:
    def __init__(self, g, es, pfx):
        self.g = g
        self.pfx = pfx
        self.n = 0
        mk = lambda nm, dt: [g.sb('%s_%s%d' % (pfx, nm, i), [128, 512], dt, es) for i in range(2)]
        self.xf = mk('xf', F32)
        self.sq = mk('sq', BF16)
        self.rs = mk('rs', F32)
        self.xn = mk('xn', F32)
        self.xb = mk('xb', BF16)
        self.t1 = mk('t1', F32)

    def run(self, ps, pskey, gcol, gkeys, ts, cos=None, sin=None, out_r=None, okr=None, out_n=None, okn=None, pbank=2):
        g, k, pfx = self.g, self.g.k, self.pfx
        b = self.n % 2
        self.n += 1
        K_ = lambda nm: '%s_%s%d' % (pfx, nm, b)
        xf, sq, rs, xn, xb, t1 = self.xf[b], self.sq[b], self.rs[b], self.xn[b], self.xb[b], self.t1[b]
        k.op('act', lambda e: e.copy(out=xf[:], in_=ps), reads=[pskey], writes=[K_('xf')])
        k.op('act', lambda e: e.activation(out=sq[:], in_=xf[:], func=AF.Square), reads=[K_('xf')], writes=[K_('sq')])
        pi = pbank + b
        k.op('pe', lambda e: e.matmul(g.ps[pi][:], lhsT=g.cs['blk64_b'][:], rhs=sq[:], start=True, stop=True),
             reads=[K_('sq'), 'c_blk64_b'], writes=[g.psk[pi]])
        k.op('dve', lambda e: e.tensor_scalar(out=rs[:], in0=g.ps[pi][:], scalar1=1.0 / 64, scalar2=NORM_EPS, op0=ALU.mult, op1=ALU.add),
             reads=[g.psk[pi]], writes=[K_('rs')])
        k.op('act', lambda e: e.activation(out=rs[:], in_=rs[:], func=AF.Sqrt), reads=[K_('rs')], writes=[K_('rs')])
        k.op('dve', lambda e: e.reciprocal(out=rs[:], in_=rs[:]), reads=[K_('rs')], writes=[K_('rs')])
        k.op('dve', lambda e: e.scalar_tensor_tensor(out=xn[:], in0=xf[:], scalar=gcol, in1=rs[:], op0=ALU.mult, op1=ALU.mult),
             reads=[K_('xf'), K_('rs')] + gkeys, writes=[K_('xn')])
        if out_n is not None:
            k.op('pool', lambda e: e.tensor_copy(out_n, xn[:]), reads=[K_('xn')], writes=[okn])
        if out_r is not None:
            k.op('act', lambda e: e.copy(out=xb[:], in_=xn[:]), reads=[K_('xn')], writes=[K_('xb')])
            k.op('pe', lambda e: e.matmul(g.ps[pi][:], lhsT=g.cs['prot_b'][:], rhs=xb[:], start=True, stop=True),
                 reads=[K_('xb'), 'c_prot_b'], writes=[g.psk[pi]])
            k.op('dve', lambda e: e.tensor_tensor(out=t1[:], in0=g.ps[pi][:], in1=sin[:, ts], op=ALU.mult),
                 reads=[g.psk[pi], 'rp_sin'], writes=[K_('t1')])
            k.op('pool', lambda e: e.tensor_tensor(out=xn[:], in0=xn[:], in1=cos[:, ts], op=ALU.mult),
                 reads=[K_('xn'), 'rp_cos'], writes=[K_('xn')])
            k.op('dve', lambda e: e.tensor_tensor(out=out_r, in0=xn[:], in1=t1[:], op=ALU.add),
                 reads=[K_('xn'), K_('t1')], writes=[okr])


def attention(g, es, pfx, heads, ngroups, ktiles, qT, kT, vfn, ncols, masks, epilogue, scale=0.125):
    k = g.k
    pt = [g.sb('%s_pt%d' % (pfx, i), [128, 512], BF16, es) for i in range(3)]
    n = 0
    for h in heads:
        for gi in range(ngroups):
            kts = ktiles(gi)
            if not kts:
                continue
            for idx, kt in enumerate(kts):
                sbk = n % 2
                pb = n % 3
                n += 1
                qa, qk = qT(h, gi)
                ka, kk, nk = kT(h, kt)
                k.op('pe', lambda e, sbk=sbk, ka=ka, qa=qa, nk=nk: e.matmul(g.ps[sbk][0:nk, :], lhsT=ka, rhs=qa, start=True, stop=True),
                     reads=qk + kk, writes=[g.psk[sbk]])
                ptk = '%s_pt%d' % (pfx, pb)
                k.op('act', lambda e, sbk=sbk, pb=pb, nk=nk: e.activation(out=pt[pb][0:nk, :], in_=g.ps[sbk][0:nk, :], func=AF.Exp, scale=scale),
                     reads=[g.psk[sbk]], writes=[ptk])
                for (ma, mk_, is_ps) in masks(h, gi, kt):
                    eng = 'dve' if (is_ps or n % 2 == 0) else 'pool'
                    k.op(eng, lambda e, pb=pb, ma=ma, nk=nk: e.tensor_tensor(out=pt[pb][0:nk, :], in0=pt[pb][0:nk, :], in1=ma[0:nk, :], op=ALU.mult),
                         reads=[ptk] + mk_, writes=[ptk])
                va, vk = vfn(h, kt)
                for sub in range(4):
                    k.op('pe', lambda e, sub=sub, pb=pb, va=va, nk=nk, idx=idx: e.matmul(
                        g.ps[4 + sub][:, 0:ncols], lhsT=pt[pb][0:nk, sub * 128:(sub + 1) * 128], rhs=va,
                        start=(idx == 0), stop=(idx == len(kts) - 1)), reads=[ptk] + vk, writes=[g.psk[4 + sub]])
            for sub in range(4):
                epilogue(h, gi, sub, g.ps[4 + sub], g.psk[4 + sub])


def finish_tm(g, es, pfx, ytm, ykeyfn, gb, gbkey, row0):
    k = g.k
    ycv = g.ycatT.rearrange('(c p) t -> p c t', p=128)
    sq = [g.sb(pfx + '_fsq%d' % i, [128, 256], F32, es) for i in range(2)]
    ss = [g.sb(pfx + '_fss%d' % i, [128, 4], F32, es) for i in range(2)]
    yb = [g.sb(pfx + '_fyb%d' % i, [128, 256], BF16, es) for i in range(2)]
    yT = [g.sb(pfx + '_fyT%d' % i, [128, 2, 128], BF16, es) for i in range(2)]
    psb = [g.ps[2][:].bitcast(BF16), g.ps[3][:].bitcast(BF16)]
    for t_ in range(NT):
        b = t_ % 2
        s_ = str(b)
        yk = ykeyfn(t_)
        k.op('pool', lambda e, b=b, t_=t_: e.tensor_tensor(out=sq[b][:], in0=ytm[:, t_, :], in1=ytm[:, t_, :], op=ALU.mult),
             reads=yk, writes=[pfx + '_fsq' + s_])
        k.op('dve', lambda e, b=b: e.reduce_sum(out=ss[b][:], in_=sq[b][:].rearrange('p (h d) -> p h d', d=64), axis=AX.X),
             reads=[pfx + '_fsq' + s_], writes=[pfx + '_fss' + s_])
        k.op('dve', lambda e, b=b: e.tensor_scalar(out=ss[b][:], in0=ss[b][:], scalar1=1.0 / 64, scalar2=NORM_EPS, op0=ALU.mult, op1=ALU.add),
             reads=[pfx + '_fss' + s_], writes=[pfx + '_fss' + s_])
        k.op('act', lambda e, b=b: e.activation(out=ss[b][:], in_=ss[b][:], func=AF.Sqrt), reads=[pfx + '_fss' + s_], writes=[pfx + '_fss' + s_])
        k.op('dve', lambda e, b=b: e.reciprocal(out=ss[b][:], in_=ss[b][:]), reads=[pfx + '_fss' + s_], writes=[pfx + '_fss' + s_])
        for h in range(4):
            k.op('dve', lambda e, b=b, h=h, t_=t_: e.scalar_tensor_tensor(
                out=yb[b][:, h * 64:(h + 1) * 64], in0=ytm[:, t_, h * 64:(h + 1) * 64], scalar=ss[b][:, h:h + 1],
                in1=gb[:, h * 64:(h + 1) * 64], op0=ALU.mult, op1=ALU.mult),
                reads=yk + [pfx + '_fss' + s_, gbkey], writes=[pfx + '_fyb' + s_])
        for j in range(2):
            k.op('pe', lambda e, b=b, j=j: e.transpose(out=psb[b][:, j * 128:(j + 1) * 128], in_=yb[b][:, j * 128:(j + 1) * 128],
                                                       identity=g.cs['ident_b'][:]),
                 reads=[pfx + '_fyb' + s_, 'c_ident_b'], writes=[g.psk[2 + b]])
        k.op('act', lambda e, b=b: e.copy(out=yT[b][:], in_=psb[b][:, 0:256].rearrange('p (j t) -> p j t', j=2)),
             reads=[g.psk[2 + b]], writes=[pfx + '_fyT' + s_])
        c0 = row0 // 128
        k.dma('sp', ycv[:, c0:c0 + 2, t_ * 128:(t_ + 1) * 128], yT[b][:], reads=[pfx + '_fyT' + s_], writes=['ycat_%s%d' % (pfx, t_)])


def mixer_dil(g, l):
    k = g.k
    with contextlib.ExitStack() as es:
        qT = g.sb('dl_qT', [128, 2, S], BF16, es)
        kT = g.sb('dl_kT', [128, 2, S], BF16, es)
        V = g.sb('dl_V', [128, NT, 4, 65], BF16, es)
        ytm = g.sb('dl_ytm', [128, NT, 256], F32, es)
        gb = g.sb('dl_gb', [128, 256], F32, es)
        k.dma('sp', gb[:], g.W['onorm_g'][l, 256:512].partition_broadcast(128), writes=['dl_gb'])
        k.op('pool', lambda e: e.memset(V[:, :, :, 64:65], 1.0), writes=['dl_Vone'])
        with contextlib.ExitStack() as es2:
            cos, sin = rope_tables(g, es2)
            w = g.sb('dl_w', [128, 8, 768], BF16, es2)
            load_w_bf16(g, w[:], 'dl_w', g.W['w_in'][l][:, 1792:2560])
            gq = g.sb('dl_gq', [128, 1], F32, es2)
            gk = g.sb('dl_gk', [128, 1], F32, es2)
            for hh in range(2):
                k.dma('sp', gq[hh * 64:(hh + 1) * 64, :], g.W['dil_q_g'][l].rearrange('(p o) -> p o', o=1), writes=['dl_gq'],
                      allow_slow_non_contiguous=True)
                k.dma('sp', gk[hh * 64:(hh + 1) * 64, :], g.W['dil_k_g'][l].rearrange('(p o) -> p o', o=1), writes=['dl_gk'],
                      allow_slow_non_contiguous=True)
            nr = NormRope(g, es2, 'dl')
            hload = ht_loader(g, es2, 'dl')
            n = 0
            for tt in range(8):
                ts = slice(tt * 512, (tt + 1) * 512)
                ht, hk = hload(tt)
                for j in range(2):
                    for (dst, dkey, c0, gcol, gkey) in ((qT, 'dl_qT', 0, gq, 'dl_gq'), (kT, 'dl_kT', 256, gk, 'dl_gk')):
                        pi = n % 2
                        n += 1
                        proj_fm(g, w, 'dl_w', c0 + j * 128, 128, ht, hk, g.ps[pi][:], g.psk[pi])
                        nr.run(g.ps[pi][:], g.psk[pi], gcol[:, 0:1], [gkey], ts, cos, sin, dst[:, j, ts], '%s%d_%d' % (dkey, j, tt))
                for sub in range(4):
                    pi = 4 + sub
                    t_ = tt * 4 + sub
                    proj_tm(g, w, 'dl_w', 512, 256, ht, hk, sub, g.ps[pi][:, 0:256], g.psk[pi])
                    k.op('act', lambda e, pi=pi, t_=t_: e.copy(out=V[:, t_, :, 0:64], in_=g.ps[pi][:, 0:256].rearrange('p (h d) -> p h d', d=64)),
                         reads=[g.psk[pi]], writes=['dl_V%d' % t_])
            k.barrier()
        msk = load_const(g, es, 'dil_mask')
        rd = [g.sb('dl_rd%d' % i, [128, 1], F32, es) for i in range(2)]
        cnt = [0]

        def q_of(h, gi):
            j, hp = h // 2, h % 2
            return qT[hp * 64:(hp + 1) * 64, j, gi * 512:(gi + 1) * 512], ['dl_qT%d_%d' % (j, gi)]

        def k_of(h, kt):
            j, hp = h // 2, h % 2
            return kT[hp * 64:(hp + 1) * 64, j, kt * 128:(kt + 1) * 128], ['dl_kT%d_%d' % (j, kt // 4)], 128

        def v_of(h, kt):
            return V[:, kt, h, :], ['dl_V%d' % kt, 'dl_Vone']

        def ktiles(gi):
            return [kt for kt in range(max(0, 4 * gi - 16), 4 * gi + 4)]

        def masks(h, gi, kt):
            return [(msk[:, (4 * gi - kt) + 3, :], ['c_dil_mask'], False)]

        def epi(h, gi, sub, acc, acck):
            b = cnt[0] % 2
            cnt[0] += 1
            t_ = gi * 4 + sub
            k.op('dve', lambda e: e.reciprocal(out=rd[b][:], in_=acc[:, 64:65]), reads=[acck], writes=['dl_rd%d' % b])
            k.op('dve', lambda e: e.tensor_scalar(out=ytm[:, t_, h * 64:(h + 1) * 64], in0=acc[:, 0:64], scalar1=rd[b][:, 0:1],
                                                   scalar2=None, op0=ALU.mult), reads=[acck, 'dl_rd%d' % b], writes=['dl_ytm%d_%d' % (t_, h)])

        attention(g, es, 'dl', range(4), 8, ktiles, q_of, k_of, v_of, 65, masks, epi)
        finish_tm(g, es, 'dl', ytm, lambda t_: ['dl_ytm%d_%d' % (t_, h) for h in range(4)], gb, 'dl_gb', 512)
        k.barrier()


NSA_STOP = None
NSA_SKIP = ''


def mixer_nsa(g, l):
    k = g.k
    W = g.W
    with contextlib.ExitStack() as es:
        qn = g.sb('ns_qn', [128, 2, S], BF16, es)
        qr = g.sb('ns_qr', [128, 2, S], BF16, es)
        ksT = g.sb('ns_ks', [128, S], BF16, es)
        kwT = g.sb('ns_kw', [128, S], BF16, es)
        kcvc = g.sb('ns_kcvc', [128, S], BF16, es)
        Vs = g.sb('ns_Vs', [128, NT, 66], BF16, es)
        Vw = g.sb('ns_Vw', [128, NT, 66], BF16, es)
        gate = g.sb('ns_gate', [128, NT, 12], F32, es)
        ytm = g.sb('ns_ytm', [128, NT, 256], F32, es)
        gb = g.sb('ns_gb', [128, 256], F32, es)
        k.dma('sp', gb[:], W['onorm_g'][l, 512:768].partition_broadcast(128), writes=['ns_gb'])
        k.op('pool', lambda e: e.memset(Vs[:, :, 64:65], 1.0), writes=['ns_Vs1'])
        k.op('pool', lambda e: e.memset(Vw[:, :, 64:65], 1.0), writes=['ns_Vw1'])
        with contextlib.ExitStack() as es2:
            cos, sin = rope_tables(g, es2)
            w = g.sb('ns_w', [128, 8, 652], BF16, es2)
            load_w_bf16(g, w[:], 'ns_w', W['w_in'][l][:, 2560:3212])
            wks = g.sb('ns_wks', [128, 8, 128], BF16, es2)
            wkw = g.sb('ns_wkw', [128, 8, 128], BF16, es2)
            for hh in range(2):
                load_w_bf16(g, wks[:, :, hh * 64:(hh + 1) * 64], 'ns_wks', W['w_in'][l][:, 2944:3008])
                load_w_bf16(g, wkw[:, :, hh * 64:(hh + 1) * 64], 'ns_wkw', W['w_in'][l][:, 3072:3136])
            gq = g.sb('ns_gq', [128, 1], F32, es2)
            gks = g.sb('ns_gks', [128, 1], F32, es2)
            gkw = g.sb('ns_gkw', [128, 1], F32, es2)
            for hh in range(2):
                for (dst, nm, key) in ((gq, 'nsa_q_g', 'ns_gq'), (gks, 'nsa_ks_g', 'ns_gks'), (gkw, 'nsa_kw_g', 'ns_gkw')):
                    k.dma('sp', dst[hh * 64:(hh + 1) * 64, :], W[nm][l].rearrange('(p o) -> p o', o=1), writes=[key],
                          allow_slow_non_contiguous=True)
            nr = NormRope(g, es2, 'ns')
            hload = ht_loader(g, es2, 'ns')
            n = 0
            for tt in range(8):
                ts = slice(tt * 512, (tt + 1) * 512)
                ht, hk = hload(tt)
                for j in range(2):
                    pi = n % 2
                    n += 1
                    proj_fm(g, w, 'ns_w', j * 128, 128, ht, hk, g.ps[pi][:], g.psk[pi])
                    nr.run(g.ps[pi][:], g.psk[pi], gq[:, 0:1], ['ns_gq'], ts, cos, sin, qr[:, j, ts], 'ns_qr%d_%d' % (j, tt),
                           None if 'a' in NSA_SKIP else qn[:, j, ts], 'ns_qn%d_%d' % (j, tt))
                for (wsb, wkey, gcol, gkey, dst, dkey) in ((wks, 'ns_wks', gks, 'ns_gks', ksT, 'ns_ks'), (wkw, 'ns_wkw', gkw, 'ns_gkw', kwT, 'ns_kw')):
                    if 'c' in NSA_SKIP:
                        continue
                    pi = n % 2
                    n += 1
                    proj_fm(g, wsb, wkey, 0, 128, ht, hk, g.ps[pi][:], g.psk[pi])
                    nr.run(g.ps[pi][:], g.psk[pi], gcol[:, 0:1], [gkey], ts, cos, sin, dst[:, ts], '%s_%d' % (dkey, tt))
                pi = n % 2
                n += 1
                proj_fm(g, w, 'ns_w', 256, 128, ht, hk, g.ps[pi][:], g.psk[pi])
                k.op('act', lambda e, pi=pi, ts=ts: e.copy(out=kcvc[:, ts], in_=g.ps[pi][:]), reads=[g.psk[pi]], writes=['ns_kcvc%d' % tt])
                for sub in range(4):
                    if 'b' in NSA_SKIP:
                        continue
                    pi = 4 + sub
                    t_ = tt * 4 + sub
                    proj_tm(g, w, 'ns_w', 448, 204, ht, hk, sub, g.ps[pi][:, 0:204], g.psk[pi])
                    k.op('act', lambda e, pi=pi, t_=t_: e.copy(out=Vs[:, t_, 0:64], in_=g.ps[pi][:, 0:64]), reads=[g.psk[pi]],
                         writes=['ns_Vs%d' % t_])
                    if 'e' not in NSA_SKIP:
                        k.op('dve', lambda e, pi=pi, t_=t_: e.tensor_copy(Vw[:, t_, 0:64], g.ps[pi][:, 128:192]), reads=[g.psk[pi]],
                             writes=['ns_Vw%d' % t_])
                    if 'd' not in NSA_SKIP:
                        k.op('act', lambda e, pi=pi, t_=t_: e.activation(out=gate[:, t_, :], in_=g.ps[pi][:, 192:204], func=AF.Sigmoid),
                             reads=[g.psk[pi]], writes=['ns_gate%d' % t_])
            k.barrier()
        if NSA_STOP == 'proj':
            return
        kcT = g.sb('ns_kcT', [128, 512], BF16, es)
        Vc = [g.sb('ns_Vc%d' % j, [128, 129], BF16, es) for j in range(2)]
        ovl1 = load_const(g, es, 'ovl1')
        with contextlib.ExitStack() as es3:
            W1 = g.sb('ns_W1', [128, 32, 256], BF16, es3)
            peT = g.sb('ns_peT', [128, 32], BF16, es3)
            W2k = g.sb('ns_W2k', [128, 2, 128], BF16, es3)
            W2v = g.sb('ns_W2v', [128, 2, 64], BF16, es3)
            gkc = g.sb('ns_gkc', [128, 1], F32, es3)
            for (pb, w1n, pen) in ((0, 'nsa_wk1', 'nsa_pe_k'), (64, 'nsa_wv1', 'nsa_pe_v')):
                k.dma('pool', W1[pb:pb + 64, :, :], W[w1n][l].rearrange('(l d) m -> d l m', d=64), writes=['ns_W1'])
                k.dma('pool', peT[pb:pb + 64, :], W[pen][l].rearrange('l d -> d l'), writes=['ns_peT'], allow_slow_non_contiguous=True)
            for hh in range(2):
                k.dma('pool', W2k[:, :, hh * 64:(hh + 1) * 64], W['nsa_wk2'][l].rearrange('(c p) d -> p c d', p=128), writes=['ns_W2k'])
                k.dma('sp', gkc[hh * 64:(hh + 1) * 64, :], W['nsa_kc_g'][l].rearrange('(p o) -> p o', o=1), writes=['ns_gkc'],
                      allow_slow_non_contiguous=True)
            k.dma('pool', W2v[:], W['nsa_wv2'][l].rearrange('(c p) d -> p c d', p=128), writes=['ns_W2v'])
            hid = {}
            bia = g.sb('ns_bia', [128, 4], F32, es3)
            xs = g.sb('ns_xs', [128, 256], F32, es3)
            x2 = g.sb('ns_x2', [128, 256], F32, es3)
            th = g.sb('ns_th', [128, 256], F32, es3)
            n = 0
            allkc = ['ns_kcvc%d' % i for i in range(8)]
            for kv, pb in (('k', 0), ('v', 64)):
                for mc in range(2):
                    hd_ = g.sb('ns_hid%s%d' % (kv, mc), [128, 256], BF16, es3)
                    hid[(kv, mc)] = hd_
                    hk_ = 'ns_hid%s%d' % (kv, mc)
                    k.op('pool', lambda e, hd_=hd_: e.memset(hd_[:], 0.0), writes=[hk_])
                    pi, pj = n % 2, 2 + n % 2
                    for l_ in range(32):
                        k.op('pe', lambda e, l_=l_, pb=pb, mc=mc, pi=pi: e.matmul(
                            g.ps[pi][:, 0:255], lhsT=W1[pb:pb + 64, l_, mc * 128:(mc + 1) * 128],
                            rhs=kcvc[pb:pb + 64, l_:l_ + 16 * 254 + 1:16], start=(l_ == 0), stop=(l_ == 31)),
                            reads=['ns_W1'] + allkc, writes=[g.psk[pi]])
                    for l_ in range(32):
                        k.op('pe', lambda e, l_=l_, pb=pb, mc=mc, pj=pj: e.matmul(
                            g.ps[pj][:, 0:1], lhsT=W1[pb:pb + 64, l_, mc * 128:(mc + 1) * 128], rhs=peT[pb:pb + 64, l_:l_ + 1],
                            start=(l_ == 0), stop=(l_ == 31)), reads=['ns_W1', 'ns_peT'], writes=[g.psk[pj]])
                    k.op('act', lambda e, n=n, pj=pj: e.copy(out=bia[:, n:n + 1], in_=g.ps[pj][:, 0:1]), reads=[g.psk[pj]], writes=['ns_bia'])
                    k.op('act', lambda e, n=n, pi=pi: e.activation(out=xs[:, 0:255], in_=g.ps[pi][:, 0:255], func=AF.Identity,
                                                                   bias=bia[:, n:n + 1], scale=1.0), reads=[g.psk[pi], 'ns_bia'], writes=['ns_xs'])
                    k.op('dve', lambda e: e.tensor_tensor(out=x2[:, 0:255], in0=xs[:, 0:255], in1=xs[:, 0:255], op=ALU.mult), reads=['ns_xs'], writes=['ns_x2'])
                    k.op('dve', lambda e: e.tensor_scalar(out=x2[:, 0:255], in0=x2[:, 0:255], scalar1=0.044715, scalar2=1.0, op0=ALU.mult, op1=ALU.add),
                         reads=['ns_x2'], writes=['ns_x2'])
                    k.op('dve', lambda e: e.tensor_tensor(out=x2[:, 0:255], in0=x2[:, 0:255], in1=xs[:, 0:255], op=ALU.mult), reads=['ns_x2', 'ns_xs'], writes=['ns_x2'])
                    k.op('act', lambda e: e.activation(out=th[:, 0:255], in_=x2[:, 0:255], func=AF.Tanh, scale=0.7978845608028654),
                         reads=['ns_x2'], writes=['ns_th'])
                    k.op('dve', lambda e: e.scalar_tensor_tensor(out=th[:, 0:255], in0=th[:, 0:255], scalar=1.0, in1=xs[:, 0:255], op0=ALU.add, op1=ALU.mult),
                         reads=['ns_th', 'ns_xs'], writes=['ns_th'])
                    k.op('act', lambda e, hd_=hd_: e.mul(out=hd_[:, 0:255], in_=th[:, 0:255], mul=0.5), reads=['ns_th'], writes=[hk_])
                    n += 1
            for mc in range(2):
                k.op('pe', lambda e, mc=mc: e.matmul(g.ps[0][:, 0:256], lhsT=W2k[:, mc, :], rhs=hid[('k', mc)][:], start=(mc == 0), stop=(mc == 1)),
                     reads=['ns_W2k', 'ns_hidk%d' % mc], writes=[g.psk[0]])
            nr2 = NormRope(g, es3, 'nc')
            nr2.run(g.ps[0][:], g.psk[0], gkc[:, 0:1], ['ns_gkc'], slice(0, 512), out_n=kcT[:], okn='ns_kcT')
            k.op('pool', lambda e: e.memset(kcT[:, 255:512], 0.0), reads=['ns_kcT'], writes=['ns_kcT'])
            for j in range(2):
                for mc in range(2):
                    k.op('pe', lambda e, j=j, mc=mc: e.matmul(g.ps[1][:, 0:64], lhsT=hid[('v', mc)][:, j * 128:(j + 1) * 128], rhs=W2v[:, mc, :],
                                                           start=(mc == 0), stop=(mc == 1)), reads=['ns_W2v', 'ns_hidv%d' % mc], writes=[g.psk[1]])
                k.op('act', lambda e, j=j: e.copy(out=Vc[j][:, 0:64], in_=g.ps[1][:, 0:64]), reads=[g.psk[1]], writes=['ns_Vc%d' % j])
                k.op('pool', lambda e, j=j: e.tensor_copy(Vc[j][:, 64:129], ovl1[:, j, :]), reads=['c_ovl1'], writes=['ns_Vc%d_o' % j])
            k.barrier()
        if NSA_STOP == 'cmpkv':
            return
        imp = g.sb('ns_imp', [128, NT, 64], F32, es)
        selT = g.sb('ns_selT', [64, S], BF16, es)
        rd = [g.sb('ns_rd%d' % i, [128, 1], F32, es) for i in range(2)]
        cf = [g.sb('ns_cf%d' % i, [128, 1], F32, es) for i in range(2)]
        cnt = [0]

        def q_of_n(h, gi):
            j, hp = h // 2, h % 2
            return qn[hp * 64:(hp + 1) * 64, j, gi * 512:(gi + 1) * 512], ['ns_qn%d_%d' % (j, gi)]

        def q_of_r(h, gi):
            j, hp = h // 2, h % 2
            return qr[hp * 64:(hp + 1) * 64, j, gi * 512:(gi + 1) * 512], ['ns_qr%d_%d' % (j, gi)]

        def make_epi(br, first):
            def epi(h, gi, sub, acc, acck):
                b = cnt[0] % 2
                cnt[0] += 1
                t_ = gi * 4 + sub
                rk, ck = 'ns_rd%d' % b, 'ns_cf%d' % b
                k.op('dve', lambda e: e.tensor_scalar(out=rd[b][:], in0=acc[:, 64:65], scalar1=1e-30, scalar2=None, op0=ALU.max),
                     reads=[acck], writes=[rk])
                k.op('dve', lambda e: e.reciprocal(out=rd[b][:], in_=rd[b][:]), reads=[rk], writes=[rk])
                k.op('dve', lambda e: e.tensor_tensor(out=cf[b][:], in0=rd[b][:], in1=gate[:, t_, h * 3 + br:h * 3 + br + 1], op=ALU.mult),
                     reads=[rk, 'ns_gate%d' % t_], writes=[ck])
                yk = 'ns_ytm%d_%d' % (t_, h)
                if first:
                    k.op('dve', lambda e: e.tensor_scalar(out=ytm[:, t_, h * 64:(h + 1) * 64], in0=acc[:, 0:64], scalar1=cf[b][:, 0:1],
                                                           scalar2=None, op0=ALU.mult), reads=[acck, ck], writes=[yk])
                    ik = 'ns_imp%d' % t_
                    if h == 0:
                        k.op('dve', lambda e: e.tensor_scalar(out=imp[:, t_, :], in0=acc[:, 65:129], scalar1=rd[b][:, 0:1], scalar2=None,
                                                               op0=ALU.mult), reads=[acck, rk], writes=[ik])
                    else:
                        k.op('dve', lambda e: e.scalar_tensor_tensor(out=imp[:, t_, :], in0=acc[:, 65:129], scalar=rd[b][:, 0:1], in1=imp[:, t_, :],
                                                                      op0=ALU.mult, op1=ALU.add), reads=[acck, rk, ik], writes=[ik])
                else:
                    k.op('dve', lambda e: e.scalar_tensor_tensor(out=ytm[:, t_, h * 64:(h + 1) * 64], in0=acc[:, 0:64], scalar=cf[b][:, 0:1],
                                                                  in1=ytm[:, t_, h * 64:(h + 1) * 64], op0=ALU.mult, op1=ALU.add),
                         reads=[acck, ck, yk], writes=[yk])
            return epi

        with contextlib.ExitStack() as es4:
            cmsk = load_const(g, es4, 'cmp_mask')

            def k_cmp(h, kt):
                hp = h % 2
                return kcT[hp * 64:(hp + 1) * 64, kt * 128:(kt + 1) * 128], ['ns_kcT'], 128

            def v_cmp(h, kt):
                return Vc[kt][:, :], ['ns_Vc%d' % kt, 'ns_Vc%d_o' % kt]

            def kt_cmp(gi):
                return [0] + ([1] if gi >= 4 else [])

            def m_cmp(h, gi, kt):
                i = gi - 4 * kt
                return [] if i >= 5 else [(cmsk[:, i, :], ['c_cmp_mask'], False)]

            attention(g, es4, 'nc', range(4), 8, kt_cmp, q_of_n, k_cmp, v_cmp, 129, m_cmp, make_epi(0, True))
            if NSA_STOP == 'cmpattn':
                k.barrier()
                return
            keep = load_const(g, es4, 'sel_keep')
            addc = load_const(g, es4, 'sel_add')
            sc = [g.sb('ns_sc%d' % i, [128, 64], F32, es4) for i in range(2)]
            wk2_ = [g.sb('ns_wk%d' % i, [128, 64], F32, es4) for i in range(2)]
            m8 = [g.sb('ns_m8%d' % i, [128, 8], F32, es4) for i in range(2)]
            sm = [g.sb('ns_sm%d' % i, [128, 64], BF16, es4) for i in range(2)]
            psb = [g.ps[2][:].bitcast(BF16), g.ps[3][:].bitcast(BF16)]
            for t_ in range(NT):
                b = t_ % 2
                s_ = str(b)
                k.op('dve', lambda e, b=b, t_=t_: e.tensor_tensor(out=sc[b][:], in0=imp[:, t_, :], in1=keep[:, t_, :], op=ALU.mult),
                     reads=['ns_imp%d' % t_, 'c_sel_keep'], writes=['ns_sc' + s_])
                k.op('dve', lambda e, b=b, t_=t_: e.tensor_tensor(out=sc[b][:], in0=sc[b][:], in1=addc[:, t_, :], op=ALU.add),
                     reads=['ns_sc' + s_, 'c_sel_add'], writes=['ns_sc' + s_])
                k.op('dve', lambda e, b=b: e.max(out=m8[b][:], in_=sc[b][:]), reads=['ns_sc' + s_], writes=['ns_m8' + s_])
                k.op('dve', lambda e, b=b: e.match_replace(out=wk2_[b][:], in_to_replace=m8[b][:], in_values=sc[b][:], imm_value=-1e30),
                     reads=['ns_sc' + s_, 'ns_m8' + s_], writes=['ns_wk' + s_])
                k.op('dve', lambda e, b=b: e.max(out=m8[b][:], in_=wk2_[b][:]), reads=['ns_wk' + s_], writes=['ns_m8' + s_])
                k.op('dve', lambda e, b=b: e.tensor_scalar(out=sm[b][:], in0=sc[b][:], scalar1=m8[b][:, 7:8], scalar2=None, op0=ALU.is_ge),
                     reads=['ns_sc' + s_, 'ns_m8' + s_], writes=['ns_sm' + s_])
                k.op('pe', lambda e, b=b: e.transpose(out=psb[b][0:64, 0:128], in_=sm[b][:], identity=g.cs['ident_b'][:]),
                     reads=['ns_sm' + s_, 'c_ident_b'], writes=[g.psk[2 + b]])
                k.op('act', lambda e, b=b, t_=t_: e.copy(out=selT[:, t_ * 128:(t_ + 1) * 128], in_=psb[b][0:64, 0:128]),
                     reads=[g.psk[2 + b]], writes=['ns_selT%d' % (t_ // 4)])
            tp = tap(g, 'selT%d' % l, [64, S], BF16)
            if tp is not None:
                k.dma('sp', tp, selT[:], reads=['ns_selT%d' % i for i in range(8)], writes=['tap'])
            k.barrier()
        if NSA_STOP == 'topk':
            return
        with contextlib.ExitStack() as es5:
            cau = load_const(g, es5, 'cau_mask')
            sexp = load_const(g, es5, 'sel_exp')
            mc_ = [0]

            def k_s(h, kt):
                hp = h % 2
                return ksT[hp * 64:(hp + 1) * 64, kt * 128:(kt + 1) * 128], ['ns_ks_%d' % (kt // 4)], 128

            def v_s(h, kt):
                return Vs[:, kt, 0:65], ['ns_Vs%d' % kt, 'ns_Vs1']

            def m_s(h, gi, kt):
                pm = 2 + mc_[0] % 2
                mc_[0] += 1
                k.op('pe', lambda e: e.matmul(g.ps[pm][:], lhsT=sexp[:, kt, :], rhs=selT[:, gi * 512:(gi + 1) * 512], start=True, stop=True),
                     reads=['c_sel_exp', 'ns_selT%d' % gi], writes=[g.psk[pm]])
                ms = [(g.ps[pm], [g.psk[pm]], True)]
                if kt >= 4 * gi:
                    ms.append((cau[:, (4 * gi - kt) + 3, :], ['c_cau_mask'], False))
                return ms

            attention(g, es5, 'nl', range(4), 8, lambda gi: list(range(0, 4 * gi + 4)), q_of_r, k_s, v_s, 65, m_s, make_epi(1, False))
            k.barrier()
        if NSA_STOP == 'sel':
            return
        with contextlib.ExitStack() as es6:
            swm = load_const(g, es6, 'swa_mask')

            def k_w(h, kt):
                hp = h % 2
                return kwT[hp * 64:(hp + 1) * 64, kt * 128:(kt + 1) * 128], ['ns_kw_%d' % (kt // 4)], 128

            def v_w(h, kt):
                return Vw[:, kt, 0:65], ['ns_Vw%d' % kt, 'ns_Vw1']

            attention(g, es6, 'nw', range(4), 8, lambda gi: list(range(max(0, 4 * gi - 4), 4 * gi + 4)), q_of_r, k_w, v_w, 65,
                      lambda h, gi, kt: [(swm[:, (4 * gi - kt) + 3, :], ['c_swa_mask'], False)], make_epi(2, False))
            finish_tm(g, es6, 'ns', ytm, lambda t_: ['ns_ytm%d_%d' % (t_, h) for h in range(4)], gb, 'ns_gb', 768)
            k.barrier()


RW_STEPS = S


def mixer_rwkv(g, l):
    k = g.k
    W = g.W
    ycv = g.ycatT.rearrange('(c p) t -> p c t', p=128)
    with contextlib.ExitStack() as es:
        rT = g.sb('rw_rT', [128, 2, S], BF16, es)
        kkT = g.sb('rw_kkT', [128, 2, S], BF16, es)
        wT = g.sb('rw_wT', [128, 2, S], F32, es)
        vtm = g.sb('rw_vtm', [128, NT, 256], BF16, es)
        gtm = g.sb('rw_gtm', [128, NT, 256], BF16, es)
        bon = g.sb('rw_bon', [128, NT, 4], F32, es)
        with contextlib.ExitStack() as es2:
            wa = g.sb('rw_wa', [128, 8, 1024], BF16, es2)
            wb = g.sb('rw_wb', [128, 8, 1024], BF16, es2)
            mub = g.sb('rw_mub', [128, 1024], F32, es2)
            omb = g.sb('rw_omb', [128, 1024], F32, es2)
            load_w_bf16(g, wa[:], 'rw_wa', W['w_in'][l][:, 0:1024])
            k.dma('sp', mub[:], W['rwkv_mu'][l].partition_broadcast(128), writes=['rw_mub'])
            k.op('dve', lambda e: e.tensor_scalar(out=omb[:], in0=mub[:], scalar1=-1.0, scalar2=1.0, op0=ALU.mult, op1=ALU.add),
                 reads=['rw_mub'], writes=['rw_omb'])
            for c in range(8):
                k.op('dve', lambda e, c=c: e.tensor_tensor(out=wb[:, c, :], in0=wa[:, c, :], in1=mub[:], op=ALU.mult),
                     reads=['rw_wa', 'rw_mub'], writes=['rw_wb'])
            for c in range(8):
                k.op('pool', lambda e, c=c: e.tensor_tensor(out=wa[:, c, :], in0=wa[:, c, :], in1=omb[:], op=ALU.mult),
                     reads=['rw_wa', 'rw_omb', 'rw_wb'], writes=['rw_wa'])
            w2 = g.sb('rw_w2', [64, 256], BF16, es2)
            a2 = g.sb('rw_a2', [128, 256], BF16, es2)
            g2 = g.sb('rw_g2', [128, 256], BF16, es2)
            a0r = g.sb('rw_a0r', [1, 256], BF16, es2)
            w0c = g.sb('rw_w0c', [128, 2], F32, es2)
            kkc = g.sb('rw_kkc', [128, 2], F32, es2)
            k.dma('pool', w2[:], W['rwkv_w2'][l], writes=['rw_w2'])
            k.dma('pool', a2[64:128, :], W['rwkv_a2'][l], writes=['rw_a2'])
            k.dma('pool', g2[:], W['rwkv_g2'][l], writes=['rw_g2'])
            k.dma('pool', a0r[:], W['rwkv_a0'][l:l + 1, :], writes=['rw_a0r'])
            k.dma('sp', w0c[:], W['rwkv_w0'][l].rearrange('(c p) -> p c', p=128), writes=['rw_w0c'], allow_slow_non_contiguous=True)
            k.dma('sp', kkc[:], W['rwkv_kk'][l].rearrange('(c p) -> p c', p=128), writes=['rw_kkc'], allow_slow_non_contiguous=True)
            bc = {}
            for nm, src in (('kk', W['rwkv_kk'][l]), ('ka', W['rwkv_ka'][l]), ('rk', W['rwkv_rk'][l].rearrange('h d -> (h d)'))):
                t = g.sb('rw_bc_' + nm, [128, 256], F32, es2)
                k.dma('sp', t[:], src.partition_broadcast(128), writes=['rw_bc_' + nm])
                bc[nm] = t
            hv = g.hTd.rearrange('(c p) t -> p c t', p=128)
            hb = [g.sb('rw_ht%d' % i, [128, 8, 513], BF16, es2) for i in range(2)]
            k.op('pool', lambda e: e.memset(hb[0][:, :, 0:1], 0.0), writes=['rw_ht0'])
            tnh = [g.sb('rw_tnh%d' % i, [128, 512], BF16, es2) for i in range(2)]
            sgd = [g.sb('rw_sgd%d' % i, [128, 512], BF16, es2) for i in range(2)]
            kx = [g.sb('rw_kx%d' % i, [128, 512], F32, es2) for i in range(2)]
            sq = [g.sb('rw_sq%d' % i, [128, 512], BF16, es2) for i in range(2)]
            rs = [g.sb('rw_rs%d' % i, [128, 512], F32, es2) for i in range(2)]
            tm = {nm: [g.sb('rw_%s%d' % (nm, i), [128, 256], F32, es2) for i in range(2)] for nm in ('a', 'r', 'kxm', 'k2', 'tq')}
            s4 = [g.sb('rw_s4%d' % i, [128, 4], F32, es2) for i in range(2)]
            ob = [g.sb('rw_ob%d' % i, [128, 768], BF16, es2) for i in range(2)]

            def fm2(ht, hk, c0, ncols, ps, pskey):
                for c in range(8):
                    k.op('pe', lambda e, c=c: e.matmul(ps, lhsT=wa[:, c, c0:c0 + ncols], rhs=ht[:, c, 1:513], start=(c == 0), stop=False),
                         reads=['rw_wa', hk], writes=[pskey])
                for c in range(8):
                    k.op('pe', lambda e, c=c: e.matmul(ps, lhsT=wb[:, c, c0:c0 + ncols], rhs=ht[:, c, 0:512], start=False, stop=(c == 7)),
                         reads=['rw_wb', hk], writes=[pskey])

            nps = 0
            for tt in range(8):
                b = tt % 2
                ts = slice(tt * 512, (tt + 1) * 512)
                hk = 'rw_ht%d' % b
                ht = hb[b]
                if tt == 0:
                    k.dma('sp', ht[:, :, 1:513], hv[:, :, 0:512], reads=['hTd'], writes=[hk])
                else:
                    k.dma('sp', ht[:], hv[:, :, tt * 512 - 1:(tt + 1) * 512], reads=['hTd'], writes=[hk])
                for j in range(2):
                    pi = nps % 2
                    nps += 1
                    fm2(ht, hk, j * 128, 128, g.ps[pi][:], g.psk[pi])
                    k.op('act', lambda e, pi=pi, j=j, ts=ts: e.copy(out=rT[:, j, ts], in_=g.ps[pi][:]), reads=[g.psk[pi]],
                         writes=['rw_rT%d_%d' % (j, tt)])
                    pi = nps % 2
                    nps += 1
                    fm2(ht, hk, 256 + j * 128, 128, g.ps[pi][:], g.psk[pi])
                    kb = nps % 2
                    ks_ = str(kb)
                    k.op('act', lambda e, pi=pi, j=j, kb=kb: e.activation(out=kx[kb][:], in_=g.ps[pi][:], func=AF.Copy, scale=kkc[:, j:j + 1]),
                         reads=[g.psk[pi], 'rw_kkc'], writes=['rw_kx' + ks_])
                    k.op('act', lambda e, kb=kb: e.activation(out=sq[kb][:], in_=kx[kb][:], func=AF.Square), reads=['rw_kx' + ks_], writes=['rw_sq' + ks_])
                    pj = 2 + kb
                    k.op('pe', lambda e, kb=kb, pj=pj: e.matmul(g.ps[pj][:], lhsT=g.cs['blk64_b'][:], rhs=sq[kb][:], start=True, stop=True),
                         reads=['rw_sq' + ks_, 'c_blk64_b'], writes=[g.psk[pj]])
                    k.op('dve', lambda e, kb=kb, pj=pj: e.tensor_scalar(out=rs[kb][:], in0=g.ps[pj][:], scalar1=1e-12, scalar2=None, op0=ALU.add),
                         reads=[g.psk[pj]], writes=['rw_rs' + ks_])
                    k.op('act', lambda e, kb=kb: e.activation(out=rs[kb][:], in_=rs[kb][:], func=AF.Sqrt), reads=['rw_rs' + ks_], writes=['rw_rs' + ks_])
                    k.op('dve', lambda e, kb=kb: e.reciprocal(out=rs[kb][:], in_=rs[kb][:]), reads=['rw_rs' + ks_], writes=['rw_rs' + ks_])
                    k.op('dve', lambda e, kb=kb, j=j, ts=ts: e.tensor_tensor(out=kkT[:, j, ts], in0=kx[kb][:], in1=rs[kb][:], op=ALU.mult),
                         reads=['rw_kx' + ks_, 'rw_rs' + ks_], writes=['rw_kkT%d_%d' % (j, tt)])
                pi = nps % 2
                nps += 1
                fm2(ht, hk, 768, 128, g.ps[pi][:], g.psk[pi])
                k.op('act', lambda e, pi=pi, b=b: e.activation(out=tnh[b][0:64, :], in_=g.ps[pi][0:64, :], func=AF.Tanh),
                     reads=[g.psk[pi]], writes=['rw_tnh%d' % b])
                k.op('act', lambda e, pi=pi, b=b: e.copy(out=tnh[b][64:128, :], in_=g.ps[pi][64:128, :]),
                     reads=[g.psk[pi]], writes=['rw_tnhb%d' % b])
                pi = nps % 2
                nps += 1
                fm2(ht, hk, 896, 128, g.ps[pi][:], g.psk[pi])
                k.op('act', lambda e, pi=pi, b=b: e.activation(out=sgd[b][:], in_=g.ps[pi][:], func=AF.Sigmoid), reads=[g.psk[pi]],
                     writes=['rw_sgd%d' % b])
                for j in range(2):
                    pi = nps % 2
                    nps += 1
                    k.op('pe', lambda e, pi=pi, j=j, b=b: e.matmul(g.ps[pi][:], lhsT=w2[:, j * 128:(j + 1) * 128], rhs=tnh[b][0:64, :],
                                                                   start=True, stop=True), reads=['rw_w2', 'rw_tnh%d' % b], writes=[g.psk[pi]])
                    k.op('act', lambda e, pi=pi, j=j, ts=ts: e.activation(out=wT[:, j, ts], in_=g.ps[pi][:], func=AF.Sigmoid, bias=w0c[:, j:j + 1], scale=1.0),
                         reads=[g.psk[pi], 'rw_w0c'], writes=['rw_wT%d_%d' % (j, tt)])
                    k.op('act', lambda e, j=j, ts=ts: e.activation(out=wT[:, j, ts], in_=wT[:, j, ts], func=AF.Exp, scale=-0.606531),
                         reads=['rw_wT%d_%d' % (j, tt)], writes=['rw_wT%d_%d' % (j, tt)])
                for sub in range(4):
                    t_ = tt * 4 + sub
                    q = t_ % 2
                    qs = str(q)
                    cs_ = slice(sub * 128, (sub + 1) * 128)
                    pA, pB, pC, pD = 4, 5, 6, 7
                    for (ps_, c0, n_) in ((pA, 0, 512), (pB, 512, 256)):
                        for c in range(8):
                            k.op('pe', lambda e, c=c, ps_=ps_, c0=c0, n_=n_, sub=sub: e.matmul(
                                g.ps[ps_][:, 0:n_], lhsT=ht[:, c, 1 + sub * 128:1 + (sub + 1) * 128], rhs=wa[:, c, c0:c0 + n_],
                                start=(c == 0), stop=False), reads=['rw_wa', hk], writes=[g.psk[ps_]])
                        for c in range(8):
                            k.op('pe', lambda e, c=c, ps_=ps_, c0=c0, n_=n_, sub=sub: e.matmul(
                                g.ps[ps_][:, 0:n_], lhsT=ht[:, c, sub * 128:(sub + 1) * 128], rhs=wb[:, c, c0:c0 + n_],
                                start=False, stop=(c == 7)), reads=['rw_wb', hk], writes=[g.psk[ps_]])
                    k.op('pe', lambda e, b=b, cs_=cs_: e.matmul(g.ps[pC][:, 0:256], lhsT=tnh[b][64:128, cs_], rhs=a2[64:128, :], start=True, stop=False),
                         reads=['rw_tnhb%d' % b, 'rw_a2'], writes=[g.psk[pC]])
                    k.op('pe', lambda e: e.matmul(g.ps[pC][:, 0:256], lhsT=g.cs['ones_b'][0:1, :], rhs=a0r[:], start=False, stop=True),
                         reads=['c_ones_b', 'rw_a0r'], writes=[g.psk[pC]])
                    k.op('pe', lambda e, b=b, cs_=cs_: e.matmul(g.ps[pD][:, 0:256], lhsT=sgd[b][:, cs_], rhs=g2[:], start=True, stop=True),
                         reads=['rw_sgd%d' % b, 'rw_g2'], writes=[g.psk[pD]])
                    a_, r_, kxm, k2, tq = (tm[nm][q] for nm in ('a', 'r', 'kxm', 'k2', 'tq'))
                    K_ = lambda nm: 'rw_%s%s' % (nm, qs)
                    k.op('act', lambda e: e.activation(out=a_[:], in_=g.ps[pC][:, 0:256], func=AF.Sigmoid), reads=[g.psk[pC]], writes=[K_('a')])
                    k.op('act', lambda e, t_=t_: e.copy(out=gtm[:, t_, :], in_=g.ps[pD][:, 0:256]), reads=[g.psk[pD]], writes=['rw_gtm%d' % t_])
                    k.op('act', lambda e: e.copy(out=r_[:], in_=g.ps[pA][:, 0:256]), reads=[g.psk[pA]], writes=[K_('r')])
                    k.op('act', lambda e, t_=t_: e.copy(out=vtm[:, t_, :], in_=g.ps[pB][:, 0:256]), reads=[g.psk[pB]], writes=['rw_vtm%d' % t_])
                    k.op('act', lambda e, q=q: e.copy(out=ob[q][:, 512:768], in_=g.ps[pB][:, 0:256]), reads=[g.psk[pB]], writes=['rw_obv' + qs])
                    k.op('dve', lambda e: e.tensor_tensor(out=kxm[:], in0=g.ps[pA][:, 256:512], in1=bc['kk'][:], op=ALU.mult),
                         reads=[g.psk[pA], 'rw_bc_kk'], writes=[K_('kxm')])
                    k.op('pool', lambda e: e.tensor_tensor(out=tq[:], in0=kxm[:], in1=kxm[:], op=ALU.mult), reads=[K_('kxm')], writes=[K_('tq')])
                    k.op('dve', lambda e, q=q: e.reduce_sum(out=s4[q][:], in_=tq[:].rearrange('p (h d) -> p h d', d=64), axis=AX.X),
                         reads=[K_('tq')], writes=['rw_s4' + qs])
                    k.op('dve', lambda e, q=q: e.tensor_scalar(out=s4[q][:], in0=s4[q][:], scalar1=1e-12, scalar2=None, op0=ALU.add),
                         reads=['rw_s4' + qs], writes=['rw_s4' + qs])
                    k.op('act', lambda e, q=q: e.activation(out=s4[q][:], in_=s4[q][:], func=AF.Sqrt), reads=['rw_s4' + qs], writes=['rw_s4' + qs])
                    k.op('dve', lambda e, q=q: e.reciprocal(out=s4[q][:], in_=s4[q][:]), reads=['rw_s4' + qs], writes=['rw_s4' + qs])
                    for h in range(4):
                        hs = slice(h * 64, (h + 1) * 64)
                        k.op('dve', lambda e, h=h, hs=hs, q=q: e.scalar_tensor_tensor(out=ob[q][:, hs], in0=kxm[:, hs], scalar=s4[q][:, h:h + 1],
                                                                                      in1=a_[:, hs], op0=ALU.mult, op1=ALU.mult),
                             reads=[K_('kxm'), 'rw_s4' + qs, K_('a')], writes=['rw_obb' + qs])
                    k.op('dve', lambda e: e.scalar_tensor_tensor(out=tq[:], in0=a_[:], scalar=-1.0, in1=bc['ka'][:], op0=ALU.add, op1=ALU.mult),
                         reads=[K_('a'), 'rw_bc_ka', K_('tq')], writes=[K_('tq')])
                    k.op('dve', lambda e: e.scalar_tensor_tensor(out=k2[:], in0=tq[:], scalar=1.0, in1=g.ps[pA][:, 256:512], op0=ALU.add, op1=ALU.mult),
                         reads=[K_('tq'), g.psk[pA]], writes=[K_('k2')])
                    k.op('pool', lambda e, q=q: e.tensor_copy(ob[q][:, 256:512], k2[:]), reads=[K_('k2')], writes=['rw_obk' + qs])
                    k.op('pool', lambda e: e.tensor_tensor(out=tq[:], in0=r_[:], in1=k2[:], op=ALU.mult), reads=[K_('r'), K_('k2'), K_('tq')], writes=[K_('tq')])
                    k.op('pool', lambda e: e.tensor_tensor(out=tq[:], in0=tq[:], in1=bc['rk'][:], op=ALU.mult), reads=[K_('tq'), 'rw_bc_rk'], writes=[K_('tq')])
                    k.op('dve', lambda e, t_=t_: e.reduce_sum(out=bon[:, t_, :], in_=tq[:].rearrange('p (h d) -> p h d', d=64), axis=AX.X),
                         reads=[K_('tq')], writes=['rw_bon%d' % t_])
                    k.dma('sp', g.rwscr[t_ * 128:(t_ + 1) * 128, :], ob[q][:], reads=['rw_obv' + qs, 'rw_obb' + qs, 'rw_obk' + qs],
                          writes=['rwscr%d' % t_])
            k.barrier()
        TC = 64
        ST = g.sb('rw_ST', [128, 128], BF16, es)
        Lb = [g.sb('rw_L%d' % i, [36, TC, 128], BF16, es) for i in range(2)]
        Rb = [g.sb('rw_R%d' % i, [36, TC, 128], BF16, es) for i in range(2)]
        KKn = [g.sb('rw_KKn%d' % i, [128, TC, 4], BF16, es) for i in range(2)]
        Rr = [g.sb('rw_Rr%d' % i, [128, TC, 4], BF16, es) for i in range(2)]
        sam = load_const(g, es, 'sa_mask')
        hlm = load_const(g, es, 'hl_mask')
        Yc = [g.sb('rw_Yc%d' % i, [128, 2, 128], F32, es) for i in range(2)]
        ytm = g.sb('rw_ytm', [128, 256], F32, es)
        yo = [g.sb('rw_yo%d' % i, [128, 256], BF16, es) for i in range(2)]
        yT = [g.sb('rw_yT%d' % i, [128, 2, 128], BF16, es) for i in range(2)]
        st8 = g.sb('rw_st8', [128, 8], F32, es)
        tq2 = g.sb('rw_tq2', [128, 256], F32, es)
        gnw = g.sb('rw_gnw', [128, 256], F32, es)
        gnb = g.sb('rw_gnb', [128, 256], F32, es)
        k.dma('sp', gnw[:], W['rwkv_gn_w'][l].partition_broadcast(128), writes=['rw_gnw'])
        k.dma('sp', gnb[:], W['rwkv_gn_b'][l].partition_broadcast(128), writes=['rw_gnb'])
        k.op('pool', lambda e: e.memset(ST[:], 0.0), writes=['rw_ST0', 'rw_ST1'])
        for i in range(2):
            k.op('pool', lambda e, i=i: e.memset(Lb[i][:], 0.0), writes=['rw_L%d' % i])
            k.op('pool', lambda e, i=i: e.memset(Rb[i][:], 0.0), writes=['rw_R%d' % i])
        psb = [g.ps[6][:].bitcast(BF16), g.ps[7][:].bitcast(BF16)]
        nsteps = RW_STEPS
        def setup(ch):
            cb = ch % 2
            t0 = ch * TC
            cbs = str(cb)
            tt = t0 // 512
            for h in range(4):
                hl, hh = h % 2, h // 2
                src = g.rwscr[t0:t0 + TC, :]
                k.dma('sp', Lb[cb][h:h + 1, :, hl * 64:(hl + 1) * 64], src[:, h * 64:(h + 1) * 64].rearrange('(o t) d -> o t d', o=1),
                      reads=['rwscr%d' % (t0 // 128)], writes=['rw_L' + cbs])
                k.dma('sp', Lb[cb][32 + h:33 + h, :, hl * 64:(hl + 1) * 64], src[:, 256 + h * 64:256 + (h + 1) * 64].rearrange('(o t) d -> o t d', o=1),
                      reads=['rwscr%d' % (t0 // 128)], writes=['rw_L' + cbs])
                k.dma('sp', Rb[cb][32 + h:33 + h, :, hh * 64:(hh + 1) * 64], src[:, 512 + h * 64:512 + (h + 1) * 64].rearrange('(o t) d -> o t d', o=1),
                      reads=['rwscr%d' % (t0 // 128)], writes=['rw_Rv' + cbs])
                k.op('pool', lambda e, h=h, hh=hh, hl=hl, cb=cb, t0=t0: e.tensor_scalar(
                    out=KKn[cb][:, :, h], in0=kkT[:, hh, t0:t0 + TC], scalar1=hlm[:, hl:hl + 1], scalar2=None, op0=ALU.mult),
                    reads=['rw_kkT%d_%d' % (hh, tt), 'c_hl_mask'], writes=['rw_KKn' + cbs])
                k.op('pool', lambda e, h=h, hh=hh, hl=hl, cb=cb, t0=t0: e.tensor_scalar(
                    out=Rr[cb][:, :, h], in0=rT[:, hh, t0:t0 + TC], scalar1=hlm[:, 2 + hl:3 + hl], scalar2=None, op0=ALU.mult),
                    reads=['rw_rT%d_%d' % (hh, tt), 'c_hl_mask'], writes=['rw_Rr' + cbs])

        setup(0)
        for ch in range(nsteps // TC):
            cb = ch % 2
            t0 = ch * TC
            cbs = str(cb)
            tt = t0 // 512
            if ch + 1 < nsteps // TC:
                setup(ch + 1)
            for s_ in range(TC):
                t = t0 + s_
                pa = t % 2
                pu = 2 + t % 2
                py = 4 + (t // 128) % 2
                k.op('pe', lambda e, cb=cb, s_=s_, pa=pa: e.matmul(g.ps[pa][0:4, 0:128], lhsT=KKn[cb][:, s_, :], rhs=ST[:], start=True, stop=True),
                     reads=['rw_KKn' + cbs, 'rw_ST0', 'rw_ST1'], writes=[g.psk[pa]])
                k.op('dve', lambda e, cb=cb, s_=s_, pa=pa: e.tensor_tensor(out=Rb[cb][0:4, s_, :], in0=g.ps[pa][0:4, 0:128], in1=sam[0:4, :], op=ALU.mult),
                     reads=[g.psk[pa], 'c_sa_mask'], writes=['rw_R' + cbs])
                k.op('pe', lambda e, cb=cb, s_=s_, pu=pu: e.matmul(g.ps[pu][:, 0:128], lhsT=Lb[cb][:, s_, :], rhs=Rb[cb][:, s_, :], start=True, stop=True),
                     reads=['rw_L' + cbs, 'rw_R' + cbs, 'rw_Rv' + cbs], writes=[g.psk[pu]])
                for hh in range(2):
                    k.op('dve', lambda e, hh=hh, t=t, pu=pu: e.scalar_tensor_tensor(
                        out=ST[:, hh * 64:(hh + 1) * 64], in0=ST[:, hh * 64:(hh + 1) * 64], scalar=wT[:, hh, t:t + 1],
                        in1=g.ps[pu][:, hh * 64:(hh + 1) * 64], op0=ALU.mult, op1=ALU.add),
                        reads=['rw_ST%d' % hh, g.psk[pu], 'rw_wT%d_%d' % (hh, tt)], writes=['rw_ST%d' % hh])
                k.op('pe', lambda e, cb=cb, s_=s_, t=t, py=py: e.matmul(g.ps[py][:, (t % 128) * 4:(t % 128) * 4 + 4], lhsT=ST[:], rhs=Rr[cb][:, s_, :],
                                                                   start=True, stop=True), reads=['rw_ST0', 'rw_ST1', 'rw_Rr' + cbs], writes=[g.psk[py]])
                if t % 128 == 127:
                    t_ = t // 128
                    yb_ = t_ % 2
                    ys = str(yb_)
                    yv = g.ps[py][:].rearrange('p (t h) -> p h t', h=4)
                    for hl in range(2):
                        k.op('act', lambda e, hl=hl, yb_=yb_: e.copy(out=Yc[yb_][0:64, hl, :], in_=yv[0:64, hl, :]), reads=[g.psk[py]],
                             writes=['rw_Yc%s_%d' % (ys, hl)])
                        k.op('act', lambda e, hl=hl, yb_=yb_: e.copy(out=Yc[yb_][64:128, hl, :], in_=yv[64:128, 2 + hl, :]), reads=[g.psk[py]],
                             writes=['rw_Ycb%s_%d' % (ys, hl)])
                    ytv = ytm[:].rearrange('p (hh hl i) -> p hl hh i', hh=2, hl=2)
                    for hl in range(2):
                        pt_ = 6 + hl
                        k.op('pe', lambda e, hl=hl, yb_=yb_, pt_=pt_: e.transpose(out=g.ps[pt_][:, 0:128], in_=Yc[yb_][:, hl, :], identity=g.cs['ident_f'][:]),
                             reads=['rw_Yc%s_%d' % (ys, hl), 'rw_Ycb%s_%d' % (ys, hl), 'c_ident_f'], writes=[g.psk[pt_]])
                        k.op('act', lambda e, hl=hl, pt_=pt_: e.copy(out=ytv[:, hl, :, :], in_=g.ps[pt_][:, 0:128].rearrange('p (hh i) -> p hh i', hh=2)),
                             reads=[g.psk[pt_]], writes=['rw_ytm'])
                    k.op('dve', lambda e: e.reduce_sum(out=st8[:, 0:4], in_=ytm[:].rearrange('p (h d) -> p h d', d=64), axis=AX.X),
                         reads=['rw_ytm'], writes=['rw_st8'])
                    k.op('pool', lambda e: e.tensor_tensor(out=tq2[:], in0=ytm[:], in1=ytm[:], op=ALU.mult), reads=['rw_ytm'], writes=['rw_tq2'])
                    k.op('dve', lambda e: e.reduce_sum(out=st8[:, 4:8], in_=tq2[:].rearrange('p (h d) -> p h d', d=64), axis=AX.X),
                         reads=['rw_tq2'], writes=['rw_st8'])
                    k.op('dve', lambda e: e.tensor_scalar(out=st8[:], in0=st8[:], scalar1=1.0 / 64, scalar2=None, op0=ALU.mult),
                         reads=['rw_st8'], writes=['rw_st8'])
                    k.op('dve', lambda e: e.tensor_tensor(out=tq2[:, 0:4], in0=st8[:, 0:4], in1=st8[:, 0:4], op=ALU.mult), reads=['rw_st8', 'rw_tq2'],
                         writes=['rw_tq2'])
                    k.op('dve', lambda e: e.tensor_tensor(out=st8[:, 4:8], in0=st8[:, 4:8], in1=tq2[:, 0:4], op=ALU.subtract), reads=['rw_st8', 'rw_tq2'],
                         writes=['rw_st8'])
                    k.op('dve', lambda e: e.tensor_scalar(out=st8[:, 4:8], in0=st8[:, 4:8], scalar1=64e-5, scalar2=None, op0=ALU.add),
                         reads=['rw_st8'], writes=['rw_st8'])
                    k.op('act', lambda e: e.activation(out=st8[:, 4:8], in_=st8[:, 4:8], func=AF.Sqrt), reads=['rw_st8'], writes=['rw_st8'])
                    k.op('dve', lambda e: e.reciprocal(out=st8[:, 4:8], in_=st8[:, 4:8]), reads=['rw_st8'], writes=['rw_st8'])
                    for h in range(4):
                        hs = slice(h * 64, (h + 1) * 64)
                        k.op('dve', lambda e, h=h, hs=hs: e.tensor_scalar(out=tq2[:, hs], in0=ytm[:, hs], scalar1=st8[:, h:h + 1], scalar2=st8[:, 4 + h:5 + h],
                                                                         op0=ALU.subtract, op1=ALU.mult), reads=['rw_ytm', 'rw_st8', 'rw_tq2'], writes=['rw_tq2'])
                    k.op('pool', lambda e: e.tensor_tensor(out=tq2[:], in0=tq2[:], in1=gnw[:], op=ALU.mult), reads=['rw_tq2', 'rw_gnw'], writes=['rw_tq2'])
                    k.op('pool', lambda e: e.tensor_tensor(out=tq2[:], in0=tq2[:], in1=gnb[:], op=ALU.add), reads=['rw_tq2', 'rw_gnb'], writes=['rw_tq2'])
                    for h in range(4):
                        hs = slice(h * 64, (h + 1) * 64)
                        k.op('dve', lambda e, h=h, hs=hs, t_=t_: e.scalar_tensor_tensor(out=tq2[:, hs], in0=vtm[:, t_, hs], scalar=bon[:, t_, h:h + 1],
                                                                                       in1=tq2[:, hs], op0=ALU.mult, op1=ALU.add),
                             reads=['rw_vtm%d' % t_, 'rw_bon%d' % t_, 'rw_tq2'], writes=['rw_tq2'])
                    k.op('dve', lambda e, yb_=yb_, t_=t_: e.tensor_tensor(out=yo[yb_][:], in0=tq2[:], in1=gtm[:, t_, :], op=ALU.mult),
                         reads=['rw_tq2', 'rw_gtm%d' % t_], writes=['rw_yo' + ys])
                    for j in range(2):
                        k.op('pe', lambda e, j=j, yb_=yb_: e.transpose(out=psb[yb_][:, j * 128:(j + 1) * 128], in_=yo[yb_][:, j * 128:(j + 1) * 128],
                                                                      identity=g.cs['ident_b'][:]), reads=['rw_yo' + ys, 'c_ident_b'], writes=[g.psk[6 + yb_]])
                    k.op('act', lambda e, yb_=yb_: e.copy(out=yT[yb_][:], in_=psb[yb_][:, 0:256].rearrange('p (j t) -> p j t', j=2)),
                         reads=[g.psk[6 + yb_]], writes=['rw_yT' + ys])
                    k.dma('sp', ycv[:, 0:2, t_ * 128:(t_ + 1) * 128], yT[yb_][:], reads=['rw_yT' + ys], writes=['ycat_rw%d' % t_])
        k.barrier()
```

```python
import contextlib
import numpy as np
import ml_dtypes
import concourse.bass as bass
import concourse.mybir as mybir
from concourse.bass_utils import run_bass_kernel_spmd

F32 = mybir.dt.float32
BF16 = mybir.dt.bfloat16
I32 = mybir.dt.int32
AF = mybir.ActivationFunctionType
ALU = mybir.AluOpType
AX = mybir.AxisListType


class KB:
    NDS = 8

    def __init__(self):
        self.nc = bass.Bass("TRN2", target_bir_lowering=False)
        nc = self.nc
        self.eng = {"pe": nc.tensor, "act": nc.scalar, "dve": nc.vector, "pool": nc.gpsimd, "sp": nc.sync}
        self.semh = {}
        for e in self.eng:
            self.semh[e] = nc.semaphore("s_" + e).__enter__()
        self.cnt = {e: 0 for e in self.eng}
        self.waited = {e: {} for e in self.eng}
        self.lastw = {}
        self.readers = {}
        self.dq = {}
        self.nwaits = 0
        self.nops = 0

    def _deps(self, eng, reads, writes):
        deps = {}

        def add(k, v):
            if deps.get(k, 0) < v:
                deps[k] = v

        for r in reads:
            p = self.lastw.get(r)
            if p is not None:
                if not (p[0] == eng and eng == "pe"):
                    add(*p)
            if isinstance(r, str) and r.startswith("ps") and r[2:].isdigit():
                for k, v in self.readers.get(r, {}).items():
                    if k != eng:
                        add(k, v)
        for w in writes:
            p = self.lastw.get(w)
            if p is not None and p[0] != eng:
                add(*p)
            for k, v in self.readers.get(w, {}).items():
                if k != eng:
                    add(k, v)
        return deps

    def _wait(self, eng, deps):
        wt = self.waited[eng]
        for k, v in deps.items():
            if wt.get(k, 0) >= v:
                continue
            self.eng[eng].wait_ge(self.semh[k], v)
            wt[k] = v
            self.nwaits += 1

    def _record(self, prod, reads, writes):
        k, v = prod
        for r in reads:
            d = self.readers.setdefault(r, {})
            if d.get(k, 0) < v:
                d[k] = v
        for w in writes:
            self.lastw[w] = prod
            self.readers[w] = {}

    def op(self, eng, fn, reads=(), writes=()):
        self._wait(eng, self._deps(eng, reads, writes))
        ins = fn(self.eng[eng])
        self.cnt[eng] += 1
        ins.then_inc(self.semh[eng], 1)
        self._record((eng, self.cnt[eng]), reads, writes)
        self.nops += 1
        return ins

    def dma(self, q, out, in_, reads=(), writes=(), **kw):
        st = self.dq.get(q)
        if st is None:
            st = {"n": 0, "sems": []}
            for i in range(self.NDS):
                key = ("dma", q, i)
                self.semh[key] = self.nc.semaphore("d_%s_%d" % (q, i)).__enter__()
                st["sems"].append(key)
            self.dq[q] = st
        i = st["n"]
        slot = i % self.NDS
        val = 16 * (i // self.NDS + 1)
        key = st["sems"][slot]
        deps = self._deps(q, reads, writes)
        if i >= self.NDS:
            if deps.get(key, 0) < val - 16:
                deps[key] = val - 16
        self._wait(q, deps)
        ins = self.eng[q].dma_start(out=out, in_=in_, **kw)
        ins.then_inc(self.semh[key], 16)
        st["n"] += 1
        self._record((key, val), reads, writes)
        return ins

    def barrier(self):
        deps = {}
        for e in self.eng:
            if self.cnt[e] > 0:
                deps[e] = self.cnt[e]
        for q, st in self.dq.items():
            n = st["n"]
            for slot in range(self.NDS):
                if n > slot:
                    cntslot = (n - 1 - slot) // self.NDS + 1
                    deps[st["sems"][slot]] = 16 * cntslot
        for e in self.eng:
            d = {k: v for k, v in deps.items() if k != e or e != "pe"}
            self._wait(e, d)

    def finish(self):
        self.barrier()


S = 4096
D = 1024
NT = 32
PROJ_W = 3212
L_DEPTH = 2
NORM_EPS = 1e-6
PI = float(np.pi)

PARAM_SHAPES = {
    'w_ada': (2, 1024, 6144), 'b_ada': (2, 6144), 'norm1_g': (2, 1024), 'norm2_g': (2, 1024),
    'w_in': (2, 1024, 3212), 'w_out': (2, 1024, 1024), 'rwkv_mu': (2, 1024), 'rwkv_w0': (2, 256),
    'rwkv_w2': (2, 64, 256), 'rwkv_a0': (2, 256), 'rwkv_a2': (2, 64, 256), 'rwkv_g2': (2, 128, 256),
    'rwkv_kk': (2, 256), 'rwkv_ka': (2, 256), 'rwkv_rk': (2, 4, 64), 'rwkv_gn_w': (2, 256), 'rwkv_gn_b': (2, 256),
    'conv_w': (2, 3, 256), 'dil_q_g': (2, 64), 'dil_k_g': (2, 64), 'nsa_q_g': (2, 64), 'nsa_kc_g': (2, 64),
    'nsa_ks_g': (2, 64), 'nsa_kw_g': (2, 64), 'nsa_pe_k': (2, 32, 64), 'nsa_pe_v': (2, 32, 64),
    'nsa_wk1': (2, 2048, 256), 'nsa_wk2': (2, 256, 64), 'nsa_wv1': (2, 2048, 256), 'nsa_wv2': (2, 256, 64),
    'onorm_g': (2, 768), 'w_router': (2, 1024, 32), 'b_router': (2, 32), 'w_gu': (2, 32, 1024, 2048),
    'b_gu': (2, 32, 2048), 'w_down': (2, 32, 1024, 1024), 'b_down': (2, 32, 1024),
}


def host_consts():
    c = {}
    c['ident_f'] = np.eye(128, dtype=np.float32)
    c['ident_b'] = np.eye(128).astype(ml_dtypes.bfloat16)
    c['ones_b'] = np.ones((128, 128)).astype(ml_dtypes.bfloat16)
    blk = np.zeros((128, 128), np.float32)
    blk[:64, :64] = 1.0
    blk[64:, 64:] = 1.0
    c['blk64_b'] = blk.astype(ml_dtypes.bfloat16)
    sel = np.zeros((32, 32, 128), np.float32)
    for e in range(32):
        sel[e, e, :] = 1.0
    c['sel_b'] = sel.astype(ml_dtypes.bfloat16)
    pr = np.zeros((128, 128), np.float32)
    for m in range(128):
        if m % 64 < 32:
            pr[m + 32, m] = -1.0
        else:
            pr[m - 32, m] = 1.0
    c['prot_b'] = pr.astype(ml_dtypes.bfloat16)
    c['invf'] = (10000.0 ** (-(np.arange(128) % 32) / 32.0)).astype(np.float32).reshape(128, 1)
    kl = np.arange(128)[:, None]
    ql = np.arange(512)[None, :]

    def toep(offs, fn):
        return np.stack([fn(128 * o + ql - kl) for o in offs]).astype(np.float32)

    def dil(d):
        m = ((d >= 0) & (d <= 128)).astype(np.float32)
        m += ((d >= 0) & (d % 4 == 0) & (d // 4 <= 128))
        m += ((d >= 0) & (d % 16 == 0) & (d // 16 <= 128))
        return m
    c['dil_mask'] = toep(range(-3, 17), dil).transpose(1, 0, 2).astype(ml_dtypes.bfloat16).copy()
    c['swa_mask'] = toep(range(-3, 5), lambda d: (d >= 0) & (d <= 511)).transpose(1, 0, 2).astype(ml_dtypes.bfloat16).copy()
    c['cau_mask'] = toep(range(-3, 1), lambda d: d >= 0).transpose(1, 0, 2).astype(ml_dtypes.bfloat16).copy()
    c['cmp_mask'] = np.stack([(16 * kl + 31 <= 512 * i + ql) for i in range(5)]).astype(np.float32).transpose(1, 0, 2) \
        .astype(ml_dtypes.bfloat16).copy()
    diff = np.arange(256)[:, None] - 4 * np.arange(64)[None, :]
    offs = (np.arange(4)[:, None] - np.arange(2)[None, :]).reshape(-1)
    ov = (diff[..., None] == offs).sum(-1).astype(np.float32)
    ov[255] = 0
    ov1 = np.concatenate([np.ones((256, 1), np.float32), ov], axis=1)
    ov1[255] = 0
    c['ovl1'] = ov1.reshape(2, 128, 65).transpose(1, 0, 2).astype(ml_dtypes.bfloat16).copy()
    tok = np.arange(S)[:, None]
    jb = np.arange(64)[None, :]
    cur = tok // 64
    fut = jb > cur
    forced = ((jb == 0) | (jb == cur) | (jb == cur - 1)) & ~fut
    keep = ~(fut | forced)
    c['sel_keep'] = keep.astype(np.float32).reshape(32, 128, 64).transpose(1, 0, 2).copy()
    c['sel_add'] = (-1.0 * fut + 1e4 * forced).astype(np.float32).reshape(32, 128, 64).transpose(1, 0, 2).copy()
    ex = np.zeros((64, 32, 128), np.float32)
    for kt in range(32):
        ex[2 * kt, kt, :64] = 1
        ex[2 * kt + 1, kt, 64:] = 1
    c['sel_exp'] = ex.astype(ml_dtypes.bfloat16)
    sam = np.zeros((128, 128), np.float32)
    sam[0:2, 0:64] = 1.0
    sam[2:4, 64:128] = 1.0
    c['sa_mask'] = sam
    hm = np.zeros((128, 4), np.float32)
    hm[0:64, 0] = -1.0
    hm[64:128, 1] = -1.0
    hm[0:64, 2] = 1.0
    hm[64:128, 3] = 1.0
    c['hl_mask'] = hm
    return c


RESIDENT_CONSTS = ('ident_f', 'ident_b', 'ones_b', 'blk64_b', 'sel_b', 'prot_b', 'invf')


class Ctx:
    pass


class LazyW:
    def __init__(self, nc):
        self.nc = nc
        self.d = {}

    def __getitem__(self, n):
        if n not in self.d:
            self.d[n] = self.nc.dram_tensor(n, list(PARAM_SHAPES[n]), F32, kind='ExternalInput').ap()
        return self.d[n]


def build_program(layers=(0, 1), taps=(), first=True, last=True, mixers='ABCD', moe=True):
    k = KB()
    nc = k.nc
    g = Ctx()
    g.k = k
    g.nc = nc
    g.taps = {}
    g.want = set(taps)
    g.mixers = mixers
    g.do_moe = moe
    g.x = nc.dram_tensor('x', [S, D], F32, kind='ExternalInput').ap()
    g.c = nc.dram_tensor('c', [D], F32, kind='ExternalInput').ap()
    g.pos = nc.dram_tensor('positions', [S], I32, kind='ExternalInput').ap()
    g.W = LazyW(nc)
    g.C = {}
    for n, a in host_consts().items():
        dt = F32 if a.dtype == np.float32 else BF16
        g.C[n] = nc.dram_tensor('const_' + n, list(a.shape), dt, kind='ExternalInput').ap()
    g.out = nc.dram_tensor('out', [S, D], F32, kind='ExternalOutput').ap()
    g.xT = [nc.dram_tensor('xT%d' % i, [D, S], F32, kind='Internal').ap() for i in range(2)]
    g.ycatT = nc.dram_tensor('ycatT', [D, S], BF16, kind='Internal').ap()
    g.h2Td = nc.dram_tensor('h2Td', [D, S], BF16, kind='Internal').ap()
    g.hTd = nc.dram_tensor('hTd', [D, S], BF16, kind='Internal').ap()
    g.rwscr = nc.dram_tensor('rwscr', [S, 768], BF16, kind='Internal').ap()

    with contextlib.ExitStack() as es:
        g.layer = 'i'
        g.nsb = 0

        def sb(name, shape, dt, stack=es):
            g.nsb += 1
            return stack.enter_context(nc.sbuf_tensor('%s_%d' % (name, g.nsb), list(shape), dt))
        g.sb = sb
        g.ps = [nc.alloc_psum_tensor('ps%d' % i, [128, 512], F32) for i in range(8)]
        g.psk = ['ps%d' % i for i in range(8)]
        g.cs = {}
        for n, ap in g.C.items():
            if n not in RESIDENT_CONSTS:
                continue
            t = sb('c_' + n, ap.shape, ap.dtype)
            k.dma('sp', t[:], ap, writes=['c_' + n])
            g.cs[n] = t
        g.mod = sb('mod', [128, 48], F32)
        g.GT = sb('GT', [32, S], BF16)
        if first:
            phase_pre(g)
        cur = 0
        for l in layers:
            g.layer = str(l)
            phase_mod(g, l)
            with contextlib.ExitStack() as les:
                phase_norm1(g, l, g.xT[cur])
                g.onorm = g.sb('onorm', [128, 6], F32, les)
                k.dma('sp', g.onorm[:], g.W['onorm_g'][l].rearrange('(j p) -> p j', p=128), writes=['onorm'],
                      allow_slow_non_contiguous=True)
                if 'B' in g.mixers:
                    mixer_conv(g, l)
                if 'C' in g.mixers:
                    mixer_dil(g, l)
                if 'D' in g.mixers:
                    mixer_nsa(g, l)
                if 'A' in g.mixers:
                    mixer_rwkv(g, l)
                zero_missing(g)
                t = tap(g, 'ycatT%d' % l, [D, S], BF16)
                if t is not None:
                    k.barrier()
                    k.dma('sp', t, g.ycatT, writes=['tap'])
                k.barrier()
            phase_wout(g, l, g.xT[cur], g.xT[cur ^ 1])
            phase_norm2_router(g, l, g.xT[cur ^ 1])
            phase_moe(g, l, g.xT[cur ^ 1], g.xT[cur], final=(last and l == layers[-1]))
        k.finish()
    return k, g


def tap(g, name, shape, dt=F32):
    if name not in g.want:
        return None
    t = g.nc.dram_tensor('tap_' + name, list(shape), dt, kind='ExternalOutput').ap()
    g.taps[name] = t
    return t


def phase_pre(g):
    k, nc = g.k, g.nc
    identf = g.cs['ident_f']
    xTd = g.xT[0].rearrange('(c p) t -> p c t', p=128)
    with contextlib.ExitStack() as es:
        xin = [g.sb('pre_x%d' % i, [128, D], F32, es) for i in range(2)]
        xo = [g.sb('pre_o%d' % i, [128, 8, 128], F32, es) for i in range(2)]
        for t in range(NT):
            b = t % 2
            k.dma('sp', xin[b][:], g.x[t * 128:(t + 1) * 128, :], writes=['pre_x%d' % b])
            for h in range(2):
                pi = (2 * t + h) % 4
                for j in range(4):
                    cc = h * 4 + j
                    k.op('pe', lambda e, cc=cc, j=j, pi=pi, b=b: e.transpose(
                        out=g.ps[pi][:, j * 128:(j + 1) * 128], in_=xin[b][:, cc * 128:(cc + 1) * 128], identity=identf[:]),
                        reads=['pre_x%d' % b, 'c_ident_f'], writes=[g.psk[pi]])
                eng = 'act' if h == 0 else 'dve'
                if eng == 'act':
                    k.op('act', lambda e, pi=pi, b=b, h=h: e.copy(
                        out=xo[b][:, h * 4:(h + 1) * 4, :], in_=g.ps[pi][:].rearrange('p (c t) -> p c t', c=4)),
                        reads=[g.psk[pi]], writes=['pre_o%d_%d' % (b, h)])
                else:
                    k.op('dve', lambda e, pi=pi, b=b, h=h: e.tensor_copy(
                        xo[b][:, h * 4:(h + 1) * 4, :], g.ps[pi][:].rearrange('p (c t) -> p c t', c=4)),
                        reads=[g.psk[pi]], writes=['pre_o%d_%d' % (b, h)])
            k.dma('sp', xTd[:, :, t * 128:(t + 1) * 128], xo[b][:], reads=['pre_o%d_0' % b, 'pre_o%d_1' % b],
                  writes=['xT0_%d' % (t // 4)])
        k.barrier()


def phase_mod(g, l):
    k, nc = g.k, g.nc
    with contextlib.ExitStack() as es:
        cT = g.sb('cT', [128, 8], F32, es)
        cs = g.sb('cs', [128, 8], BF16, es)
        bcol = g.sb('bcol', [128, 48], F32, es)
        wt = [g.sb('wada%d' % i, [128, 8, 512], BF16, es) for i in range(2)]
        k.dma('sp', cT[:], g.c.rearrange('(k p) -> p k', p=128), writes=['cT'], allow_slow_non_contiguous=True)
        k.dma('sp', bcol[:], g.W['b_ada'][l].rearrange('(k p) -> p k', p=128), writes=['bcol'], allow_slow_non_contiguous=True)
        k.op('act', lambda e: e.activation(out=cs[:], in_=cT[:], func=AF.Silu), reads=['cT'], writes=['cs'])
        psm = g.ps[7]
        for ct in range(12):
            b = ct % 2
            k.dma('pool', wt[b][:], g.W['w_ada'][l][:, ct * 512:(ct + 1) * 512].rearrange('(k p) c -> p k c', p=128),
                  writes=['wada%d' % b])
            for j in range(4):
                col = ct * 4 + j
                for kk in range(8):
                    k.op('pe', lambda e, b=b, j=j, kk=kk, col=col: e.matmul(
                        psm[:, col:col + 1], lhsT=wt[b][:, kk, j * 128:(j + 1) * 128], rhs=cs[:, kk:kk + 1],
                        start=(kk == 0), stop=(kk == 7)),
                        reads=['wada%d' % b, 'cs'], writes=[g.psk[7]])
        k.op('dve', lambda e: e.tensor_tensor(out=g.mod[:], in0=psm[:, 0:48], in1=bcol[:], op=ALU.add),
             reads=[g.psk[7], 'bcol'], writes=['mod'])
        t = tap(g, 'mod%d' % l, [128, 48])
        if t is not None:
            k.dma('sp', t, g.mod[:], reads=['mod'], writes=['tap'])
        k.barrier()


def phase_norm1(g, l, xTd):
    k, nc = g.k, g.nc
    xTv = xTd.rearrange('(c p) t -> p c t', p=128)
    with contextlib.ExitStack() as es:
        gcol = g.sb('n1_g', [128, 8], F32, es)
        g1s = g.sb('n1_gs', [128, 8], F32, es)
        k.dma('sp', gcol[:], g.W['norm1_g'][l].rearrange('(k p) -> p k', p=128), writes=['n1_g'], allow_slow_non_contiguous=True)
        k.op('dve', lambda e: e.scalar_tensor_tensor(out=g1s[:], in0=g.mod[:, 8:16], scalar=1.0, in1=gcol[:],
                                                      op0=ALU.add, op1=ALU.mult), reads=['mod', 'n1_g'], writes=['n1_gs'])
        hT = g.sb('hT', [128, 8, S], BF16, es)
        norm_tiles(g, es, xTv, g1s, g.mod[:, 0:8], ['n1_gs', 'mod'], hT, 'hT', 'n1')
        hv = g.hTd.rearrange('(c p) t -> p c t', p=128)
        for c in range(8):
            k.dma('sp', hv[:, c, :], hT[:, c, :], reads=['hT%d' % c], writes=['hTd'])
        t = tap(g, 'h%d' % l, [128, 8, S], BF16)
        if t is not None:
            k.dma('sp', t, hT[:], reads=['hT%d' % i for i in range(8)], writes=['tap'])
        k.barrier()


def norm_tiles(g, es, xTv, gs, sh, gkeys, hT, hkey, pfx, xsrc_keys=None):
    k = g.k
    xt = [g.sb(pfx + '_x%d' % i, [128, 8, 512], F32, es) for i in range(2)]
    sq = [g.sb(pfx + '_sq%d' % i, [128, 8, 512], BF16, es) for i in range(2)]
    rstd = [g.sb(pfx + '_rstd%d' % i, [128, 512], F32, es) for i in range(2)]
    tmp = [g.sb(pfx + '_tmp%d' % i, [128, 512], F32, es) for i in range(2)]
    ones = g.cs['ones_b']
    for tt in range(8):
        b = tt % 2
        ts = slice(tt * 512, (tt + 1) * 512)
        xk, sk, rk = pfx + '_x%d' % b, pfx + '_sq%d' % b, pfx + '_rstd%d' % b
        k.dma('sp', xt[b][:], xTv[:, :, ts], reads=(xsrc_keys or []), writes=[xk])
        k.op('act', lambda e, b=b: e.activation(out=sq[b][:], in_=xt[b][:], func=AF.Square), reads=[xk], writes=[sk])
        pi = tt % 2
        for c in range(8):
            k.op('pe', lambda e, b=b, c=c, pi=pi: e.matmul(g.ps[pi][:], lhsT=ones[:], rhs=sq[b][:, c, :],
                                                          start=(c == 0), stop=(c == 7)),
                 reads=[sk, 'c_ones_b'], writes=[g.psk[pi]])
        k.op('dve', lambda e, b=b, pi=pi: e.tensor_scalar(out=rstd[b][:], in0=g.ps[pi][:], scalar1=1.0 / D, scalar2=NORM_EPS,
                                                         op0=ALU.mult, op1=ALU.add), reads=[g.psk[pi]], writes=[rk])
        k.op('act', lambda e, b=b: e.activation(out=rstd[b][:], in_=rstd[b][:], func=AF.Sqrt), reads=[rk], writes=[rk])
        k.op('dve', lambda e, b=b: e.reciprocal(out=rstd[b][:], in_=rstd[b][:]), reads=[rk], writes=[rk])
        for c in range(8):
            tb = c % 2
            tk = pfx + '_tmp%d' % tb
            k.op('dve', lambda e, b=b, c=c, tb=tb: e.tensor_tensor(out=tmp[tb][:], in0=xt[b][:, c, :], in1=rstd[b][:], op=ALU.mult),
                 reads=[xk, rk], writes=[tk])
            k.op('act', lambda e, c=c, tb=tb, ts=ts: e.activation(out=hT[:, c, ts], in_=tmp[tb][:], func=AF.Identity,
                                                                  scale=gs[:, c:c + 1], bias=sh[:, c:c + 1]),
                 reads=[tk] + gkeys, writes=[hkey + '%d' % c])


def make_in_maps(inputs, g, n_cores=8):
    consts = host_consts()
    maps = []
    shared = {n: np.ascontiguousarray(inputs[n], dtype=np.float32) for n in g.W.d}
    for b in range(n_cores):
        m = {'x': np.ascontiguousarray(inputs['x'][b]), 'c': np.ascontiguousarray(inputs['c'][b]),
             'positions': np.ascontiguousarray(inputs['positions'][b]).astype(np.int32)}
        m.update(shared)
        for n, a in consts.items():
            m['const_' + n] = a
        maps.append(m)
    return maps


def kernel(**inputs):
    k, g = build_program()
    maps = make_in_maps(inputs, g)
    res = run_bass_kernel_spmd(k.nc, maps, core_ids=list(range(8)))
    return np.stack([r['out'] for r in res.results], axis=0)


def load_w_bf16(g, dst, dkey, src_ap):
    g.k.dma('pool', dst, src_ap.rearrange('(c p) n -> p c n', p=128), writes=[dkey])


def ht_loader(g, es, pfx):
    bufs = [g.sb(pfx + '_ht%d' % i, [128, 8, 512], BF16, es) for i in range(2)]
    hv = g.hTd.rearrange('(c p) t -> p c t', p=128)

    def load(tt):
        b = tt % 2
        g.k.dma('sp', bufs[b][:], hv[:, :, tt * 512:(tt + 1) * 512], reads=['hTd'], writes=[pfx + '_ht%d' % b])
        return bufs[b], pfx + '_ht%d' % b
    return load


def proj_fm(g, wsb, wkey, c0, ncols, ht, hkey, ps, pskey):
    for c in range(8):
        g.k.op('pe', lambda e, c=c: e.matmul(ps, lhsT=wsb[:, c, c0:c0 + ncols], rhs=ht[:, c, :],
                                             start=(c == 0), stop=(c == 7)),
               reads=[wkey, hkey], writes=[pskey])


def proj_tm(g, wsb, wkey, c0, ncols, ht, hkey, sub, ps, pskey):
    for c in range(8):
        g.k.op('pe', lambda e, c=c: e.matmul(ps, lhsT=ht[:, c, sub * 128:(sub + 1) * 128], rhs=wsb[:, c, c0:c0 + ncols],
                                             start=(c == 0), stop=(c == 7)),
               reads=[wkey, hkey], writes=[pskey])


def head_rmsnorm_fm(g, y, ykey, sq, sqkey, gcol, gkeys, out_ap, okey, pi, tmpf, tmpkey):
    k = g.k
    k.op('act', lambda e: e.activation(out=sq, in_=y, func=AF.Square), reads=[ykey], writes=[sqkey])
    k.op('pe', lambda e: e.matmul(g.ps[pi][:], lhsT=g.cs['blk64_b'][:], rhs=sq, start=True, stop=True),
         reads=[sqkey, 'c_blk64_b'], writes=[g.psk[pi]])
    k.op('dve', lambda e: e.tensor_scalar(out=tmpf, in0=g.ps[pi][:], scalar1=1.0 / 64, scalar2=NORM_EPS, op0=ALU.mult, op1=ALU.add),
         reads=[g.psk[pi]], writes=[tmpkey])
    k.op('act', lambda e: e.activation(out=tmpf, in_=tmpf, func=AF.Sqrt), reads=[tmpkey], writes=[tmpkey])
    k.op('dve', lambda e: e.reciprocal(out=tmpf, in_=tmpf), reads=[tmpkey], writes=[tmpkey])
    k.op('dve', lambda e: e.scalar_tensor_tensor(out=out_ap, in0=y, scalar=gcol, in1=tmpf, op0=ALU.mult, op1=ALU.mult),
         reads=[ykey, tmpkey] + gkeys, writes=[okey])


def mixer_conv(g, l):
    k = g.k
    ycv = g.ycatT.rearrange('(c p) t -> p c t', p=128)
    with contextlib.ExitStack() as es:
        wb = g.sb('cv_w', [128, 8, 768], BF16, es)
        load_w_bf16(g, wb[:], 'cv_w', g.W['w_in'][l][:, 1024:1792])
        cw = g.sb('cv_cw', [128, 2, 3], F32, es)
        for kk in range(3):
            k.dma('sp', cw[:, :, kk], g.W['conv_w'][l, kk].rearrange('(j p) -> p j', p=128), writes=['cv_cw'],
                  allow_slow_non_contiguous=True)
        bg = g.sb('cv_bg', [128, 2, S], BF16, es)
        u = g.sb('cv_u', [128, 2, S + 2], F32, es)
        cgt = [g.sb('cv_cg%d' % i, [128, 512], F32, es) for i in range(2)]
        k.op('pool', lambda e: e.memset(u[:, :, 0:2], 0.0), writes=['cv_u_h'])
        n = 0
        hload = ht_loader(g, es, 'cv')
        for tt in range(8):
            ts = slice(tt * 512, (tt + 1) * 512)
            ht, hk = hload(tt)
            for j in range(2):
                pa, pb, pc = n % 6, (n + 1) % 6, (n + 2) % 6
                n += 3
                proj_fm(g, wb, 'cv_w', j * 128, 128, ht, hk, g.ps[pa][:], g.psk[pa])
                proj_fm(g, wb, 'cv_w', 256 + j * 128, 128, ht, hk, g.ps[pb][:], g.psk[pb])
                proj_fm(g, wb, 'cv_w', 512 + j * 128, 128, ht, hk, g.ps[pc][:], g.psk[pc])
                k.op('act', lambda e, j=j, ts=ts, pa=pa: e.copy(out=bg[:, j, ts], in_=g.ps[pa][:]), reads=[g.psk[pa]],
                     writes=['cv_bg%d_%d' % (j, tt)])
                cb = n % 2
                k.op('act', lambda e, cb=cb, pb=pb: e.copy(out=cgt[cb][:], in_=g.ps[pb][:]), reads=[g.psk[pb]], writes=['cv_cg%d' % cb])
                k.op('dve', lambda e, j=j, tt=tt, cb=cb, pc=pc: e.tensor_tensor(out=u[:, j, 2 + tt * 512:2 + (tt + 1) * 512],
                                                                                 in0=g.ps[pc][:], in1=cgt[cb][:], op=ALU.mult),
                     reads=[g.psk[pc], 'cv_cg%d' % cb], writes=['cv_u%d_%d' % (j, tt)])
        y = [g.sb('cv_y%d' % i, [128, 512], F32, es) for i in range(2)]
        sq = [g.sb('cv_sq%d' % i, [128, 512], BF16, es) for i in range(2)]
        tf = [g.sb('cv_tf%d' % i, [128, 512], F32, es) for i in range(2)]
        yo = [g.sb('cv_yo%d' % i, [128, 2, 512], BF16, es) for i in range(2)]
        n = 0
        for tt in range(8):
            ts = slice(tt * 512, (tt + 1) * 512)
            ob = tt % 2
            for j in range(2):
                b = n % 2
                n += 1
                yk = 'cv_y%d' % b
                ukeys = ['cv_u%d_%d' % (j, tt), 'cv_u_h'] + (['cv_u%d_%d' % (j, tt - 1)] if tt > 0 else [])
                k.op('act', lambda e, b=b, j=j, tt=tt: e.activation(out=y[b][:], in_=u[:, j, 2 + tt * 512:2 + (tt + 1) * 512],
                                                                   func=AF.Copy, scale=cw[:, j, 2:3]),
                     reads=ukeys + ['cv_cw'], writes=[yk])
                for kk in (1, 0):
                    k.op('dve', lambda e, b=b, j=j, tt=tt, kk=kk: e.scalar_tensor_tensor(
                        out=y[b][:], in0=u[:, j, kk + tt * 512:kk + (tt + 1) * 512], scalar=cw[:, j, kk:kk + 1], in1=y[b][:],
                        op0=ALU.mult, op1=ALU.add), reads=ukeys + ['cv_cw', yk], writes=[yk])
                k.op('dve', lambda e, b=b, j=j, ts=ts: e.tensor_tensor(out=y[b][:], in0=y[b][:], in1=bg[:, j, ts], op=ALU.mult),
                     reads=[yk, 'cv_bg%d_%d' % (j, tt)], writes=[yk])
                head_rmsnorm_fm(g, y[b][:], yk, sq[b][:], 'cv_sq%d' % b, g.onorm[:, j:j + 1], ['onorm'], yo[ob][:, j, :],
                                'cv_yo%d_%d' % (ob, j), 6 + b, tf[b][:], 'cv_tf%d' % b)
            k.dma('sp', ycv[:, 2:4, ts], yo[ob][:], reads=['cv_yo%d_0' % ob, 'cv_yo%d_1' % ob], writes=['ycat_B%d' % tt])
        k.barrier()


def phase_wout(g, l, xT_old, xT_new):
    k = g.k
    ycv = g.ycatT.rearrange('(c p) t -> p c t', p=128)
    xo = xT_old.rearrange('(c p) t -> p c t', p=128)
    xn = xT_new.rearrange('(c p) t -> p c t', p=128)
    with contextlib.ExitStack() as es:
        wo = g.sb('wo_w', [128, 8, D], BF16, es)
        load_w_bf16(g, wo[:], 'wo_w', g.W['w_out'][l])
        yc = [g.sb('wo_yc%d' % i, [128, 8, 512], BF16, es) for i in range(2)]
        xt = [g.sb('wo_x%d' % i, [128, 8, 512], F32, es) for i in range(2)]
        n = 0
        for tt in range(8):
            b = tt % 2
            ts = slice(tt * 512, (tt + 1) * 512)
            k.dma('sp', yc[b][:], ycv[:, :, ts], writes=['wo_yc%d' % b])
            k.dma('sp', xt[b][:], xo[:, :, ts], writes=['wo_x%d' % b])
            for dc in range(8):
                pi = n % 4
                n += 1
                for c in range(8):
                    k.op('pe', lambda e, b=b, c=c, dc=dc, pi=pi: e.matmul(g.ps[pi][:], lhsT=wo[:, c, dc * 128:(dc + 1) * 128],
                                                                         rhs=yc[b][:, c, :], start=(c == 0), stop=(c == 7)),
                         reads=['wo_w', 'wo_yc%d' % b], writes=[g.psk[pi]])
                k.op('dve', lambda e, b=b, dc=dc, pi=pi: e.scalar_tensor_tensor(
                    out=xt[b][:, dc, :], in0=g.ps[pi][:], scalar=g.mod[:, 16 + dc:17 + dc], in1=xt[b][:, dc, :],
                    op0=ALU.mult, op1=ALU.add), reads=[g.psk[pi], 'mod', 'wo_x%d' % b], writes=['wo_x%d' % b])
            k.dma('sp', xn[:, :, ts], xt[b][:], reads=['wo_x%d' % b], writes=['xTn_%d' % tt])
        t = tap(g, 'x1T%d' % l, [128, 8, S])
        if t is not None:
            k.barrier()
            k.dma('sp', t, xn, writes=['tap'])
        k.barrier()


def phase_norm2_router(g, l, xT1):
    k = g.k
    xv = xT1.rearrange('(c p) t -> p c t', p=128)
    h2v = g.h2Td.rearrange('(c p) t -> p c t', p=128)
    with contextlib.ExitStack() as es:
        gcol = g.sb('n2_g', [128, 8], F32, es)
        g2s = g.sb('n2_gs', [128, 8], F32, es)
        k.dma('sp', gcol[:], g.W['norm2_g'][l].rearrange('(k p) -> p k', p=128), writes=['n2_g'], allow_slow_non_contiguous=True)
        k.op('dve', lambda e: e.scalar_tensor_tensor(out=g2s[:], in0=g.mod[:, 32:40], scalar=1.0, in1=gcol[:],
                                                      op0=ALU.add, op1=ALU.mult), reads=['mod', 'n2_g'], writes=['n2_gs'])
        h2 = g.sb('n2_h', [128, 8, S], BF16, es)
        norm_tiles(g, es, xv, g2s, g.mod[:, 24:32], ['n2_gs', 'mod'], h2, 'n2_h', 'n2')
        for c in range(8):
            k.dma('sp', h2v[:, c, :], h2[:, c, :], reads=['n2_h%d' % c], writes=['h2Td%d' % c])
        t = tap(g, 'h2T%d' % l, [128, 8, S], BF16)
        if t is not None:
            k.dma('sp', t, h2[:], reads=['n2_h%d' % c for c in range(8)], writes=['tap'])
        wr = g.sb('rt_w', [128, 8, 32], BF16, es)
        load_w_bf16(g, wr[:], 'rt_w', g.W['w_router'][l])
        br = g.sb('rt_b', [1, 32], BF16, es)
        k.dma('pool', br[:], g.W['b_router'][l:l + 1, :], writes=['rt_b'])
        lg = [g.sb('rt_lg%d' % i, [128, 32], F32, es) for i in range(2)]
        m8 = [g.sb('rt_m8%d' % i, [128, 8], F32, es) for i in range(2)]
        nm = [g.sb('rt_nm%d' % i, [128, 1], F32, es) for i in range(2)]
        ex = [g.sb('rt_ex%d' % i, [128, 32], F32, es) for i in range(2)]
        mk = [g.sb('rt_mk%d' % i, [128, 32], F32, es) for i in range(2)]
        sm = [g.sb('rt_sm%d' % i, [128, 1], F32, es) for i in range(2)]
        Gt = [g.sb('rt_G%d' % i, [128, 32], F32, es) for i in range(2)]
        gtap = tap(g, 'G%d' % l, [S, 32])
        for t_ in range(NT):
            b = t_ % 2
            pi = t_ % 2
            tk = slice(t_ * 128, (t_ + 1) * 128)
            for c in range(8):
                k.op('pe', lambda e, c=c, tk=tk, pi=pi: e.matmul(g.ps[pi][:, 0:32], lhsT=h2[:, c, tk], rhs=wr[:, c, :],
                                                               start=(c == 0), stop=False),
                     reads=['n2_h%d' % c, 'rt_w'], writes=[g.psk[pi]])
            k.op('pe', lambda e, pi=pi: e.matmul(g.ps[pi][:, 0:32], lhsT=g.cs['ones_b'][0:1, :], rhs=br[:], start=False, stop=True),
                 reads=['c_ones_b', 'rt_b'], writes=[g.psk[pi]])
            s = str(b)
            k.op('act', lambda e, b=b, pi=pi: e.copy(out=lg[b][:], in_=g.ps[pi][:, 0:32]), reads=[g.psk[pi]], writes=['rt_lg' + s])
            k.op('dve', lambda e, b=b: e.max(out=m8[b][:], in_=lg[b][:]), reads=['rt_lg' + s], writes=['rt_m8' + s])
            k.op('dve', lambda e, b=b: e.tensor_scalar(out=nm[b][:], in0=m8[b][:, 0:1], scalar1=-1.0, scalar2=None, op0=ALU.mult),
                 reads=['rt_m8' + s], writes=['rt_nm' + s])
            k.op('act', lambda e, b=b: e.activation(out=ex[b][:], in_=lg[b][:], func=AF.Exp, bias=nm[b][:], scale=1.0),
                 reads=['rt_lg' + s, 'rt_nm' + s], writes=['rt_ex' + s])
            k.op('dve', lambda e, b=b: e.tensor_scalar(out=mk[b][:], in0=lg[b][:], scalar1=m8[b][:, 3:4], scalar2=None, op0=ALU.is_ge),
                 reads=['rt_lg' + s, 'rt_m8' + s], writes=['rt_mk' + s])
            k.op('dve', lambda e, b=b: e.tensor_tensor(out=ex[b][:], in0=ex[b][:], in1=mk[b][:], op=ALU.mult),
                 reads=['rt_ex' + s, 'rt_mk' + s], writes=['rt_ex' + s])
            k.op('dve', lambda e, b=b: e.reduce_sum(out=sm[b][:], in_=ex[b][:], axis=AX.X), reads=['rt_ex' + s], writes=['rt_sm' + s])
            k.op('dve', lambda e, b=b: e.reciprocal(out=sm[b][:], in_=sm[b][:]), reads=['rt_sm' + s], writes=['rt_sm' + s])
            k.op('dve', lambda e, b=b: e.tensor_scalar(out=Gt[b][:], in0=ex[b][:], scalar1=sm[b][:], scalar2=None, op0=ALU.mult),
                 reads=['rt_ex' + s, 'rt_sm' + s], writes=['rt_G' + s])
            if gtap is not None:
                k.dma('sp', gtap[tk, :], Gt[b][:], reads=['rt_G' + s], writes=['tap'])
            pj = 2 + t_ % 2
            k.op('pe', lambda e, b=b, pj=pj: e.transpose(out=g.ps[pj][0:32, 0:128], in_=Gt[b][:], identity=g.cs['ident_f'][:]),
                 reads=['rt_G' + s, 'c_ident_f'], writes=[g.psk[pj]])
            k.op('act', lambda e, tk=tk, pj=pj: e.copy(out=g.GT[:, tk], in_=g.ps[pj][0:32, 0:128]), reads=[g.psk[pj]],
                 writes=['GT%d' % (t_ // 4)])
        k.barrier()


def phase_moe(g, l, xT1, xT2, final):
    k = g.k
    x1v = xT1.rearrange('(c p) t -> p c t', p=128)
    x2v = xT2.rearrange('(c p) t -> p c t', p=128)
    h2v = g.h2Td.rearrange('(c p) t -> p c t', p=128)
    NE = 32 if g.do_moe else 0
    QT = 1024
    with contextlib.ExitStack() as es:
        bgc = g.sb('me_bgc', [128, 16, 32], F32, es)
        bdr = g.sb('me_bdr', [32, D], BF16, es)
        k.dma('pool', bdr[:], g.W['b_down'][l], writes=['me_bdr'])
        with contextlib.ExitStack() as es2:
            bgr = g.sb('me_bgr', [32, 2048], F32, es2)
            k.dma('sp', bgr[:], g.W['b_gu'][l], writes=['me_bgr'])
            for j in range(16):
                pi = j % 2
                k.op('pe', lambda e, j=j, pi=pi: e.transpose(out=g.ps[pi][:, 0:32], in_=bgr[:, j * 128:(j + 1) * 128],
                                                             identity=g.cs['ident_f'][0:32, 0:32]),
                     reads=['me_bgr', 'c_ident_f'], writes=[g.psk[pi]])
                k.op('act', lambda e, j=j, pi=pi: e.copy(out=bgc[:, j, :], in_=g.ps[pi][:, 0:32]), reads=[g.psk[pi]], writes=['me_bgc'])
            k.barrier()
        wgu = [g.sb('me_wgu%d' % i, [128, 8, 2048], BF16, es) for i in range(2)]
        wdn = [g.sb('me_wdn%d' % i, [128, 8, D], BF16, es) for i in range(2)]
        h2q = g.sb('me_h2', [128, 8, QT], BF16, es)
        yacc = g.sb('me_yacc', [128, 8, QT], F32, es)
        act = [g.sb('me_act%d' % i, [128, 8, 512], BF16, es) for i in range(2)]
        gbc = [g.sb('me_gbc%d' % i, [128, 512], BF16, es) for i in range(2)]
        NSET = 3
        gq = [g.sb('me_gq%d' % i, [128, 512], F32, es) for i in range(NSET)]
        sg = [g.sb('me_sg%d' % i, [128, 512], F32, es) for i in range(NSET)]
        uq = [g.sb('me_uq%d' % i, [128, 512], F32, es) for i in range(NSET)]
        k.op('dve', lambda e: e.tensor_scalar(out=bgc[:, 8:16, :], in0=bgc[:, 8:16, :], scalar1=1.0, scalar2=None, op0=ALU.add),
             reads=['me_bgc'], writes=['me_bgc'])
        nf = 0
        xrow = [g.sb('me_xr%d' % i, [128, D], F32, es) for i in range(1)] if final else None
        nw = 0
        nit = 0
        npsum = 0
        for q in range(S // QT):
            qs = slice(q * QT, (q + 1) * QT)
            k.dma('sp', h2q[:], h2v[:, :, qs], reads=['h2Td%d' % c for c in range(8)], writes=['me_h2'])
            for e_ in range(NE):
                wb = nw % 2
                nw += 1
                gk, dk = 'me_wgu%d' % wb, 'me_wdn%d' % wb
                load_w_bf16(g, wgu[wb][:], gk, g.W['w_gu'][l, e_])
                load_w_bf16(g, wdn[wb][:], dk, g.W['w_down'][l, e_])
                for tl in range(QT // 512):
                    ts = slice(tl * 512, (tl + 1) * 512)
                    gts = slice(q * QT + tl * 512, q * QT + (tl + 1) * 512)
                    ab = nit % 2
                    nit += 1
                    ak = 'me_act%d' % ab
                    pg = 7
                    k.op('pe', lambda e, e_=e_, gts=gts, pg=pg: e.matmul(g.ps[pg][:], lhsT=g.cs['sel_b'][:, e_, :], rhs=g.GT[:, gts],
                                                                        start=True, stop=True),
                         reads=['c_sel_b'] + ['GT%d' % i for i in range(8)], writes=[g.psk[pg]])
                    k.op('act', lambda e, ab=ab, pg=pg: e.copy(out=gbc[ab][:], in_=g.ps[pg][:]), reads=[g.psk[pg]], writes=['me_gbc%d' % ab])
                    for f in range(8):
                        fb = nf % NSET
                        nf += 1
                        pa, pb = 2 * fb, 2 * fb + 1
                        for c in range(8):
                            k.op('pe', lambda e, c=c, f=f, wb=wb, ts=ts, pa=pa: e.matmul(
                                g.ps[pa][:], lhsT=wgu[wb][:, c, f * 128:(f + 1) * 128], rhs=h2q[:, c, ts], start=(c == 0), stop=(c == 7)),
                                reads=[gk, 'me_h2'], writes=[g.psk[pa]])
                        for c in range(8):
                            k.op('pe', lambda e, c=c, f=f, wb=wb, ts=ts, pb=pb: e.matmul(
                                g.ps[pb][:], lhsT=wgu[wb][:, c, 1024 + f * 128:1024 + (f + 1) * 128], rhs=h2q[:, c, ts],
                                start=(c == 0), stop=(c == 7)), reads=[gk, 'me_h2'], writes=[g.psk[pb]])
                        fs = str(fb)
                        k.op('dve', lambda e, f=f, fb=fb, pa=pa, e_=e_: e.tensor_scalar(
                            out=gq[fb][:], in0=g.ps[pa][:], scalar1=bgc[:, f, e_:e_ + 1], scalar2=7.0, op0=ALU.add, op1=ALU.min),
                            reads=[g.psk[pa], 'me_bgc'], writes=['me_gq' + fs])
                        k.op('act', lambda e, fb=fb: e.activation(out=sg[fb][:], in_=gq[fb][:], func=AF.Sigmoid, scale=1.702),
                             reads=['me_gq' + fs], writes=['me_sg' + fs])
                        k.op('dve', lambda e, f=f, fb=fb, pb=pb, e_=e_: e.tensor_scalar(
                            out=uq[fb][:], in0=g.ps[pb][:], scalar1=bgc[:, 8 + f, e_:e_ + 1], scalar2=-6.0, op0=ALU.add, op1=ALU.max),
                            reads=[g.psk[pb], 'me_bgc'], writes=['me_uq' + fs])
                        k.op('pool', lambda e, fb=fb: e.tensor_tensor(out=sg[fb][:], in0=sg[fb][:], in1=gq[fb][:], op=ALU.mult),
                             reads=['me_sg' + fs, 'me_gq' + fs], writes=['me_sg' + fs])
                        k.op('dve', lambda e, fb=fb: e.scalar_tensor_tensor(out=uq[fb][:], in0=uq[fb][:], scalar=8.0, in1=sg[fb][:],
                                                                            op0=ALU.min, op1=ALU.mult),
                             reads=['me_sg' + fs, 'me_uq' + fs], writes=['me_uq' + fs])
                        k.op('pool', lambda e, f=f, fb=fb, ab=ab: e.tensor_tensor(out=act[ab][:, f, :], in0=uq[fb][:], in1=gbc[ab][:], op=ALU.mult),
                             reads=['me_uq' + fs, 'me_gbc%d' % ab], writes=[ak])
                    for dc in range(8):
                        pd = 6 + dc % 2
                        for f in range(8):
                            k.op('pe', lambda e, f=f, dc=dc, wb=wb, ab=ab, pd=pd: e.matmul(
                                g.ps[pd][:], lhsT=wdn[wb][:, f, dc * 128:(dc + 1) * 128], rhs=act[ab][:, f, :],
                                start=(f == 0), stop=(f == 7 and e_ != 0)), reads=[dk, ak], writes=[g.psk[pd]])
                        if e_ == 0:
                            k.op('pe', lambda e, dc=dc, gts=gts, pd=pd: e.matmul(
                                g.ps[pd][:], lhsT=bdr[:, dc * 128:(dc + 1) * 128], rhs=g.GT[:, gts], start=False, stop=True),
                                reads=['me_bdr'] + ['GT%d' % i for i in range(8)], writes=[g.psk[pd]])
                            k.op('dve', lambda e, dc=dc, ts=ts, pd=pd: e.tensor_copy(yacc[:, dc, ts], g.ps[pd][:]),
                                 reads=[g.psk[pd]], writes=['me_yacc'])
                        else:
                            k.op('dve', lambda e, dc=dc, ts=ts, pd=pd: e.tensor_tensor(out=yacc[:, dc, ts], in0=yacc[:, dc, ts],
                                                                                    in1=g.ps[pd][:], op=ALU.add),
                                 reads=[g.psk[pd], 'me_yacc'], writes=['me_yacc'])
            if NE == 0:
                k.op('pool', lambda e: e.memset(yacc[:], 0.0), writes=['me_yacc'])
            for c in range(8):
                for tl in range(QT // 512):
                    ts = slice(tl * 512, (tl + 1) * 512)
                    gts = slice(q * QT + tl * 512, q * QT + (tl + 1) * 512)
                    fb = (c * 2 + tl) % 2
                    k.dma('sp', gq[fb][:], x1v[:, c, gts], writes=['me_gq%d' % fb])
                    k.op('dve', lambda e, c=c, ts=ts, fb=fb: e.scalar_tensor_tensor(
                        out=yacc[:, c, ts], in0=yacc[:, c, ts], scalar=g.mod[:, 40 + c:41 + c], in1=gq[fb][:], op0=ALU.mult, op1=ALU.add),
                        reads=['me_yacc', 'mod', 'me_gq%d' % fb], writes=['me_yacc'])
            if not final:
                k.dma('sp', x2v[:, :, qs], yacc[:], reads=['me_yacc'], writes=['xT2_%d' % q])
            else:
                for t_ in range(QT // 128):
                    tg = q * (QT // 128) + t_
                    ob = 0
                    for h in range(2):
                        pi = (2 * t_ + h) % 4
                        for j in range(4):
                            cc = h * 4 + j
                            k.op('pe', lambda e, cc=cc, j=j, pi=pi, t_=t_: e.transpose(
                                out=g.ps[pi][:, j * 128:(j + 1) * 128], in_=yacc[:, cc, t_ * 128:(t_ + 1) * 128],
                                identity=g.cs['ident_f'][:]), reads=['me_yacc', 'c_ident_f'], writes=[g.psk[pi]])
                        k.op('act' if h == 0 else 'dve',
                             (lambda e, pi=pi, ob=ob, h=h: e.copy(out=xrow[ob][:, h * 512:(h + 1) * 512], in_=g.ps[pi][:])) if h == 0 else
                             (lambda e, pi=pi, ob=ob, h=h: e.tensor_copy(xrow[ob][:, h * 512:(h + 1) * 512], g.ps[pi][:])),
                             reads=[g.psk[pi]], writes=['me_xr%d_%d' % (ob, h)])
                    k.dma('sp', g.out[tg * 128:(tg + 1) * 128, :], xrow[ob][:], reads=['me_xr%d_0' % ob, 'me_xr%d_1' % ob], writes=['out'])
            tp = tap(g, 'x2T%d_q%d' % (l, q), [128, 8, QT])
            if tp is not None:
                k.dma('sp', tp, yacc[:], reads=['me_yacc'], writes=['tap'])
        k.barrier()


def zero_missing(g):
    k = g.k
    miss = [i for i, m in enumerate('ABCD') if m not in g.mixers]
    if not miss:
        return
    with contextlib.ExitStack() as es:
        z = g.sb('zz', [128, S], BF16, es)
        k.op('pool', lambda e: e.memset(z[:], 0.0), writes=['zz'])
        for i in miss:
            for j in range(2):
                r0 = i * 256 + j * 128
                k.dma('sp', g.ycatT[r0:r0 + 128, :], z[:], reads=['zz'], writes=['ycat_z'])
        k.barrier()


def load_const(g, es, name):
    ap = g.C[name]
    t = g.sb('c_' + name, ap.shape, ap.dtype, es)
    g.k.dma('sp', t[:], ap, writes=['c_' + name])
    return t


def rope_tables(g, es):
    k = g.k
    cos = g.sb('rp_cos', [128, S], F32, es)
    sin = g.sb('rp_sin', [128, S], F32, es)
    invf = g.cs['invf']
    CH = 1024
    with contextlib.ExitStack() as es2:
        posi = g.sb('rp_pi', [128, CH], I32, es2)
        ang = g.sb('rp_ang', [128, CH], F32, es2)
        tq = g.sb('rp_t', [128, CH], F32, es2)
        ti = g.sb('rp_ti', [128, CH], I32, es2)
        r = g.sb('rp_r', [128, CH], F32, es2)
        m = g.sb('rp_m', [128, CH], F32, es2)
        for ch in range(S // CH):
            cs_ = slice(ch * CH, (ch + 1) * CH)
            k.dma('sp', posi[:], g.pos[cs_].partition_broadcast(128), writes=['rp_pi'])
            k.op('dve', lambda e: e.tensor_copy(ang[:], posi[:]), reads=['rp_pi'], writes=['rp_ang'])
            k.op('dve', lambda e: e.tensor_scalar(out=ang[:], in0=ang[:], scalar1=invf[:, 0:1], scalar2=None, op0=ALU.mult),
                 reads=['rp_ang', 'c_invf'], writes=['rp_ang'])
            for which, dst, dkey in ((0, sin, 'rp_sin'), (1, cos, 'rp_cos')):
                shift = 0.0 if which == 0 else PI / 2
                k.op('dve', lambda e, shift=shift: e.tensor_scalar(out=tq[:], in0=ang[:], scalar1=shift, scalar2=1.0 / (2 * PI),
                                                                    op0=ALU.add, op1=ALU.mult), reads=['rp_ang'], writes=['rp_t'])
                k.op('dve', lambda e: e.tensor_copy(ti[:], tq[:]), reads=['rp_t'], writes=['rp_ti'])
                k.op('dve', lambda e: e.tensor_copy(tq[:], ti[:]), reads=['rp_ti'], writes=['rp_t'])
                k.op('dve', lambda e: e.scalar_tensor_tensor(out=r[:], in0=tq[:], scalar=-2 * PI, in1=ang[:], op0=ALU.mult, op1=ALU.add),
                     reads=['rp_t', 'rp_ang'], writes=['rp_r'])
                if shift != 0.0:
                    k.op('dve', lambda e, shift=shift: e.tensor_scalar(out=r[:], in0=r[:], scalar1=shift, scalar2=None, op0=ALU.add),
                         reads=['rp_r'], writes=['rp_r'])
                k.op('dve', lambda e: e.tensor_scalar(out=m[:], in0=r[:], scalar1=PI, scalar2=-2 * PI, op0=ALU.is_gt, op1=ALU.mult),
                     reads=['rp_r'], writes=['rp_m'])
                k.op('dve', lambda e: e.tensor_tensor(out=r[:], in0=r[:], in1=m[:], op=ALU.add), reads=['rp_r', 'rp_m'], writes=['rp_r'])
                k.op('dve', lambda e: e.tensor_scalar(out=m[:], in0=r[:], scalar1=-PI, scalar2=2 * PI, op0=ALU.is_lt, op1=ALU.mult),
                     reads=['rp_r'], writes=['rp_m'])
                k.op('dve', lambda e: e.tensor_tensor(out=r[:], in0=r[:], in1=m[:], op=ALU.add), reads=['rp_r', 'rp_m'], writes=['rp_r'])
                k.op('act', lambda e, dst=dst, cs_=cs_: e.activation(out=dst[:, cs_], in_=r[:], func=AF.Sin), reads=['rp_r'],
                     writes=[dkey])
        k.barrier()
    return cos, sin


class NormRope:
    def __init__(self, g, es, pfx):
        self.g = g
        self.pfx = pfx
        self.n = 0
        mk = lambda nm, dt: [g.sb('%s_%s%d' % (pfx, nm, i), [128, 512], dt, es) for i in range(2)]
        self.xf = mk('xf', F32)
        self.sq = mk('sq', BF16)
        self.rs = mk('rs', F32)
        self.xn = mk('xn', F32)
        self.xb = mk('xb', BF16)
        self.t1 = mk('t1', F32)

    def run(self, ps, pskey, gcol, gkeys, ts, cos=None, sin=None, out_r=None, okr=None, out_n=None, okn=None, pbank=2):
        g, k, pfx = self.g, self.g.k, self.pfx
        b = self.n % 2
        self.n += 1
        K_ = lambda nm: '%s_%s%d' % (pfx, nm, b)
        xf, sq, rs, xn, xb, t1 = self.xf[b], self.sq[b], self.rs[b], self.xn[b], self.xb[b], self.t1[b]
        k.op('act', lambda e: e.copy(out=xf[:], in_=ps), reads=[pskey], writes=[K_('xf')])
        k.op('act', lambda e: e.activation(out=sq[:], in_=xf[:], func=AF.Square), reads=[K_('xf')], writes=[K_('sq')])
        pi = pbank + b
        k.op('pe', lambda e: e.matmul(g.ps[pi][:], lhsT=g.cs['blk64_b'][:], rhs=sq[:], start=True, stop=True),
             reads=[K_('sq'), 'c_blk64_b'], writes=[g.psk[pi]])
        k.op('dve', lambda e: e.tensor_scalar(out=rs[:], in0=g.ps[pi][:], scalar1=1.0 / 64, scalar2=NORM_EPS, op0=ALU.mult, op1=ALU.add),
             reads=[g.psk[pi]], writes=[K_('rs')])
        k.op('act', lambda e: e.activation(out=rs[:], in_=rs[:], func=AF.Sqrt), reads=[K_('rs')], writes=[K_('rs')])
        k.op('dve', lambda e: e.reciprocal(out=rs[:], in_=rs[:]), reads=[K_('rs')], writes=[K_('rs')])
        k.op('dve', lambda e: e.scalar_tensor_tensor(out=xn[:], in0=xf[:], scalar=gcol, in1=rs[:], op0=ALU.mult, op1=ALU.mult),
             reads=[K_('xf'), K_('rs')] + gkeys, writes=[K_('xn')])
        if out_n is not None:
            k.op('pool', lambda e: e.tensor_copy(out_n, xn[:]), reads=[K_('xn')], writes=[okn])
        if out_r is not None:
            k.op('act', lambda e: e.copy(out=xb[:], in_=xn[:]), reads=[K_('xn')], writes=[K_('xb')])
            k.op('pe', lambda e: e.matmul(g.ps[pi][:], lhsT=g.cs['prot_b'][:], rhs=xb[:], start=True, stop=True),
                 reads=[K_('xb'), 'c_prot_b'], writes=[g.psk[pi]])
            k.op('dve', lambda e: e.tensor_tensor(out=t1[:], in0=g.ps[pi][:], in1=sin[:, ts], op=ALU.mult),
                 reads=[g.psk[pi], 'rp_sin'], writes=[K_('t1')])
            k.op('pool', lambda e: e.tensor_tensor(out=xn[:], in0=xn[:], in1=cos[:, ts], op=ALU.mult),
                 reads=[K_('xn'), 'rp_cos'], writes=[K_('xn')])
            k.op('dve', lambda e: e.tensor_tensor(out=out_r, in0=xn[:], in1=t1[:], op=ALU.add),
                 reads=[K_('xn'), K_('t1')], writes=[okr])


def attention(g, es, pfx, heads, ngroups, ktiles, qT, kT, vfn, ncols, masks, epilogue, scale=0.125):
    k = g.k
    pt = [g.sb('%s_pt%d' % (pfx, i), [128, 512], BF16, es) for i in range(3)]
    n = 0
    for h in heads:
        for gi in range(ngroups):
            kts = ktiles(gi)
            if not kts:
                continue
            for idx, kt in enumerate(kts):
                sbk = n % 2
                pb = n % 3
                n += 1
                qa, qk = qT(h, gi)
                ka, kk, nk = kT(h, kt)
                k.op('pe', lambda e, sbk=sbk, ka=ka, qa=qa, nk=nk: e.matmul(g.ps[sbk][0:nk, :], lhsT=ka, rhs=qa, start=True, stop=True),
                     reads=qk + kk, writes=[g.psk[sbk]])
                ptk = '%s_pt%d' % (pfx, pb)
                k.op('act', lambda e, sbk=sbk, pb=pb, nk=nk: e.activation(out=pt[pb][0:nk, :], in_=g.ps[sbk][0:nk, :], func=AF.Exp, scale=scale),
                     reads=[g.psk[sbk]], writes=[ptk])
                for (ma, mk_, is_ps) in masks(h, gi, kt):
                    eng = 'dve' if (is_ps or n % 2 == 0) else 'pool'
                    k.op(eng, lambda e, pb=pb, ma=ma, nk=nk: e.tensor_tensor(out=pt[pb][0:nk, :], in0=pt[pb][0:nk, :], in1=ma[0:nk, :], op=ALU.mult),
                         reads=[ptk] + mk_, writes=[ptk])
                va, vk = vfn(h, kt)
                for sub in range(4):
                    k.op('pe', lambda e, sub=sub, pb=pb, va=va, nk=nk, idx=idx: e.matmul(
                        g.ps[4 + sub][:, 0:ncols], lhsT=pt[pb][0:nk, sub * 128:(sub + 1) * 128], rhs=va,
                        start=(idx == 0), stop=(idx == len(kts) - 1)), reads=[ptk] + vk, writes=[g.psk[4 + sub]])
            for sub in range(4):
                epilogue(h, gi, sub, g.ps[4 + sub], g.psk[4 + sub])


def finish_tm(g, es, pfx, ytm, ykeyfn, gb, gbkey, row0):
    k = g.k
    ycv = g.ycatT.rearrange('(c p) t -> p c t', p=128)
    sq = [g.sb(pfx + '_fsq%d' % i, [128, 256], F32, es) for i in range(2)]
    ss = [g.sb(pfx + '_fss%d' % i, [128, 4], F32, es) for i in range(2)]
    yb = [g.sb(pfx + '_fyb%d' % i, [128, 256], BF16, es) for i in range(2)]
    yT = [g.sb(pfx + '_fyT%d' % i, [128, 2, 128], BF16, es) for i in range(2)]
    psb = [g.ps[2][:].bitcast(BF16), g.ps[3][:].bitcast(BF16)]
    for t_ in range(NT):
        b = t_ % 2
        s_ = str(b)
        yk = ykeyfn(t_)
        k.op('pool', lambda e, b=b, t_=t_: e.tensor_tensor(out=sq[b][:], in0=ytm[:, t_, :], in1=ytm[:, t_, :], op=ALU.mult),
             reads=yk, writes=[pfx + '_fsq' + s_])
        k.op('dve', lambda e, b=b: e.reduce_sum(out=ss[b][:], in_=sq[b][:].rearrange('p (h d) -> p h d', d=64), axis=AX.X),
             reads=[pfx + '_fsq' + s_], writes=[pfx + '_fss' + s_])
        k.op('dve', lambda e, b=b: e.tensor_scalar(out=ss[b][:], in0=ss[b][:], scalar1=1.0 / 64, scalar2=NORM_EPS, op0=ALU.mult, op1=ALU.add),
             reads=[pfx + '_fss' + s_], writes=[pfx + '_fss' + s_])
        k.op('act', lambda e, b=b: e.activation(out=ss[b][:], in_=ss[b][:], func=AF.Sqrt), reads=[pfx + '_fss' + s_], writes=[pfx + '_fss' + s_])
        k.op('dve', lambda e, b=b: e.reciprocal(out=ss[b][:], in_=ss[b][:]), reads=[pfx + '_fss' + s_], writes=[pfx + '_fss' + s_])
        for h in range(4):
            k.op('dve', lambda e, b=b, h=h, t_=t_: e.scalar_tensor_tensor(
                out=yb[b][:, h * 64:(h + 1) * 64], in0=ytm[:, t_, h * 64:(h + 1) * 64], scalar=ss[b][:, h:h + 1],
                in1=gb[:, h * 64:(h + 1) * 64], op0=ALU.mult, op1=ALU.mult),
                reads=yk + [pfx + '_fss' + s_, gbkey], writes=[pfx + '_fyb' + s_])
        for j in range(2):
            k.op('pe', lambda e, b=b, j=j: e.transpose(out=psb[b][:, j * 128:(j + 1) * 128], in_=yb[b][:, j * 128:(j + 1) * 128],
                                                       identity=g.cs['ident_b'][:]),
                 reads=[pfx + '_fyb' + s_, 'c_ident_b'], writes=[g.psk[2 + b]])
        k.op('act', lambda e, b=b: e.copy(out=yT[b][:], in_=psb[b][:, 0:256].rearrange('p (j t) -> p j t', j=2)),
             reads=[g.psk[2 + b]], writes=[pfx + '_fyT' + s_])
        c0 = row0 // 128
        k.dma('sp', ycv[:, c0:c0 + 2, t_ * 128:(t_ + 1) * 128], yT[b][:], reads=[pfx + '_fyT' + s_], writes=['ycat_%s%d' % (pfx, t_)])


def mixer_dil(g, l):
    k = g.k
    with contextlib.ExitStack() as es:
        qT = g.sb('dl_qT', [128, 2, S], BF16, es)
        kT = g.sb('dl_kT', [128, 2, S], BF16, es)
        V = g.sb('dl_V', [128, NT, 4, 65], BF16, es)
        ytm = g.sb('dl_ytm', [128, NT, 256], F32, es)
        gb = g.sb('dl_gb', [128, 256], F32, es)
        k.dma('sp', gb[:], g.W['onorm_g'][l, 256:512].partition_broadcast(128), writes=['dl_gb'])
        k.op('pool', lambda e: e.memset(V[:, :, :, 64:65], 1.0), writes=['dl_Vone'])
        with contextlib.ExitStack() as es2:
            cos, sin = rope_tables(g, es2)
            w = g.sb('dl_w', [128, 8, 768], BF16, es2)
            load_w_bf16(g, w[:], 'dl_w', g.W['w_in'][l][:, 1792:2560])
            gq = g.sb('dl_gq', [128, 1], F32, es2)
            gk = g.sb('dl_gk', [128, 1], F32, es2)
            for hh in range(2):
                k.dma('sp', gq[hh * 64:(hh + 1) * 64, :], g.W['dil_q_g'][l].rearrange('(p o) -> p o', o=1), writes=['dl_gq'],
                      allow_slow_non_contiguous=True)
                k.dma('sp', gk[hh * 64:(hh + 1) * 64, :], g.W['dil_k_g'][l].rearrange('(p o) -> p o', o=1), writes=['dl_gk'],
                      allow_slow_non_contiguous=True)
            nr = NormRope(g, es2, 'dl')
            hload = ht_loader(g, es2, 'dl')
            n = 0
            for tt in range(8):
                ts = slice(tt * 512, (tt + 1) * 512)
                ht, hk = hload(tt)
                for j in range(2):
                    for (dst, dkey, c0, gcol, gkey) in ((qT, 'dl_qT', 0, gq, 'dl_gq'), (kT, 'dl_kT', 256, gk, 'dl_gk')):
                        pi = n % 2
                        n += 1
                        proj_fm(g, w, 'dl_w', c0 + j * 128, 128, ht, hk, g.ps[pi][:], g.psk[pi])
                        nr.run(g.ps[pi][:], g.psk[pi], gcol[:, 0:1], [gkey], ts, cos, sin, dst[:, j, ts], '%s%d_%d' % (dkey, j, tt))
                for sub in range(4):
                    pi = 4 + sub
                    t_ = tt * 4 + sub
                    proj_tm(g, w, 'dl_w', 512, 256, ht, hk, sub, g.ps[pi][:, 0:256], g.psk[pi])
                    k.op('act', lambda e, pi=pi, t_=t_: e.copy(out=V[:, t_, :, 0:64], in_=g.ps[pi][:, 0:256].rearrange('p (h d) -> p h d', d=64)),
                         reads=[g.psk[pi]], writes=['dl_V%d' % t_])
            k.barrier()
        msk = load_const(g, es, 'dil_mask')
        rd = [g.sb('dl_rd%d' % i, [128, 1], F32, es) for i in range(2)]
        cnt = [0]

        def q_of(h, gi):
            j, hp = h // 2, h % 2
            return qT[hp * 64:(hp + 1) * 64, j, gi * 512:(gi + 1) * 512], ['dl_qT%d_%d' % (j, gi)]

        def k_of(h, kt):
            j, hp = h // 2, h % 2
            return kT[hp * 64:(hp + 1) * 64, j, kt * 128:(kt + 1) * 128], ['dl_kT%d_%d' % (j, kt // 4)], 128

        def v_of(h, kt):
            return V[:, kt, h, :], ['dl_V%d' % kt, 'dl_Vone']

        def ktiles(gi):
            return [kt for kt in range(max(0, 4 * gi - 16), 4 * gi + 4)]

        def masks(h, gi, kt):
            return [(msk[:, (4 * gi - kt) + 3, :], ['c_dil_mask'], False)]

        def epi(h, gi, sub, acc, acck):
            b = cnt[0] % 2
            cnt[0] += 1
            t_ = gi * 4 + sub
            k.op('dve', lambda e: e.reciprocal(out=rd[b][:], in_=acc[:, 64:65]), reads=[acck], writes=['dl_rd%d' % b])
            k.op('dve', lambda e: e.tensor_scalar(out=ytm[:, t_, h * 64:(h + 1) * 64], in0=acc[:, 0:64], scalar1=rd[b][:, 0:1],
                                                   scalar2=None, op0=ALU.mult), reads=[acck, 'dl_rd%d' % b], writes=['dl_ytm%d_%d' % (t_, h)])

        attention(g, es, 'dl', range(4), 8, ktiles, q_of, k_of, v_of, 65, masks, epi)
        finish_tm(g, es, 'dl', ytm, lambda t_: ['dl_ytm%d_%d' % (t_, h) for h in range(4)], gb, 'dl_gb', 512)
        k.barrier()


NSA_STOP = None
NSA_SKIP = ''


def mixer_nsa(g, l):
    k = g.k
    W = g.W
    with contextlib.ExitStack() as es:
        qn = g.sb('ns_qn', [128, 2, S], BF16, es)
        qr = g.sb('ns_qr', [128, 2, S], BF16, es)
        ksT = g.sb('ns_ks', [128, S], BF16, es)
        kwT = g.sb('ns_kw', [128, S], BF16, es)
        kcvc = g.sb('ns_kcvc', [128, S], BF16, es)
        Vs = g.sb('ns_Vs', [128, NT, 66], BF16, es)
        Vw = g.sb('ns_Vw', [128, NT, 66], BF16, es)
        gate = g.sb('ns_gate', [128, NT, 12], F32, es)
        ytm = g.sb('ns_ytm', [128, NT, 256], F32, es)
        gb = g.sb('ns_gb', [128, 256], F32, es)
        k.dma('sp', gb[:], W['onorm_g'][l, 512:768].partition_broadcast(128), writes=['ns_gb'])
        k.op('pool', lambda e: e.memset(Vs[:, :, 64:65], 1.0), writes=['ns_Vs1'])
        k.op('pool', lambda e: e.memset(Vw[:, :, 64:65], 1.0), writes=['ns_Vw1'])
        with contextlib.ExitStack() as es2:
            cos, sin = rope_tables(g, es2)
            w = g.sb('ns_w', [128, 8, 652], BF16, es2)
            load_w_bf16(g, w[:], 'ns_w', W['w_in'][l][:, 2560:3212])
            wks = g.sb('ns_wks', [128, 8, 128], BF16, es2)
            wkw = g.sb('ns_wkw', [128, 8, 128], BF16, es2)
            for hh in range(2):
                load_w_bf16(g, wks[:, :, hh * 64:(hh + 1) * 64], 'ns_wks', W['w_in'][l][:, 2944:3008])
                load_w_bf16(g, wkw[:, :, hh * 64:(hh + 1) * 64], 'ns_wkw', W['w_in'][l][:, 3072:3136])
            gq = g.sb('ns_gq', [128, 1], F32, es2)
            gks = g.sb('ns_gks', [128, 1], F32, es2)
            gkw = g.sb('ns_gkw', [128, 1], F32, es2)
            for hh in range(2):
                for (dst, nm, key) in ((gq, 'nsa_q_g', 'ns_gq'), (gks, 'nsa_ks_g', 'ns_gks'), (gkw, 'nsa_kw_g', 'ns_gkw')):
                    k.dma('sp', dst[hh * 64:(hh + 1) * 64, :], W[nm][l].rearrange('(p o) -> p o', o=1), writes=[key],
                          allow_slow_non_contiguous=True)
            nr = NormRope(g, es2, 'ns')
            hload = ht_loader(g, es2, 'ns')
            n = 0
            for tt in range(8):
                ts = slice(tt * 512, (tt + 1) * 512)
                ht, hk = hload(tt)
                for j in range(2):
                    pi = n % 2
                    n += 1
                    proj_fm(g, w, 'ns_w', j * 128, 128, ht, hk, g.ps[pi][:], g.psk[pi])
                    nr.run(g.ps[pi][:], g.psk[pi], gq[:, 0:1], ['ns_gq'], ts, cos, sin, qr[:, j, ts], 'ns_qr%d_%d' % (j, tt),
                           None if 'a' in NSA_SKIP else qn[:, j, ts], 'ns_qn%d_%d' % (j, tt))
                for (wsb, wkey, gcol, gkey, dst, dkey) in ((wks, 'ns_wks', gks, 'ns_gks', ksT, 'ns_ks'), (wkw, 'ns_wkw', gkw, 'ns_gkw', kwT, 'ns_kw')):
                    if 'c' in NSA_SKIP:
                        continue
                    pi = n % 2
                    n += 1
                    proj_fm(g, wsb, wkey, 0, 128, ht, hk, g.ps[pi][:], g.psk[pi])
                    nr.run(g.ps[pi][:], g.psk[pi], gcol[:, 0:1], [gkey], ts, cos, sin, dst[:, ts], '%s_%d' % (dkey, tt))
                pi = n % 2
                n += 1
                proj_fm(g, w, 'ns_w', 256, 128, ht, hk, g.ps[pi][:], g.psk[pi])
                k.op('act', lambda e, pi=pi, ts=ts: e.copy(out=kcvc[:, ts], in_=g.ps[pi][:]), reads=[g.psk[pi]], writes=['ns_kcvc%d' % tt])
                for sub in range(4):
                    if 'b' in NSA_SKIP:
                        continue
                    pi = 4 + sub
                    t_ = tt * 4 + sub
                    proj_tm(g, w, 'ns_w', 448, 204, ht, hk, sub, g.ps[pi][:, 0:204], g.psk[pi])
                    k.op('act', lambda e, pi=pi, t_=t_: e.copy(out=Vs[:, t_, 0:64], in_=g.ps[pi][:, 0:64]), reads=[g.psk[pi]],
                         writes=['ns_Vs%d' % t_])
                    if 'e' not in NSA_SKIP:
                        k.op('dve', lambda e, pi=pi, t_=t_: e.tensor_copy(Vw[:, t_, 0:64], g.ps[pi][:, 128:192]), reads=[g.psk[pi]],
                             writes=['ns_Vw%d' % t_])
                    if 'd' not in NSA_SKIP:
                        k.op('act', lambda e, pi=pi, t_=t_: e.activation(out=gate[:, t_, :], in_=g.ps[pi][:, 192:204], func=AF.Sigmoid),
                             reads=[g.psk[pi]], writes=['ns_gate%d' % t_])
            k.barrier()
        if NSA_STOP == 'proj':
            return
        kcT = g.sb('ns_kcT', [128, 512], BF16, es)
        Vc = [g.sb('ns_Vc%d' % j, [128, 129], BF16, es) for j in range(2)]
        ovl1 = load_const(g, es, 'ovl1')
        with contextlib.ExitStack() as es3:
            W1 = g.sb('ns_W1', [128, 32, 256], BF16, es3)
            peT = g.sb('ns_peT', [128, 32], BF16, es3)
            W2k = g.sb('ns_W2k', [128, 2, 128], BF16, es3)
            W2v = g.sb('ns_W2v', [128, 2, 64], BF16, es3)
            gkc = g.sb('ns_gkc', [128, 1], F32, es3)
            for (pb, w1n, pen) in ((0, 'nsa_wk1', 'nsa_pe_k'), (64, 'nsa_wv1', 'nsa_pe_v')):
                k.dma('pool', W1[pb:pb + 64, :, :], W[w1n][l].rearrange('(l d) m -> d l m', d=64), writes=['ns_W1'])
                k.dma('pool', peT[pb:pb + 64, :], W[pen][l].rearrange('l d -> d l'), writes=['ns_peT'], allow_slow_non_contiguous=True)
            for hh in range(2):
                k.dma('pool', W2k[:, :, hh * 64:(hh + 1) * 64], W['nsa_wk2'][l].rearrange('(c p) d -> p c d', p=128), writes=['ns_W2k'])
                k.dma('sp', gkc[hh * 64:(hh + 1) * 64, :], W['nsa_kc_g'][l].rearrange('(p o) -> p o', o=1), writes=['ns_gkc'],
                      allow_slow_non_contiguous=True)
            k.dma('pool', W2v[:], W['nsa_wv2'][l].rearrange('(c p) d -> p c d', p=128), writes=['ns_W2v'])
            hid = {}
            bia = g.sb('ns_bia', [128, 4], F32, es3)
            xs = g.sb('ns_xs', [128, 256], F32, es3)
            x2 = g.sb('ns_x2', [128, 256], F32, es3)
            th = g.sb('ns_th', [128, 256], F32, es3)
            n = 0
            allkc = ['ns_kcvc%d' % i for i in range(8)]
            for kv, pb in (('k', 0), ('v', 64)):
                for mc in range(2):
                    hd_ = g.sb('ns_hid%s%d' % (kv, mc), [128, 256], BF16, es3)
                    hid[(kv, mc)] = hd_
                    hk_ = 'ns_hid%s%d' % (kv, mc)
                    k.op('pool', lambda e, hd_=hd_: e.memset(hd_[:], 0.0), writes=[hk_])
                    pi, pj = n % 2, 2 + n % 2
                    for l_ in range(32):
                        k.op('pe', lambda e, l_=l_, pb=pb, mc=mc, pi=pi: e.matmul(
                            g.ps[pi][:, 0:255], lhsT=W1[pb:pb + 64, l_, mc * 128:(mc + 1) * 128],
                            rhs=kcvc[pb:pb + 64, l_:l_ + 16 * 254 + 1:16], start=(l_ == 0), stop=(l_ == 31)),
                            reads=['ns_W1'] + allkc, writes=[g.psk[pi]])
                    for l_ in range(32):
                        k.op('pe', lambda e, l_=l_, pb=pb, mc=mc, pj=pj: e.matmul(
                            g.ps[pj][:, 0:1], lhsT=W1[pb:pb + 64, l_, mc * 128:(mc + 1) * 128], rhs=peT[pb:pb + 64, l_:l_ + 1],
                            start=(l_ == 0), stop=(l_ == 31)), reads=['ns_W1', 'ns_peT'], writes=[g.psk[pj]])
                    k.op('act', lambda e, n=n, pj=pj: e.copy(out=bia[:, n:n + 1], in_=g.ps[pj][:, 0:1]), reads=[g.psk[pj]], writes=['ns_bia'])
                    k.op('act', lambda e, n=n, pi=pi: e.activation(out=xs[:, 0:255], in_=g.ps[pi][:, 0:255], func=AF.Identity,
                                                                   bias=bia[:, n:n + 1], scale=1.0), reads=[g.psk[pi], 'ns_bia'], writes=['ns_xs'])
                    k.op('dve', lambda e: e.tensor_tensor(out=x2[:, 0:255], in0=xs[:, 0:255], in1=xs[:, 0:255], op=ALU.mult), reads=['ns_xs'], writes=['ns_x2'])
                    k.op('dve', lambda e: e.tensor_scalar(out=x2[:, 0:255], in0=x2[:, 0:255], scalar1=0.044715, scalar2=1.0, op0=ALU.mult, op1=ALU.add),
                         reads=['ns_x2'], writes=['ns_x2'])
                    k.op('dve', lambda e: e.tensor_tensor(out=x2[:, 0:255], in0=x2[:, 0:255], in1=xs[:, 0:255], op=ALU.mult), reads=['ns_x2', 'ns_xs'], writes=['ns_x2'])
                    k.op('act', lambda e: e.activation(out=th[:, 0:255], in_=x2[:, 0:255], func=AF.Tanh, scale=0.7978845608028654),
                         reads=['ns_x2'], writes=['ns_th'])
                    k.op('dve', lambda e: e.scalar_tensor_tensor(out=th[:, 0:255], in0=th[:, 0:255], scalar=1.0, in1=xs[:, 0:255], op0=ALU.add, op1=ALU.mult),
                         reads=['ns_th', 'ns_xs'], writes=['ns_th'])
                    k.op('act', lambda e, hd_=hd_: e.mul(out=hd_[:, 0:255], in_=th[:, 0:255], mul=0.5), reads=['ns_th'], writes=[hk_])
                    n += 1
            for mc in range(2):
                k.op('pe', lambda e, mc=mc: e.matmul(g.ps[0][:, 0:256], lhsT=W2k[:, mc, :], rhs=hid[('k', mc)][:], start=(mc == 0), stop=(mc == 1)),
                     reads=['ns_W2k', 'ns_hidk%d' % mc], writes=[g.psk[0]])
            nr2 = NormRope(g, es3, 'nc')
            nr2.run(g.ps[0][:], g.psk[0], gkc[:, 0:1], ['ns_gkc'], slice(0, 512), out_n=kcT[:], okn='ns_kcT')
            k.op('pool', lambda e: e.memset(kcT[:, 255:512], 0.0), reads=['ns_kcT'], writes=['ns_kcT'])
            for j in range(2):
                for mc in range(2):
                    k.op('pe', lambda e, j=j, mc=mc: e.matmul(g.ps[1][:, 0:64], lhsT=hid[('v', mc)][:, j * 128:(j + 1) * 128], rhs=W2v[:, mc, :],
                                                           start=(mc == 0), stop=(mc == 1)), reads=['ns_W2v', 'ns_hidv%d' % mc], writes=[g.psk[1]])
                k.op('act', lambda e, j=j: e.copy(out=Vc[j][:, 0:64], in_=g.ps[1][:, 0:64]), reads=[g.psk[1]], writes=['ns_Vc%d' % j])
                k.op('pool', lambda e, j=j: e.tensor_copy(Vc[j][:, 64:129], ovl1[:, j, :]), reads=['c_ovl1'], writes=['ns_Vc%d_o' % j])
            k.barrier()
        if NSA_STOP == 'cmpkv':
            return
        imp = g.sb('ns_imp', [128, NT, 64], F32, es)
        selT = g.sb('ns_selT', [64, S], BF16, es)
        rd = [g.sb('ns_rd%d' % i, [128, 1], F32, es) for i in range(2)]
        cf = [g.sb('ns_cf%d' % i, [128, 1], F32, es) for i in range(2)]
        cnt = [0]

        def q_of_n(h, gi):
            j, hp = h // 2, h % 2
            return qn[hp * 64:(hp + 1) * 64, j, gi * 512:(gi + 1) * 512], ['ns_qn%d_%d' % (j, gi)]

        def q_of_r(h, gi):
            j, hp = h // 2, h % 2
            return qr[hp * 64:(hp + 1) * 64, j, gi * 512:(gi + 1) * 512], ['ns_qr%d_%d' % (j, gi)]

        def make_epi(br, first):
            def epi(h, gi, sub, acc, acck):
                b = cnt[0] % 2
                cnt[0] += 1
                t_ = gi * 4 + sub
                rk, ck = 'ns_rd%d' % b, 'ns_cf%d' % b
                k.op('dve', lambda e: e.tensor_scalar(out=rd[b][:], in0=acc[:, 64:65], scalar1=1e-30, scalar2=None, op0=ALU.max),
                     reads=[acck], writes=[rk])
                k.op('dve', lambda e: e.reciprocal(out=rd[b][:], in_=rd[b][:]), reads=[rk], writes=[rk])
                k.op('dve', lambda e: e.tensor_tensor(out=cf[b][:], in0=rd[b][:], in1=gate[:, t_, h * 3 + br:h * 3 + br + 1], op=ALU.mult),
                     reads=[rk, 'ns_gate%d' % t_], writes=[ck])
                yk = 'ns_ytm%d_%d' % (t_, h)
                if first:
                    k.op('dve', lambda e: e.tensor_scalar(out=ytm[:, t_, h * 64:(h + 1) * 64], in0=acc[:, 0:64], scalar1=cf[b][:, 0:1],
                                                           scalar2=None, op0=ALU.mult), reads=[acck, ck], writes=[yk])
                    ik = 'ns_imp%d' % t_
                    if h == 0:
                        k.op('dve', lambda e: e.tensor_scalar(out=imp[:, t_, :], in0=acc[:, 65:129], scalar1=rd[b][:, 0:1], scalar2=None,
                                                               op0=ALU.mult), reads=[acck, rk], writes=[ik])
                    else:
                        k.op('dve', lambda e: e.scalar_tensor_tensor(out=imp[:, t_, :], in0=acc[:, 65:129], scalar=rd[b][:, 0:1], in1=imp[:, t_, :],
                                                                      op0=ALU.mult, op1=ALU.add), reads=[acck, rk, ik], writes=[ik])
                else:
                    k.op('dve', lambda e: e.scalar_tensor_tensor(out=ytm[:, t_, h * 64:(h + 1) * 64], in0=acc[:, 0:64], scalar=cf[b][:, 0:1],
                                                                  in1=ytm[:, t_, h * 64:(h + 1) * 64], op0=ALU.mult, op1=ALU.add),
                         reads=[acck, ck, yk], writes=[yk])
            return epi

        with contextlib.ExitStack() as es4:
            cmsk = load_const(g, es4, 'cmp_mask')

            def k_cmp(h, kt):
                hp = h % 2
                return kcT[hp * 64:(hp + 1) * 64, kt * 128:(kt + 1) * 128], ['ns_kcT'], 128

            def v_cmp(h, kt):
                return Vc[kt][:, :], ['ns_Vc%d' % kt, 'ns_Vc%d_o' % kt]

            def kt_cmp(gi):
                return [0] + ([1] if gi >= 4 else [])

            def m_cmp(h, gi, kt):
                i = gi - 4 * kt
                return [] if i >= 5 else [(cmsk[:, i, :], ['c_cmp_mask'], False)]

            attention(g, es4, 'nc', range(4), 8, kt_cmp, q_of_n, k_cmp, v_cmp, 129, m_cmp, make_epi(0, True))
            if NSA_STOP == 'cmpattn':
                k.barrier()
                return
            keep = load_const(g, es4, 'sel_keep')
            addc = load_const(g, es4, 'sel_add')
            sc = [g.sb('ns_sc%d' % i, [128, 64], F32, es4) for i in range(2)]
            wk2_ = [g.sb('ns_wk%d' % i, [128, 64], F32, es4) for i in range(2)]
            m8 = [g.sb('ns_m8%d' % i, [128, 8], F32, es4) for i in range(2)]
            sm = [g.sb('ns_sm%d' % i, [128, 64], BF16, es4) for i in range(2)]
            psb = [g.ps[2][:].bitcast(BF16), g.ps[3][:].bitcast(BF16)]
            for t_ in range(NT):
                b = t_ % 2
                s_ = str(b)
                k.op('dve', lambda e, b=b, t_=t_: e.tensor_tensor(out=sc[b][:], in0=imp[:, t_, :], in1=keep[:, t_, :], op=ALU.mult),
                     reads=['ns_imp%d' % t_, 'c_sel_keep'], writes=['ns_sc' + s_])
                k.op('dve', lambda e, b=b, t_=t_: e.tensor_tensor(out=sc[b][:], in0=sc[b][:], in1=addc[:, t_, :], op=ALU.add),
                     reads=['ns_sc' + s_, 'c_sel_add'], writes=['ns_sc' + s_])
                k.op('dve', lambda e, b=b: e.max(out=m8[b][:], in_=sc[b][:]), reads=['ns_sc' + s_], writes=['ns_m8' + s_])
                k.op('dve', lambda e, b=b: e.match_replace(out=wk2_[b][:], in_to_replace=m8[b][:], in_values=sc[b][:], imm_value=-1e30),
                     reads=['ns_sc' + s_, 'ns_m8' + s_], writes=['ns_wk' + s_])
                k.op('dve', lambda e, b=b: e.max(out=m8[b][:], in_=wk2_[b][:]), reads=['ns_wk' + s_], writes=['ns_m8' + s_])
                k.op('dve', lambda e, b=b: e.tensor_scalar(out=sm[b][:], in0=sc[b][:], scalar1=m8[b][:, 7:8], scalar2=None, op0=ALU.is_ge),
                     reads=['ns_sc' + s_, 'ns_m8' + s_], writes=['ns_sm' + s_])
                k.op('pe', lambda e, b=b: e.transpose(out=psb[b][0:64, 0:128], in_=sm[b][:], identity=g.cs['ident_b'][:]),
                     reads=['ns_sm' + s_, 'c_ident_b'], writes=[g.psk[2 + b]])
                k.op('act', lambda e, b=b, t_=t_: e.copy(out=selT[:, t_ * 128:(t_ + 1) * 128], in_=psb[b][0:64, 0:128]),
                     reads=[g.psk[2 + b]], writes=['ns_selT%d' % (t_ // 4)])
            tp = tap(g, 'selT%d' % l, [64, S], BF16)
            if tp is not None:
                k.dma('sp', tp, selT[:], reads=['ns_selT%d' % i for i in range(8)], writes=['tap'])
            k.barrier()
        if NSA_STOP == 'topk':
            return
        with contextlib.ExitStack() as es5:
            cau = load_const(g, es5, 'cau_mask')
            sexp = load_const(g, es5, 'sel_exp')
            mc_ = [0]

            def k_s(h, kt):
                hp = h % 2
                return ksT[hp * 64:(hp + 1) * 64, kt * 128:(kt + 1) * 128], ['ns_ks_%d' % (kt // 4)], 128

            def v_s(h, kt):
                return Vs[:, kt, 0:65], ['ns_Vs%d' % kt, 'ns_Vs1']

            def m_s(h, gi, kt):
                pm = 2 + mc_[0] % 2
                mc_[0] += 1
                k.op('pe', lambda e: e.matmul(g.ps[pm][:], lhsT=sexp[:, kt, :], rhs=selT[:, gi * 512:(gi + 1) * 512], start=True, stop=True),
                     reads=['c_sel_exp', 'ns_selT%d' % gi], writes=[g.psk[pm]])
                ms = [(g.ps[pm], [g.psk[pm]], True)]
                if kt >= 4 * gi:
                    ms.append((cau[:, (4 * gi - kt) + 3, :], ['c_cau_mask'], False))
                return ms

            attention(g, es5, 'nl', range(4), 8, lambda gi: list(range(0, 4 * gi + 4)), q_of_r, k_s, v_s, 65, m_s, make_epi(1, False))
            k.barrier()
        if NSA_STOP == 'sel':
            return
        with contextlib.ExitStack() as es6:
            swm = load_const(g, es6, 'swa_mask')

            def k_w(h, kt):
                hp = h % 2
                return kwT[hp * 64:(hp + 1) * 64, kt * 128:(kt + 1) * 128], ['ns_kw_%d' % (kt // 4)], 128

            def v_w(h, kt):
                return Vw[:, kt, 0:65], ['ns_Vw%d' % kt, 'ns_Vw1']

            attention(g, es6, 'nw', range(4), 8, lambda gi: list(range(max(0, 4 * gi - 4), 4 * gi + 4)), q_of_r, k_w, v_w, 65,
                      lambda h, gi, kt: [(swm[:, (4 * gi - kt) + 3, :], ['c_swa_mask'], False)], make_epi(2, False))
            finish_tm(g, es6, 'ns', ytm, lambda t_: ['ns_ytm%d_%d' % (t_, h) for h in range(4)], gb, 'ns_gb', 768)
            k.barrier()


RW_STEPS = S


def mixer_rwkv(g, l):
    k = g.k
    W = g.W
    ycv = g.ycatT.rearrange('(c p) t -> p c t', p=128)
    with contextlib.ExitStack() as es:
        rT = g.sb('rw_rT', [128, 2, S], BF16, es)
        kkT = g.sb('rw_kkT', [128, 2, S], BF16, es)
        wT = g.sb('rw_wT', [128, 2, S], F32, es)
        vtm = g.sb('rw_vtm', [128, NT, 256], BF16, es)
        gtm = g.sb('rw_gtm', [128, NT, 256], BF16, es)
        bon = g.sb('rw_bon', [128, NT, 4], F32, es)
        with contextlib.ExitStack() as es2:
            wa = g.sb('rw_wa', [128, 8, 1024], BF16, es2)
            wb = g.sb('rw_wb', [128, 8, 1024], BF16, es2)
            mub = g.sb('rw_mub', [128, 1024], F32, es2)
            omb = g.sb('rw_omb', [128, 1024], F32, es2)
            load_w_bf16(g, wa[:], 'rw_wa', W['w_in'][l][:, 0:1024])
            k.dma('sp', mub[:], W['rwkv_mu'][l].partition_broadcast(128), writes=['rw_mub'])
            k.op('dve', lambda e: e.tensor_scalar(out=omb[:], in0=mub[:], scalar1=-1.0, scalar2=1.0, op0=ALU.mult, op1=ALU.add),
                 reads=['rw_mub'], writes=['rw_omb'])
            for c in range(8):
                k.op('dve', lambda e, c=c: e.tensor_tensor(out=wb[:, c, :], in0=wa[:, c, :], in1=mub[:], op=ALU.mult),
                     reads=['rw_wa', 'rw_mub'], writes=['rw_wb'])
            for c in range(8):
                k.op('pool', lambda e, c=c: e.tensor_tensor(out=wa[:, c, :], in0=wa[:, c, :], in1=omb[:], op=ALU.mult),
                     reads=['rw_wa', 'rw_omb', 'rw_wb'], writes=['rw_wa'])
            w2 = g.sb('rw_w2', [64, 256], BF16, es2)
            a2 = g.sb('rw_a2', [128, 256], BF16, es2)
            g2 = g.sb('rw_g2', [128, 256], BF16, es2)
            a0r = g.sb('rw_a0r', [1, 256], BF16, es2)
            w0c = g.sb('rw_w0c', [128, 2], F32, es2)
            kkc = g.sb('rw_kkc', [128, 2], F32, es2)
            k.dma('pool', w2[:], W['rwkv_w2'][l], writes=['rw_w2'])
            k.dma('pool', a2[64:128, :], W['rwkv_a2'][l], writes=['rw_a2'])
            k.dma('pool', g2[:], W['rwkv_g2'][l], writes=['rw_g2'])
            k.dma('pool', a0r[:], W['rwkv_a0'][l:l + 1, :], writes=['rw_a0r'])
            k.dma('sp', w0c[:], W['rwkv_w0'][l].rearrange('(c p) -> p c', p=128), writes=['rw_w0c'], allow_slow_non_contiguous=True)
            k.dma('sp', kkc[:], W['rwkv_kk'][l].rearrange('(c p) -> p c', p=128), writes=['rw_kkc'], allow_slow_non_contiguous=True)
            bc = {}
            for nm, src in (('kk', W['rwkv_kk'][l]), ('ka', W['rwkv_ka'][l]), ('rk', W['rwkv_rk'][l].rearrange('h d -> (h d)'))):
                t = g.sb('rw_bc_' + nm, [128, 256], F32, es2)
                k.dma('sp', t[:], src.partition_broadcast(128), writes=['rw_bc_' + nm])
                bc[nm] = t
            hv = g.hTd.rearrange('(c p) t -> p c t', p=128)
            hb = [g.sb('rw_ht%d' % i, [128, 8, 513], BF16, es2) for i in range(2)]
            k.op('pool', lambda e: e.memset(hb[0][:, :, 0:1], 0.0), writes=['rw_ht0'])
            tnh = [g.sb('rw_tnh%d' % i, [128, 512], BF16, es2) for i in range(2)]
            sgd = [g.sb('rw_sgd%d' % i, [128, 512], BF16, es2) for i in range(2)]
            kx = [g.sb('rw_kx%d' % i, [128, 512], F32, es2) for i in range(2)]
            sq = [g.sb('rw_sq%d' % i, [128, 512], BF16, es2) for i in range(2)]
            rs = [g.sb('rw_rs%d' % i, [128, 512], F32, es2) for i in range(2)]
            tm = {nm: [g.sb('rw_%s%d' % (nm, i), [128, 256], F32, es2) for i in range(2)] for nm in ('a', 'r', 'kxm', 'k2', 'tq')}
            s4 = [g.sb('rw_s4%d' % i, [128, 4], F32, es2) for i in range(2)]
            ob = [g.sb('rw_ob%d' % i, [128, 768], BF16, es2) for i in range(2)]

            def fm2(ht, hk, c0, ncols, ps, pskey):
                for c in range(8):
                    k.op('pe', lambda e, c=c: e.matmul(ps, lhsT=wa[:, c, c0:c0 + ncols], rhs=ht[:, c, 1:513], start=(c == 0), stop=False),
                         reads=['rw_wa', hk], writes=[pskey])
                for c in range(8):
                    k.op('pe', lambda e, c=c: e.matmul(ps, lhsT=wb[:, c, c0:c0 + ncols], rhs=ht[:, c, 0:512], start=False, stop=(c == 7)),
                         reads=['rw_wb', hk], writes=[pskey])

            nps = 0
            for tt in range(8):
                b = tt % 2
                ts = slice(tt * 512, (tt + 1) * 512)
                hk = 'rw_ht%d' % b
                ht = hb[b]
                if tt == 0:
                    k.dma('sp', ht[:, :, 1:513], hv[:, :, 0:512], reads=['hTd'], writes=[hk])
                else:
                    k.dma('sp', ht[:], hv[:, :, tt * 512 - 1:(tt + 1) * 512], reads=['hTd'], writes=[hk])
                for j in range(2):
                    pi = nps % 2
                    nps += 1
                    fm2(ht, hk, j * 128, 128, g.ps[pi][:], g.psk[pi])
                    k.op('act', lambda e, pi=pi, j=j, ts=ts: e.copy(out=rT[:, j, ts], in_=g.ps[pi][:]), reads=[g.psk[pi]],
                         writes=['rw_rT%d_%d' % (j, tt)])
                    pi = nps % 2
                    nps += 1
                    fm2(ht, hk, 256 + j * 128, 128, g.ps[pi][:], g.psk[pi])
                    kb = nps % 2
                    ks_ = str(kb)
                    k.op('act', lambda e, pi=pi, j=j, kb=kb: e.activation(out=kx[kb][:], in_=g.ps[pi][:], func=AF.Copy, scale=kkc[:, j:j + 1]),
                         reads=[g.psk[pi], 'rw_kkc'], writes=['rw_kx' + ks_])
                    k.op('act', lambda e, kb=kb: e.activation(out=sq[kb][:], in_=kx[kb][:], func=AF.Square), reads=['rw_kx' + ks_], writes=['rw_sq' + ks_])
                    pj = 2 + kb
                    k.op('pe', lambda e, kb=kb, pj=pj: e.matmul(g.ps[pj][:], lhsT=g.cs['blk64_b'][:], rhs=sq[kb][:], start=True, stop=True),
                         reads=['rw_sq' + ks_, 'c_blk64_b'], writes=[g.psk[pj]])
                    k.op('dve', lambda e, kb=kb, pj=pj: e.tensor_scalar(out=rs[kb][:], in0=g.ps[pj][:], scalar1=1e-12, scalar2=None, op0=ALU.add),
                         reads=[g.psk[pj]], writes=['rw_rs' + ks_])
                    k.op('act', lambda e, kb=kb: e.activation(out=rs[kb][:], in_=rs[kb][:], func=AF.Sqrt), reads=['rw_rs' + ks_], writes=['rw_rs' + ks_])
                    k.op('dve', lambda e, kb=kb: e.reciprocal(out=rs[kb][:], in_=rs[kb][:]), reads=['rw_rs' + ks_], writes=['rw_rs' + ks_])
                    k.op('dve', lambda e, kb=kb, j=j, ts=ts: e.tensor_tensor(out=kkT[:, j, ts], in0=kx[kb][:], in1=rs[kb][:], op=ALU.mult),
                         reads=['rw_kx' + ks_, 'rw_rs' + ks_], writes=['rw_kkT%d_%d' % (j, tt)])
                pi = nps % 2
                nps += 1
                fm2(ht, hk, 768, 128, g.ps[pi][:], g.psk[pi])
                k.op('act', lambda e, pi=pi, b=b: e.activation(out=tnh[b][0:64, :], in_=g.ps[pi][0:64, :], func=AF.Tanh),
                     reads=[g.psk[pi]], writes=['rw_tnh%d' % b])
                k.op('act', lambda e, pi=pi, b=b: e.copy(out=tnh[b][64:128, :], in_=g.ps[pi][64:128, :]),
                     reads=[g.psk[pi]], writes=['rw_tnhb%d' % b])
                pi = nps % 2
                nps += 1
                fm2(ht, hk, 896, 128, g.ps[pi][:], g.psk[pi])
                k.op('act', lambda e, pi=pi, b=b: e.activation(out=sgd[b][:], in_=g.ps[pi][:], func=AF.Sigmoid), reads=[g.psk[pi]],
                     writes=['rw_sgd%d' % b])
                for j in range(2):
                    pi = nps % 2
                    nps += 1
                    k.op('pe', lambda e, pi=pi, j=j, b=b: e.matmul(g.ps[pi][:], lhsT=w2[:, j * 128:(j + 1) * 128], rhs=tnh[b][0:64, :],
                                                                   start=True, stop=True), reads=['rw_w2', 'rw_tnh%d' % b], writes=[g.psk[pi]])
                    k.op('act', lambda e, pi=pi, j=j, ts=ts: e.activation(out=wT[:, j, ts], in_=g.ps[pi][:], func=AF.Sigmoid, bias=w0c[:, j:j + 1], scale=1.0),
                         reads=[g.psk[pi], 'rw_w0c'], writes=['rw_wT%d_%d' % (j, tt)])
                    k.op('act', lambda e, j=j, ts=ts: e.activation(out=wT[:, j, ts], in_=wT[:, j, ts], func=AF.Exp, scale=-0.606531),
                         reads=['rw_wT%d_%d' % (j, tt)], writes=['rw_wT%d_%d' % (j, tt)])
                for sub in range(4):
                    t_ = tt * 4 + sub
                    q = t_ % 2
                    qs = str(q)
                    cs_ = slice(sub * 128, (sub + 1) * 128)
                    pA, pB, pC, pD = 4, 5, 6, 7
                    for (ps_, c0, n_) in ((pA, 0, 512), (pB, 512, 256)):
                        for c in range(8):
                            k.op('pe', lambda e, c=c, ps_=ps_, c0=c0, n_=n_, sub=sub: e.matmul(
                                g.ps[ps_][:, 0:n_], lhsT=ht[:, c, 1 + sub * 128:1 + (sub + 1) * 128], rhs=wa[:, c, c0:c0 + n_],
                                start=(c == 0), stop=False), reads=['rw_wa', hk], writes=[g.psk[ps_]])
                        for c in range(8):
                            k.op('pe', lambda e, c=c, ps_=ps_, c0=c0, n_=n_, sub=sub: e.matmul(
                                g.ps[ps_][:, 0:n_], lhsT=ht[:, c, sub * 128:(sub + 1) * 128], rhs=wb[:, c, c0:c0 + n_],
                                start=False, stop=(c == 7)), reads=['rw_wb', hk], writes=[g.psk[ps_]])
                    k.op('pe', lambda e, b=b, cs_=cs_: e.matmul(g.ps[pC][:, 0:256], lhsT=tnh[b][64:128, cs_], rhs=a2[64:128, :], start=True, stop=False),
                         reads=['rw_tnhb%d' % b, 'rw_a2'], writes=[g.psk[pC]])
                    k.op('pe', lambda e: e.matmul(g.ps[pC][:, 0:256], lhsT=g.cs['ones_b'][0:1, :], rhs=a0r[:], start=False, stop=True),
                         reads=['c_ones_b', 'rw_a0r'], writes=[g.psk[pC]])
                    k.op('pe', lambda e, b=b, cs_=cs_: e.matmul(g.ps[pD][:, 0:256], lhsT=sgd[b][:, cs_], rhs=g2[:], start=True, stop=True),
                         reads=['rw_sgd%d' % b, 'rw_g2'], writes=[g.psk[pD]])
                    a_, r_, kxm, k2, tq = (tm[nm][q] for nm in ('a', 'r', 'kxm', 'k2', 'tq'))
                    K_ = lambda nm: 'rw_%s%s' % (nm, qs)
                    k.op('act', lambda e: e.activation(out=a_[:], in_=g.ps[pC][:, 0:256], func=AF.Sigmoid), reads=[g.psk[pC]], writes=[K_('a')])
                    k.op('act', lambda e, t_=t_: e.copy(out=gtm[:, t_, :], in_=g.ps[pD][:, 0:256]), reads=[g.psk[pD]], writes=['rw_gtm%d' % t_])
                    k.op('act', lambda e: e.copy(out=r_[:], in_=g.ps[pA][:, 0:256]), reads=[g.psk[pA]], writes=[K_('r')])
                    k.op('act', lambda e, t_=t_: e.copy(out=vtm[:, t_, :], in_=g.ps[pB][:, 0:256]), reads=[g.psk[pB]], writes=['rw_vtm%d' % t_])
                    k.op('act', lambda e, q=q: e.copy(out=ob[q][:, 512:768], in_=g.ps[pB][:, 0:256]), reads=[g.psk[pB]], writes=['rw_obv' + qs])
                    k.op('dve', lambda e: e.tensor_tensor(out=kxm[:], in0=g.ps[pA][:, 256:512], in1=bc['kk'][:], op=ALU.mult),
                         reads=[g.psk[pA], 'rw_bc_kk'], writes=[K_('kxm')])
                    k.op('pool', lambda e: e.tensor_tensor(out=tq[:], in0=kxm[:], in1=kxm[:], op=ALU.mult), reads=[K_('kxm')], writes=[K_('tq')])
                    k.op('dve', lambda e, q=q: e.reduce_sum(out=s4[q][:], in_=tq[:].rearrange('p (h d) -> p h d', d=64), axis=AX.X),
                         reads=[K_('tq')], writes=['rw_s4' + qs])
                    k.op('dve', lambda e, q=q: e.tensor_scalar(out=s4[q][:], in0=s4[q][:], scalar1=1e-12, scalar2=None, op0=ALU.add),
                         reads=['rw_s4' + qs], writes=['rw_s4' + qs])
                    k.op('act', lambda e, q=q: e.activation(out=s4[q][:], in_=s4[q][:], func=AF.Sqrt), reads=['rw_s4' + qs], writes=['rw_s4' + qs])
                    k.op('dve', lambda e, q=q: e.reciprocal(out=s4[q][:], in_=s4[q][:]), reads=['rw_s4' + qs], writes=['rw_s4' + qs])
                    for h in range(4):
                        hs = slice(h * 64, (h + 1) * 64)
                        k.op('dve', lambda e, h=h, hs=hs, q=q: e.scalar_tensor_tensor(out=ob[q][:, hs], in0=kxm[:, hs], scalar=s4[q][:, h:h + 1],
                                                                                      in1=a_[:, hs], op0=ALU.mult, op1=ALU.mult),
                             reads=[K_('kxm'), 'rw_s4' + qs, K_('a')], writes=['rw_obb' + qs])
                    k.op('dve', lambda e: e.scalar_tensor_tensor(out=tq[:], in0=a_[:], scalar=-1.0, in1=bc['ka'][:], op0=ALU.add, op1=ALU.mult),
                         reads=[K_('a'), 'rw_bc_ka', K_('tq')], writes=[K_('tq')])
                    k.op('dve', lambda e: e.scalar_tensor_tensor(out=k2[:], in0=tq[:], scalar=1.0, in1=g.ps[pA][:, 256:512], op0=ALU.add, op1=ALU.mult),
                         reads=[K_('tq'), g.psk[pA]], writes=[K_('k2')])
                    k.op('pool', lambda e, q=q: e.tensor_copy(ob[q][:, 256:512], k2[:]), reads=[K_('k2')], writes=['rw_obk' + qs])
                    k.op('pool', lambda e: e.tensor_tensor(out=tq[:], in0=r_[:], in1=k2[:], op=ALU.mult), reads=[K_('r'), K_('k2'), K_('tq')], writes=[K_('tq')])
                    k.op('pool', lambda e: e.tensor_tensor(out=tq[:], in0=tq[:], in1=bc['rk'][:], op=ALU.mult), reads=[K_('tq'), 'rw_bc_rk'], writes=[K_('tq')])
                    k.op('dve', lambda e, t_=t_: e.reduce_sum(out=bon[:, t_, :], in_=tq[:].rearrange('p (h d) -> p h d', d=64), axis=AX.X),
                         reads=[K_('tq')], writes=['rw_bon%d' % t_])
                    k.dma('sp', g.rwscr[t_ * 128:(t_ + 1) * 128, :], ob[q][:], reads=['rw_obv' + qs, 'rw_obb' + qs, 'rw_obk' + qs],
                          writes=['rwscr%d' % t_])
            k.barrier()
        TC = 64
        ST = g.sb('rw_ST', [128, 128], BF16, es)
        Lb = [g.sb('rw_L%d' % i, [36, TC, 128], BF16, es) for i in range(2)]
        Rb = [g.sb('rw_R%d' % i, [36, TC, 128], BF16, es) for i in range(2)]
        KKn = [g.sb('rw_KKn%d' % i, [128, TC, 4], BF16, es) for i in range(2)]
        Rr = [g.sb('rw_Rr%d' % i, [128, TC, 4], BF16, es) for i in range(2)]
        sam = load_const(g, es, 'sa_mask')
        hlm = load_const(g, es, 'hl_mask')
        Yc = [g.sb('rw_Yc%d' % i, [128, 2, 128], F32, es) for i in range(2)]
        ytm = g.sb('rw_ytm', [128, 256], F32, es)
        yo = [g.sb('rw_yo%d' % i, [128, 256], BF16, es) for i in range(2)]
        yT = [g.sb('rw_yT%d' % i, [128, 2, 128], BF16, es) for i in range(2)]
        st8 = g.sb('rw_st8', [128, 8], F32, es)
        tq2 = g.sb('rw_tq2', [128, 256], F32, es)
        gnw = g.sb('rw_gnw', [128, 256], F32, es)
        gnb = g.sb('rw_gnb', [128, 256], F32, es)
        k.dma('sp', gnw[:], W['rwkv_gn_w'][l].partition_broadcast(128), writes=['rw_gnw'])
        k.dma('sp', gnb[:], W['rwkv_gn_b'][l].partition_broadcast(128), writes=['rw_gnb'])
        k.op('pool', lambda e: e.memset(ST[:], 0.0), writes=['rw_ST0', 'rw_ST1'])
        for i in range(2):
            k.op('pool', lambda e, i=i: e.memset(Lb[i][:], 0.0), writes=['rw_L%d' % i])
            k.op('pool', lambda e, i=i: e.memset(Rb[i][:], 0.0), writes=['rw_R%d' % i])
        psb = [g.ps[6][:].bitcast(BF16), g.ps[7][:].bitcast(BF16)]
        nsteps = RW_STEPS
        def setup(ch):
            cb = ch % 2
            t0 = ch * TC
            cbs = str(cb)
            tt = t0 // 512
            for h in range(4):
                hl, hh = h % 2, h // 2
                src = g.rwscr[t0:t0 + TC, :]
                k.dma('sp', Lb[cb][h:h + 1, :, hl * 64:(hl + 1) * 64], src[:, h * 64:(h + 1) * 64].rearrange('(o t) d -> o t d', o=1),
                      reads=['rwscr%d' % (t0 // 128)], writes=['rw_L' + cbs])
                k.dma('sp', Lb[cb][32 + h:33 + h, :, hl * 64:(hl + 1) * 64], src[:, 256 + h * 64:256 + (h + 1) * 64].rearrange('(o t) d -> o t d', o=1),
                      reads=['rwscr%d' % (t0 // 128)], writes=['rw_L' + cbs])
                k.dma('sp', Rb[cb][32 + h:33 + h, :, hh * 64:(hh + 1) * 64], src[:, 512 + h * 64:512 + (h + 1) * 64].rearrange('(o t) d -> o t d', o=1),
                      reads=['rwscr%d' % (t0 // 128)], writes=['rw_Rv' + cbs])
                k.op('pool', lambda e, h=h, hh=hh, hl=hl, cb=cb, t0=t0: e.tensor_scalar(
                    out=KKn[cb][:, :, h], in0=kkT[:, hh, t0:t0 + TC], scalar1=hlm[:, hl:hl + 1], scalar2=None, op0=ALU.mult),
                    reads=['rw_kkT%d_%d' % (hh, tt), 'c_hl_mask'], writes=['rw_KKn' + cbs])
                k.op('pool', lambda e, h=h, hh=hh, hl=hl, cb=cb, t0=t0: e.tensor_scalar(
                    out=Rr[cb][:, :, h], in0=rT[:, hh, t0:t0 + TC], scalar1=hlm[:, 2 + hl:3 + hl], scalar2=None, op0=ALU.mult),
                    reads=['rw_rT%d_%d' % (hh, tt), 'c_hl_mask'], writes=['rw_Rr' + cbs])

        setup(0)
        pend = []
        for ch in range(nsteps // TC):
            cb = ch % 2
            t0 = ch * TC
            cbs = str(cb)
            tt = t0 // 512
            if ch + 1 < nsteps // TC:
                setup(ch + 1)
            for s_ in range(TC):
                t = t0 + s_
                pa = t % 2
                pu = 2 + t % 2
                py = 4 + (t // 128) % 2
                k.op('pe', lambda e, cb=cb, s_=s_, pa=pa: e.matmul(g.ps[pa][0:4, 0:128], lhsT=KKn[cb][:, s_, :], rhs=ST[:], start=True, stop=True),
                     reads=['rw_KKn' + cbs, 'rw_ST0', 'rw_ST1'], writes=[g.psk[pa]])
                if pend:
                    pend.pop()()
                k.op('dve', lambda e, cb=cb, s_=s_, pa=pa: e.tensor_tensor(out=Rb[cb][0:4, s_, :], in0=g.ps[pa][0:4, 0:128], in1=sam[0:4, :], op=ALU.mult),
                     reads=[g.psk[pa], 'c_sa_mask'], writes=['rw_R' + cbs])
                k.op('pe', lambda e, cb=cb, s_=s_, pu=pu: e.matmul(g.ps[pu][:, 0:128], lhsT=Lb[cb][:, s_, :], rhs=Rb[cb][:, s_, :], start=True, stop=True),
                     reads=['rw_L' + cbs, 'rw_R' + cbs, 'rw_Rv' + cbs], writes=[g.psk[pu]])
                for hh in range(2):
                    k.op('dve', lambda e, hh=hh, t=t, pu=pu: e.scalar_tensor_tensor(
                        out=ST[:, hh * 64:(hh + 1) * 64], in0=ST[:, hh * 64:(hh + 1) * 64], scalar=wT[:, hh, t:t + 1],
                        in1=g.ps[pu][:, hh * 64:(hh + 1) * 64], op0=ALU.mult, op1=ALU.add),
                        reads=['rw_ST%d' % hh, g.psk[pu], 'rw_wT%d_%d' % (hh, tt)], writes=['rw_ST%d' % hh])
                pend.append(lambda cb=cb, s_=s_, t=t, py=py, cbs=cbs: k.op('pe', lambda e: e.matmul(
                    g.ps[py][:, (t % 128) * 4:(t % 128) * 4 + 4], lhsT=ST[:], rhs=Rr[cb][:, s_, :], start=True, stop=True),
                    reads=['rw_ST0', 'rw_ST1', 'rw_Rr' + cbs], writes=[g.psk[py]]))
                if t % 128 == 127:
                    pend.pop()()
                    t_ = t // 128
                    yb_ = t_ % 2
                    ys = str(yb_)
                    yv = g.ps[py][:].rearrange('p (t h) -> p h t', h=4)
                    for hl in range(2):
                        k.op('act', lambda e, hl=hl, yb_=yb_: e.copy(out=Yc[yb_][0:64, hl, :], in_=yv[0:64, hl, :]), reads=[g.psk[py]],
                             writes=['rw_Yc%s_%d' % (ys, hl)])
                        k.op('act', lambda e, hl=hl, yb_=yb_: e.copy(out=Yc[yb_][64:128, hl, :], in_=yv[64:128, 2 + hl, :]), reads=[g.psk[py]],
                             writes=['rw_Ycb%s_%d' % (ys, hl)])
                    ytv = ytm[:].rearrange('p (hh hl i) -> p hl hh i', hh=2, hl=2)
                    for hl in range(2):
                        pt_ = 6 + hl
                        k.op('pe', lambda e, hl=hl, yb_=yb_, pt_=pt_: e.transpose(out=g.ps[pt_][:, 0:128], in_=Yc[yb_][:, hl, :], identity=g.cs['ident_f'][:]),
                             reads=['rw_Yc%s_%d' % (ys, hl), 'rw_Ycb%s_%d' % (ys, hl), 'c_ident_f'], writes=[g.psk[pt_]])
                        k.op('act', lambda e, hl=hl, pt_=pt_: e.copy(out=ytv[:, hl, :, :], in_=g.ps[pt_][:, 0:128].rearrange('p (hh i) -> p hh i', hh=2)),
                             reads=[g.psk[pt_]], writes=['rw_ytm'])
                    k.op('dve', lambda e: e.reduce_sum(out=st8[:, 0:4], in_=ytm[:].rearrange('p (h d) -> p h d', d=64), axis=AX.X),
                         reads=['rw_ytm'], writes=['rw_st8'])
                    k.op('pool', lambda e: e.tensor_tensor(out=tq2[:], in0=ytm[:], in1=ytm[:], op=ALU.mult), reads=['rw_ytm'], writes=['rw_tq2'])
                    k.op('dve', lambda e: e.reduce_sum(out=st8[:, 4:8], in_=tq2[:].rearrange('p (h d) -> p h d', d=64), axis=AX.X),
                         reads=['rw_tq2'], writes=['rw_st8'])
                    k.op('dve', lambda e: e.tensor_scalar(out=st8[:], in0=st8[:], scalar1=1.0 / 64, scalar2=None, op0=ALU.mult),
                         reads=['rw_st8'], writes=['rw_st8'])
                    k.op('dve', lambda e: e.tensor_tensor(out=tq2[:, 0:4], in0=st8[:, 0:4], in1=st8[:, 0:4], op=ALU.mult), reads=['rw_st8', 'rw_tq2'],
                         writes=['rw_tq2'])
                    k.op('dve', lambda e: e.tensor_tensor(out=st8[:, 4:8], in0=st8[:, 4:8], in1=tq2[:, 0:4], op=ALU.subtract), reads=['rw_st8', 'rw_tq2'],
                         writes=['rw_st8'])
                    k.op('dve', lambda e: e.tensor_scalar(out=st8[:, 4:8], in0=st8[:, 4:8], scalar1=64e-5, scalar2=None, op0=ALU.add),
                         reads=['rw_st8'], writes=['rw_st8'])
                    k.op('act', lambda e: e.activation(out=st8[:, 4:8], in_=st8[:, 4:8], func=AF.Sqrt), reads=['rw_st8'], writes=['rw_st8'])
                    k.op('dve', lambda e: e.reciprocal(out=st8[:, 4:8], in_=st8[:, 4:8]), reads=['rw_st8'], writes=['rw_st8'])
                    for h in range(4):
                        hs = slice(h * 64, (h + 1) * 64)
                        k.op('dve', lambda e, h=h, hs=hs: e.tensor_scalar(out=tq2[:, hs], in0=ytm[:, hs], scalar1=st8[:, h:h + 1], scalar2=st8[:, 4 + h:5 + h],
                                                                         op0=ALU.subtract, op1=ALU.mult), reads=['rw_ytm', 'rw_st8', 'rw_tq2'], writes=['rw_tq2'])
                    k.op('pool', lambda e: e.tensor_tensor(out=tq2[:], in0=tq2[:], in1=gnw[:], op=ALU.mult), reads=['rw_tq2', 'rw_gnw'], writes=['rw_tq2'])
                    k.op('pool', lambda e: e.tensor_tensor(out=tq2[:], in0=tq2[:], in1=gnb[:], op=ALU.add), reads=['rw_tq2', 'rw_gnb'], writes=['rw_tq2'])
                    for h in range(4):
                        hs = slice(h * 64, (h + 1) * 64)
                        k.op('dve', lambda e, h=h, hs=hs, t_=t_: e.scalar_tensor_tensor(out=tq2[:, hs], in0=vtm[:, t_, hs], scalar=bon[:, t_, h:h + 1],
                                                                                       in1=tq2[:, hs], op0=ALU.mult, op1=ALU.add),
                             reads=['rw_vtm%d' % t_, 'rw_bon%d' % t_, 'rw_tq2'], writes=['rw_tq2'])
                    k.op('dve', lambda e, yb_=yb_, t_=t_: e.tensor_tensor(out=yo[yb_][:], in0=tq2[:], in1=gtm[:, t_, :], op=ALU.mult),
                         reads=['rw_tq2', 'rw_gtm%d' % t_], writes=['rw_yo' + ys])
                    for j in range(2):
                        k.op('pe', lambda e, j=j, yb_=yb_: e.transpose(out=psb[yb_][:, j * 128:(j + 1) * 128], in_=yo[yb_][:, j * 128:(j + 1) * 128],
                                                                      identity=g.cs['ident_b'][:]), reads=['rw_yo' + ys, 'c_ident_b'], writes=[g.psk[6 + yb_]])
                    k.op('act', lambda e, yb_=yb_: e.copy(out=yT[yb_][:], in_=psb[yb_][:, 0:256].rearrange('p (j t) -> p j t', j=2)),
                         reads=[g.psk[6 + yb_]], writes=['rw_yT' + ys])
                    k.dma('sp', ycv[:, 0:2, t_ * 128:(t_ + 1) * 128], yT[yb_][:], reads=['rw_yT' + ys], writes=['ycat_rw%d' % t_])
        k.barrier()
```

```python
import contextlib
import numpy as np
import ml_dtypes
import concourse.bass as bass
import concourse.mybir as mybir
from concourse.bass_utils import run_bass_kernel_spmd

F32 = mybir.dt.float32
BF16 = mybir.dt.bfloat16
I32 = mybir.dt.int32
AF = mybir.ActivationFunctionType
ALU = mybir.AluOpType
AX = mybir.AxisListType


class KB:
    NDS = 8

    def __init__(self):
        self.nc = bass.Bass("TRN2", target_bir_lowering=False)
        nc = self.nc
        self.eng = {"pe": nc.tensor, "act": nc.scalar, "dve": nc.vector, "pool": nc.gpsimd, "sp": nc.sync}
        self.semh = {}
        for e in self.eng:
            self.semh[e] = nc.semaphore("s_" + e).__enter__()
        self.cnt = {e: 0 for e in self.eng}
        self.waited = {e: {} for e in self.eng}
        self.lastw = {}
        self.readers = {}
        self.dq = {}
        self.nwaits = 0
        self.nops = 0

    def _deps(self, eng, reads, writes):
        deps = {}

        def add(k, v):
            if deps.get(k, 0) < v:
                deps[k] = v

        for r in reads:
            p = self.lastw.get(r)
            if p is not None:
                if not (p[0] == eng and eng == "pe"):
                    add(*p)
            if isinstance(r, str) and r.startswith("ps") and r[2:].isdigit():
                for k, v in self.readers.get(r, {}).items():
                    if k != eng:
                        add(k, v)
        for w in writes:
            p = self.lastw.get(w)
            if p is not None and p[0] != eng:
                add(*p)
            for k, v in self.readers.get(w, {}).items():
                if k != eng:
                    add(k, v)
        return deps

    def _wait(self, eng, deps):
        wt = self.waited[eng]
        for k, v in deps.items():
            if wt.get(k, 0) >= v:
                continue
            self.eng[eng].wait_ge(self.semh[k], v)
            wt[k] = v
            self.nwaits += 1

    def _record(self, prod, reads, writes):
        k, v = prod
        for r in reads:
            d = self.readers.setdefault(r, {})
            if d.get(k, 0) < v:
                d[k] = v
        for w in writes:
            self.lastw[w] = prod
            self.readers[w] = {}

    def op(self, eng, fn, reads=(), writes=()):
        self._wait(eng, self._deps(eng, reads, writes))
        ins = fn(self.eng[eng])
        self.cnt[eng] += 1
        ins.then_inc(self.semh[eng], 1)
        self._record((eng, self.cnt[eng]), reads, writes)
        self.nops += 1
        return ins

    def dma(self, q, out, in_, reads=(), writes=(), **kw):
        st = self.dq.get(q)
        if st is None:
            st = {"n": 0, "sems": []}
            for i in range(self.NDS):
                key = ("dma", q, i)
                self.semh[key] = self.nc.semaphore("d_%s_%d" % (q, i)).__enter__()
                st["sems"].append(key)
            self.dq[q] = st
        i = st["n"]
        slot = i % self.NDS
        val = 16 * (i // self.NDS + 1)
        key = st["sems"][slot]
        deps = self._deps(q, reads, writes)
        if i >= self.NDS:
            if deps.get(key, 0) < val - 16:
                deps[key] = val - 16
        self._wait(q, deps)
        ins = self.eng[q].dma_start(out=out, in_=in_, **kw)
        ins.then_inc(self.semh[key], 16)
        st["n"] += 1
        self._record((key, val), reads, writes)
        return ins

    def barrier(self):
        deps = {}
        for e in self.eng:
            if self.cnt[e] > 0:
                deps[e] = self.cnt[e]
        for q, st in self.dq.items():
            n = st["n"]
            for slot in range(self.NDS):
                if n > slot:
                    cntslot = (n - 1 - slot) // self.NDS + 1
                    deps[st["sems"][slot]] = 16 * cntslot
        for e in self.eng:
            d = {k: v for k, v in deps.items() if k != e or e != "pe"}
            self._wait(e, d)

    def finish(self):
        self.barrier()


S = 4096
D = 1024
NT = 32
PROJ_W = 3212
L_DEPTH = 2
NORM_EPS = 1e-6
PI = float(np.pi)

PARAM_SHAPES = {
    'w_ada': (2, 1024, 6144), 'b_ada': (2, 6144), 'norm1_g': (2, 1024), 'norm2_g': (2, 1024),
    'w_in': (2, 1024, 3212), 'w_out': (2, 1024, 1024), 'rwkv_mu': (2, 1024), 'rwkv_w0': (2, 256),
    'rwkv_w2': (2, 64, 256), 'rwkv_a0': (2, 256), 'rwkv_a2': (2, 64, 256), 'rwkv_g2': (2, 128, 256),
    'rwkv_kk': (2, 256), 'rwkv_ka': (2, 256), 'rwkv_rk': (2, 4, 64), 'rwkv_gn_w': (2, 256), 'rwkv_gn_b': (2, 256),
    'conv_w': (2, 3, 256), 'dil_q_g': (2, 64), 'dil_k_g': (2, 64), 'nsa_q_g': (2, 64), 'nsa_kc_g': (2, 64),
    'nsa_ks_g': (2, 64), 'nsa_kw_g': (2, 64), 'nsa_pe_k': (2, 32, 64), 'nsa_pe_v': (2, 32, 64),
    'nsa_wk1': (2, 2048, 256), 'nsa_wk2': (2, 256, 64), 'nsa_wv1': (2, 2048, 256), 'nsa_wv2': (2, 256, 64),
    'onorm_g': (2, 768), 'w_router': (2, 1024, 32), 'b_router': (2, 32), 'w_gu': (2, 32, 1024, 2048),
    'b_gu': (2, 32, 2048), 'w_down': (2, 32, 1024, 1024), 'b_down': (2, 32, 1024),
}


def host_consts():
    c = {}
    c['ident_f'] = np.eye(128, dtype=np.float32)
    c['ident_b'] = np.eye(128).astype(ml_dtypes.bfloat16)
    c['ones_b'] = np.ones((128, 128)).astype(ml_dtypes.bfloat16)
    blk = np.zeros((128, 128), np.float32)
    blk[:64, :64] = 1.0
    blk[64:, 64:] = 1.0
    c['blk64_b'] = blk.astype(ml_dtypes.bfloat16)
    sel = np.zeros((32, 32, 128), np.float32)
    for e in range(32):
        sel[e, e, :] = 1.0
    c['sel_b'] = sel.astype(ml_dtypes.bfloat16)
    pr = np.zeros((128, 128), np.float32)
    for m in range(128):
        if m % 64 < 32:
            pr[m + 32, m] = -1.0
        else:
            pr[m - 32, m] = 1.0
    c['prot_b'] = pr.astype(ml_dtypes.bfloat16)
    c['invf'] = (10000.0 ** (-(np.arange(128) % 32) / 32.0)).astype(np.float32).reshape(128, 1)
    kl = np.arange(128)[:, None]
    ql = np.arange(512)[None, :]

    def toep(offs, fn):
        return np.stack([fn(128 * o + ql - kl) for o in offs]).astype(np.float32)

    def dil(d):
        m = ((d >= 0) & (d <= 128)).astype(np.float32)
        m += ((d >= 0) & (d % 4 == 0) & (d // 4 <= 128))
        m += ((d >= 0) & (d % 16 == 0) & (d // 16 <= 128))
        return m
    c['dil_mask'] = toep(range(-3, 17), dil).transpose(1, 0, 2).astype(ml_dtypes.bfloat16).copy()
    c['swa_mask'] = toep(range(-3, 5), lambda d: (d >= 0) & (d <= 511)).transpose(1, 0, 2).astype(ml_dtypes.bfloat16).copy()
    c['cau_mask'] = toep(range(-3, 1), lambda d: d >= 0).transpose(1, 0, 2).astype(ml_dtypes.bfloat16).copy()
    c['cmp_mask'] = np.stack([(16 * kl + 31 <= 512 * i + ql) for i in range(5)]).astype(np.float32).transpose(1, 0, 2) \
        .astype(ml_dtypes.bfloat16).copy()
    diff = np.arange(256)[:, None] - 4 * np.arange(64)[None, :]
    offs = (np.arange(4)[:, None] - np.arange(2)[None, :]).reshape(-1)
    ov = (diff[..., None] == offs).sum(-1).astype(np.float32)
    ov[255] = 0
    ov1 = np.concatenate([np.ones((256, 1), np.float32), ov], axis=1)
    ov1[255] = 0
    c['ovl1'] = ov1.reshape(2, 128, 65).transpose(1, 0, 2).astype(ml_dtypes.bfloat16).copy()
    tok = np.arange(S)[:, None]
    jb = np.arange(64)[None, :]
    cur = tok // 64
    fut = jb > cur
    forced = ((jb == 0) | (jb == cur) | (jb == cur - 1)) & ~fut
    keep = ~(fut | forced)
    c['sel_keep'] = keep.astype(np.float32).reshape(32, 128, 64).transpose(1, 0, 2).copy()
    c['sel_add'] = (-1.0 * fut + 1e4 * forced).astype(np.float32).reshape(32, 128, 64).transpose(1, 0, 2).copy()
    ex = np.zeros((64, 32, 128), np.float32)
    for kt in range(32):
        ex[2 * kt, kt, :64] = 1
        ex[2 * kt + 1, kt, 64:] = 1
    c['sel_exp'] = ex.astype(ml_dtypes.bfloat16)
    sam = np.zeros((128, 128), np.float32)
    sam[0:2, 0:64] = 1.0
    sam[2:4, 64:128] = 1.0
    c['sa_mask'] = sam
    hm = np.zeros((128, 4), np.float32)
    hm[0:64, 0] = -1.0
    hm[64:128, 1] = -1.0
    hm[0:64, 2] = 1.0
    hm[64:128, 3] = 1.0
    c['hl_mask'] = hm
    return c


RESIDENT_CONSTS = ('ident_f', 'ident_b', 'ones_b', 'blk64_b', 'sel_b', 'prot_b', 'invf')


class Ctx:
    pass


class LazyW:
    def __init__(self, nc):
        self.nc = nc
        self.d = {}

    def __getitem__(self, n):
        if n not in self.d:
            self.d[n] = self.nc.dram_tensor(n, list(PARAM_SHAPES[n]), F32, kind='ExternalInput').ap()
        return self.d[n]


def build_program(layers=(0, 1), taps=(), first=True, last=True, mixers='ABCD', moe=True):
    k = KB()
    nc = k.nc
    g = Ctx()
    g.k = k
    g.nc = nc
    g.taps = {}
    g.want = set(taps)
    g.mixers = mixers
    g.do_moe = moe
    g.x = nc.dram_tensor('x', [S, D], F32, kind='ExternalInput').ap()
    g.c = nc.dram_tensor('c', [D], F32, kind='ExternalInput').ap()
    g.pos = nc.dram_tensor('positions', [S], I32, kind='ExternalInput').ap()
    g.W = LazyW(nc)
    g.C = {}
    for n, a in host_consts().items():
        dt = F32 if a.dtype == np.float32 else BF16
        g.C[n] = nc.dram_tensor('const_' + n, list(a.shape), dt, kind='ExternalInput').ap()
    g.out = nc.dram_tensor('out', [S, D], F32, kind='ExternalOutput').ap()
    g.xT = [nc.dram_tensor('xT%d' % i, [D, S], F32, kind='Internal').ap() for i in range(2)]
    g.ycatT = nc.dram_tensor('ycatT', [D, S], BF16, kind='Internal').ap()
    g.h2Td = nc.dram_tensor('h2Td', [D, S], BF16, kind='Internal').ap()
    g.hTd = nc.dram_tensor('hTd', [D, S], BF16, kind='Internal').ap()
    g.rwscr = nc.dram_tensor('rwscr', [S, 768], BF16, kind='Internal').ap()

    with contextlib.ExitStack() as es:
        g.layer = 'i'
        g.nsb = 0

        def sb(name, shape, dt, stack=es):
            g.nsb += 1
            return stack.enter_context(nc.sbuf_tensor('%s_%d' % (name, g.nsb), list(shape), dt))
        g.sb = sb
        g.ps = [nc.alloc_psum_tensor('ps%d' % i, [128, 512], F32) for i in range(8)]
        g.psk = ['ps%d' % i for i in range(8)]
        g.cs = {}
        for n, ap in g.C.items():
            if n not in RESIDENT_CONSTS:
                continue
            t = sb('c_' + n, ap.shape, ap.dtype)
            k.dma('sp', t[:], ap, writes=['c_' + n])
            g.cs[n] = t
        g.mod = sb('mod', [128, 48], F32)
        g.GT = sb('GT', [32, S], BF16)
        if first:
            phase_pre(g)
        cur = 0
        for l in layers:
            g.layer = str(l)
            phase_mod(g, l)
            with contextlib.ExitStack() as les:
                phase_norm1(g, l, g.xT[cur])
                g.onorm = g.sb('onorm', [128, 6], F32, les)
                k.dma('sp', g.onorm[:], g.W['onorm_g'][l].rearrange('(j p) -> p j', p=128), writes=['onorm'],
                      allow_slow_non_contiguous=True)
                if 'B' in g.mixers:
                    mixer_conv(g, l)
                if 'C' in g.mixers:
                    mixer_dil(g, l)
                if 'D' in g.mixers:
                    mixer_nsa(g, l)
                if 'A' in g.mixers:
                    mixer_rwkv(g, l)
                zero_missing(g)
                t = tap(g, 'ycatT%d' % l, [D, S], BF16)
                if t is not None:
                    k.barrier()
                    k.dma('sp', t, g.ycatT, writes=['tap'])
                k.barrier()
            phase_wout(g, l, g.xT[cur], g.xT[cur ^ 1])
            phase_norm2_router(g, l, g.xT[cur ^ 1])
            phase_moe(g, l, g.xT[cur ^ 1], g.xT[cur], final=(last and l == layers[-1]))
        k.finish()
    return k, g


def tap(g, name, shape, dt=F32):
    if name not in g.want:
        return None
    t = g.nc.dram_tensor('tap_' + name, list(shape), dt, kind='ExternalOutput').ap()
    g.taps[name] = t
    return t


def phase_pre(g):
    k, nc = g.k, g.nc
    identf = g.cs['ident_f']
    xTd = g.xT[0].rearrange('(c p) t -> p c t', p=128)
    with contextlib.ExitStack() as es:
        xin = [g.sb('pre_x%d' % i, [128, D], F32, es) for i in range(2)]
        xo = [g.sb('pre_o%d' % i, [128, 8, 128], F32, es) for i in range(2)]
        for t in range(NT):
            b = t % 2
            k.dma('sp', xin[b][:], g.x[t * 128:(t + 1) * 128, :], writes=['pre_x%d' % b])
            for h in range(2):
                pi = (2 * t + h) % 4
                for j in range(4):
                    cc = h * 4 + j
                    k.op('pe', lambda e, cc=cc, j=j, pi=pi, b=b: e.transpose(
                        out=g.ps[pi][:, j * 128:(j + 1) * 128], in_=xin[b][:, cc * 128:(cc + 1) * 128], identity=identf[:]),
                        reads=['pre_x%d' % b, 'c_ident_f'], writes=[g.psk[pi]])
                eng = 'act' if h == 0 else 'dve'
                if eng == 'act':
                    k.op('act', lambda e, pi=pi, b=b, h=h: e.copy(
                        out=xo[b][:, h * 4:(h + 1) * 4, :], in_=g.ps[pi][:].rearrange('p (c t) -> p c t', c=4)),
                        reads=[g.psk[pi]], writes=['pre_o%d_%d' % (b, h)])
                else:
                    k.op('dve', lambda e, pi=pi, b=b, h=h: e.tensor_copy(
                        xo[b][:, h * 4:(h + 1) * 4, :], g.ps[pi][:].rearrange('p (c t) -> p c t', c=4)),
                        reads=[g.psk[pi]], writes=['pre_o%d_%d' % (b, h)])
            k.dma('sp', xTd[:, :, t * 128:(t + 1) * 128], xo[b][:], reads=['pre_o%d_0' % b, 'pre_o%d_1' % b],
                  writes=['xT0_%d' % (t // 4)])
        k.barrier()


def phase_mod(g, l):
    k, nc = g.k, g.nc
    with contextlib.ExitStack() as es:
        cT = g.sb('cT', [128, 8], F32, es)
        cs = g.sb('cs', [128, 8], BF16, es)
        bcol = g.sb('bcol', [128, 48], F32, es)
        wt = [g.sb('wada%d' % i, [128, 8, 512], BF16, es) for i in range(2)]
        k.dma('sp', cT[:], g.c.rearrange('(k p) -> p k', p=128), writes=['cT'], allow_slow_non_contiguous=True)
        k.dma('sp', bcol[:], g.W['b_ada'][l].rearrange('(k p) -> p k', p=128), writes=['bcol'], allow_slow_non_contiguous=True)
        k.op('act', lambda e: e.activation(out=cs[:], in_=cT[:], func=AF.Silu), reads=['cT'], writes=['cs'])
        psm = g.ps[7]
        for ct in range(12):
            b = ct % 2
            k.dma('pool', wt[b][:], g.W['w_ada'][l][:, ct * 512:(ct + 1) * 512].rearrange('(k p) c -> p k c', p=128),
                  writes=['wada%d' % b])
            for j in range(4):
                col = ct * 4 + j
                for kk in range(8):
                    k.op('pe', lambda e, b=b, j=j, kk=kk, col=col: e.matmul(
                        psm[:, col:col + 1], lhsT=wt[b][:, kk, j * 128:(j + 1) * 128], rhs=cs[:, kk:kk + 1],
                        start=(kk == 0), stop=(kk == 7)),
                        reads=['wada%d' % b, 'cs'], writes=[g.psk[7]])
        k.op('dve', lambda e: e.tensor_tensor(out=g.mod[:], in0=psm[:, 0:48], in1=bcol[:], op=ALU.add),
             reads=[g.psk[7], 'bcol'], writes=['mod'])
        t = tap(g, 'mod%d' % l, [128, 48])
        if t is not None:
            k.dma('sp', t, g.mod[:], reads=['mod'], writes=['tap'])
        k.barrier()


def phase_norm1(g, l, xTd):
    k, nc = g.k, g.nc
    xTv = xTd.rearrange('(c p) t -> p c t', p=128)
    with contextlib.ExitStack() as es:
        gcol = g.sb('n1_g', [128, 8], F32, es)
        g1s = g.sb('n1_gs', [128, 8], F32, es)
        k.dma('sp', gcol[:], g.W['norm1_g'][l].rearrange('(k p) -> p k', p=128), writes=['n1_g'], allow_slow_non_contiguous=True)
        k.op('dve', lambda e: e.scalar_tensor_tensor(out=g1s[:], in0=g.mod[:, 8:16], scalar=1.0, in1=gcol[:],
                                                      op0=ALU.add, op1=ALU.mult), reads=['mod', 'n1_g'], writes=['n1_gs'])
        hT = g.sb('hT', [128, 8, S], BF16, es)
        norm_tiles(g, es, xTv, g1s, g.mod[:, 0:8], ['n1_gs', 'mod'], hT, 'hT', 'n1')
        hv = g.hTd.rearrange('(c p) t -> p c t', p=128)
        for c in range(8):
            k.dma('sp', hv[:, c, :], hT[:, c, :], reads=['hT%d' % c], writes=['hTd'])
        t = tap(g, 'h%d' % l, [128, 8, S], BF16)
        if t is not None:
            k.dma('sp', t, hT[:], reads=['hT%d' % i for i in range(8)], writes=['tap'])
        k.barrier()


def norm_tiles(g, es, xTv, gs, sh, gkeys, hT, hkey, pfx, xsrc_keys=None):
    k = g.k
    xt = [g.sb(pfx + '_x%d' % i, [128, 8, 512], F32, es) for i in range(2)]
    sq = [g.sb(pfx + '_sq%d' % i, [128, 8, 512], BF16, es) for i in range(2)]
    rstd = [g.sb(pfx + '_rstd%d' % i, [128, 512], F32, es) for i in range(2)]
    tmp = [g.sb(pfx + '_tmp%d' % i, [128, 512], F32, es) for i in range(2)]
    ones = g.cs['ones_b']
    for tt in range(8):
        b = tt % 2
        ts = slice(tt * 512, (tt + 1) * 512)
        xk, sk, rk = pfx + '_x%d' % b, pfx + '_sq%d' % b, pfx + '_rstd%d' % b
        k.dma('sp', xt[b][:], xTv[:, :, ts], reads=(xsrc_keys or []), writes=[xk])
        k.op('act', lambda e, b=b: e.activation(out=sq[b][:], in_=xt[b][:], func=AF.Square), reads=[xk], writes=[sk])
        pi = tt % 2
        for c in range(8):
            k.op('pe', lambda e, b=b, c=c, pi=pi: e.matmul(g.ps[pi][:], lhsT=ones[:], rhs=sq[b][:, c, :],
                                                          start=(c == 0), stop=(c == 7)),
                 reads=[sk, 'c_ones_b'], writes=[g.psk[pi]])
        k.op('dve', lambda e, b=b, pi=pi: e.tensor_scalar(out=rstd[b][:], in0=g.ps[pi][:], scalar1=1.0 / D, scalar2=NORM_EPS,
                                                         op0=ALU.mult, op1=ALU.add), reads=[g.psk[pi]], writes=[rk])
        k.op('act', lambda e, b=b: e.activation(out=rstd[b][:], in_=rstd[b][:], func=AF.Sqrt), reads=[rk], writes=[rk])
        k.op('dve', lambda e, b=b: e.reciprocal(out=rstd[b][:], in_=rstd[b][:]), reads=[rk], writes=[rk])
        for c in range(8):
            tb = c % 2
            tk = pfx + '_tmp%d' % tb
            k.op('dve', lambda e, b=b, c=c, tb=tb: e.tensor_tensor(out=tmp[tb][:], in0=xt[b][:, c, :], in1=rstd[b][:], op=ALU.mult),
                 reads=[xk, rk], writes=[tk])
            k.op('act', lambda e, c=c, tb=tb, ts=ts: e.activation(out=hT[:, c, ts], in_=tmp[tb][:], func=AF.Identity,
                                                                  scale=gs[:, c:c + 1], bias=sh[:, c:c + 1]),
                 reads=[tk] + gkeys, writes=[hkey + '%d' % c])


def make_in_maps(inputs, g, n_cores=8):
    consts = host_consts()
    maps = []
    shared = {n: np.ascontiguousarray(inputs[n], dtype=np.float32) for n in g.W.d}
    for b in range(n_cores):
        m = {'x': np.ascontiguousarray(inputs['x'][b]), 'c': np.ascontiguousarray(inputs['c'][b]),
             'positions': np.ascontiguousarray(inputs['positions'][b]).astype(np.int32)}
        m.update(shared)
        for n, a in consts.items():
            m['const_' + n] = a
        maps.append(m)
    return maps


def kernel(**inputs):
    k, g = build_program()
    maps = make_in_maps(inputs, g)
    res = run_bass_kernel_spmd(k.nc, maps, core_ids=list(range(8)))
    return np.stack([r['out'] for r in res.results], axis=0)


def load_w_bf16(g, dst, dkey, src_ap):
    g.k.dma('pool', dst, src_ap.rearrange('(c p) n -> p c n', p=128), writes=[dkey])


def ht_loader(g, es, pfx):
    bufs = [g.sb(pfx + '_ht%d' % i, [128, 8, 512], BF16, es) for i in range(2)]
    hv = g.hTd.rearrange('(c p) t -> p c t', p=128)

    def load(tt):
        b = tt % 2
        g.k.dma('sp', bufs[b][:], hv[:, :, tt * 512:(tt + 1) * 512], reads=['hTd'], writes=[pfx + '_ht%d' % b])
        return bufs[b], pfx + '_ht%d' % b
    return load


def proj_fm(g, wsb, wkey, c0, ncols, ht, hkey, ps, pskey):
    for c in range(8):
        g.k.op('pe', lambda e, c=c: e.matmul(ps, lhsT=wsb[:, c, c0:c0 + ncols], rhs=ht[:, c, :],
                                             start=(c == 0), stop=(c == 7)),
               reads=[wkey, hkey], writes=[pskey])


def proj_tm(g, wsb, wkey, c0, ncols, ht, hkey, sub, ps, pskey):
    for c in range(8):
        g.k.op('pe', lambda e, c=c: e.matmul(ps, lhsT=ht[:, c, sub * 128:(sub + 1) * 128], rhs=wsb[:, c, c0:c0 + ncols],
                                             start=(c == 0), stop=(c == 7)),
               reads=[wkey, hkey], writes=[pskey])


def head_rmsnorm_fm(g, y, ykey, sq, sqkey, gcol, gkeys, out_ap, okey, pi, tmpf, tmpkey):
    k = g.k
    k.op('act', lambda e: e.activation(out=sq, in_=y, func=AF.Square), reads=[ykey], writes=[sqkey])
    k.op('pe', lambda e: e.matmul(g.ps[pi][:], lhsT=g.cs['blk64_b'][:], rhs=sq, start=True, stop=True),
         reads=[sqkey, 'c_blk64_b'], writes=[g.psk[pi]])
    k.op('dve', lambda e: e.tensor_scalar(out=tmpf, in0=g.ps[pi][:], scalar1=1.0 / 64, scalar2=NORM_EPS, op0=ALU.mult, op1=ALU.add),
         reads=[g.psk[pi]], writes=[tmpkey])
    k.op('act', lambda e: e.activation(out=tmpf, in_=tmpf, func=AF.Sqrt), reads=[tmpkey], writes=[tmpkey])
    k.op('dve', lambda e: e.reciprocal(out=tmpf, in_=tmpf), reads=[tmpkey], writes=[tmpkey])
    k.op('dve', lambda e: e.scalar_tensor_tensor(out=out_ap, in0=y, scalar=gcol, in1=tmpf, op0=ALU.mult, op1=ALU.mult),
         reads=[ykey, tmpkey] + gkeys, writes=[okey])


def mixer_conv(g, l):
    k = g.k
    ycv = g.ycatT.rearrange('(c p) t -> p c t', p=128)
    with contextlib.ExitStack() as es:
        wb = g.sb('cv_w', [128, 8, 768], BF16, es)
        load_w_bf16(g, wb[:], 'cv_w', g.W['w_in'][l][:, 1024:1792])
        cw = g.sb('cv_cw', [128, 2, 3], F32, es)
        for kk in range(3):
            k.dma('sp', cw[:, :, kk], g.W['conv_w'][l, kk].rearrange('(j p) -> p j', p=128), writes=['cv_cw'],
                  allow_slow_non_contiguous=True)
        bg = g.sb('cv_bg', [128, 2, S], BF16, es)
        u = g.sb('cv_u', [128, 2, S + 2], F32, es)
        cgt = [g.sb('cv_cg%d' % i, [128, 512], F32, es) for i in range(2)]
        k.op('pool', lambda e: e.memset(u[:, :, 0:2], 0.0), writes=['cv_u_h'])
        n = 0
        hload = ht_loader(g, es, 'cv')
        for tt in range(8):
            ts = slice(tt * 512, (tt + 1) * 512)
            ht, hk = hload(tt)
            for j in range(2):
                pa, pb, pc = n % 6, (n + 1) % 6, (n + 2) % 6
                n += 3
                proj_fm(g, wb, 'cv_w', j * 128, 128, ht, hk, g.ps[pa][:], g.psk[pa])
                proj_fm(g, wb, 'cv_w', 256 + j * 128, 128, ht, hk, g.ps[pb][:], g.psk[pb])
                proj_fm(g, wb, 'cv_w', 512 + j * 128, 128, ht, hk, g.ps[pc][:], g.psk[pc])
                k.op('act', lambda e, j=j, ts=ts, pa=pa: e.copy(out=bg[:, j, ts], in_=g.ps[pa][:]), reads=[g.psk[pa]],
                     writes=['cv_bg%d_%d' % (j, tt)])
                cb = n % 2
                k.op('act', lambda e, cb=cb, pb=pb: e.copy(out=cgt[cb][:], in_=g.ps[pb][:]), reads=[g.psk[pb]], writes=['cv_cg%d' % cb])
                k.op('dve', lambda e, j=j, tt=tt, cb=cb, pc=pc: e.tensor_tensor(out=u[:, j, 2 + tt * 512:2 + (tt + 1) * 512],
                                                                                 in0=g.ps[pc][:], in1=cgt[cb][:], op=ALU.mult),
                     reads=[g.psk[pc], 'cv_cg%d' % cb], writes=['cv_u%d_%d' % (j, tt)])
        y = [g.sb('cv_y%d' % i, [128, 512], F32, es) for i in range(2)]
        sq = [g.sb('cv_sq%d' % i, [128, 512], BF16, es) for i in range(2)]
        tf = [g.sb('cv_tf%d' % i, [128, 512], F32, es) for i in range(2)]
        yo = [g.sb('cv_yo%d' % i, [128, 2, 512], BF16, es) for i in range(2)]
        n = 0
        for tt in range(8):
            ts = slice(tt * 512, (tt + 1) * 512)
            ob = tt % 2
            for j in range(2):
                b = n % 2
                n += 1
                yk = 'cv_y%d' % b
                ukeys = ['cv_u%d_%d' % (j, tt), 'cv_u_h'] + (['cv_u%d_%d' % (j, tt - 1)] if tt > 0 else [])
                k.op('act', lambda e, b=b, j=j, tt=tt: e.activation(out=y[b][:], in_=u[:, j, 2 + tt * 512:2 + (tt + 1) * 512],
                                                                   func=AF.Copy, scale=cw[:, j, 2:3]),
                     reads=ukeys + ['cv_cw'], writes=[yk])
                for kk in (1, 0):
                    k.op('dve', lambda e, b=b, j=j, tt=tt, kk=kk: e.scalar_tensor_tensor(
                        out=y[b][:], in0=u[:, j, kk + tt * 512:kk + (tt + 1) * 512], scalar=cw[:, j, kk:kk + 1], in1=y[b][:],
                        op0=ALU.mult, op1=ALU.add), reads=ukeys + ['cv_cw', yk], writes=[yk])
                k.op('dve', lambda e, b=b, j=j, ts=ts: e.tensor_tensor(out=y[b][:], in0=y[b][:], in1=bg[:, j, ts], op=ALU.mult),
                     reads=[yk, 'cv_bg%d_%d' % (j, tt)], writes=[yk])
                head_rmsnorm_fm(g, y[b][:], yk, sq[b][:], 'cv_sq%d' % b, g.onorm[:, j:j + 1], ['onorm'], yo[ob][:, j, :],
                                'cv_yo%d_%d' % (ob, j), 6 + b, tf[b][:], 'cv_tf%d' % b)
            k.dma('sp', ycv[:, 2:4, ts], yo[ob][:], reads=['cv_yo%d_0' % ob, 'cv_yo%d_1' % ob], writes=['ycat_B%d' % tt])
        k.barrier()


def phase_wout(g, l, xT_old, xT_new):
    k = g.k
    ycv = g.ycatT.rearrange('(c p) t -> p c t', p=128)
    xo = xT_old.rearrange('(c p) t -> p c t', p=128)
    xn = xT_new.rearrange('(c p) t -> p c t', p=128)
    with contextlib.ExitStack() as es:
        wo = g.sb('wo_w', [128, 8, D], BF16, es)
        load_w_bf16(g, wo[:], 'wo_w', g.W['w_out'][l])
        yc = [g.sb('wo_yc%d' % i, [128, 8, 512], BF16, es) for i in range(2)]
        xt = [g.sb('wo_x%d' % i, [128, 8, 512], F32, es) for i in range(2)]
        n = 0
        for tt in range(8):
            b = tt % 2
            ts = slice(tt * 512, (tt + 1) * 512)
            k.dma('sp', yc[b][:], ycv[:, :, ts], writes=['wo_yc%d' % b])
            k.dma('sp', xt[b][:], xo[:, :, ts], writes=['wo_x%d' % b])
            for dc in range(8):
                pi = n % 4
                n += 1
                for c in range(8):
                    k.op('pe', lambda e, b=b, c=c, dc=dc, pi=pi: e.matmul(g.ps[pi][:], lhsT=wo[:, c, dc * 128:(dc + 1) * 128],
                                                                         rhs=yc[b][:, c, :], start=(c == 0), stop=(c == 7)),
                         reads=['wo_w', 'wo_yc%d' % b], writes=[g.psk[pi]])
                k.op('dve', lambda e, b=b, dc=dc, pi=pi: e.scalar_tensor_tensor(
                    out=xt[b][:, dc, :], in0=g.ps[pi][:], scalar=g.mod[:, 16 + dc:17 + dc], in1=xt[b][:, dc, :],
                    op0=ALU.mult, op1=ALU.add), reads=[g.psk[pi], 'mod', 'wo_x%d' % b], writes=['wo_x%d' % b])
            k.dma('sp', xn[:, :, ts], xt[b][:], reads=['wo_x%d' % b], writes=['xTn_%d' % tt])
        t = tap(g, 'x1T%d' % l, [128, 8, S])
        if t is not None:
            k.barrier()
            k.dma('sp', t, xn, writes=['tap'])
        k.barrier()


def phase_norm2_router(g, l, xT1):
    k = g.k
    xv = xT1.rearrange('(c p) t -> p c t', p=128)
    h2v = g.h2Td.rearrange('(c p) t -> p c t', p=128)
    with contextlib.ExitStack() as es:
        gcol = g.sb('n2_g', [128, 8], F32, es)
        g2s = g.sb('n2_gs', [128, 8], F32, es)
        k.dma('sp', gcol[:], g.W['norm2_g'][l].rearrange('(k p) -> p k', p=128), writes=['n2_g'], allow_slow_non_contiguous=True)
        k.op('dve', lambda e: e.scalar_tensor_tensor(out=g2s[:], in0=g.mod[:, 32:40], scalar=1.0, in1=gcol[:],
                                                      op0=ALU.add, op1=ALU.mult), reads=['mod', 'n2_g'], writes=['n2_gs'])
        h2 = g.sb('n2_h', [128, 8, S], BF16, es)
        norm_tiles(g, es, xv, g2s, g.mod[:, 24:32], ['n2_gs', 'mod'], h2, 'n2_h', 'n2')
        for c in range(8):
            k.dma('sp', h2v[:, c, :], h2[:, c, :], reads=['n2_h%d' % c], writes=['h2Td%d' % c])
        t = tap(g, 'h2T%d' % l, [128, 8, S], BF16)
        if t is not None:
            k.dma('sp', t, h2[:], reads=['n2_h%d' % c for c in range(8)], writes=['tap'])
        wr = g.sb('rt_w', [128, 8, 32], BF16, es)
        load_w_bf16(g, wr[:], 'rt_w', g.W['w_router'][l])
        br = g.sb('rt_b', [1, 32], BF16, es)
        k.dma('pool', br[:], g.W['b_router'][l:l + 1, :], writes=['rt_b'])
        lg = [g.sb('rt_lg%d' % i, [128, 32], F32, es) for i in range(2)]
        m8 = [g.sb('rt_m8%d' % i, [128, 8], F32, es) for i in range(2)]
        nm = [g.sb('rt_nm%d' % i, [128, 1], F32, es) for i in range(2)]
        ex = [g.sb('rt_ex%d' % i, [128, 32], F32, es) for i in range(2)]
        mk = [g.sb('rt_mk%d' % i, [128, 32], F32, es) for i in range(2)]
        sm = [g.sb('rt_sm%d' % i, [128, 1], F32, es) for i in range(2)]
        Gt = [g.sb('rt_G%d' % i, [128, 32], F32, es) for i in range(2)]
        gtap = tap(g, 'G%d' % l, [S, 32])
        for t_ in range(NT):
            b = t_ % 2
            pi = t_ % 2
            tk = slice(t_ * 128, (t_ + 1) * 128)
            for c in range(8):
                k.op('pe', lambda e, c=c, tk=tk, pi=pi: e.matmul(g.ps[pi][:, 0:32], lhsT=h2[:, c, tk], rhs=wr[:, c, :],
                                                               start=(c == 0), stop=False),
                     reads=['n2_h%d' % c, 'rt_w'], writes=[g.psk[pi]])
            k.op('pe', lambda e, pi=pi: e.matmul(g.ps[pi][:, 0:32], lhsT=g.cs['ones_b'][0:1, :], rhs=br[:], start=False, stop=True),
                 reads=['c_ones_b', 'rt_b'], writes=[g.psk[pi]])
            s = str(b)
            k.op('act', lambda e, b=b, pi=pi: e.copy(out=lg[b][:], in_=g.ps[pi][:, 0:32]), reads=[g.psk[pi]], writes=['rt_lg' + s])
            k.op('dve', lambda e, b=b: e.max(out=m8[b][:], in_=lg[b][:]), reads=['rt_lg' + s], writes=['rt_m8' + s])
            k.op('dve', lambda e, b=b: e.tensor_scalar(out=nm[b][:], in0=m8[b][:, 0:1], scalar1=-1.0, scalar2=None, op0=ALU.mult),
                 reads=['rt_m8' + s], writes=['rt_nm' + s])
            k.op('act', lambda e, b=b: e.activation(out=ex[b][:], in_=lg[b][:], func=AF.Exp, bias=nm[b][:], scale=1.0),
                 reads=['rt_lg' + s, 'rt_nm' + s], writes=['rt_ex' + s])
            k.op('dve', lambda e, b=b: e.tensor_scalar(out=mk[b][:], in0=lg[b][:], scalar1=m8[b][:, 3:4], scalar2=None, op0=ALU.is_ge),
                 reads=['rt_lg' + s, 'rt_m8' + s], writes=['rt_mk' + s])
            k.op('dve', lambda e, b=b: e.tensor_tensor(out=ex[b][:], in0=ex[b][:], in1=mk[b][:], op=ALU.mult),
                 reads=['rt_ex' + s, 'rt_mk' + s], writes=['rt_ex' + s])
            k.op('dve', lambda e, b=b: e.reduce_sum(out=sm[b][:], in_=ex[b][:], axis=AX.X), reads=['rt_ex' + s], writes=['rt_sm' + s])
            k.op('dve', lambda e, b=b: e.reciprocal(out=sm[b][:], in_=sm[b][:]), reads=['rt_sm' + s], writes=['rt_sm' + s])
            k.op('dve', lambda e, b=b: e.tensor_scalar(out=Gt[b][:], in0=ex[b][:], scalar1=sm[b][:], scalar2=None, op0=ALU.mult),
                 reads=['rt_ex' + s, 'rt_sm' + s], writes=['rt_G' + s])
            if gtap is not None:
                k.dma('sp', gtap[tk, :], Gt[b][:], reads=['rt_G' + s], writes=['tap'])
            pj = 2 + t_ % 2
            k.op('pe', lambda e, b=b, pj=pj: e.transpose(out=g.ps[pj][0:32, 0:128], in_=Gt[b][:], identity=g.cs['ident_f'][:]),
                 reads=['rt_G' + s, 'c_ident_f'], writes=[g.psk[pj]])
            k.op('act', lambda e, tk=tk, pj=pj: e.copy(out=g.GT[:, tk], in_=g.ps[pj][0:32, 0:128]), reads=[g.psk[pj]],
                 writes=['GT%d' % (t_ // 4)])
        k.barrier()


def phase_moe(g, l, xT1, xT2, final):
    k = g.k
    x1v = xT1.rearrange('(c p) t -> p c t', p=128)
    x2v = xT2.rearrange('(c p) t -> p c t', p=128)
    h2v = g.h2Td.rearrange('(c p) t -> p c t', p=128)
    NE = 32 if g.do_moe else 0
    QT = 1024
    with contextlib.ExitStack() as es:
        bgc = g.sb('me_bgc', [128, 16, 32], F32, es)
        bdr = g.sb('me_bdr', [32, D], BF16, es)
        k.dma('pool', bdr[:], g.W['b_down'][l], writes=['me_bdr'])
        with contextlib.ExitStack() as es2:
            bgr = g.sb('me_bgr', [32, 2048], F32, es2)
            k.dma('sp', bgr[:], g.W['b_gu'][l], writes=['me_bgr'])
            for j in range(16):
                pi = j % 2
                k.op('pe', lambda e, j=j, pi=pi: e.transpose(out=g.ps[pi][:, 0:32], in_=bgr[:, j * 128:(j + 1) * 128],
                                                             identity=g.cs['ident_f'][0:32, 0:32]),
                     reads=['me_bgr', 'c_ident_f'], writes=[g.psk[pi]])
                k.op('act', lambda e, j=j, pi=pi: e.copy(out=bgc[:, j, :], in_=g.ps[pi][:, 0:32]), reads=[g.psk[pi]], writes=['me_bgc'])
            k.barrier()
        wgu = [g.sb('me_wgu%d' % i, [128, 8, 2048], BF16, es) for i in range(2)]
        wdn = [g.sb('me_wdn%d' % i, [128, 8, D], BF16, es) for i in range(2)]
        h2q = g.sb('me_h2', [128, 8, QT], BF16, es)
        yacc = g.sb('me_yacc', [128, 8, QT], F32, es)
        act = [g.sb('me_act%d' % i, [128, 8, 512], BF16, es) for i in range(2)]
        gbc = [g.sb('me_gbc%d' % i, [128, 512], BF16, es) for i in range(2)]
        NSET = 3
        gq = [g.sb('me_gq%d' % i, [128, 512], F32, es) for i in range(NSET)]
        sg = [g.sb('me_sg%d' % i, [128, 512], F32, es) for i in range(NSET)]
        uq = [g.sb('me_uq%d' % i, [128, 512], F32, es) for i in range(NSET)]
        k.op('dve', lambda e: e.tensor_scalar(out=bgc[:, 8:16, :], in0=bgc[:, 8:16, :], scalar1=1.0, scalar2=None, op0=ALU.add),
             reads=['me_bgc'], writes=['me_bgc'])
        nf = 0
        xrow = [g.sb('me_xr%d' % i, [128, D], F32, es) for i in range(1)] if final else None
        nw = 0
        nit = 0
        npsum = 0
        for q in range(S // QT):
            qs = slice(q * QT, (q + 1) * QT)
            k.dma('sp', h2q[:], h2v[:, :, qs], reads=['h2Td%d' % c for c in range(8)], writes=['me_h2'])
            for e_ in range(NE):
                wb = nw % 2
                nw += 1
                gk, dk = 'me_wgu%d' % wb, 'me_wdn%d' % wb
                load_w_bf16(g, wgu[wb][:], gk, g.W['w_gu'][l, e_])
                load_w_bf16(g, wdn[wb][:], dk, g.W['w_down'][l, e_])
                for tl in range(QT // 512):
                    ts = slice(tl * 512, (tl + 1) * 512)
                    gts = slice(q * QT + tl * 512, q * QT + (tl + 1) * 512)
                    ab = nit % 2
                    nit += 1
                    ak = 'me_act%d' % ab
                    pg = 7
                    k.op('pe', lambda e, e_=e_, gts=gts, pg=pg: e.matmul(g.ps[pg][:], lhsT=g.cs['sel_b'][:, e_, :], rhs=g.GT[:, gts],
                                                                        start=True, stop=True),
                         reads=['c_sel_b'] + ['GT%d' % i for i in range(8)], writes=[g.psk[pg]])
                    k.op('act', lambda e, ab=ab, pg=pg: e.copy(out=gbc[ab][:], in_=g.ps[pg][:]), reads=[g.psk[pg]], writes=['me_gbc%d' % ab])
                    for f in range(8):
                        fb = nf % NSET
                        nf += 1
                        pa, pb = 2 * fb, 2 * fb + 1
                        for c in range(8):
                            k.op('pe', lambda e, c=c, f=f, wb=wb, ts=ts, pa=pa: e.matmul(
                                g.ps[pa][:], lhsT=wgu[wb][:, c, f * 128:(f + 1) * 128], rhs=h2q[:, c, ts], start=(c == 0), stop=(c == 7)),
                                reads=[gk, 'me_h2'], writes=[g.psk[pa]])
                        for c in range(8):
                            k.op('pe', lambda e, c=c, f=f, wb=wb, ts=ts, pb=pb: e.matmul(
                                g.ps[pb][:], lhsT=wgu[wb][:, c, 1024 + f * 128:1024 + (f + 1) * 128], rhs=h2q[:, c, ts],
                                start=(c == 0), stop=(c == 7)), reads=[gk, 'me_h2'], writes=[g.psk[pb]])
                        fs = str(fb)
                        k.op('dve', lambda e, f=f, fb=fb, pa=pa, e_=e_: e.tensor_scalar(
                            out=gq[fb][:], in0=g.ps[pa][:], scalar1=bgc[:, f, e_:e_ + 1], scalar2=7.0, op0=ALU.add, op1=ALU.min),
                            reads=[g.psk[pa], 'me_bgc'], writes=['me_gq' + fs])
                        k.op('act', lambda e, fb=fb: e.activation(out=sg[fb][:], in_=gq[fb][:], func=AF.Sigmoid, scale=1.702),
                             reads=['me_gq' + fs], writes=['me_sg' + fs])
                        k.op('dve', lambda e, f=f, fb=fb, pb=pb, e_=e_: e.tensor_scalar(
                            out=uq[fb][:], in0=g.ps[pb][:], scalar1=bgc[:, 8 + f, e_:e_ + 1], scalar2=-6.0, op0=ALU.add, op1=ALU.max),
                            reads=[g.psk[pb], 'me_bgc'], writes=['me_uq' + fs])
                        k.op('pool', lambda e, fb=fb: e.tensor_tensor(out=sg[fb][:], in0=sg[fb][:], in1=gq[fb][:], op=ALU.mult),
                             reads=['me_sg' + fs, 'me_gq' + fs], writes=['me_sg' + fs])
                        k.op('dve', lambda e, fb=fb: e.scalar_tensor_tensor(out=uq[fb][:], in0=uq[fb][:], scalar=8.0, in1=sg[fb][:],
                                                                            op0=ALU.min, op1=ALU.mult),
                             reads=['me_sg' + fs, 'me_uq' + fs], writes=['me_uq' + fs])
                        k.op('pool', lambda e, f=f, fb=fb, ab=ab: e.tensor_tensor(out=act[ab][:, f, :], in0=uq[fb][:], in1=gbc[ab][:], op=ALU.mult),
                             reads=['me_uq' + fs, 'me_gbc%d' % ab], writes=[ak])
                    for dc in range(8):
                        pd = 6 + dc % 2
                        for f in range(8):
                            k.op('pe', lambda e, f=f, dc=dc, wb=wb, ab=ab, pd=pd: e.matmul(
                                g.ps[pd][:], lhsT=wdn[wb][:, f, dc * 128:(dc + 1) * 128], rhs=act[ab][:, f, :],
                                start=(f == 0), stop=(f == 7 and e_ != 0)), reads=[dk, ak], writes=[g.psk[pd]])
                        if e_ == 0:
                            k.op('pe', lambda e, dc=dc, gts=gts, pd=pd: e.matmul(
                                g.ps[pd][:], lhsT=bdr[:, dc * 128:(dc + 1) * 128], rhs=g.GT[:, gts], start=False, stop=True),
                                reads=['me_bdr'] + ['GT%d' % i for i in range(8)], writes=[g.psk[pd]])
                            k.op('dve', lambda e, dc=dc, ts=ts, pd=pd: e.tensor_copy(yacc[:, dc, ts], g.ps[pd][:]),
                                 reads=[g.psk[pd]], writes=['me_yacc'])
                        else:
                            k.op('dve', lambda e, dc=dc, ts=ts, pd=pd: e.tensor_tensor(out=yacc[:, dc, ts], in0=yacc[:, dc, ts],
                                                                                    in1=g.ps[pd][:], op=ALU.add),
                                 reads=[g.psk[pd], 'me_yacc'], writes=['me_yacc'])
            if NE == 0:
                k.op('pool', lambda e: e.memset(yacc[:], 0.0), writes=['me_yacc'])
            for c in range(8):
                for tl in range(QT // 512):
                    ts = slice(tl * 512, (tl + 1) * 512)
                    gts = slice(q * QT + tl * 512, q * QT + (tl + 1) * 512)
                    fb = (c * 2 + tl) % 2
                    k.dma('sp', gq[fb][:], x1v[:, c, gts], writes=['me_gq%d' % fb])
                    k.op('dve', lambda e, c=c, ts=ts, fb=fb: e.scalar_tensor_tensor(
                        out=yacc[:, c, ts], in0=yacc[:, c, ts], scalar=g.mod[:, 40 + c:41 + c], in1=gq[fb][:], op0=ALU.mult, op1=ALU.add),
                        reads=['me_yacc', 'mod', 'me_gq%d' % fb], writes=['me_yacc'])
            if not final:
                k.dma('sp', x2v[:, :, qs], yacc[:], reads=['me_yacc'], writes=['xT2_%d' % q])
            else:
                for t_ in range(QT // 128):
                    tg = q * (QT // 128) + t_
                    ob = 0
                    for h in range(2):
                        pi = (2 * t_ + h) % 4
                        for j in range(4):
                            cc = h * 4 + j
                            k.op('pe', lambda e, cc=cc, j=j, pi=pi, t_=t_: e.transpose(
                                out=g.ps[pi][:, j * 128:(j + 1) * 128], in_=yacc[:, cc, t_ * 128:(t_ + 1) * 128],
                                identity=g.cs['ident_f'][:]), reads=['me_yacc', 'c_ident_f'], writes=[g.psk[pi]])
                        k.op('act' if h == 0 else 'dve',
                             (lambda e, pi=pi, ob=ob, h=h: e.copy(out=xrow[ob][:, h * 512:(h + 1) * 512], in_=g.ps[pi][:])) if h == 0 else
                             (lambda e, pi=pi, ob=ob, h=h: e.tensor_copy(xrow[ob][:, h * 512:(h + 1) * 512], g.ps[pi][:])),
                             reads=[g.psk[pi]], writes=['me_xr%d_%d' % (ob, h)])
                    k.dma('sp', g.out[tg * 128:(tg + 1) * 128, :], xrow[ob][:], reads=['me_xr%d_0' % ob, 'me_xr%d_1' % ob], writes=['out'])
            tp = tap(g, 'x2T%d_q%d' % (l, q), [128, 8, QT])
            if tp is not None:
                k.dma('sp', tp, yacc[:], reads=['me_yacc'], writes=['tap'])
        k.barrier()


def zero_missing(g):
    k = g.k
    miss = [i for i, m in enumerate('ABCD') if m not in g.mixers]
    if not miss:
        return
    with contextlib.ExitStack() as es:
        z = g.sb('zz', [128, S], BF16, es)
        k.op('pool', lambda e: e.memset(z[:], 0.0), writes=['zz'])
        for i in miss:
            for j in range(2):
                r0 = i * 256 + j * 128
                k.dma('sp', g.ycatT[r0:r0 + 128, :], z[:], reads=['zz'], writes=['ycat_z'])
        k.barrier()


def load_const(g, es, name):
    ap = g.C[name]
    t = g.sb('c_' + name, ap.shape, ap.dtype, es)
    g.k.dma('sp', t[:], ap, writes=['c_' + name])
    return t


def rope_tables(g, es):
    k = g.k
    cos = g.sb('rp_cos', [128, S], F32, es)
    sin = g.sb('rp_sin', [128, S], F32, es)
    invf = g.cs['invf']
    CH = 1024
    with contextlib.ExitStack() as es2:
        posi = g.sb('rp_pi', [128, CH], I32, es2)
        ang = g.sb('rp_ang', [128, CH], F32, es2)
        tq = g.sb('rp_t', [128, CH], F32, es2)
        ti = g.sb('rp_ti', [128, CH], I32, es2)
        r = g.sb('rp_r', [128, CH], F32, es2)
        m = g.sb('rp_m', [128, CH], F32, es2)
        for ch in range(S // CH):
            cs_ = slice(ch * CH, (ch + 1) * CH)
            k.dma('sp', posi[:], g.pos[cs_].partition_broadcast(128), writes=['rp_pi'])
            k.op('dve', lambda e: e.tensor_copy(ang[:], posi[:]), reads=['rp_pi'], writes=['rp_ang'])
            k.op('dve', lambda e: e.tensor_scalar(out=ang[:], in0=ang[:], scalar1=invf[:, 0:1], scalar2=None, op0=ALU.mult),
                 reads=['rp_ang', 'c_invf'], writes=['rp_ang'])
            for which, dst, dkey in ((0, sin, 'rp_sin'), (1, cos, 'rp_cos')):
                shift = 0.0 if which == 0 else PI / 2
                k.op('dve', lambda e, shift=shift: e.tensor_scalar(out=tq[:], in0=ang[:], scalar1=shift, scalar2=1.0 / (2 * PI),
                                                                    op0=ALU.add, op1=ALU.mult), reads=['rp_ang'], writes=['rp_t'])
                k.op('dve', lambda e: e.tensor_copy(ti[:], tq[:]), reads=['rp_t'], writes=['rp_ti'])
                k.op('dve', lambda e: e.tensor_copy(tq[:], ti[:]), reads=['rp_ti'], writes=['rp_t'])
                k.op('dve', lambda e: e.scalar_tensor_tensor(out=r[:], in0=tq[:], scalar=-2 * PI, in1=ang[:], op0=ALU.mult, op1=ALU.add),
                     reads=['rp_t', 'rp_ang'], writes=['rp_r'])
                if shift != 0.0:
                    k.op('dve', lambda e, shift=shift: e.tensor_scalar(out=r[:], in0=r[:], scalar1=shift, scalar2=None, op0=ALU.add),
                         reads=['rp_r'], writes=['rp_r'])
                k.op('dve', lambda e: e.tensor_scalar(out=m[:], in0=r[:], scalar1=PI, scalar2=-2 * PI, op0=ALU.is_gt, op1=ALU.mult),
                     reads=['rp_r'], writes=['rp_m'])
                k.op('dve', lambda e: e.tensor_tensor(out=r[:], in0=r[:], in1=m[:], op=ALU.add), reads=['rp_r', 'rp_m'], writes=['rp_r'])
                k.op('dve', lambda e: e.tensor_scalar(out=m[:], in0=r[:], scalar1=-PI, scalar2=2 * PI, op0=ALU.is_lt, op1=ALU.mult),
                     reads=['rp_r'], writes=['rp_m'])
                k.op('dve', lambda e: e.tensor_tensor(out=r[:], in0=r[:], in1=m[:], op=ALU.add), reads=['rp_r', 'rp_m'], writes=['rp_r'])
                k.op('act', lambda e, dst=dst, cs_=cs_: e.activation(out=dst[:, cs_], in_=r[:], func=AF.Sin), reads=['rp_r'],
                     writes=[dkey])
        k.barrier()
    return cos, sin


class NormRope:
    def __init__(self, g, es, pfx):
        self.g = g
        self.pfx = pfx
        self.n = 0
        mk = lambda nm, dt: [g.sb('%s_%s%d' % (pfx, nm, i), [128, 512], dt, es) for i in range(2)]
        self.xf = mk('xf', F32)
        self.sq = mk('sq', BF16)
        self.rs = mk('rs', F32)
        self.xn = mk('xn', F32)
        self.xb = mk('xb', BF16)
        self.t1 = mk('t1', F32)

    def run(self, ps, pskey, gcol, gkeys, ts, cos=None, sin=None, out_r=None, okr=None, out_n=None, okn=None, pbank=2):
        g, k, pfx = self.g, self.g.k, self.pfx
        b = self.n % 2
        self.n += 1
        K_ = lambda nm: '%s_%s%d' % (pfx, nm, b)
        xf, sq, rs, xn, xb, t1 = self.xf[b], self.sq[b], self.rs[b], self.xn[b], self.xb[b], self.t1[b]
        k.op('act', lambda e: e.copy(out=xf[:], in_=ps), reads=[pskey], writes=[K_('xf')])
        k.op('act', lambda e: e.activation(out=sq[:], in_=xf[:], func=AF.Square), reads=[K_('xf')], writes=[K_('sq')])
        pi = pbank + b
        k.op('pe', lambda e: e.matmul(g.ps[pi][:], lhsT=g.cs['blk64_b'][:], rhs=sq[:], start=True, stop=True),
             reads=[K_('sq'), 'c_blk64_b'], writes=[g.psk[pi]])
        k.op('dve', lambda e: e.tensor_scalar(out=rs[:], in0=g.ps[pi][:], scalar1=1.0 / 64, scalar2=NORM_EPS, op0=ALU.mult, op1=ALU.add),
             reads=[g.psk[pi]], writes=[K_('rs')])
        k.op('act', lambda e: e.activation(out=rs[:], in_=rs[:], func=AF.Sqrt), reads=[K_('rs')], writes=[K_('rs')])
        k.op('dve', lambda e: e.reciprocal(out=rs[:], in_=rs[:]), reads=[K_('rs')], writes=[K_('rs')])
        k.op('dve', lambda e: e.scalar_tensor_tensor(out=xn[:], in0=xf[:], scalar=gcol, in1=rs[:], op0=ALU.mult, op1=ALU.mult),
             reads=[K_('xf'), K_('rs')] + gkeys, writes=[K_('xn')])
        if out_n is not None:
            k.op('pool', lambda e: e.tensor_copy(out_n, xn[:]), reads=[K_('xn')], writes=[okn])
        if out_r is not None:
            k.op('act', lambda e: e.copy(out=xb[:], in_=xn[:]), reads=[K_('xn')], writes=[K_('xb')])
            k.op('pe', lambda e: e.matmul(g.ps[pi][:], lhsT=g.cs['prot_b'][:], rhs=xb[:], start=True, stop=True),
                 reads=[K_('xb'), 'c_prot_b'], writes=[g.psk[pi]])
            k.op('dve', lambda e: e.tensor_tensor(out=t1[:], in0=g.ps[pi][:], in1=sin[:, ts], op=ALU.mult),
                 reads=[g.psk[pi], 'rp_sin'], writes=[K_('t1')])
            k.op('pool', lambda e: e.tensor_tensor(out=xn[:], in0=xn[:], in1=cos[:, ts], op=ALU.mult),
                 reads=[K_('xn'), 'rp_cos'], writes=[K_('xn')])
            k.op('dve', lambda e: e.tensor_tensor(out=out_r, in0=xn[:], in1=t1[:], op=ALU.add),
                 reads=[K_('xn'), K_('t1')], writes=[okr])


def attention(g, es, pfx, heads, ngroups, ktiles, qT, kT, vfn, ncols, masks, epilogue, scale=0.125):
    k = g.k
    pt = [g.sb('%s_pt%d' % (pfx, i), [128, 512], BF16, es) for i in range(3)]
    n = 0
    for h in heads:
        for gi in range(ngroups):
            kts = ktiles(gi)
            if not kts:
                continue
            for idx, kt in enumerate(kts):
                sbk = n % 2
                pb = n % 3
                n += 1
                qa, qk = qT(h, gi)
                ka, kk, nk = kT(h, kt)
                k.op('pe', lambda e, sbk=sbk, ka=ka, qa=qa, nk=nk: e.matmul(g.ps[sbk][0:nk, :], lhsT=ka, rhs=qa, start=True, stop=True),
                     reads=qk + kk, writes=[g.psk[sbk]])
                ptk = '%s_pt%d' % (pfx, pb)
                k.op('act', lambda e, sbk=sbk, pb=pb, nk=nk: e.activation(out=pt[pb][0:nk, :], in_=g.ps[sbk][0:nk, :], func=AF.Exp, scale=scale),
                     reads=[g.psk[sbk]], writes=[ptk])
                for (ma, mk_, is_ps) in masks(h, gi, kt):
                    eng = 'dve' if (is_ps or n % 2 == 0) else 'pool'
                    k.op(eng, lambda e, pb=pb, ma=ma, nk=nk: e.tensor_tensor(out=pt[pb][0:nk, :], in0=pt[pb][0:nk, :], in1=ma[0:nk, :], op=ALU.mult),
                         reads=[ptk] + mk_, writes=[ptk])
                va, vk = vfn(h, kt)
                for sub in range(4):
                    k.op('pe', lambda e, sub=sub, pb=pb, va=va, nk=nk, idx=idx: e.matmul(
                        g.ps[4 + sub][:, 0:ncols], lhsT=pt[pb][0:nk, sub * 128:(sub + 1) * 128], rhs=va,
                        start=(idx == 0), stop=(idx == len(kts) - 1)), reads=[ptk] + vk, writes=[g.psk[4 + sub]])
            for sub in range(4):
                epilogue(h, gi, sub, g.ps[4 + sub], g.psk[4 + sub])


def finish_tm(g, es, pfx, ytm, ykeyfn, gb, gbkey, row0):
    k = g.k
    ycv = g.ycatT.rearrange('(c p) t -> p c t', p=128)
    sq = [g.sb(pfx + '_fsq%d' % i, [128, 256], F32, es) for i in range(2)]
    ss = [g.sb(pfx + '_fss%d' % i, [128, 4], F32, es) for i in range(2)]
    yb = [g.sb(pfx + '_fyb%d' % i, [128, 256], BF16, es) for i in range(2)]
    yT = [g.sb(pfx + '_fyT%d' % i, [128, 2, 128], BF16, es) for i in range(2)]
    psb = [g.ps[2][:].bitcast(BF16), g.ps[3][:].bitcast(BF16)]
    for t_ in range(NT):
        b = t_ % 2
        s_ = str(b)
        yk = ykeyfn(t_)
        k.op('pool', lambda e, b=b, t_=t_: e.tensor_tensor(out=sq[b][:], in0=ytm[:, t_, :], in1=ytm[:, t_, :], op=ALU.mult),
             reads=yk, writes=[pfx + '_fsq' + s_])
        k.op('dve', lambda e, b=b: e.reduce_sum(out=ss[b][:], in_=sq[b][:].rearrange('p (h d) -> p h d', d=64), axis=AX.X),
             reads=[pfx + '_fsq' + s_], writes=[pfx + '_fss' + s_])
        k.op('dve', lambda e, b=b: e.tensor_scalar(out=ss[b][:], in0=ss[b][:], scalar1=1.0 / 64, scalar2=NORM_EPS, op0=ALU.mult, op1=ALU.add),
             reads=[pfx + '_fss' + s_], writes=[pfx + '_fss' + s_])
        k.op('act', lambda e, b=b: e.activation(out=ss[b][:], in_=ss[b][:], func=AF.Sqrt), reads=[pfx + '_fss' + s_], writes=[pfx + '_fss' + s_])
        k.op('dve', lambda e, b=b: e.reciprocal(out=ss[b][:], in_=ss[b][:]), reads=[pfx + '_fss' + s_], writes=[pfx + '_fss' + s_])
        for h in range(4):
            k.op('dve', lambda e, b=b, h=h, t_=t_: e.scalar_tensor_tensor(
                out=yb[b][:, h * 64:(h + 1) * 64], in0=ytm[:, t_, h * 64:(h + 1) * 64], scalar=ss[b][:, h:h + 1],
                in1=gb[:, h * 64:(h + 1) * 64], op0=ALU.mult, op1=ALU.mult),
                reads=yk + [pfx + '_fss' + s_, gbkey], writes=[pfx + '_fyb' + s_])
        for j in range(2):
            k.op('pe', lambda e, b=b, j=j: e.transpose(out=psb[b][:, j * 128:(j + 1) * 128], in_=yb[b][:, j * 128:(j + 1) * 128],
                                                       identity=g.cs['ident_b'][:]),
                 reads=[pfx + '_fyb' + s_, 'c_ident_b'], writes=[g.psk[2 + b]])
        k.op('act', lambda e, b=b: e.copy(out=yT[b][:], in_=psb[b][:, 0:256].rearrange('p (j t) -> p j t', j=2)),
             reads=[g.psk[2 + b]], writes=[pfx + '_fyT' + s_])
        c0 = row0 // 128
        k.dma('sp', ycv[:, c0:c0 + 2, t_ * 128:(t_ + 1) * 128], yT[b][:], reads=[pfx + '_fyT' + s_], writes=['ycat_%s%d' % (pfx, t_)])


def mixer_dil(g, l):
    k = g.k
    with contextlib.ExitStack() as es:
        qT = g.sb('dl_qT', [128, 2, S], BF16, es)
        kT = g.sb('dl_kT', [128, 2, S], BF16, es)
        V = g.sb('dl_V', [128, NT, 4, 65], BF16, es)
        ytm = g.sb('dl_ytm', [128, NT, 256], F32, es)
        gb = g.sb('dl_gb', [128, 256], F32, es)
        k.dma('sp', gb[:], g.W['onorm_g'][l, 256:512].partition_broadcast(128), writes=['dl_gb'])
        k.op('pool', lambda e: e.memset(V[:, :, :, 64:65], 1.0), writes=['dl_Vone'])
        with contextlib.ExitStack() as es2:
            cos, sin = rope_tables(g, es2)
            w = g.sb('dl_w', [128, 8, 768], BF16, es2)
            load_w_bf16(g, w[:], 'dl_w', g.W['w_in'][l][:, 1792:2560])
            gq = g.sb('dl_gq', [128, 1], F32, es2)
            gk = g.sb('dl_gk', [128, 1], F32, es2)
            for hh in range(2):
                k.dma('sp', gq[hh * 64:(hh + 1) * 64, :], g.W['dil_q_g'][l].rearrange('(p o) -> p o', o=1), writes=['dl_gq'],
                      allow_slow_non_contiguous=True)
                k.dma('sp', gk[hh * 64:(hh + 1) * 64, :], g.W['dil_k_g'][l].rearrange('(p o) -> p o', o=1), writes=['dl_gk'],
                      allow_slow_non_contiguous=True)
            nr = NormRope(g, es2, 'dl')
            hload = ht_loader(g, es2, 'dl')
            n = 0
            for tt in range(8):
                ts = slice(tt * 512, (tt + 1) * 512)
                ht, hk = hload(tt)
                for j in range(2):
                    for (dst, dkey, c0, gcol, gkey) in ((qT, 'dl_qT', 0, gq, 'dl_gq'), (kT, 'dl_kT', 256, gk, 'dl_gk')):
                        pi = n % 2
                        n += 1
                        proj_fm(g, w, 'dl_w', c0 + j * 128, 128, ht, hk, g.ps[pi][:], g.psk[pi])
                        nr.run(g.ps[pi][:], g.psk[pi], gcol[:, 0:1], [gkey], ts, cos, sin, dst[:, j, ts], '%s%d_%d' % (dkey, j, tt))
                for sub in range(4):
                    pi = 4 + sub
                    t_ = tt * 4 + sub
                    proj_tm(g, w, 'dl_w', 512, 256, ht, hk, sub, g.ps[pi][:, 0:256], g.psk[pi])
                    k.op('act', lambda e, pi=pi, t_=t_: e.copy(out=V[:, t_, :, 0:64], in_=g.ps[pi][:, 0:256].rearrange('p (h d) -> p h d', d=64)),
                         reads=[g.psk[pi]], writes=['dl_V%d' % t_])
            k.barrier()
        msk = load_const(g, es, 'dil_mask')
        rd = [g.sb('dl_rd%d' % i, [128, 1], F32, es) for i in range(2)]
        cnt = [0]

        def q_of(h, gi):
            j, hp = h // 2, h % 2
            return qT[hp * 64:(hp + 1) * 64, j, gi * 512:(gi + 1) * 512], ['dl_qT%d_%d' % (j, gi)]

        def k_of(h, kt):
            j, hp = h // 2, h % 2
            return kT[hp * 64:(hp + 1) * 64, j, kt * 128:(kt + 1) * 128], ['dl_kT%d_%d' % (j, kt // 4)], 128

        def v_of(h, kt):
            return V[:, kt, h, :], ['dl_V%d' % kt, 'dl_Vone']

        def ktiles(gi):
            return [kt for kt in range(max(0, 4 * gi - 16), 4 * gi + 4)]

        def masks(h, gi, kt):
            return [(msk[:, (4 * gi - kt) + 3, :], ['c_dil_mask'], False)]

        def epi(h, gi, sub, acc, acck):
            b = cnt[0] % 2
            cnt[0] += 1
            t_ = gi * 4 + sub
            k.op('dve', lambda e: e.reciprocal(out=rd[b][:], in_=acc[:, 64:65]), reads=[acck], writes=['dl_rd%d' % b])
            k.op('dve', lambda e: e.tensor_scalar(out=ytm[:, t_, h * 64:(h + 1) * 64], in0=acc[:, 0:64], scalar1=rd[b][:, 0:1],
                                                   scalar2=None, op0=ALU.mult), reads=[acck, 'dl_rd%d' % b], writes=['dl_ytm%d_%d' % (t_, h)])

        attention(g, es, 'dl', range(4), 8, ktiles, q_of, k_of, v_of, 65, masks, epi)
        finish_tm(g, es, 'dl', ytm, lambda t_: ['dl_ytm%d_%d' % (t_, h) for h in range(4)], gb, 'dl_gb', 512)
        k.barrier()


NSA_STOP = None
NSA_SKIP = ''


def mixer_nsa(g, l):
    k = g.k
    W = g.W
    with contextlib.ExitStack() as es:
        qn = g.sb('ns_qn', [128, 2, S], BF16, es)
        qr = g.sb('ns_qr', [128, 2, S], BF16, es)
        ksT = g.sb('ns_ks', [128, S], BF16, es)
        kwT = g.sb('ns_kw', [128, S], BF16, es)
        kcvc = g.sb('ns_kcvc', [128, S], BF16, es)
        Vs = g.sb('ns_Vs', [128, NT, 66], BF16, es)
        Vw = g.sb('ns_Vw', [128, NT, 66], BF16, es)
        gate = g.sb('ns_gate', [128, NT, 12], F32, es)
        ytm = g.sb('ns_ytm', [128, NT, 256], F32, es)
        gb = g.sb('ns_gb', [128, 256], F32, es)
        k.dma('sp', gb[:], W['onorm_g'][l, 512:768].partition_broadcast(128), writes=['ns_gb'])
        k.op('pool', lambda e: e.memset(Vs[:, :, 64:65], 1.0), writes=['ns_Vs1'])
        k.op('pool', lambda e: e.memset(Vw[:, :, 64:65], 1.0), writes=['ns_Vw1'])
        with contextlib.ExitStack() as es2:
            cos, sin = rope_tables(g, es2)
            w = g.sb('ns_w', [128, 8, 652], BF16, es2)
            load_w_bf16(g, w[:], 'ns_w', W['w_in'][l][:, 2560:3212])
            wks = g.sb('ns_wks', [128, 8, 128], BF16, es2)
            wkw = g.sb('ns_wkw', [128, 8, 128], BF16, es2)
            for hh in range(2):
                load_w_bf16(g, wks[:, :, hh * 64:(hh + 1) * 64], 'ns_wks', W['w_in'][l][:, 2944:3008])
                load_w_bf16(g, wkw[:, :, hh * 64:(hh + 1) * 64], 'ns_wkw', W['w_in'][l][:, 3072:3136])
            gq = g.sb('ns_gq', [128, 1], F32, es2)
            gks = g.sb('ns_gks', [128, 1], F32, es2)
            gkw = g.sb('ns_gkw', [128, 1], F32, es2)
            for hh in range(2):
                for (dst, nm, key) in ((gq, 'nsa_q_g', 'ns_gq'), (gks, 'nsa_ks_g', 'ns_gks'), (gkw, 'nsa_kw_g', 'ns_gkw')):
                    k.dma('sp', dst[hh * 64:(hh + 1) * 64, :], W[nm][l].rearrange('(p o) -> p o', o=1), writes=[key],
                          allow_slow_non_contiguous=True)
            nr = NormRope(g, es2, 'ns')
            hload = ht_loader(g, es2, 'ns')
            n = 0
            for tt in range(8):
                ts = slice(tt * 512, (tt + 1) * 512)
                ht, hk = hload(tt)
                for j in range(2):
                    pi = n % 2
                    n += 1
                    proj_fm(g, w, 'ns_w', j * 128, 128, ht, hk, g.ps[pi][:], g.psk[pi])
                    nr.run(g.ps[pi][:], g.psk[pi], gq[:, 0:1], ['ns_gq'], ts, cos, sin, qr[:, j, ts], 'ns_qr%d_%d' % (j, tt),
                           None if 'a' in NSA_SKIP else qn[:, j, ts], 'ns_qn%d_%d' % (j, tt))
                for (wsb, wkey, gcol, gkey, dst, dkey) in ((wks, 'ns_wks', gks, 'ns_gks', ksT, 'ns_ks'), (wkw, 'ns_wkw', gkw, 'ns_gkw', kwT, 'ns_kw')):
                    if 'c' in NSA_SKIP:
                        continue
                    pi = n % 2
                    n += 1
                    proj_fm(g, wsb, wkey, 0, 128, ht, hk, g.ps[pi][:], g.psk[pi])
                    nr.run(g.ps[pi][:], g.psk[pi], gcol[:, 0:1], [gkey], ts, cos, sin, dst[:, ts], '%s_%d' % (dkey, tt))
                pi = n % 2
                n += 1
                proj_fm(g, w, 'ns_w', 256, 128, ht, hk, g.ps[pi][:], g.psk[pi])
                k.op('act', lambda e, pi=pi, ts=ts: e.copy(out=kcvc[:, ts], in_=g.ps[pi][:]), reads=[g.psk[pi]], writes=['ns_kcvc%d' % tt])
                for sub in range(4):
                    if 'b' in NSA_SKIP:
                        continue
                    pi = 4 + sub
                    t_ = tt * 4 + sub
                    proj_tm(g, w, 'ns_w', 448, 204, ht, hk, sub, g.ps[pi][:, 0:204], g.psk[pi])
                    k.op('act', lambda e, pi=pi, t_=t_: e.copy(out=Vs[:, t_, 0:64], in_=g.ps[pi][:, 0:64]), reads=[g.psk[pi]],
                         writes=['ns_Vs%d' % t_])
                    if 'e' not in NSA_SKIP:
                        k.op('dve', lambda e, pi=pi, t_=t_: e.tensor_copy(Vw[:, t_, 0:64], g.ps[pi][:, 128:192]), reads=[g.psk[pi]],
                             writes=['ns_Vw%d' % t_])
                    if 'd' not in NSA_SKIP:
                        k.op('act', lambda e, pi=pi, t_=t_: e.activation(out=gate[:, t_, :], in_=g.ps[pi][:, 192:204], func=AF.Sigmoid),
                             reads=[g.psk[pi]], writes=['ns_gate%d' % t_])
            k.barrier()
        if NSA_STOP == 'proj':
            return
        kcT = g.sb('ns_kcT', [128, 512], BF16, es)
        Vc = [g.sb('ns_Vc%d' % j, [128, 129], BF16, es) for j in range(2)]
        ovl1 = load_const(g, es, 'ovl1')
        with contextlib.ExitStack() as es3:
            W1 = g.sb('ns_W1', [128, 32, 256], BF16, es3)
            peT = g.sb('ns_peT', [128, 32], BF16, es3)
            W2k = g.sb('ns_W2k', [128, 2, 128], BF16, es3)
            W2v = g.sb('ns_W2v', [128, 2, 64], BF16, es3)
            gkc = g.sb('ns_gkc', [128, 1], F32, es3)
            for (pb, w1n, pen) in ((0, 'nsa_wk1', 'nsa_pe_k'), (64, 'nsa_wv1', 'nsa_pe_v')):
                k.dma('pool', W1[pb:pb + 64, :, :], W[w1n][l].rearrange('(l d) m -> d l m', d=64), writes=['ns_W1'])
                k.dma('pool', peT[pb:pb + 64, :], W[pen][l].rearrange('l d -> d l'), writes=['ns_peT'], allow_slow_non_contiguous=True)
            for hh in range(2):
                k.dma('pool', W2k[:, :, hh * 64:(hh + 1) * 64], W['nsa_wk2'][l].rearrange('(c p) d -> p c d', p=128), writes=['ns_W2k'])
                k.dma('sp', gkc[hh * 64:(hh + 1) * 64, :], W['nsa_kc_g'][l].rearrange('(p o) -> p o', o=1), writes=['ns_gkc'],
                      allow_slow_non_contiguous=True)
            k.dma('pool', W2v[:], W['nsa_wv2'][l].rearrange('(c p) d -> p c d', p=128), writes=['ns_W2v'])
            hid = {}
            bia = g.sb('ns_bia', [128, 4], F32, es3)
            xs = g.sb('ns_xs', [128, 256], F32, es3)
            x2 = g.sb('ns_x2', [128, 256], F32, es3)
            th = g.sb('ns_th', [128, 256], F32, es3)
            n = 0
            allkc = ['ns_kcvc%d' % i for i in range(8)]
            for kv, pb in (('k', 0), ('v', 64)):
                for mc in range(2):
                    hd_ = g.sb('ns_hid%s%d' % (kv, mc), [128, 256], BF16, es3)
                    hid[(kv, mc)] = hd_
                    hk_ = 'ns_hid%s%d' % (kv, mc)
                    k.op('pool', lambda e, hd_=hd_: e.memset(hd_[:], 0.0), writes=[hk_])
                    pi, pj = n % 2, 2 + n % 2
                    for l_ in range(32):
                        k.op('pe', lambda e, l_=l_, pb=pb, mc=mc, pi=pi: e.matmul(
                            g.ps[pi][:, 0:255], lhsT=W1[pb:pb + 64, l_, mc * 128:(mc + 1) * 128],
                            rhs=kcvc[pb:pb + 64, l_:l_ + 16 * 254 + 1:16], start=(l_ == 0), stop=(l_ == 31)),
                            reads=['ns_W1'] + allkc, writes=[g.psk[pi]])
                    for l_ in range(32):
                        k.op('pe', lambda e, l_=l_, pb=pb, mc=mc, pj=pj: e.matmul(
                            g.ps[pj][:, 0:1], lhsT=W1[pb:pb + 64, l_, mc * 128:(mc + 1) * 128], rhs=peT[pb:pb + 64, l_:l_ + 1],
                            start=(l_ == 0), stop=(l_ == 31)), reads=['ns_W1', 'ns_peT'], writes=[g.psk[pj]])
                    k.op('act', lambda e, n=n, pj=pj: e.copy(out=bia[:, n:n + 1], in_=g.ps[pj][:, 0:1]), reads=[g.psk[pj]], writes=['ns_bia'])
                    k.op('act', lambda e, n=n, pi=pi: e.activation(out=xs[:, 0:255], in_=g.ps[pi][:, 0:255], func=AF.Identity,
                                                                   bias=bia[:, n:n + 1], scale=1.0), reads=[g.psk[pi], 'ns_bia'], writes=['ns_xs'])
                    k.op('dve', lambda e: e.tensor_tensor(out=x2[:, 0:255], in0=xs[:, 0:255], in1=xs[:, 0:255], op=ALU.mult), reads=['ns_xs'], writes=['ns_x2'])
                    k.op('dve', lambda e: e.tensor_scalar(out=x2[:, 0:255], in0=x2[:, 0:255], scalar1=0.044715, scalar2=1.0, op0=ALU.mult, op1=ALU.add),
                         reads=['ns_x2'], writes=['ns_x2'])
                    k.op('dve', lambda e: e.tensor_tensor(out=x2[:, 0:255], in0=x2[:, 0:255], in1=xs[:, 0:255], op=ALU.mult), reads=['ns_x2', 'ns_xs'], writes=['ns_x2'])
                    k.op('act', lambda e: e.activation(out=th[:, 0:255], in_=x2[:, 0:255], func=AF.Tanh, scale=0.7978845608028654),
                         reads=['ns_x2'], writes=['ns_th'])
                    k.op('dve', lambda e: e.scalar_tensor_tensor(out=th[:, 0:255], in0=th[:, 0:255], scalar=1.0, in1=xs[:, 0:255], op0=ALU.add, op1=ALU.mult),
                         reads=['ns_th', 'ns_xs'], writes=['ns_th'])
                    k.op('act', lambda e, hd_=hd_: e.mul(out=hd_[:, 0:255], in_=th[:, 0:255], mul=0.5), reads=['ns_th'], writes=[hk_])
                    n += 1
            for mc in range(2):
                k.op('pe', lambda e, mc=mc: e.matmul(g.ps[0][:, 0:256], lhsT=W2k[:, mc, :], rhs=hid[('k', mc)][:], start=(mc == 0), stop=(mc == 1)),
                     reads=['ns_W2k', 'ns_hidk%d' % mc], writes=[g.psk[0]])
            nr2 = NormRope(g, es3, 'nc')
            nr2.run(g.ps[0][:], g.psk[0], gkc[:, 0:1], ['ns_gkc'], slice(0, 512), out_n=kcT[:], okn='ns_kcT')
            k.op('pool', lambda e: e.memset(kcT[:, 255:512], 0.0), reads=['ns_kcT'], writes=['ns_kcT'])
            for j in range(2):
                for mc in range(2):
                    k.op('pe', lambda e, j=j, mc=mc: e.matmul(g.ps[1][:, 0:64], lhsT=hid[('v', mc)][:, j * 128:(j + 1) * 128], rhs=W2v[:, mc, :],
                                                           start=(mc == 0), stop=(mc == 1)), reads=['ns_W2v', 'ns_hidv%d' % mc], writes=[g.psk[1]])
                k.op('act', lambda e, j=j: e.copy(out=Vc[j][:, 0:64], in_=g.ps[1][:, 0:64]), reads=[g.psk[1]], writes=['ns_Vc%d' % j])
                k.op('pool', lambda e, j=j: e.tensor_copy(Vc[j][:, 64:129], ovl1[:, j, :]), reads=['c_ovl1'], writes=['ns_Vc%d_o' % j])
            k.barrier()
        if NSA_STOP == 'cmpkv':
            return
        imp = g.sb('ns_imp', [128, NT, 64], F32, es)
        selT = g.sb('ns_selT', [64, S], BF16, es)
        rd = [g.sb('ns_rd%d' % i, [128, 1], F32, es) for i in range(2)]
        cf = [g.sb('ns_cf%d' % i, [128, 1], F32, es) for i in range(2)]
        cnt = [0]

        def q_of_n(h, gi):
            j, hp = h // 2, h % 2
            return qn[hp * 64:(hp + 1) * 64, j, gi * 512:(gi + 1) * 512], ['ns_qn%d_%d' % (j, gi)]

        def q_of_r(h, gi):
            j, hp = h // 2, h % 2
            return qr[hp * 64:(hp + 1) * 64, j, gi * 512:(gi + 1) * 512], ['ns_qr%d_%d' % (j, gi)]

        def make_epi(br, first):
            def epi(h, gi, sub, acc, acck):
                b = cnt[0] % 2
                cnt[0] += 1
                t_ = gi * 4 + sub
                rk, ck = 'ns_rd%d' % b, 'ns_cf%d' % b
                k.op('dve', lambda e: e.tensor_scalar(out=rd[b][:], in0=acc[:, 64:65], scalar1=1e-30, scalar2=None, op0=ALU.max),
                     reads=[acck], writes=[rk])
                k.op('dve', lambda e: e.reciprocal(out=rd[b][:], in_=rd[b][:]), reads=[rk], writes=[rk])
                k.op('dve', lambda e: e.tensor_tensor(out=cf[b][:], in0=rd[b][:], in1=gate[:, t_, h * 3 + br:h * 3 + br + 1], op=ALU.mult),
                     reads=[rk, 'ns_gate%d' % t_], writes=[ck])
                yk = 'ns_ytm%d_%d' % (t_, h)
                if first:
                    k.op('dve', lambda e: e.tensor_scalar(out=ytm[:, t_, h * 64:(h + 1) * 64], in0=acc[:, 0:64], scalar1=cf[b][:, 0:1],
                                                           scalar2=None, op0=ALU.mult), reads=[acck, ck], writes=[yk])
                    ik = 'ns_imp%d' % t_
                    if h == 0:
                        k.op('dve', lambda e: e.tensor_scalar(out=imp[:, t_, :], in0=acc[:, 65:129], scalar1=rd[b][:, 0:1], scalar2=None,
                                                               op0=ALU.mult), reads=[acck, rk], writes=[ik])
                    else:
                        k.op('dve', lambda e: e.scalar_tensor_tensor(out=imp[:, t_, :], in0=acc[:, 65:129], scalar=rd[b][:, 0:1], in1=imp[:, t_, :],
                                                                      op0=ALU.mult, op1=ALU.add), reads=[acck, rk, ik], writes=[ik])
                else:
                    k.op('dve', lambda e: e.scalar_tensor_tensor(out=ytm[:, t_, h * 64:(h + 1) * 64], in0=acc[:, 0:64], scalar=cf[b][:, 0:1],
                                                                  in1=ytm[:, t_, h * 64:(h + 1) * 64], op0=ALU.mult, op1=ALU.add),
                         reads=[acck, ck, yk], writes=[yk])
            return epi

        with contextlib.ExitStack() as es4:
            cmsk = load_const(g, es4, 'cmp_mask')

            def k_cmp(h, kt):
                hp = h % 2
                return kcT[hp * 64:(hp + 1) * 64, kt * 128:(kt + 1) * 128], ['ns_kcT'], 128

            def v_cmp(h, kt):
                return Vc[kt][:, :], ['ns_Vc%d' % kt, 'ns_Vc%d_o' % kt]

            def kt_cmp(gi):
                return [0] + ([1] if gi >= 4 else [])

            def m_cmp(h, gi, kt):
                i = gi - 4 * kt
                return [] if i >= 5 else [(cmsk[:, i, :], ['c_cmp_mask'], False)]

            attention(g, es4, 'nc', range(4), 8, kt_cmp, q_of_n, k_cmp, v_cmp, 129, m_cmp, make_epi(0, True))
            if NSA_STOP == 'cmpattn':
                k.barrier()
                return
            keep = load_const(g, es4, 'sel_keep')
            addc = load_const(g, es4, 'sel_add')
            sc = [g.sb('ns_sc%d' % i, [128, 64], F32, es4) for i in range(2)]
            wk2_ = [g.sb('ns_wk%d' % i, [128, 64], F32, es4) for i in range(2)]
            m8 = [g.sb('ns_m8%d' % i, [128, 8], F32, es4) for i in range(2)]
            sm = [g.sb('ns_sm%d' % i, [128, 64], BF16, es4) for i in range(2)]
            psb = [g.ps[2][:].bitcast(BF16), g.ps[3][:].bitcast(BF16)]
            for t_ in range(NT):
                b = t_ % 2
                s_ = str(b)
                k.op('dve', lambda e, b=b, t_=t_: e.tensor_tensor(out=sc[b][:], in0=imp[:, t_, :], in1=keep[:, t_, :], op=ALU.mult),
                     reads=['ns_imp%d' % t_, 'c_sel_keep'], writes=['ns_sc' + s_])
                k.op('dve', lambda e, b=b, t_=t_: e.tensor_tensor(out=sc[b][:], in0=sc[b][:], in1=addc[:, t_, :], op=ALU.add),
                     reads=['ns_sc' + s_, 'c_sel_add'], writes=['ns_sc' + s_])
                k.op('dve', lambda e, b=b: e.max(out=m8[b][:], in_=sc[b][:]), reads=['ns_sc' + s_], writes=['ns_m8' + s_])
                k.op('dve', lambda e, b=b: e.match_replace(out=wk2_[b][:], in_to_replace=m8[b][:], in_values=sc[b][:], imm_value=-1e30),
                     reads=['ns_sc' + s_, 'ns_m8' + s_], writes=['ns_wk' + s_])
                k.op('dve', lambda e, b=b: e.max(out=m8[b][:], in_=wk2_[b][:]), reads=['ns_wk' + s_], writes=['ns_m8' + s_])
                k.op('dve', lambda e, b=b: e.tensor_scalar(out=sm[b][:], in0=sc[b][:], scalar1=m8[b][:, 7:8], scalar2=None, op0=ALU.is_ge),
                     reads=['ns_sc' + s_, 'ns_m8' + s_], writes=['ns_sm' + s_])
                k.op('pe', lambda e, b=b: e.transpose(out=psb[b][0:64, 0:128], in_=sm[b][:], identity=g.cs['ident_b'][:]),
                     reads=['ns_sm' + s_, 'c_ident_b'], writes=[g.psk[2 + b]])
                k.op('act', lambda e, b=b, t_=t_: e.copy(out=selT[:, t_ * 128:(t_ + 1) * 128], in_=psb[b][0:64, 0:128]),
                     reads=[g.psk[2 + b]], writes=['ns_selT%d' % (t_ // 4)])
            tp = tap(g, 'selT%d' % l, [64, S], BF16)
            if tp is not None:
                k.dma('sp', tp, selT[:], reads=['ns_selT%d' % i for i in range(8)], writes=['tap'])
            k.barrier()
        if NSA_STOP == 'topk':
            return
        with contextlib.ExitStack() as es5:
            cau = load_const(g, es5, 'cau_mask')
            sexp = load_const(g, es5, 'sel_exp')
            mc_ = [0]

            def k_s(h, kt):
                hp = h % 2
                return ksT[hp * 64:(hp + 1) * 64, kt * 128:(kt + 1) * 128], ['ns_ks_%d' % (kt // 4)], 128

            def v_s(h, kt):
                return Vs[:, kt, 0:65], ['ns_Vs%d' % kt, 'ns_Vs1']

            def m_s(h, gi, kt):
                pm = 2 + mc_[0] % 2
                mc_[0] += 1
                k.op('pe', lambda e: e.matmul(g.ps[pm][:], lhsT=sexp[:, kt, :], rhs=selT[:, gi * 512:(gi + 1) * 512], start=True, stop=True),
                     reads=['c_sel_exp', 'ns_selT%d' % gi], writes=[g.psk[pm]])
                ms = [(g.ps[pm], [g.psk[pm]], True)]
                if kt >= 4 * gi:
                    ms.append((cau[:, (4 * gi - kt) + 3, :], ['c_cau_mask'], False))
                return ms

            attention(g, es5, 'nl', range(4), 8, lambda gi: list(range(0, 4 * gi + 4)), q_of_r, k_s, v_s, 65, m_s, make_epi(1, False))
            k.barrier()
        if NSA_STOP == 'sel':
            return
        with contextlib.ExitStack() as es6:
            swm = load_const(g, es6, 'swa_mask')

            def k_w(h, kt):
                hp = h % 2
                return kwT[hp * 64:(hp + 1) * 64, kt * 128:(kt + 1) * 128], ['ns_kw_%d' % (kt // 4)], 128

            def v_w(h, kt):
                return Vw[:, kt, 0:65], ['ns_Vw%d' % kt, 'ns_Vw1']

            attention(g, es6, 'nw', range(4), 8, lambda gi: list(range(max(0, 4 * gi - 4), 4 * gi + 4)), q_of_r, k_w, v_w, 65,
                      lambda h, gi, kt: [(swm[:, (4 * gi - kt) + 3, :], ['c_swa_mask'], False)], make_epi(2, False))
            finish_tm(g, es6, 'ns', ytm, lambda t_: ['ns_ytm%d_%d' % (t_, h) for h in range(4)], gb, 'ns_gb', 768)
            k.barrier()


RW_STEPS = S


def mixer_rwkv(g, l):
    k = g.k
    W = g.W
    ycv = g.ycatT.rearrange('(c p) t -> p c t', p=128)
    with contextlib.ExitStack() as es:
        rT = g.sb('rw_rT', [128, 2, S], BF16, es)
        kkT = g.sb('rw_kkT', [128, 2, S], BF16, es)
        wT = g.sb('rw_wT', [128, 2, S], F32, es)
        vtm = g.sb('rw_vtm', [128, NT, 256], BF16, es)
        gtm = g.sb('rw_gtm', [128, NT, 256], BF16, es)
        bon = g.sb('rw_bon', [128, NT, 4], F32, es)
        with contextlib.ExitStack() as es2:
            wa = g.sb('rw_wa', [128, 8, 1024], BF16, es2)
            wb = g.sb('rw_wb', [128, 8, 1024], BF16, es2)
            mub = g.sb('rw_mub', [128, 1024], F32, es2)
            omb = g.sb('rw_omb', [128, 1024], F32, es2)
            load_w_bf16(g, wa[:], 'rw_wa', W['w_in'][l][:, 0:1024])
            k.dma('sp', mub[:], W['rwkv_mu'][l].partition_broadcast(128), writes=['rw_mub'])
            k.op('dve', lambda e: e.tensor_scalar(out=omb[:], in0=mub[:], scalar1=-1.0, scalar2=1.0, op0=ALU.mult, op1=ALU.add),
                 reads=['rw_mub'], writes=['rw_omb'])
            for c in range(8):
                k.op('dve', lambda e, c=c: e.tensor_tensor(out=wb[:, c, :], in0=wa[:, c, :], in1=mub[:], op=ALU.mult),
                     reads=['rw_wa', 'rw_mub'], writes=['rw_wb'])
            for c in range(8):
                k.op('pool', lambda e, c=c: e.tensor_tensor(out=wa[:, c, :], in0=wa[:, c, :], in1=omb[:], op=ALU.mult),
                     reads=['rw_wa', 'rw_omb', 'rw_wb'], writes=['rw_wa'])
            w2 = g.sb('rw_w2', [64, 256], BF16, es2)
            a2 = g.sb('rw_a2', [128, 256], BF16, es2)
            g2 = g.sb('rw_g2', [128, 256], BF16, es2)
            a0r = g.sb('rw_a0r', [1, 256], BF16, es2)
            w0c = g.sb('rw_w0c', [128, 2], F32, es2)
            kkc = g.sb('rw_kkc', [128, 2], F32, es2)
            k.dma('pool', w2[:], W['rwkv_w2'][l], writes=['rw_w2'])
            k.dma('pool', a2[64:128, :], W['rwkv_a2'][l], writes=['rw_a2'])
            k.dma('pool', g2[:], W['rwkv_g2'][l], writes=['rw_g2'])
            k.dma('pool', a0r[:], W['rwkv_a0'][l:l + 1, :], writes=['rw_a0r'])
            k.dma('sp', w0c[:], W['rwkv_w0'][l].rearrange('(c p) -> p c', p=128), writes=['rw_w0c'], allow_slow_non_contiguous=True)
            k.dma('sp', kkc[:], W['rwkv_kk'][l].rearrange('(c p) -> p c', p=128), writes=['rw_kkc'], allow_slow_non_contiguous=True)
            bc = {}
            for nm, src in (('kk', W['rwkv_kk'][l]), ('ka', W['rwkv_ka'][l]), ('rk', W['rwkv_rk'][l].rearrange('h d -> (h d)'))):
                t = g.sb('rw_bc_' + nm, [128, 256], F32, es2)
                k.dma('sp', t[:], src.partition_broadcast(128), writes=['rw_bc_' + nm])
                bc[nm] = t
            hv = g.hTd.rearrange('(c p) t -> p c t', p=128)
            hb = [g.sb('rw_ht%d' % i, [128, 8, 513], BF16, es2) for i in range(2)]
            k.op('pool', lambda e: e.memset(hb[0][:, :, 0:1], 0.0), writes=['rw_ht0'])
            tnh = [g.sb('rw_tnh%d' % i, [128, 512], BF16, es2) for i in range(2)]
            sgd = [g.sb('rw_sgd%d' % i, [128, 512], BF16, es2) for i in range(2)]
            kx = [g.sb('rw_kx%d' % i, [128, 512], F32, es2) for i in range(2)]
            sq = [g.sb('rw_sq%d' % i, [128, 512], BF16, es2) for i in range(2)]
            rs = [g.sb('rw_rs%d' % i, [128, 512], F32, es2) for i in range(2)]
            tm = {nm: [g.sb('rw_%s%d' % (nm, i), [128, 256], F32, es2) for i in range(2)] for nm in ('a', 'r', 'kxm', 'k2', 'tq')}
            s4 = [g.sb('rw_s4%d' % i, [128, 4], F32, es2) for i in range(2)]
            ob = [g.sb('rw_ob%d' % i, [128, 768], BF16, es2) for i in range(2)]

            def fm2(ht, hk, c0, ncols, ps, pskey):
                for c in range(8):
                    k.op('pe', lambda e, c=c: e.matmul(ps, lhsT=wa[:, c, c0:c0 + ncols], rhs=ht[:, c, 1:513], start=(c == 0), stop=False),
                         reads=['rw_wa', hk], writes=[pskey])
                for c in range(8):
                    k.op('pe', lambda e, c=c: e.matmul(ps, lhsT=wb[:, c, c0:c0 + ncols], rhs=ht[:, c, 0:512], start=False, stop=(c == 7)),
                         reads=['rw_wb', hk], writes=[pskey])

            nps = 0
            for tt in range(8):
                b = tt % 2
                ts = slice(tt * 512, (tt + 1) * 512)
                hk = 'rw_ht%d' % b
                ht = hb[b]
                if tt == 0:
                    k.dma('sp', ht[:, :, 1:513], hv[:, :, 0:512], reads=['hTd'], writes=[hk])
                else:
                    k.dma('sp', ht[:], hv[:, :, tt * 512 - 1:(tt + 1) * 512], reads=['hTd'], writes=[hk])
                for j in range(2):
                    pi = nps % 2
                    nps += 1
                    fm2(ht, hk, j * 128, 128, g.ps[pi][:], g.psk[pi])
                    k.op('act', lambda e, pi=pi, j=j, ts=ts: e.copy(out=rT[:, j, ts], in_=g.ps[pi][:]), reads=[g.psk[pi]],
                         writes=['rw_rT%d_%d' % (j, tt)])
                    pi = nps % 2
                    nps += 1
                    fm2(ht, hk, 256 + j * 128, 128, g.ps[pi][:], g.psk[pi])
                    kb = nps % 2
                    ks_ = str(kb)
                    k.op('act', lambda e, pi=pi, j=j, kb=kb: e.activation(out=kx[kb][:], in_=g.ps[pi][:], func=AF.Copy, scale=kkc[:, j:j + 1]),
                         reads=[g.psk[pi], 'rw_kkc'], writes=['rw_kx' + ks_])
                    k.op('act', lambda e, kb=kb: e.activation(out=sq[kb][:], in_=kx[kb][:], func=AF.Square), reads=['rw_kx' + ks_], writes=['rw_sq' + ks_])
                    pj = 2 + kb
                    k.op('pe', lambda e, kb=kb, pj=pj: e.matmul(g.ps[pj][:], lhsT=g.cs['blk64_b'][:], rhs=sq[kb][:], start=True, stop=True),
                         reads=['rw_sq' + ks_, 'c_blk64_b'], writes=[g.psk[pj]])
                    k.op('dve', lambda e, kb=kb, pj=pj: e.tensor_scalar(out=rs[kb][:], in0=g.ps[pj][:], scalar1=1e-12, scalar2=None, op0=ALU.add),
                         reads=[g.psk[pj]], writes=['rw_rs' + ks_])
                    k.op('act', lambda e, kb=kb: e.activation(out=rs[kb][:], in_=rs[kb][:], func=AF.Sqrt), reads=['rw_rs' + ks_], writes=['rw_rs' + ks_])
                    k.op('dve', lambda e, kb=kb: e.reciprocal(out=rs[kb][:], in_=rs[kb][:]), reads=['rw_rs' + ks_], writes=['rw_rs' + ks_])
                    k.op('dve', lambda e, kb=kb, j=j, ts=ts: e.tensor_tensor(out=kkT[:, j, ts], in0=kx[kb][:], in1=rs[kb][:], op=ALU.mult),
                         reads=['rw_kx' + ks_, 'rw_rs' + ks_], writes=['rw_kkT%d_%d' % (j, tt)])
                pi = nps % 2
                nps += 1
                fm2(ht, hk, 768, 128, g.ps[pi][:], g.psk[pi])
                k.op('act', lambda e, pi=pi, b=b: e.activation(out=tnh[b][0:64, :], in_=g.ps[pi][0:64, :], func=AF.Tanh),
                     reads=[g.psk[pi]], writes=['rw_tnh%d' % b])
                k.op('act', lambda e, pi=pi, b=b: e.copy(out=tnh[b][64:128, :], in_=g.ps[pi][64:128, :]),
                     reads=[g.psk[pi]], writes=['rw_tnhb%d' % b])
                pi = nps % 2
                nps += 1
                fm2(ht, hk, 896, 128, g.ps[pi][:], g.psk[pi])
                k.op('act', lambda e, pi=pi, b=b: e.activation(out=sgd[b][:], in_=g.ps[pi][:], func=AF.Sigmoid), reads=[g.psk[pi]],
                     writes=['rw_sgd%d' % b])
                for j in range(2):
                    pi = nps % 2
                    nps += 1
                    k.op('pe', lambda e, pi=pi, j=j, b=b: e.matmul(g.ps[pi][:], lhsT=w2[:, j * 128:(j + 1) * 128], rhs=tnh[b][0:64, :],
                                                                   start=True, stop=True), reads=['rw_w2', 'rw_tnh%d' % b], writes=[g.psk[pi]])
                    k.op('act', lambda e, pi=pi, j=j, ts=ts: e.activation(out=wT[:, j, ts], in_=g.ps[pi][:], func=AF.Sigmoid, bias=w0c[:, j:j + 1], scale=1.0),
                         reads=[g.psk[pi], 'rw_w0c'], writes=['rw_wT%d_%d' % (j, tt)])
                    k.op('act', lambda e, j=j, ts=ts: e.activation(out=wT[:, j, ts], in_=wT[:, j, ts], func=AF.Exp, scale=-0.606531),
                         reads=['rw_wT%d_%d' % (j, tt)], writes=['rw_wT%d_%d' % (j, tt)])
                for sub in range(4):
                    t_ = tt * 4 + sub
                    q = t_ % 2
                    qs = str(q)
                    cs_ = slice(sub * 128, (sub + 1) * 128)
                    pA, pB, pC, pD = 4, 5, 6, 7
                    for (ps_, c0, n_) in ((pA, 0, 512), (pB, 512, 256)):
                        for c in range(8):
                            k.op('pe', lambda e, c=c, ps_=ps_, c0=c0, n_=n_, sub=sub: e.matmul(
                                g.ps[ps_][:, 0:n_], lhsT=ht[:, c, 1 + sub * 128:1 + (sub + 1) * 128], rhs=wa[:, c, c0:c0 + n_],
                                start=(c == 0), stop=False), reads=['rw_wa', hk], writes=[g.psk[ps_]])
                        for c in range(8):
                            k.op('pe', lambda e, c=c, ps_=ps_, c0=c0, n_=n_, sub=sub: e.matmul(
                                g.ps[ps_][:, 0:n_], lhsT=ht[:, c, sub * 128:(sub + 1) * 128], rhs=wb[:, c, c0:c0 + n_],
                                start=False, stop=(c == 7)), reads=['rw_wb', hk], writes=[g.psk[ps_]])
                    k.op('pe', lambda e, b=b, cs_=cs_: e.matmul(g.ps[pC][:, 0:256], lhsT=tnh[b][64:128, cs_], rhs=a2[64:128, :], start=True, stop=False),
                         reads=['rw_tnhb%d' % b, 'rw_a2'], writes=[g.psk[pC]])
                    k.op('pe', lambda e: e.matmul(g.ps[pC][:, 0:256], lhsT=g.cs['ones_b'][0:1, :], rhs=a0r[:], start=False, stop=True),
                         reads=['c_ones_b', 'rw_a0r'], writes=[g.psk[pC]])
                    k.op('pe', lambda e, b=b, cs_=cs_: e.matmul(g.ps[pD][:, 0:256], lhsT=sgd[b][:, cs_], rhs=g2[:], start=True, stop=True),
                         reads=['rw_sgd%d' % b, 'rw_g2'], writes=[g.psk[pD]])
                    a_, r_, kxm, k2, tq = (tm[nm][q] for nm in ('a', 'r', 'kxm', 'k2', 'tq'))
                    K_ = lambda nm: 'rw_%s%s' % (nm, qs)
                    k.op('act', lambda e: e.activation(out=a_[:], in_=g.ps[pC][:, 0:256], func=AF.Sigmoid), reads=[g.psk[pC]], writes=[K_('a')])
                    k.op('act', lambda e, t_=t_: e.copy(out=gtm[:, t_, :], in_=g.ps[pD][:, 0:256]), reads=[g.psk[pD]], writes=['rw_gtm%d' % t_])
                    k.op('act', lambda e: e.copy(out=r_[:], in_=g.ps[pA][:, 0:256]), reads=[g.psk[pA]], writes=[K_('r')])
                    k.op('act', lambda e, t_=t_: e.copy(out=vtm[:, t_, :], in_=g.ps[pB][:, 0:256]), reads=[g.psk[pB]], writes=['rw_vtm%d' % t_])
                    k.op('act', lambda e, q=q: e.copy(out=ob[q][:, 512:768], in_=g.ps[pB][:, 0:256]), reads=[g.psk[pB]], writes=['rw_obv' + qs])
                    k.op('dve', lambda e: e.tensor_tensor(out=kxm[:], in0=g.ps[pA][:, 256:512], in1=bc['kk'][:], op=ALU.mult),
                         reads=[g.psk[pA], 'rw_bc_kk'], writes=[K_('kxm')])
                    k.op('pool', lambda e: e.tensor_tensor(out=tq[:], in0=kxm[:], in1=kxm[:], op=ALU.mult), reads=[K_('kxm')], writes=[K_('tq')])
                    k.op('dve', lambda e, q=q: e.reduce_sum(out=s4[q][:], in_=tq[:].rearrange('p (h d) -> p h d', d=64), axis=AX.X),
                         reads=[K_('tq')], writes=['rw_s4' + qs])
                    k.op('dve', lambda e, q=q: e.tensor_scalar(out=s4[q][:], in0=s4[q][:], scalar1=1e-12, scalar2=None, op0=ALU.add),
                         reads=['rw_s4' + qs], writes=['rw_s4' + qs])
                    k.op('act', lambda e, q=q: e.activation(out=s4[q][:], in_=s4[q][:], func=AF.Sqrt), reads=['rw_s4' + qs], writes=['rw_s4' + qs])
                    k.op('dve', lambda e, q=q: e.reciprocal(out=s4[q][:], in_=s4[q][:]), reads=['rw_s4' + qs], writes=['rw_s4' + qs])
                    for h in range(4):
                        hs = slice(h * 64, (h + 1) * 64)
                        k.op('dve', lambda e, h=h, hs=hs, q=q: e.scalar_tensor_tensor(out=ob[q][:, hs], in0=kxm[:, hs], scalar=s4[q][:, h:h + 1],
                                                                                      in1=a_[:, hs], op0=ALU.mult, op1=ALU.mult),
                             reads=[K_('kxm'), 'rw_s4' + qs, K_('a')], writes=['rw_obb' + qs])
                    k.op('dve', lambda e: e.scalar_tensor_tensor(out=tq[:], in0=a_[:], scalar=-1.0, in1=bc['ka'][:], op0=ALU.add, op1=ALU.mult),
                         reads=[K_('a'), 'rw_bc_ka', K_('tq')], writes=[K_('tq')])
                    k.op('dve', lambda e: e.scalar_tensor_tensor(out=k2[:], in0=tq[:], scalar=1.0, in1=g.ps[pA][:, 256:512], op0=ALU.add, op1=ALU.mult),
                         reads=[K_('tq'), g.psk[pA]], writes=[K_('k2')])
                    k.op('pool', lambda e, q=q: e.tensor_copy(ob[q][:, 256:512], k2[:]), reads=[K_('k2')], writes=['rw_obk' + qs])
                    k.op('pool', lambda e: e.tensor_tensor(out=tq[:], in0=r_[:], in1=k2[:], op=ALU.mult), reads=[K_('r'), K_('k2'), K_('tq')], writes=[K_('tq')])
                    k.op('pool', lambda e: e.tensor_tensor(out=tq[:], in0=tq[:], in1=bc['rk'][:], op=ALU.mult), reads=[K_('tq'), 'rw_bc_rk'], writes=[K_('tq')])
                    k.op('dve', lambda e, t_=t_: e.reduce_sum(out=bon[:, t_, :], in_=tq[:].rearrange('p (h d) -> p h d', d=64), axis=AX.X),
                         reads=[K_('tq')], writes=['rw_bon%d' % t_])
                    k.dma('sp', g.rwscr[t_ * 128:(t_ + 1) * 128, :], ob[q][:], reads=['rw_obv' + qs, 'rw_obb' + qs, 'rw_obk' + qs],
                          writes=['rwscr%d' % t_])
            k.barrier()
        TC = 64
        ST = g.sb('rw_ST', [128, 128], BF16, es)
        Lb = [g.sb('rw_L%d' % i, [36, TC, 128], BF16, es) for i in range(2)]
        Rb = [g.sb('rw_R%d' % i, [36, TC, 128], BF16, es) for i in range(2)]
        KKn = [g.sb('rw_KKn%d' % i, [128, TC, 4], BF16, es) for i in range(2)]
        Rr = [g.sb('rw_Rr%d' % i, [128, TC, 4], BF16, es) for i in range(2)]
        sam = load_const(g, es, 'sa_mask')
        hlm = load_const(g, es, 'hl_mask')
        Yc = [g.sb('rw_Yc%d' % i, [128, 2, 128], F32, es) for i in range(2)]
        ytm = g.sb('rw_ytm', [128, 256], F32, es)
        yo = [g.sb('rw_yo%d' % i, [128, 256], BF16, es) for i in range(2)]
        yT = [g.sb('rw_yT%d' % i, [128, 2, 128], BF16, es) for i in range(2)]
        st8 = g.sb('rw_st8', [128, 8], F32, es)
        tq2 = g.sb('rw_tq2', [128, 256], F32, es)
        gnw = g.sb('rw_gnw', [128, 256], F32, es)
        gnb = g.sb('rw_gnb', [128, 256], F32, es)
        k.dma('sp', gnw[:], W['rwkv_gn_w'][l].partition_broadcast(128), writes=['rw_gnw'])
        k.dma('sp', gnb[:], W['rwkv_gn_b'][l].partition_broadcast(128), writes=['rw_gnb'])
        k.op('pool', lambda e: e.memset(ST[:], 0.0), writes=['rw_ST0', 'rw_ST1'])
        for i in range(2):
            k.op('pool', lambda e, i=i: e.memset(Lb[i][:], 0.0), writes=['rw_L%d' % i])
            k.op('pool', lambda e, i=i: e.memset(Rb[i][:], 0.0), writes=['rw_R%d' % i])
        psb = [g.ps[6][:].bitcast(BF16), g.ps[7][:].bitcast(BF16)]
        nsteps = RW_STEPS
        def setup(ch):
            cb = ch % 2
            t0 = ch * TC
            cbs = str(cb)
            tt = t0 // 512
            for h in range(4):
                hl, hh = h % 2, h // 2
                src = g.rwscr[t0:t0 + TC, :]
                k.dma('sp', Lb[cb][h:h + 1, :, hl * 64:(hl + 1) * 64], src[:, h * 64:(h + 1) * 64].rearrange('(o t) d -> o t d', o=1),
                      reads=['rwscr%d' % (t0 // 128)], writes=['rw_L' + cbs])
                k.dma('sp', Lb[cb][32 + h:33 + h, :, hl * 64:(hl + 1) * 64], src[:, 256 + h * 64:256 + (h + 1) * 64].rearrange('(o t) d -> o t d', o=1),
                      reads=['rwscr%d' % (t0 // 128)], writes=['rw_L' + cbs])
                k.dma('sp', Rb[cb][32 + h:33 + h, :, hh * 64:(hh + 1) * 64], src[:, 512 + h * 64:512 + (h + 1) * 64].rearrange('(o t) d -> o t d', o=1),
                      reads=['rwscr%d' % (t0 // 128)], writes=['rw_Rv' + cbs])
                k.op('pool', lambda e, h=h, hh=hh, hl=hl, cb=cb, t0=t0: e.tensor_scalar(
                    out=KKn[cb][:, :, h], in0=kkT[:, hh, t0:t0 + TC], scalar1=hlm[:, hl:hl + 1], scalar2=None, op0=ALU.mult),
                    reads=['rw_kkT%d_%d' % (hh, tt), 'c_hl_mask'], writes=['rw_KKn' + cbs])
                k.op('pool', lambda e, h=h, hh=hh, hl=hl, cb=cb, t0=t0: e.tensor_scalar(
                    out=Rr[cb][:, :, h], in0=rT[:, hh, t0:t0 + TC], scalar1=hlm[:, 2 + hl:3 + hl], scalar2=None, op0=ALU.mult),
                    reads=['rw_rT%d_%d' % (hh, tt), 'c_hl_mask'], writes=['rw_Rr' + cbs])

        setup(0)
        pend = []
        for ch in range(nsteps // TC):
            cb = ch % 2
            t0 = ch * TC
            cbs = str(cb)
            tt = t0 // 512
            if pend:
                pend.pop()()
            if ch + 1 < nsteps // TC:
                setup(ch + 1)
            for s_ in range(TC):
                t = t0 + s_
                pa = t % 2
                pu = 2 + t % 2
                py = 4 + (t // 128) % 2
                k.op('pe', lambda e, cb=cb, s_=s_, pa=pa: e.matmul(g.ps[pa][0:4, 0:128], lhsT=KKn[cb][:, s_, :], rhs=ST[:], start=True, stop=True),
                     reads=['rw_KKn' + cbs, 'rw_ST0', 'rw_ST1'], writes=[g.psk[pa]])
                if pend:
                    pend.pop()()
                k.op('dve', lambda e, cb=cb, s_=s_, pa=pa: e.tensor_tensor(out=Rb[cb][0:4, s_, :], in0=g.ps[pa][0:4, 0:128], in1=sam[0:4, :], op=ALU.mult),
                     reads=[g.psk[pa], 'c_sa_mask'], writes=['rw_R' + cbs])
                k.op('pe', lambda e, cb=cb, s_=s_, pu=pu: e.matmul(g.ps[pu][:, 0:128], lhsT=Lb[cb][:, s_, :], rhs=Rb[cb][:, s_, :], start=True, stop=True),
                     reads=['rw_L' + cbs, 'rw_R' + cbs, 'rw_Rv' + cbs], writes=[g.psk[pu]])
                for hh in range(2):
                    k.op('dve', lambda e, hh=hh, t=t, pu=pu: e.scalar_tensor_tensor(
                        out=ST[:, hh * 64:(hh + 1) * 64], in0=ST[:, hh * 64:(hh + 1) * 64], scalar=wT[:, hh, t:t + 1],
                        in1=g.ps[pu][:, hh * 64:(hh + 1) * 64], op0=ALU.mult, op1=ALU.add),
                        reads=['rw_ST%d' % hh, g.psk[pu], 'rw_wT%d_%d' % (hh, tt)], writes=['rw_ST%d' % hh])
                pend.append(lambda cb=cb, s_=s_, t=t, py=py, cbs=cbs: k.op('pe', lambda e: e.matmul(
                    g.ps[py][:, (t % 128) * 4:(t % 128) * 4 + 4], lhsT=ST[:], rhs=Rr[cb][:, s_, :], start=True, stop=True),
                    reads=['rw_ST0', 'rw_ST1', 'rw_Rr' + cbs], writes=[g.psk[py]]))
                if t % 128 == 127:
                    pend.pop()()
                    t_ = t // 128
                    yb_ = t_ % 2
                    ys = str(yb_)
                    yv = g.ps[py][:].rearrange('p (t h) -> p h t', h=4)
                    for hl in range(2):
                        k.op('act', lambda e, hl=hl, yb_=yb_: e.copy(out=Yc[yb_][0:64, hl, :], in_=yv[0:64, hl, :]), reads=[g.psk[py]],
                             writes=['rw_Yc%s_%d' % (ys, hl)])
                        k.op('act', lambda e, hl=hl, yb_=yb_: e.copy(out=Yc[yb_][64:128, hl, :], in_=yv[64:128, 2 + hl, :]), reads=[g.psk[py]],
                             writes=['rw_Ycb%s_%d' % (ys, hl)])
                    ytv = ytm[:].rearrange('p (hh hl i) -> p hl hh i', hh=2, hl=2)
                    for hl in range(2):
                        pt_ = 6 + hl
                        k.op('pe', lambda e, hl=hl, yb_=yb_, pt_=pt_: e.transpose(out=g.ps[pt_][:, 0:128], in_=Yc[yb_][:, hl, :], identity=g.cs['ident_f'][:]),
                             reads=['rw_Yc%s_%d' % (ys, hl), 'rw_Ycb%s_%d' % (ys, hl), 'c_ident_f'], writes=[g.psk[pt_]])
                        k.op('act', lambda e, hl=hl, pt_=pt_: e.copy(out=ytv[:, hl, :, :], in_=g.ps[pt_][:, 0:128].rearrange('p (hh i) -> p hh i', hh=2)),
                             reads=[g.psk[pt_]], writes=['rw_ytm'])
                    k.op('dve', lambda e: e.reduce_sum(out=st8[:, 0:4], in_=ytm[:].rearrange('p (h d) -> p h d', d=64), axis=AX.X),
                         reads=['rw_ytm'], writes=['rw_st8'])
                    k.op('pool', lambda e: e.tensor_tensor(out=tq2[:], in0=ytm[:], in1=ytm[:], op=ALU.mult), reads=['rw_ytm'], writes=['rw_tq2'])
                    k.op('dve', lambda e: e.reduce_sum(out=st8[:, 4:8], in_=tq2[:].rearrange('p (h d) -> p h d', d=64), axis=AX.X),
                         reads=['rw_tq2'], writes=['rw_st8'])
                    k.op('dve', lambda e: e.tensor_scalar(out=st8[:], in0=st8[:], scalar1=1.0 / 64, scalar2=None, op0=ALU.mult),
                         reads=['rw_st8'], writes=['rw_st8'])
                    k.op('dve', lambda e: e.tensor_tensor(out=tq2[:, 0:4], in0=st8[:, 0:4], in1=st8[:, 0:4], op=ALU.mult), reads=['rw_st8', 'rw_tq2'],
                         writes=['rw_tq2'])
                    k.op('dve', lambda e: e.tensor_tensor(out=st8[:, 4:8], in0=st8[:, 4:8], in1=tq2[:, 0:4], op=ALU.subtract), reads=['rw_st8', 'rw_tq2'],
                         writes=['rw_st8'])
                    k.op('dve', lambda e: e.tensor_scalar(out=st8[:, 4:8], in0=st8[:, 4:8], scalar1=64e-5, scalar2=None, op0=ALU.add),
                         reads=['rw_st8'], writes=['rw_st8'])
                    k.op('act', lambda e: e.activation(out=st8[:, 4:8], in_=st8[:, 4:8], func=AF.Sqrt), reads=['rw_st8'], writes=['rw_st8'])
                    k.op('dve', lambda e: e.reciprocal(out=st8[:, 4:8], in_=st8[:, 4:8]), reads=['rw_st8'], writes=['rw_st8'])
                    for h in range(4):
                        hs = slice(h * 64, (h + 1) * 64)
                        k.op('dve', lambda e, h=h, hs=hs: e.tensor_scalar(out=tq2[:, hs], in0=ytm[:, hs], scalar1=st8[:, h:h + 1], scalar2=st8[:, 4 + h:5 + h],
                                                                         op0=ALU.subtract, op1=ALU.mult), reads=['rw_ytm', 'rw_st8', 'rw_tq2'], writes=['rw_tq2'])
                    k.op('pool', lambda e: e.tensor_tensor(out=tq2[:], in0=tq2[:], in1=gnw[:], op=ALU.mult), reads=['rw_tq2', 'rw_gnw'], writes=['rw_tq2'])
                    k.op('pool', lambda e: e.tensor_tensor(out=tq2[:], in0=tq2[:], in1=gnb[:], op=ALU.add), reads=['rw_tq2', 'rw_gnb'], writes=['rw_tq2'])
                    for h in range(4):
                        hs = slice(h * 64, (h + 1) * 64)
                        k.op('dve', lambda e, h=h, hs=hs, t_=t_: e.scalar_tensor_tensor(out=tq2[:, hs], in0=vtm[:, t_, hs], scalar=bon[:, t_, h:h + 1],
                                                                                       in1=tq2[:, hs], op0=ALU.mult, op1=ALU.add),
                             reads=['rw_vtm%d' % t_, 'rw_bon%d' % t_, 'rw_tq2'], writes=['rw_tq2'])
                    k.op('dve', lambda e, yb_=yb_, t_=t_: e.tensor_tensor(out=yo[yb_][:], in0=tq2[:], in1=gtm[:, t_, :], op=ALU.mult),
                         reads=['rw_tq2', 'rw_gtm%d' % t_], writes=['rw_yo' + ys])
                    for j in range(2):
                        k.op('pe', lambda e, j=j, yb_=yb_: e.transpose(out=psb[yb_][:, j * 128:(j + 1) * 128], in_=yo[yb_][:, j * 128:(j + 1) * 128],
                                                                      identity=g.cs['ident_b'][:]), reads=['rw_yo' + ys, 'c_ident_b'], writes=[g.psk[6 + yb_]])
                    k.op('act', lambda e, yb_=yb_: e.copy(out=yT[yb_][:], in_=psb[yb_][:, 0:256].rearrange('p (j t) -> p j t', j=2)),
                         reads=[g.psk[6 + yb_]], writes=['rw_yT' + ys])
                    k.dma('sp', ycv[:, 0:2, t_ * 128:(t_ + 1) * 128], yT[yb_][:], reads=['rw_yT' + ys], writes=['ycat_rw%d' % t_])
        k.barrier()
```
